# Optimizing a Trainium2 kernel written in Bass

```python
import math
import jax, jax.numpy as jnp
from jax import lax
import numpy as np

D_MODEL = 2048
BATCH = 4
SEQ = 2048
DEPTH = 4

N_MIXERS = 4
HEAD_DIM = 64
N_HEADS = D_MODEL // HEAD_DIM
N_KV_HEADS = N_HEADS // 4
GQA_GROUP = N_HEADS // N_KV_HEADS
WINDOW = 128
Q_BLOCK = 128
DIFF_HEADS = D_MODEL // (2 * HEAD_DIM)
DIFF_V_DIM = 2 * HEAD_DIM
HGRN_HEAD_DIM = 128
HGRN_HEADS = D_MODEL // HGRN_HEAD_DIM
HGRN_CHUNK = 64
IDX_HEADS = 16
IDX_DIM = 64
TOPK_MAX = 256
D_FF = 5632
CONV_WIDTH = 3
NUM_BUCKETS = 32
MAX_DISTANCE = 128
RMS_EPS = 1e-6
N_SWA = len(range(0, DEPTH, N_MIXERS))
N_DIFF = len(range(1, DEPTH, N_MIXERS))
N_HGRN = len(range(2, DEPTH, N_MIXERS))
N_DSA = len(range(3, DEPTH, N_MIXERS))
SWA_SPLITS = (N_HEADS * HEAD_DIM, N_KV_HEADS * HEAD_DIM, N_KV_HEADS * HEAD_DIM)
SWA_IN = sum(SWA_SPLITS)
DIFF_SPLITS = (DIFF_HEADS * 2 * HEAD_DIM, DIFF_HEADS * 2 * HEAD_DIM, DIFF_HEADS * DIFF_V_DIM)
DIFF_IN = sum(DIFF_SPLITS)
HGRN_IN = 4 * HGRN_HEADS * HGRN_HEAD_DIM
DSA_SPLITS = (N_HEADS * HEAD_DIM, N_KV_HEADS * HEAD_DIM, N_KV_HEADS * HEAD_DIM,
              IDX_HEADS * IDX_DIM, IDX_DIM, IDX_HEADS)
DSA_IN = sum(DSA_SPLITS)

kernel_name = 'hybrid_swa_diff_hgrn2_dsa_convffn'


def split_cols(z, sizes):
    return jnp.split(z, [int(c) for c in np.cumsum(sizes)[:-1]], axis=-1)


def rms_norm(x, g):
    xf = x.astype(jnp.float32)
    y = xf * lax.rsqrt(jnp.mean(xf * xf, axis=-1, keepdims=True) + RMS_EPS)
    return (y * g.astype(jnp.float32)).astype(x.dtype)


def rel_position_bias(dist, table):
    n = jnp.maximum(dist, 0)
    max_exact = NUM_BUCKETS // 2
    log_ratio = jnp.log(jnp.maximum(n, 1).astype(jnp.float32) / max_exact) / math.log(MAX_DISTANCE / max_exact)
    large = jnp.minimum(max_exact + (log_ratio * (NUM_BUCKETS - max_exact)).astype(jnp.int32), NUM_BUCKETS - 1)
    bucket = jnp.where(n < max_exact, n, large)
    return table.astype(jnp.float32)[bucket]


def swa_sink_attention(h, w_in, w_out, sinks, bias_table):
    B, S, _ = h.shape
    nb = S // Q_BLOCK
    q, k, v = split_cols(h @ w_in, SWA_SPLITS)
    q = q.reshape(B, nb, Q_BLOCK, N_KV_HEADS, GQA_GROUP, HEAD_DIM)
    pad = ((0, 0), (Q_BLOCK, 0), (0, 0))
    kp = jnp.pad(k, pad).reshape(B, nb + 1, Q_BLOCK, N_KV_HEADS, HEAD_DIM)
    vp = jnp.pad(v, pad).reshape(B, nb + 1, Q_BLOCK, N_KV_HEADS, HEAD_DIM)
    kb = jnp.concatenate([kp[:, :-1], kp[:, 1:]], axis=2)
    vb = jnp.concatenate([vp[:, :-1], vp[:, 1:]], axis=2)
    qi = jnp.arange(Q_BLOCK)
    kj = jnp.arange(2 * Q_BLOCK) - Q_BLOCK
    dist = qi[:, None] - kj[None, :]
    in_band = (dist >= 0) & (dist < WINDOW)
    key_exists = (jnp.arange(nb)[:, None] > 0) | (kj[None, :] >= 0)
    mask = in_band[None] & key_exists[:, None, :]
    bias = rel_position_bias(dist, bias_table).transpose(2, 0, 1).reshape(
        N_KV_HEADS, GQA_GROUP, Q_BLOCK, 2 * Q_BLOCK)
    s = jnp.einsum('bnqhgd,bnshd->bnhgqs', q, kb).astype(jnp.float32) * HEAD_DIM ** -0.5 + bias
    s = jnp.where(mask[None, :, None, None], s, -jnp.inf)
    sink = jnp.broadcast_to(sinks.astype(jnp.float32).reshape(N_KV_HEADS, GQA_GROUP, 1, 1),
                            s.shape[:-1] + (1,))
    p = jax.nn.softmax(jnp.concatenate([s, sink], axis=-1), axis=-1)[..., :-1]
    o = jnp.einsum('bnhgqs,bnshd->bnqhgd', p.astype(vb.dtype), vb)
    return o.reshape(B, S, N_HEADS * HEAD_DIM) @ w_out


def diff_attention(h, w_in, w_out, lambdas, subln_g, bias_table, layer_idx):
    B, S, _ = h.shape
    nb = S // Q_BLOCK
    q, k, v = split_cols(h @ w_in, DIFF_SPLITS)
    q = q.reshape(B, nb, Q_BLOCK, DIFF_HEADS, 2, HEAD_DIM).transpose(1, 0, 2, 3, 4, 5)
    k = k.reshape(B, S, DIFF_HEADS, 2, HEAD_DIM)
    v = v.reshape(B, S, DIFF_HEADS, DIFF_V_DIM)
    lam_init = 0.8 - 0.6 * math.exp(-0.3 * layer_idx)
    lf = lambdas.astype(jnp.float32)
    lam = jnp.exp(jnp.sum(lf[0] * lf[1])) - jnp.exp(jnp.sum(lf[2] * lf[3])) + lam_init
    kpos = jnp.arange(S)

    def block(args):
        qb, start = args
        dist = (start + jnp.arange(Q_BLOCK))[:, None] - kpos[None, :]
        bias = rel_position_bias(dist, bias_table).reshape(Q_BLOCK, S, DIFF_HEADS, 2).transpose(2, 3, 0, 1)
        s = jnp.einsum('bqhmd,bshmd->bhmqs', qb, k).astype(jnp.float32) * HEAD_DIM ** -0.5 + bias
        p = jax.nn.softmax(jnp.where(dist >= 0, s, -jnp.inf), axis=-1)
        a = p[:, :, 0] - lam * p[:, :, 1]
        return jnp.einsum('bhqs,bshe->bqhe', a.astype(v.dtype), v)

    o = lax.map(block, (q, jnp.arange(nb) * Q_BLOCK))
    o = o.transpose(1, 0, 2, 3, 4).reshape(B, S, DIFF_HEADS, DIFF_V_DIM)
    o = rms_norm(o, subln_g) * (1.0 - lam_init)
    return o.reshape(B, S, DIFF_HEADS * DIFF_V_DIM) @ w_out


def hgrn2_recurrence(h, w_in, w_out, lb, norm_g):
    B, S, _ = h.shape
    nc = S // HGRN_CHUNK
    f32 = jnp.float32
    q, fz, i, g = jnp.split(h @ w_in, 4, axis=-1)

    def chunks(t):
        return t.astype(f32).reshape(B, nc, HGRN_CHUNK, HGRN_HEADS, HGRN_HEAD_DIM).transpose(1, 0, 3, 2, 4)

    lbf = lb.astype(f32)
    log_f = jnp.logaddexp(jnp.log(lbf), jnp.log1p(-lbf) + jax.nn.log_sigmoid(fz.astype(f32)))
    key = -jnp.expm1(log_f)
    qs, ks, vs, gs = chunks(jax.nn.silu(q.astype(f32))), chunks(key), chunks(i), chunks(log_f)
    causal = jnp.tril(jnp.ones((HGRN_CHUNK, HGRN_CHUNK), dtype=bool))

    def step(state, inp):
        qc, kc, vc, gc = inp
        b = jnp.cumsum(gc, axis=2)
        o_inter = jnp.einsum('bhtk,bhkv->bhtv', qc * jnp.exp(b), state)
        rel = jnp.where(causal[:, :, None], b[:, :, :, None, :] - b[:, :, None, :, :], -jnp.inf)
        att = jnp.einsum('bhtsk,bhsk->bhts', qc[:, :, :, None, :] * jnp.exp(rel), kc)
        o_intra = jnp.einsum('bhts,bhsv->bhtv', att, vc)
        b_end = b[:, :, -1:, :]
        state = state * jnp.exp(b_end[:, :, 0, :, None]) + jnp.einsum(
            'bhsk,bhsv->bhkv', kc * jnp.exp(b_end - b), vc)
        return state, o_inter + o_intra

    state0 = jnp.zeros((B, HGRN_HEADS, HGRN_HEAD_DIM, HGRN_HEAD_DIM), f32)
    _, o = lax.scan(step, state0, (qs, ks, vs, gs))
    o = o.transpose(1, 0, 3, 2, 4).reshape(B, S, HGRN_HEADS, HGRN_HEAD_DIM)
    gate = jax.nn.silu(g.astype(f32)).reshape(B, S, HGRN_HEADS, HGRN_HEAD_DIM)
    o = (rms_norm(o, norm_g) * gate).astype(h.dtype)
    return o.reshape(B, S, HGRN_HEADS * HGRN_HEAD_DIM) @ w_out


def dsa_sparse_attention(h, w_in, w_out, bias_table):
    B, S, _ = h.shape
    nb = S // Q_BLOCK
    n_sel = min(TOPK_MAX, S // 4)
    f32 = jnp.float32
    q, k, v, qi, ki, wi = split_cols(h @ w_in, DSA_SPLITS)
    q = q.reshape(B, nb, Q_BLOCK, N_KV_HEADS, GQA_GROUP, HEAD_DIM).transpose(1, 0, 2, 3, 4, 5)
    qi = qi.reshape(B, nb, Q_BLOCK, IDX_HEADS, IDX_DIM).transpose(1, 0, 2, 3, 4)
    wi = wi.reshape(B, nb, Q_BLOCK, IDX_HEADS).transpose(1, 0, 2, 3)
    k = k.reshape(B, S, N_KV_HEADS, HEAD_DIM)
    v = v.reshape(B, S, N_KV_HEADS, HEAD_DIM)
    kpos = jnp.arange(S)
    gather = jax.vmap(lambda t, idx: t[idx])

    def block(args):
        qb, qib, wib, start = args
        qpos = start + jnp.arange(Q_BLOCK)
        logits = jnp.einsum('bqhd,bsd->bqhs', qib, ki).astype(f32) * IDX_DIM ** -0.5
        score = jnp.einsum('bqh,bqhs->bqs', wib.astype(f32) * IDX_HEADS ** -0.5, jax.nn.relu(logits))
        score = jnp.where(qpos[:, None] >= kpos[None, :], score, -jnp.inf)
        _, idx = lax.top_k(score, n_sel)
        valid = idx <= qpos[None, :, None]
        kg, vg = gather(k, idx), gather(v, idx)
        s = jnp.einsum('bqhgd,bqkhd->bhgqk', qb, kg).astype(f32) * HEAD_DIM ** -0.5
        bias = rel_position_bias(qpos[None, :, None] - idx, bias_table).transpose(0, 3, 1, 2).reshape(
            B, N_KV_HEADS, GQA_GROUP, Q_BLOCK, n_sel)
        s = jnp.where(valid[:, None, None], s + bias, -jnp.inf)
        p = jax.nn.softmax(s, axis=-1)
        return jnp.einsum('bhgqk,bqkhd->bqhgd', p.astype(vg.dtype), vg)

    o = lax.map(block, (q, qi, wi, jnp.arange(nb) * Q_BLOCK))
    return o.transpose(1, 0, 2, 3, 4, 5).reshape(B, S, N_HEADS * HEAD_DIM) @ w_out


def conv_ffn(h, w_up, conv_w, w_down):
    S = h.shape[1]
    u = h @ w_up
    up = jnp.pad(u, ((0, 0), (CONV_WIDTH - 1, 0), (0, 0)))
    c = conv_w[0] * up[:, 0:S]
    for j in range(1, CONV_WIDTH):
        c = c + conv_w[j] * up[:, j:j + S]
    gate, val = jnp.split(c, 2, axis=-1)
    return (jax.nn.silu(gate) * val) @ w_down


def setup_inputs(seed: int = 0) -> dict:
    key = jax.random.key(seed)
    ks = jax.random.split(key, 20)
    f32 = jnp.float32

    def dense(k, shape, fan_in):
        return jax.random.normal(k, shape, f32) * fan_in ** -0.5

    return {
        'x': jax.random.normal(ks[0], (BATCH, SEQ, D_MODEL), f32),
        'rel_bias_table': 0.3 * jax.random.normal(ks[1], (NUM_BUCKETS, N_HEADS), f32),
        'hgrn_lb_logits': 0.5 * jax.random.normal(ks[2], (DEPTH, HGRN_HEADS * HGRN_HEAD_DIM), f32),
        'norm_g': 1.0 + 0.02 * jax.random.normal(ks[3], (DEPTH, 4, D_MODEL), f32),
        'ffn_w_up': dense(ks[4], (DEPTH, D_MODEL, 2 * D_FF), D_MODEL),
        'ffn_conv': dense(ks[5], (DEPTH, CONV_WIDTH, 2 * D_FF), CONV_WIDTH),
        'ffn_w_down': dense(ks[6], (DEPTH, D_FF, D_MODEL), D_FF),
        'swa_w_in': dense(ks[7], (N_SWA, D_MODEL, SWA_IN), D_MODEL),
        'swa_w_out': dense(ks[8], (N_SWA, N_HEADS * HEAD_DIM, D_MODEL), N_HEADS * HEAD_DIM),
        'swa_sinks': 0.5 * jax.random.normal(ks[9], (N_SWA, N_HEADS), f32),
        'diff_w_in': dense(ks[10], (N_DIFF, D_MODEL, DIFF_IN), D_MODEL),
        'diff_w_out': dense(ks[11], (N_DIFF, DIFF_HEADS * DIFF_V_DIM, D_MODEL), DIFF_HEADS * DIFF_V_DIM),
        'diff_lambda': 0.1 * jax.random.normal(ks[12], (N_DIFF, 4, HEAD_DIM), f32),
        'diff_subln_g': 1.0 + 0.02 * jax.random.normal(ks[13], (N_DIFF, DIFF_V_DIM), f32),
        'hgrn_w_in': dense(ks[14], (N_HGRN, D_MODEL, HGRN_IN), D_MODEL),
        'hgrn_w_out': dense(ks[15], (N_HGRN, HGRN_HEADS * HGRN_HEAD_DIM, D_MODEL), HGRN_HEADS * HGRN_HEAD_DIM),
        'hgrn_norm_g': 1.0 + 0.02 * jax.random.normal(ks[16], (N_HGRN, HGRN_HEAD_DIM), f32),
        'dsa_w_in': dense(ks[17], (N_DSA, D_MODEL, DSA_IN), D_MODEL),
        'dsa_w_out': dense(ks[18], (N_DSA, N_HEADS * HEAD_DIM, D_MODEL), N_HEADS * HEAD_DIM),
    }


def reference(x, rel_bias_table, hgrn_lb_logits, norm_g, ffn_w_up, ffn_conv, ffn_w_down,
              swa_w_in, swa_w_out, swa_sinks, diff_w_in, diff_w_out, diff_lambda, diff_subln_g,
              hgrn_w_in, hgrn_w_out, hgrn_norm_g, dsa_w_in, dsa_w_out):
    lb_soft = jax.nn.softmax(hgrn_lb_logits.astype(jnp.float32), axis=0)
    lb_all = jnp.cumsum(lb_soft, axis=0) - lb_soft[0]
    for i in range(DEPTH):
        kind, j = i % N_MIXERS, i // N_MIXERS
        h = rms_norm(x, norm_g[i, 0])
        if kind == 0:
            m = swa_sink_attention(h, swa_w_in[j], swa_w_out[j], swa_sinks[j], rel_bias_table)
        elif kind == 1:
            m = diff_attention(h, diff_w_in[j], diff_w_out[j], diff_lambda[j], diff_subln_g[j],
                               rel_bias_table, i)
        elif kind == 2:
            m = hgrn2_recurrence(h, hgrn_w_in[j], hgrn_w_out[j], lb_all[i], hgrn_norm_g[j])
        else:
            m = dsa_sparse_attention(h, dsa_w_in[j], dsa_w_out[j], rel_bias_table)
        x = x + rms_norm(m, norm_g[i, 1])
        h = rms_norm(x, norm_g[i, 2])
        x = x + rms_norm(conv_ffn(h, ffn_w_up[i], ffn_conv[i], ffn_w_down[i]), norm_g[i, 3])
    return x
```

```python
import math
from contextlib import ExitStack

import numpy as np
import concourse.bass as bass
import concourse.mybir as mybir
from concourse.bass_utils import run_bass_kernel_spmd

F32 = mybir.dt.float32
BF16 = mybir.dt.bfloat16
AF = mybir.ActivationFunctionType
ALU = mybir.AluOpType
AX = mybir.AxisListType

ENGS = ("pe", "act", "dve", "pool", "sp")
CENGS = ("pe", "act", "dve", "pool")

S = 2048
D = 2048
NT = 16
DFF = 5632
KFF = 44
EPS = 1e-6
MASKV = -30000.0
N_DSEM = 56


class Buf:
    __slots__ = ("name", "w", "r", "dsem")

    def __init__(self, name):
        self.name = name
        self.w = None
        self.r = {}
        self.dsem = None


class Prog:
    def __init__(self, nc, es):
        self.nc = nc
        self.ops = {e: [] for e in ENGS}
        self.sems = {}
        self.tot = {}
        self.seen = {e: {} for e in ENGS}
        for e in CENGS:
            self.sems[e] = es.enter_context(nc.semaphore("c_" + e))
            self.tot[e] = 0
        self.dfree = []
        for i in range(N_DSEM):
            k = "d%d" % i
            self.sems[k] = es.enter_context(nc.semaphore(k))
            self.tot[k] = 0
            self.dfree.append(k)
        self.dused = []
        self.phase_bufs = []
        self.nphase = 0

    def buf(self, name="b"):
        b = Buf(name)
        self.phase_bufs.append(b)
        return b

    def bufs(self, n, name="b"):
        return [self.buf(name + str(i)) for i in range(n)]

    def _dsem(self, b):
        if b.dsem is None:
            b.dsem = self.dfree.pop()
            self.dused.append(b.dsem)
        return b.dsem

    def _deps(self, eng, reads, writes):
        deps = {}

        def add(ev):
            if ev is None:
                return
            k, v = ev
            if v > deps.get(k, 0):
                deps[k] = v
        for b in reads:
            add(b.w)
        for b in writes:
            add(b.w)
            for k, v in b.r.items():
                add((k, v))
        waits = []
        for k, v in deps.items():
            if k == eng and eng == "pe":
                continue
            if k[0] == "d":
                v = self.tot[k]
            if v > self.seen[eng].get(k, 0):
                self.seen[eng][k] = v
                waits.append((k, v))
        return waits

    def op(self, eng, fn, reads=(), writes=()):
        waits = self._deps(eng, reads, writes)
        self.tot[eng] += 1
        seq = self.tot[eng]
        self.ops[eng].append((waits, fn, eng, 1))
        for b in reads:
            if b.r.get(eng, 0) < seq:
                b.r[eng] = seq
        for b in writes:
            b.w = (eng, seq)
            b.r = {}

    def dma(self, q, fn, reads=(), writes=(), owner=None):
        waits = self._deps(q, reads, writes)
        if owner is None:
            owner = writes[0] if writes else reads[0]
        k = self._dsem(owner)
        self.tot[k] += 16
        val = self.tot[k]
        self.ops[q].append((waits, fn, k, 16))
        for b in reads:
            b.r[k] = val
        for b in writes:
            b.w = (k, val)
            b.r = {}

    def end_phase(self):
        nc = self.nc
        keys = list(CENGS) + list(self.dused)
        for e in ENGS:
            waits = []
            for k in keys:
                if k == e:
                    continue
                v = self.tot[k]
                if v > self.seen[e].get(k, 0):
                    self.seen[e][k] = v
                    waits.append((k, v))
            self.ops[e].append((waits, None, None, 0))
        with nc.Block() as block:
            def mk(ename, attr):
                lst = self.ops[ename]

                def body(e):
                    for waits, fn, k, inc in lst:
                        for wk, wv in waits:
                            e.wait_ge(self.sems[wk], wv)
                        if fn is not None:
                            fn().then_inc(self.sems[k], inc)
                getattr(block, attr)(body)
            mk("sp", "sync")
            mk("pe", "tensor")
            mk("act", "scalar")
            mk("dve", "vector")
            mk("pool", "gpsimd")
        self.ops = {e: [] for e in ENGS}
        for b in self.phase_bufs:
            b.dsem = None
            b.w = None
            b.r = {}
        self.phase_bufs = []
        self.dfree.extend(self.dused)
        self.dused = []
        self.nphase += 1


class KB:
    def __init__(self, nc, P):
        self.nc = nc
        self.P = P
        self.uid = 0

    def sb(self, es, shape, dt, name="t"):
        self.uid += 1
        return es.enter_context(self.nc.sbuf_tensor("%s_%d" % (name, self.uid), list(shape), dt))

    def ps(self, es, shape, dt, name="p"):
        self.uid += 1
        return es.enter_context(self.nc.psum_tensor("%s_%d" % (name, self.uid), list(shape), dt))

    def mm(self, out, lhsT, rhs, start, stop, r, w):
        nc = self.nc
        self.P.op("pe", lambda: nc.tensor.matmul(out, lhsT=lhsT, rhs=rhs, start=start, stop=stop), r, w)

    def tr(self, out, in_, ident, r, w):
        nc = self.nc
        self.P.op("pe", lambda: nc.tensor.transpose(out, in_, ident), r, w)

    def act(self, out, in_, func, r, w, bias=None, scale=None, accum=None):
        nc = self.nc
        kw = {}
        if bias is not None:
            kw["bias"] = bias
        if scale is not None:
            kw["scale"] = scale
        if accum is not None:
            kw["accum_out"] = accum
        self.P.op("act", lambda: nc.scalar.activation(out=out, in_=in_, func=func, **kw), r, w)

    def ts(self, eng, out, in0, s1, op0, r, w, s2=None, op1=None):
        e = self.nc.vector if eng == "dve" else self.nc.gpsimd
        if op1 is None:
            self.P.op(eng, lambda: e.tensor_scalar(out=out, in0=in0, scalar1=s1, scalar2=None, op0=op0), r, w)
        else:
            self.P.op(eng, lambda: e.tensor_scalar(out=out, in0=in0, scalar1=s1, scalar2=s2, op0=op0, op1=op1), r, w)

    def tt(self, eng, out, in0, in1, op, r, w):
        e = self.nc.vector if eng == "dve" else self.nc.gpsimd
        self.P.op(eng, lambda: e.tensor_tensor(out=out, in0=in0, in1=in1, op=op), r, w)

    def stt(self, out, in0, scalar, in1, op0, op1, r, w):
        nc = self.nc
        self.P.op("dve", lambda: nc.vector.scalar_tensor_tensor(out=out, in0=in0, scalar=scalar, in1=in1, op0=op0, op1=op1), r, w)

    def cp(self, eng, out, in_, r, w):
        nc = self.nc
        if eng == "act":
            self.P.op("act", lambda: nc.scalar.copy(out=out, in_=in_), r, w)
        elif eng == "dve":
            self.P.op("dve", lambda: nc.vector.tensor_copy(out=out, in_=in_), r, w)
        else:
            self.P.op("pool", lambda: nc.gpsimd.tensor_copy(out=out, in_=in_), r, w)

    def recip(self, out, in_, r, w):
        nc = self.nc
        self.P.op("dve", lambda: nc.vector.reciprocal(out=out, in_=in_), r, w)

    def memset(self, eng, ap, val, w):
        e = self.nc.vector if eng == "dve" else self.nc.gpsimd
        self.P.op(eng, lambda: e.memset(ap, val), (), w)

    def ld(self, out, in_, r, w, q="sp", owner=None):
        nc = self.nc
        if q == "sp":
            self.P.dma("sp", lambda: nc.sync.dma_start(out=out, in_=in_), r, w, owner)
        else:
            self.P.dma("pool", lambda: nc.gpsimd.dma_start(out=out, in_=in_), r, w, owner)

    def rstd(self, es_tiles, ss, r, w):
        self.ts("dve", ss, ss, 1.0 / D, ALU.mult, r, w, s2=EPS, op1=ALU.add)
        self.act(ss, ss, AF.Sqrt, r, w)
        self.recip(ss, ss, r, w)


def phase_rn(kb, x_src, x_dst, m_src, gpost, gpre, hT, BhT, ident_d):
    nc, P = kb.nc, kb.P
    with ExitStack() as es:
        xt = [kb.sb(es, [128, D], F32, "xt") for _ in range(2)]
        Bxt = P.bufs(2, "xt")
        junk = kb.sb(es, [128, D], BF16, "junk")
        Bjunk = P.buf("junk")
        st = [kb.sb(es, [128, 2], F32, "st") for _ in range(2)]
        Bst = P.bufs(2, "st")
        if m_src is not None:
            mt = [kb.sb(es, [128, D], F32, "mt") for _ in range(2)]
            Bmt = P.bufs(2, "mt")
            gp = kb.sb(es, [128, D], F32, "gp")
            Bgp = P.buf("gp")
            kb.ld(gp[:], gpost.partition_broadcast(128), (), [Bgp])
        if gpre is not None:
            gq = kb.sb(es, [128, D], F32, "gq")
            Bgq = P.buf("gq")
            kb.ld(gq[:], gpre.partition_broadcast(128), (), [Bgq])
            hb = [kb.sb(es, [128, D], BF16, "hb") for _ in range(2)]
            Bhb = P.bufs(2, "hb")
            ident = kb.sb(es, [128, 128], BF16, "ident")
            Bid = P.buf("ident")
            kb.ld(ident[:], ident_d, (), [Bid], q="pool")
            pT = [kb.ps(es, [128, 1024], BF16, "pT") for _ in range(2)]
            BpT = P.bufs(2, "pT")
        Bxd = P.buf("xdst")
        def load_t(t_):
            if t_ >= NT:
                return
            rows_ = slice(t_ * 128, (t_ + 1) * 128)
            kb.ld(xt[t_ % 2][:], x_src[rows_, :], (), [Bxt[t_ % 2]])
            if m_src is not None:
                kb.ld(mt[t_ % 2][:], m_src[rows_, :], (), [Bmt[t_ % 2]])
        load_t(0)
        for t in range(NT):
            s = t % 2
            rows = slice(t * 128, (t + 1) * 128)
            load_t(t + 1)
            if m_src is not None:
                kb.act(junk[:], mt[s][:], AF.Square, [Bmt[s]], [Bjunk, Bst[s]], accum=st[s][:, 0:1])
                kb.rstd(None, st[s][:, 0:1], [Bst[s]], [Bst[s]])
                kb.stt(mt[s][:], mt[s][:], st[s][:, 0:1], gp[:], ALU.mult, ALU.mult, [Bmt[s], Bst[s], Bgp], [Bmt[s]])
                kb.tt("pool", xt[s][:], xt[s][:], mt[s][:], ALU.add, [Bxt[s], Bmt[s]], [Bxt[s]])
                kb.ld(x_dst[rows, :], xt[s][:], [Bxt[s]], (), owner=Bxt[s])
            if gpre is not None:
                kb.act(junk[:], xt[s][:], AF.Square, [Bxt[s]], [Bjunk, Bst[s]], accum=st[s][:, 1:2])
                kb.rstd(None, st[s][:, 1:2], [Bst[s]], [Bst[s]])
                kb.stt(hb[s][:], xt[s][:], st[s][:, 1:2], gq[:], ALU.mult, ALU.mult, [Bxt[s], Bst[s], Bgq], [Bhb[s]])
                for half in range(2):
                    for j in range(8):
                        i = half * 8 + j
                        kb.tr(pT[half][:, j * 128:(j + 1) * 128], hb[s][:, i * 128:(i + 1) * 128], ident[:], [Bhb[s], Bid], [BpT[half]])
                    src = pT[half][:].rearrange("p (j q) -> p j q", j=8)
                    dst = hT[:, half * 8:(half + 1) * 8, t * 128:(t + 1) * 128]
                    kb.cp("act" if half == 0 else "dve", dst, src, [BpT[half]], [BhT])
        P.end_phase()


def phase_proj_fm(kb, hT, BhT, w_d, coltiles, epilogue, epi_setup=None):
    nc, P = kb.nc, kb.P
    wv = w_d.rearrange("(kc p) n -> p kc n", p=128)
    with ExitStack() as es:
        NSL = 3
        wsl = [kb.sb(es, [128, 16, 256], BF16, "wsl") for _ in range(NSL)]
        Bw = P.bufs(NSL, "wsl")
        pb = [kb.ps(es, [128, 2048], F32, "pb") for _ in range(2)]
        Bpb = P.bufs(2, "pb")
        ctx = epi_setup(es) if epi_setup is not None else None
        slabs = []
        i = 0
        while i < len(coltiles):
            c0, w0 = coltiles[i]
            if i + 1 < len(coltiles) and coltiles[i + 1][0] == c0 + w0 and w0 == 128:
                slabs.append([coltiles[i], coltiles[i + 1]])
                i += 2
            else:
                slabs.append([coltiles[i]])
                i += 1
        ti = 0

        def load_slab(si):
            if si >= len(slabs):
                return
            s_ = si % NSL
            c0_ = slabs[si][0][0]
            wtot = sum(w for _, w in slabs[si])
            kb.ld(wsl[s_][:, :, 0:wtot], wv[:, :, c0_:c0_ + wtot], (), [Bw[s_]], q="pool")
        load_slab(0)
        load_slab(1)
        for si, sl in enumerate(slabs):
            s = si % NSL
            load_slab(si + 2)
            off = 0
            for (cc, w) in sl:
                pi = ti % 2
                for g in range(4):
                    for k in range(16):
                        kb.mm(pb[pi][0:w, g * 512:(g + 1) * 512], wsl[s][:, k, off:off + w], hT[:, k, g * 512:(g + 1) * 512],
                              k == 0, k == 15, [Bw[s], BhT], [Bpb[pi]])
                epilogue(ctx, ti, cc, w, pb[pi], Bpb[pi])
                off += w
                ti += 1
        P.end_phase()


def phase_proj_tok(kb, hT, BhT, w_d, colgroups, epilogue, epi_setup=None):
    nc, P = kb.nc, kb.P
    wv = w_d.rearrange("(kc p) n -> p kc n", p=128)
    with ExitStack() as es:
        wg = [kb.sb(es, [128, 16, 512], BF16, "wg") for _ in range(2)]
        Bwg = P.bufs(2, "wg")
        pb = [kb.ps(es, [128, 512], F32, "pk") for _ in range(4)]
        Bpb = P.bufs(4, "pk")
        ctx = epi_setup(es) if epi_setup is not None else None
        n = 0

        def load_g(gi):
            if gi >= len(colgroups):
                return
            c0_, w_ = colgroups[gi]
            kb.ld(wg[gi % 2][:, :, 0:w_], wv[:, :, c0_:c0_ + w_], (), [Bwg[gi % 2]], q="pool")
        load_g(0)
        for gi, (c0, w) in enumerate(colgroups):
            s = gi % 2
            load_g(gi + 1)
            for t in range(NT):
                pi = n % 4
                n += 1
                for k in range(16):
                    kb.mm(pb[pi][:, 0:w], hT[:, k, t * 128:(t + 1) * 128], wg[s][:, k, 0:w], k == 0, k == 15, [BhT, Bwg[s]], [Bpb[pi]])
                epilogue(ctx, gi, t, c0, w, pb[pi], Bpb[pi])
        P.end_phase()


def phase_up(kb, hT, BhT, w_up, conv_d, act_d):
    nc, P = kb.nc, kb.P
    wv = w_up.rearrange("(kc p) n -> p kc n", p=128)
    with ExitStack() as es:
        NSL = 3
        wsl = [kb.sb(es, [128, 16, 512], BF16, "wup") for _ in range(NSL)]
        Bw = P.bufs(NSL, "wup")
        cw = kb.sb(es, [128, 88, 3], F32, "cw")
        Bcw = P.buf("cw")
        kb.ld(cw[:], conv_d, (), [Bcw])
        psg = kb.ps(es, [128, 2048], F32, "psg")
        psv = kb.ps(es, [128, 2048], F32, "psv")
        Bpsg, Bpsv = P.buf("psg"), P.buf("psv")
        ub = [kb.sb(es, [128, 2050], F32, "ub") for _ in range(4)]
        Bub = P.bufs(4, "ub")
        cb = [kb.sb(es, [128, 2048], F32, "cb") for _ in range(4)]
        Bcb = P.bufs(4, "cb")
        ob = [kb.sb(es, [128, 2048], BF16, "ob") for _ in range(2)]
        Bob = P.bufs(2, "ob")
        Bact = P.buf("actd")
        for i in range(4):
            kb.memset("pool", ub[i][:, 0:2], 0.0, [Bub[i]])
        def load_up(sl_):
            if sl_ >= KFF // 2:
                return
            s_ = sl_ % NSL
            c_ = sl_ * 2
            kb.ld(wsl[s_][:, :, 0:256], wv[:, :, c_ * 128:c_ * 128 + 256], (), [Bw[s_]], q="pool")
            kb.ld(wsl[s_][:, :, 256:512], wv[:, :, DFF + c_ * 128:DFF + c_ * 128 + 256], (), [Bw[s_]], q="pool")
        load_up(0)
        load_up(1)
        for c in range(KFF):
            sl = c // 2
            s = sl % NSL
            if c % 2 == 0:
                load_up(sl + 2)
            par = c % 2
            for part, (pp, Bpp) in enumerate(((psg, Bpsg), (psv, Bpsv))):
                off = part * 256 + par * 128
                for g in range(4):
                    for k in range(16):
                        kb.mm(pp[:, g * 512:(g + 1) * 512], wsl[s][:, k, off:off + 128], hT[:, k, g * 512:(g + 1) * 512],
                              k == 0, k == 15, [Bw[s], BhT], [Bpp])
                u = par * 2 + part
                fi = part * KFF + c
                kb.cp("act", ub[u][:, 2:2050], pp[:], [Bpp], [Bub[u]])
                kb.ts("pool", cb[u][:], ub[u][:, 0:2048], cw[:, fi, 0:1], ALU.mult, [Bub[u], Bcw], [Bcb[u]])
                kb.stt(cb[u][:], ub[u][:, 1:2049], cw[:, fi, 1:2], cb[u][:], ALU.mult, ALU.add, [Bub[u], Bcw, Bcb[u]], [Bcb[u]])
                kb.stt(cb[u][:], ub[u][:, 2:2050], cw[:, fi, 2:3], cb[u][:], ALU.mult, ALU.add, [Bub[u], Bcw, Bcb[u]], [Bcb[u]])
                if part == 0:
                    kb.act(cb[u][:], cb[u][:], AF.Silu, [Bcb[u]], [Bcb[u]])
            ug, uv = par * 2, par * 2 + 1
            kb.tt("pool", ob[par][:], cb[ug][:], cb[uv][:], ALU.mult, [Bcb[ug], Bcb[uv]], [Bob[par]])
            kb.ld(act_d[c], ob[par][:], [Bob[par]], (), owner=Bob[par])
        P.end_phase()


def phase_down(kb, w_d, KC, act_d, m_d):
    nc, P = kb.nc, kb.P
    wv = w_d.rearrange("(kc p) n -> p kc n", p=128)
    av = act_d.rearrange("c p s -> p c s")
    TG = 256
    with ExitStack() as es:
        wres = [kb.sb(es, [128, KC, 512], BF16, "wres") for _ in range(2)]
        Bwr = [P.bufs(4, "wr%d_" % i) for i in range(2)]
        slab = [kb.sb(es, [128, KC, TG], BF16, "slab") for _ in range(2)]
        Bsl = P.bufs(2, "slab")
        pb = [kb.ps(es, [128, 512], F32, "pd") for _ in range(4)]
        Bpb = P.bufs(4, "pd")
        ot = [kb.sb(es, [128, 512], F32, "ot") for _ in range(3)]
        Bot = P.bufs(3, "ot")
        Bm = P.buf("md")
        kq = [(i * KC) // 4 for i in range(5)]
        n = 0
        ns = 0
        NTG = S // TG

        def load_w(cg_):
            if cg_ >= 4:
                return
            for pq_ in range(4):
                kb.ld(wres[cg_ % 2][:, kq[pq_]:kq[pq_ + 1], :], wv[:, kq[pq_]:kq[pq_ + 1], cg_ * 512:(cg_ + 1) * 512], (),
                      [Bwr[cg_ % 2][pq_]], q="pool")

        def load_slab(i_):
            if i_ >= 4 * NTG:
                return
            tg_ = i_ % NTG
            kb.ld(slab[i_ % 2][:], av[:, 0:KC, tg_ * TG:(tg_ + 1) * TG], (), [Bsl[i_ % 2]])
        load_w(0)
        load_slab(0)
        for cg in range(4):
            ws = cg % 2
            load_w(cg + 1)
            for tg in range(NTG):
                ss = ns % 2
                ns += 1
                load_slab(ns)
                for tt_ in range(TG // 128):
                    pi = n % 4
                    oi = n % 3
                    n += 1
                    for c in range(KC):
                        pq = 0
                        while c >= kq[pq + 1]:
                            pq += 1
                        kb.mm(pb[pi][:], slab[ss][:, c, tt_ * 128:(tt_ + 1) * 128], wres[ws][:, c, :], c == 0, c == KC - 1,
                              [Bsl[ss], Bwr[ws][pq]], [Bpb[pi]])
                    kb.cp("act" if n % 2 == 0 else "dve", ot[oi][:], pb[pi][:], [Bpb[pi]], [Bot[oi]])
                    r0 = tg * TG + tt_ * 128
                    kb.ld(m_d[r0:r0 + 128, cg * 512:(cg + 1) * 512], ot[oi][:], [Bot[oi]], (), owner=Bot[oi])
        P.end_phase()


def epi_store_setup(kb, n=2):
    def setup(es):
        P = kb.P
        return {"t": [kb.sb(es, [128, 2048], BF16, "eo") for _ in range(n)], "B": P.bufs(n, "eo"), "Bd": P.buf("qkvd"), "n": 0}
    return setup


def epi_store_fm(kb, qkv_d, tile_of):
    def epi(ctx, ti, c0, w, pb, Bpb):
        i = ctx["n"] % len(ctx["t"])
        ctx["n"] += 1
        t, B = ctx["t"][i], ctx["B"][i]
        kb.cp("act" if ti % 2 == 0 else "dve", t[0:w, :], pb[0:w, :], [Bpb], [B])
        kb.ld(qkv_d[tile_of(ti), 0:w, :], t[0:w, :], [B], (), owner=B)
    return epi


def epi_tok_setup(kb, dt=BF16):
    def setup(es):
        P = kb.P
        return {"t": [kb.sb(es, [128, 512], dt, "et") for _ in range(3)], "B": P.bufs(3, "et"), "Bd": P.buf("vtokd"), "n": 0}
    return setup


def epi_store_tok(kb, vtok_d, col_of):
    def epi(ctx, gi, t, c0, w, pb, Bpb):
        i = ctx["n"] % 3
        ctx["n"] += 1
        tl, B = ctx["t"][i], ctx["B"][i]
        kb.cp("act" if ctx["n"] % 2 == 0 else "dve", tl[:, 0:w], pb[:, 0:w], [Bpb], [B])
        d0 = col_of(gi)
        kb.ld(vtok_d[t * 128:(t + 1) * 128, d0:d0 + w], tl[:, 0:w], [B], (), owner=B)
    return epi


def phase_swa(kb, qkv_d, vtok_d, act_d, bias_sw_d, i8_d, sink_d):
    nc, P = kb.nc, kb.P
    with ExitStack() as es:
        vt = kb.sb(es, [128, 16, 512], BF16, "vt")
        Bvt = P.buf("vt")
        kb.ld(vt[:], vtok_d[:, 0:512].rearrange("(t p) c -> p t c", p=128), (), [Bvt])
        bsw = kb.sb(es, [128, 32, 256], BF16, "bsw")
        Bbsw = P.buf("bsw")
        kb.ld(bsw[:], bias_sw_d.rearrange("p (h q) -> p h q", h=32), (), [Bbsw], q="pool")
        i8 = kb.sb(es, [128, 128], BF16, "i8")
        Bi8 = P.buf("i8")
        kb.ld(i8[:], i8_d, (), [Bi8], q="pool")
        ones64 = kb.sb(es, [128, 64], BF16, "ones64")
        Bon = P.buf("ones")
        kb.memset("dve", ones64[:], 1.0, [Bon])
        skf = kb.sb(es, [1, 4096], F32, "skf")
        skb = kb.sb(es, [1, 4096], BF16, "skb")
        Bsk = P.buf("sk")
        kb.ld(skf[:], sink_d, (), [Bsk])
        kb.act(skb[:], skf[:], AF.Exp, [Bsk], [Bsk])
        kh = [kb.sb(es, [64, 2048], BF16, "kh") for _ in range(2)]
        qh = [kb.sb(es, [64, 4, 2048], BF16, "qh") for _ in range(2)]
        Bkq = P.bufs(2, "kq")
        PT = [kb.sb(es, [128, 4, 256], BF16, "PT") for _ in range(3)]
        BPT = P.bufs(3, "PT")
        oT = [kb.sb(es, [64, 4, 2048], BF16, "oT") for _ in range(2)]
        BoT = P.bufs(2, "oT")
        rec = [kb.sb(es, [64, 512], F32, "rec") for _ in range(2)]
        Brec = P.bufs(2, "rec")
        pss = [kb.ps(es, [128, 1024], F32, "pss") for _ in range(2)]
        Bpss = P.bufs(2, "pss")
        pnum = [kb.ps(es, [64, 512], F32, "pnum") for _ in range(2)]
        Bpn = P.bufs(2, "pnum")
        pden = [kb.ps(es, [64, 512], F32, "pden") for _ in range(2)]
        Bpd = P.bufs(2, "pden")
        Bact = P.buf("actd")
        nj = 0
        nb = 0
        for kv in range(8):
            s = kv % 2
            kb.ld(kh[s][:], qkv_d[16 + kv // 2, (kv % 2) * 64:(kv % 2) * 64 + 64, :], (), [Bkq[s]])
            for g in range(4):
                hq = 4 * kv + g
                kb.ld(qh[s][:, g, :], qkv_d[hq // 2, (hq % 2) * 64:(hq % 2) * 64 + 64, :], (), [Bkq[s]])
            prev = None
            for j in range(16):
                nq = 256 if j < 15 else 128
                sl = nj % 2
                pt = nj % 3
                nj += 1
                for g in range(4):
                    o = pss[sl][:, g * 256:g * 256 + nq]
                    kb.mm(o, kh[s][:, j * 128:(j + 1) * 128], qh[s][:, g, j * 128:j * 128 + nq], True, False, [Bkq[s]], [Bpss[sl]])
                    kb.mm(o, i8[:], bsw[:, 4 * kv + g, 0:nq], False, True, [Bi8, Bbsw], [Bpss[sl]])
                src = pss[sl][:].rearrange("p (g q) -> p g q", g=4)[:, :, 0:nq]
                kb.act(PT[pt][:, :, 0:nq], src, AF.Exp, [Bpss[sl]], [BPT[pt]], scale=0.125)
                b = nb % 2
                nb += 1
                rds = [Bvt, BPT[pt]] + ([BPT[prev]] if prev is not None else [])
                kb.mm(pden[b][:], ones64[0:1, 0:64], skb[0:1, 4 * kv * 128:(4 * kv + 4) * 128], True, False, [Bon, Bsk], [Bpd[b]])
                if prev is not None:
                    kb.mm(pnum[b][:], vt[:, j - 1, kv * 64:(kv + 1) * 64], PT[prev][:, :, 128:256], True, False, rds, [Bpn[b]])
                    kb.mm(pden[b][:], ones64[:], PT[prev][:, :, 128:256], False, False, rds + [Bon], [Bpd[b]])
                kb.mm(pnum[b][:], vt[:, j, kv * 64:(kv + 1) * 64], PT[pt][:, :, 0:128], prev is None, True, rds, [Bpn[b]])
                kb.mm(pden[b][:], ones64[:], PT[pt][:, :, 0:128], False, True, rds + [Bon], [Bpd[b]])
                kb.recip(rec[b][:], pden[b][:], [Bpd[b]], [Brec[b]])
                kb.tt("dve", oT[s][:, :, j * 128:(j + 1) * 128], pnum[b][:].rearrange("p (g q) -> p g q", g=4),
                      rec[b][:].rearrange("p (g q) -> p g q", g=4), ALU.mult, [Bpn[b], Brec[b]], [BoT[s]])
                prev = pt
            for g in range(4):
                hq = 4 * kv + g
                kb.ld(act_d[hq // 2, (hq % 2) * 64:(hq % 2) * 64 + 64, :], oT[s][:, g, :], [BoT[s]], (), owner=BoT[s])
        P.end_phase()


def phase_diff(kb, qkv_d, vtok_d, act_d, biasn_d, i8_d, table_d, lam_d, subg_d, layer_idx):
    nc, P = kb.nc, kb.P
    lam_init = 0.8 - 0.6 * math.exp(-0.3 * layer_idx)
    with ExitStack() as es:
        bn = kb.sb(es, [128, 32, 256], BF16, "bn")
        Bbn = P.buf("bn")
        kb.ld(bn[:], biasn_d.rearrange("p (h q) -> p h q", h=32), (), [Bbn], q="pool")
        i8 = kb.sb(es, [128, 128], BF16, "i8")
        Bi8 = P.buf("i8")
        kb.ld(i8[:], i8_d, (), [Bi8], q="pool")
        ones = kb.sb(es, [128, 128], BF16, "ones")
        onesf = kb.sb(es, [1, 128], F32, "onesf")
        Bon = P.buf("ones")
        kb.memset("dve", ones[:], 1.0, [Bon])
        kb.memset("dve", onesf[:], 1.0, [Bon])
        c31 = kb.sb(es, [128, 32], F32, "c31")
        Bc31 = P.buf("c31")
        kb.ld(c31[:], table_d[31].partition_broadcast(128), (), [Bc31])
        lamt = kb.sb(es, [1, 256], F32, "lamt")
        lsm = kb.sb(es, [1, 136], F32, "lsm")
        Blam = P.buf("lam")
        kb.ld(lamt[:], lam_d.rearrange("(o r) c -> o (r c)", o=1), (), [Blam])
        kb.tt("dve", lsm[:, 0:64], lamt[:, 0:64], lamt[:, 64:128], ALU.mult, [Blam], [Blam])
        kb.tt("dve", lsm[:, 64:128], lamt[:, 128:192], lamt[:, 192:256], ALU.mult, [Blam], [Blam])
        P.op("dve", lambda: nc.vector.tensor_reduce(out=lsm[:, 128:130], in_=lsm[:, 0:128].rearrange("p (a b) -> p a b", a=2), axis=AX.X, op=ALU.add), [Blam], [Blam])
        kb.act(lsm[:, 130:132], lsm[:, 128:130], AF.Exp, [Blam], [Blam])
        kb.tt("dve", lsm[:, 132:133], lsm[:, 130:131], lsm[:, 131:132], ALU.subtract, [Blam], [Blam])
        kb.ts("dve", lsm[:, 132:133], lsm[:, 132:133], lam_init, ALU.add, [Blam], [Blam])
        kb.cp("dve", lsm[:, 133:134], lsm[:, 132:133], [Blam], [Blam])
        pl = kb.ps(es, [128, 512], F32, "pl")
        Bpl = P.buf("pl")
        kb.mm(pl[:, 0:2], onesf[0:1, :], lsm[0:1, 132:134], True, True, [Bon, Blam], [Bpl])
        neglam = kb.sb(es, [128, 2], F32, "neglam")
        Bnl = P.buf("neglam")
        kb.ts("dve", neglam[:], pl[:, 0:2], -1.0, ALU.mult, [Bpl], [Bnl])
        gsc = kb.sb(es, [128, 1], F32, "gsc")
        Bgsc = P.buf("gsc")
        kb.ld(gsc[:], subg_d.rearrange("o e -> e o"), (), [Bgsc])
        kb.ts("dve", gsc[:], gsc[:], 1.0 - lam_init, ALU.mult, [Bgsc], [Bgsc])
        qh = [kb.sb(es, [128, 2048], BF16, "qh") for _ in range(2)]
        kh = [kb.sb(es, [128, 2048], BF16, "kh") for _ in range(2)]
        vh = [kb.sb(es, [128, 16, 128], BF16, "vh") for _ in range(2)]
        Bin = P.bufs(2, "qkv")
        PT = [kb.sb(es, [128, 512], BF16, "PT") for _ in range(3)]
        BPT = P.bufs(3, "PT")
        oTh = [kb.sb(es, [128, 2048], BF16, "oTh") for _ in range(2)]
        BoT = P.bufs(2, "oTh")
        r1 = kb.sb(es, [128, 512], F32, "r1")
        r2 = kb.sb(es, [128, 512], F32, "r2")
        t1 = kb.sb(es, [128, 512], F32, "t1")
        t2 = kb.sb(es, [128, 512], F32, "t2")
        sq = kb.sb(es, [128, 512], BF16, "sq")
        rs = kb.sb(es, [128, 512], F32, "rs")
        Bw = P.buf("work")
        Bsq = P.buf("sq")
        Brs = P.buf("rs")
        pss = [pl, kb.ps(es, [128, 512], F32, "pss")]
        Bpss = [Bpl, P.buf("pss")]
        pnum = [kb.ps(es, [128, 512], F32, "pnum") for _ in range(2)]
        pden = [kb.ps(es, [128, 512], F32, "pden") for _ in range(2)]
        Bpn = P.bufs(2, "pn")
        Bpd = P.bufs(2, "pd")
        psq = kb.ps(es, [128, 512], F32, "psq")
        Bpsq = P.buf("psq")

        def load_h(h_):
            if h_ >= 16:
                return
            s_ = h_ % 2
            kb.ld(qh[s_][:], qkv_d[h_], (), [Bin[s_]])
            kb.ld(kh[s_][:], qkv_d[16 + h_], (), [Bin[s_]])
            kb.ld(vh[s_][:], vtok_d[:, h_ * 128:(h_ + 1) * 128].rearrange("(t p) e -> p t e", p=128), (), [Bin[s_]])
        load_h(0)
        nj = 0
        for h in range(16):
            s = h % 2
            load_h(h + 1)
            for c in range(4):
                for m in range(2):
                    pr = slice(m * 64, (m + 1) * 64)
                    mp = 2 * h + m
                    jl = 4 * c + 3
                    for j in range(jl + 1):
                        lo = max(j, 4 * c)
                        N = (4 * c + 4 - lo) * 128
                        q0 = lo * 128
                        off = (lo - 4 * c) * 128
                        if j >= 4 * c:
                            nn, boff = min(N, 256), 0
                        elif j == 4 * c - 1:
                            nn, boff = 128, 128
                        else:
                            nn, boff = 0, 0
                        sl = nj % 2
                        pt = nj % 3
                        nj += 1
                        kb.mm(pss[sl][:, 0:N], kh[s][pr, j * 128:(j + 1) * 128], qh[s][pr, q0:q0 + N], True, nn == 0, [Bin[s]], [Bpss[sl]])
                        if nn:
                            kb.mm(pss[sl][:, 0:nn], i8[:], bn[:, mp, boff:boff + nn], False, True, [Bi8, Bbn], [Bpss[sl]])
                            kb.act(PT[pt][:, 0:nn], pss[sl][:, 0:nn], AF.Exp, [Bpss[sl]], [BPT[pt]], scale=0.125)
                        if N > nn:
                            kb.act(PT[pt][:, nn:N], pss[sl][:, nn:N], AF.Exp, [Bpss[sl], Bc31], [BPT[pt]], scale=0.125, bias=c31[:, mp:mp + 1])
                        kb.mm(pnum[m][:, off:512], vh[s][:, j, :], PT[pt][:, 0:N], j == 0, j == jl, [Bin[s], BPT[pt]], [Bpn[m]])
                        kb.mm(pden[m][:, off:512], ones[:], PT[pt][:, 0:N], j == 0, j == jl, [Bon, BPT[pt]], [Bpd[m]])
                kb.recip(r1[:], pden[0][:], [Bpd[0]], [Bw])
                kb.recip(r2[:], pden[1][:], [Bpd[1]], [Bw])
                kb.tt("dve", t1[:], pnum[0][:], r1[:], ALU.mult, [Bpn[0], Bw], [Bw])
                kb.tt("dve", t2[:], pnum[1][:], r2[:], ALU.mult, [Bpn[1], Bw], [Bw])
                kb.stt(t1[:], t2[:], neglam[:, 0:1], t1[:], ALU.mult, ALU.add, [Bw, Bnl], [Bw])
                kb.act(sq[:], t1[:], AF.Square, [Bw], [Bsq])
                kb.mm(psq[:], ones[:], sq[:], True, True, [Bon, Bsq], [Bpsq])
                kb.ts("dve", rs[:], psq[:], 1.0 / 128, ALU.mult, [Bpsq], [Brs], s2=EPS, op1=ALU.add)
                kb.act(rs[:], rs[:], AF.Sqrt, [Brs], [Brs])
                kb.recip(rs[:], rs[:], [Brs], [Brs])
                kb.stt(oTh[s][:, c * 512:(c + 1) * 512], t1[:], gsc[:, 0:1], rs[:], ALU.mult, ALU.mult, [Bw, Bgsc, Brs], [BoT[s]])
            kb.ld(act_d[h], oTh[s][:], [BoT[s]], (), owner=BoT[s])
        P.end_phase()


def phase_dsa_idx(kb, qkv_d, wi_d, mask_d, cmask_d, ident_d):
    nc, P = kb.nc, kb.P
    BIG = 1.0e30
    with ExitStack() as es:
        kiT = kb.sb(es, [64, 2048], BF16, "kiT")
        qi = kb.sb(es, [64, 16, 2048], BF16, "qi")
        Bqk = P.buf("qiki")
        kb.ld(kiT[:], qkv_d[28, 0:64, :], (), [Bqk])
        for h in range(16):
            kb.ld(qi[:, h, :], qkv_d[20 + h // 2, (h % 2) * 64:(h % 2) * 64 + 64, :], (), [Bqk])
        cm = kb.sb(es, [128, 128], F32, "cm")
        Bcm = P.buf("cm")
        kb.ld(cm[:], cmask_d, (), [Bcm])
        ident = kb.sb(es, [128, 128], BF16, "ident")
        Bid = P.buf("ident")
        kb.ld(ident[:], ident_d, (), [Bid], q="pool")
        zt = kb.sb(es, [128, 256], BF16, "zt")
        Bzt = P.buf("zt")
        kb.memset("pool", zt[:], 0.0, [Bzt])
        kb.ld(mask_d[0, :, 0:256], zt[:], [Bzt], (), owner=Bzt)
        kb.ld(mask_d[1, :, 0:256], zt[:], [Bzt], (), owner=Bzt)
        wi = [kb.sb(es, [128, 16], F32, "wi") for _ in range(2)]
        Bwi = P.bufs(2, "wi")
        acc = [kb.sb(es, [128, 2048], F32, "acc") for _ in range(2)]
        Bacc = P.bufs(2, "acc")
        Rb = [kb.sb(es, [128, 1024], F32, "Rb") for _ in range(2)]
        BRb = P.bufs(2, "Rb")
        work = kb.sb(es, [128, 2048], F32, "work")
        Bwork = P.buf("work")
        m8 = kb.sb(es, [128, 8], F32, "m8")
        Bm8 = P.buf("m8")
        Mb = [kb.sb(es, [128, 2048], BF16, "Mb") for _ in range(2)]
        BMb = P.bufs(2, "Mb")
        mT = [kb.sb(es, [128, 8, 128], BF16, "mT") for _ in range(2)]
        BmT = P.bufs(2, "mT")
        pl = [kb.ps(es, [128, 1024], F32, "pl") for _ in range(2)]
        Bpl = P.bufs(2, "pl")
        pT = [kb.ps(es, [128, 1024], BF16, "pT") for _ in range(2)]
        BpT = P.bufs(2, "pT")
        nl = 0
        nT = 0
        for qt in range(2, 16):
            a = qt % 2
            ns = (qt + 1) * 128
            kb.ld(wi[a][:], wi_d[qt * 128:(qt + 1) * 128, :], (), [Bwi[a]])
            for h in range(16):
                for half in range((ns + 1023) // 1024):
                    n = min(1024, ns - half * 1024)
                    sl = nl % 2
                    nl += 1
                    for sc in range((n + 511) // 512):
                        n2 = min(512, n - sc * 512)
                        c0 = half * 1024 + sc * 512
                        kb.mm(pl[sl][:, sc * 512:sc * 512 + n2], qi[:, h, qt * 128:(qt + 1) * 128], kiT[:, c0:c0 + n2], True, True, [Bqk], [Bpl[sl]])
                    kb.act(Rb[sl][:, 0:n], pl[sl][:, 0:n], AF.Relu, [Bpl[sl]], [BRb[sl]])
                    av = acc[a][:, half * 1024:half * 1024 + n]
                    if h == 0:
                        kb.ts("dve", av, Rb[sl][:, 0:n], wi[a][:, 0:1], ALU.mult, [BRb[sl], Bwi[a]], [Bacc[a]])
                    else:
                        kb.stt(av, Rb[sl][:, 0:n], wi[a][:, h:h + 1], av, ALU.mult, ALU.add, [BRb[sl], Bwi[a], Bacc[a]], [Bacc[a]])
            dg = acc[a][:, qt * 128:(qt + 1) * 128]
            kb.tt("dve", dg, dg, cm[:], ALU.add, [Bacc[a], Bcm], [Bacc[a]])
            src, Bsrc = acc[a], Bacc[a]
            for r in range(32):
                sv = src[:, 0:ns]
                P.op("dve", (lambda sv=sv: nc.vector.max(out=m8[:], in_=sv)), [Bsrc], [Bm8])
                if r < 31:
                    wv_ = work[:, 0:ns]
                    P.op("dve", (lambda sv=sv, wv_=wv_: nc.vector.match_replace(out=wv_, in_to_replace=m8[:], in_values=sv, imm_value=-BIG)), [Bsrc, Bm8], [Bwork])
                    src, Bsrc = work, Bwork
            kb.ts("dve", Mb[a][:, 0:ns], acc[a][:, 0:ns], m8[:, 7:8], ALU.is_ge, [Bacc[a], Bm8], [BMb[a]])
            for j0 in range(0, qt + 1, 8):
                njj = min(8, qt + 1 - j0)
                b = nT % 2
                nT += 1
                for i in range(njj):
                    j = j0 + i
                    kb.tr(pT[b][:, i * 128:(i + 1) * 128], Mb[a][:, j * 128:(j + 1) * 128], ident[:], [BMb[a], Bid], [BpT[b]])
                srcv = pT[b][:].rearrange("p (j q) -> p j q", j=8)[:, 0:njj, :]
                kb.ts("dve" if b == 0 else "dve", mT[b][:, 0:njj, :], srcv, -1.0, ALU.add, [BpT[b]], [BmT[b]], s2=-MASKV, op1=ALU.mult)
                kb.ld(mask_d[j0:j0 + njj, :, qt * 128:(qt + 1) * 128].rearrange("j p q -> p j q"), mT[b][:, 0:njj, :], [BmT[b]], (), owner=BmT[b])
        P.end_phase()


def phase_dsa_attn(kb, qkv_d, vtok_d, act_d, mask_d, biasn_d, i8_d, table_d):
    nc, P = kb.nc, kb.P
    with ExitStack() as es:
        bn = kb.sb(es, [128, 32, 256], BF16, "bn")
        Bbn = P.buf("bn")
        kb.ld(bn[:], biasn_d.rearrange("p (h q) -> p h q", h=32), (), [Bbn], q="pool")
        i8 = kb.sb(es, [128, 128], BF16, "i8")
        Bi8 = P.buf("i8")
        kb.ld(i8[:], i8_d, (), [Bi8], q="pool")
        ones = kb.sb(es, [128, 64], BF16, "ones")
        Bon = P.buf("ones")
        kb.memset("dve", ones[:], 1.0, [Bon])
        c31 = kb.sb(es, [128, 32], F32, "c31")
        Bc31 = P.buf("c31")
        kb.ld(c31[:], table_d[31].partition_broadcast(128), (), [Bc31])
        vt = kb.sb(es, [128, 16, 512], BF16, "vt")
        Bvt = P.buf("vt")
        kb.ld(vt[:], vtok_d[:, 0:512].rearrange("(t p) c -> p t c", p=128), (), [Bvt])
        mk = kb.sb(es, [128, 16, 2048], BF16, "mk")
        Bmk = P.buf("mk")
        for j in range(16):
            kb.ld(mk[:, j, j * 128:2048], mask_d[j, :, j * 128:2048], (), [Bmk])
        kh = [kb.sb(es, [64, 2048], BF16, "kh") for _ in range(2)]
        Bkh = P.bufs(2, "kh")
        qh = [kb.sb(es, [64, 2048], BF16, "qh") for _ in range(2)]
        Bqh = P.bufs(2, "qh")
        PT = [kb.sb(es, [128, 512], BF16, "PT") for _ in range(3)]
        BPT = P.bufs(3, "PT")
        oTh = [kb.sb(es, [64, 2048], BF16, "oTh") for _ in range(2)]
        BoT = P.bufs(2, "oTh")
        r1 = [kb.sb(es, [64, 512], F32, "r1") for _ in range(2)]
        Br1 = P.bufs(2, "r1")
        pss = [kb.ps(es, [128, 512], F32, "pss") for _ in range(2)]
        Bpss = P.bufs(2, "pss")
        pnum = [kb.ps(es, [64, 512], F32, "pnum") for _ in range(2)]
        pden = [kb.ps(es, [64, 512], F32, "pden") for _ in range(2)]
        Bpn = P.bufs(2, "pn")
        Bpd = P.bufs(2, "pd")

        def load_q(hq_):
            if hq_ >= 32:
                return
            kb.ld(qh[hq_ % 2][:], qkv_d[hq_ // 2, (hq_ % 2) * 64:(hq_ % 2) * 64 + 64, :], (), [Bqh[hq_ % 2]])

        def load_k(kv_):
            if kv_ >= 8:
                return
            kb.ld(kh[kv_ % 2][:], qkv_d[16 + kv_ // 2, (kv_ % 2) * 64:(kv_ % 2) * 64 + 64, :], (), [Bkh[kv_ % 2]])
        load_k(0)
        load_q(0)
        nj = 0
        nb = 0
        for kv in range(8):
            ks = kv % 2
            load_k(kv + 1)
            for g in range(4):
                hq = 4 * kv + g
                s = hq % 2
                load_q(hq + 1)
                for c in range(4):
                    b = nb % 2
                    nb += 1
                    jl = 4 * c + 3
                    for j in range(jl + 1):
                        lo = max(j, 4 * c)
                        N = (4 * c + 4 - lo) * 128
                        q0 = lo * 128
                        off = (lo - 4 * c) * 128
                        if j >= 4 * c:
                            nn, boff = min(N, 256), 0
                        elif j == 4 * c - 1:
                            nn, boff = 128, 128
                        else:
                            nn, boff = 0, 0
                        sl = nj % 2
                        pt = nj % 3
                        nj += 1
                        kb.mm(pss[sl][:, 0:N], kh[ks][:, j * 128:(j + 1) * 128], qh[s][:, q0:q0 + N], True, False, [Bkh[ks], Bqh[s]], [Bpss[sl]])
                        if nn:
                            kb.mm(pss[sl][:, 0:nn], i8[:], bn[:, hq, boff:boff + nn], False, False, [Bi8, Bbn], [Bpss[sl]])
                        kb.mm(pss[sl][:, 0:N], i8[:], mk[:, j, q0:q0 + N], False, True, [Bi8, Bmk], [Bpss[sl]])
                        if nn:
                            kb.act(PT[pt][:, 0:nn], pss[sl][:, 0:nn], AF.Exp, [Bpss[sl]], [BPT[pt]], scale=0.125)
                        if N > nn:
                            kb.act(PT[pt][:, nn:N], pss[sl][:, nn:N], AF.Exp, [Bpss[sl], Bc31], [BPT[pt]], scale=0.125, bias=c31[:, hq:hq + 1])
                        kb.mm(pnum[b][:, off:512], vt[:, j, kv * 64:(kv + 1) * 64], PT[pt][:, 0:N], j == 0, j == jl, [Bvt, BPT[pt]], [Bpn[b]])
                        kb.mm(pden[b][:, off:512], ones[:], PT[pt][:, 0:N], j == 0, j == jl, [Bon, BPT[pt]], [Bpd[b]])
                    kb.recip(r1[b][:], pden[b][:], [Bpd[b]], [Br1[b]])
                    kb.tt("dve", oTh[s][:, c * 512:(c + 1) * 512], pnum[b][:], r1[b][:], ALU.mult, [Bpn[b], Br1[b]], [BoT[s]])
                kb.ld(act_d[hq // 2, (hq % 2) * 64:(hq % 2) * 64 + 64, :], oTh[s][:], [BoT[s]], (), owner=BoT[s])
        P.end_phase()


def hgrn_inproj(kb, hT, BhT, w_in, lbl_d, layer_idx, hq_d, hf_d, hk_d, hv_d, qkv_d):
    nc, P = kb.nc, kb.P
    tiles = [(i * 128, 128) for i in range(16)] + [(2048 + i * 128, 128) for i in range(16)] + [(6144 + i * 128, 128) for i in range(16)]

    def setup(es):
        c = {}
        lbl = kb.sb(es, [128, 16, 4], F32, "lbl")
        Bl = P.buf("lbl")
        kb.ld(lbl[:], lbl_d.rearrange("p (t l) -> p t l", l=4), (), [Bl])
        kb.act(lbl[:], lbl[:], AF.Exp, [Bl], [Bl])
        ssum = kb.sb(es, [128, 16], F32, "ssum")
        lb = kb.sb(es, [128, 16], F32, "lb")
        oml = kb.sb(es, [128, 16], F32, "oml")
        Blb = P.buf("lb")
        P.op("dve", lambda: nc.vector.tensor_reduce(out=ssum[:], in_=lbl[:], axis=AX.X, op=ALU.add), [Bl], [Blb])
        kb.recip(ssum[:], ssum[:], [Blb], [Blb])
        kb.cp("dve", lb[:], lbl[:, :, 1], [Bl, Blb], [Blb])
        for j in range(2, layer_idx + 1):
            kb.tt("dve", lb[:], lb[:], lbl[:, :, j], ALU.add, [Bl, Blb], [Blb])
        kb.tt("dve", lb[:], lb[:], ssum[:], ALU.mult, [Blb], [Blb])
        kb.ts("dve", oml[:], lb[:], -1.0, ALU.mult, [Blb], [Blb], s2=1.0, op1=ALU.add)
        c["lb"], c["oml"], c["Blb"] = lb, oml, Blb
        c["fo"] = [kb.sb(es, [128, 2048], F32, "fo") for _ in range(2)]
        c["Bfo"] = P.bufs(2, "fo")
        c["fk"] = [kb.sb(es, [128, 2048], F32, "fk") for _ in range(2)]
        c["Bfk"] = P.bufs(2, "fk")
        c["bo"] = [kb.sb(es, [128, 2048], BF16, "bo") for _ in range(2)]
        c["Bbo"] = P.bufs(2, "bo")
        return c

    def epi(c, ti, c0, w, pb, Bpb):
        kind, h = ti // 16, ti % 16
        a = ti % 2
        if kind == 0:
            kb.act(c["fo"][a][:], pb[:], AF.Silu, [Bpb], [c["Bfo"][a]])
            kb.ld(hq_d[h], c["fo"][a][:], [c["Bfo"][a]], (), owner=c["Bfo"][a])
        elif kind == 1:
            fo, Bfo, fk, Bfk = c["fo"][a], c["Bfo"][a], c["fk"][a], c["Bfk"][a]
            kb.act(fo[:], pb[:], AF.Sigmoid, [Bpb], [Bfo])
            kb.ts("dve", fo[:], fo[:], c["oml"][:, h:h + 1], ALU.mult, [Bfo, c["Blb"]], [Bfo], s2=c["lb"][:, h:h + 1], op1=ALU.add)
            kb.ts("dve", fk[:], fo[:], -1.0, ALU.mult, [Bfo], [Bfk], s2=1.0, op1=ALU.add)
            kb.ld(hk_d[h], fk[:], [Bfk], (), owner=Bfk)
            kb.act(fo[:], fo[:], AF.Ln, [Bfo], [Bfo])
            kb.ld(hf_d[h], fo[:], [Bfo], (), owner=Bfo)
        else:
            kb.act(c["bo"][a][:], pb[:], AF.Silu, [Bpb], [c["Bbo"][a]])
            kb.ld(qkv_d[h], c["bo"][a][:], [c["Bbo"][a]], (), owner=c["Bbo"][a])
    phase_proj_fm(kb, hT, BhT, w_in, tiles, epi, setup)
    phase_proj_tok(kb, hT, BhT, w_in, [(4096 + i * 512, 512) for i in range(4)],
                   epi_store_tok(kb, hv_d, lambda gi: gi * 512), epi_tok_setup(kb, F32))


def phase_hgrn(kb, hq_d, hf_d, hk_d, hv_d, qkv_d, act_d, ng_d, ident_d, tri_d):
    nc, P = kb.nc, kb.P
    with ExitStack() as es:
        identf = kb.sb(es, [128, 128], F32, "identf")
        tri = kb.sb(es, [64, 512], F32, "tri")
        ng = kb.sb(es, [128, 1], F32, "ng")
        Bc = P.buf("consts")
        kb.ld(identf[:], ident_d, (), [Bc])
        kb.ld(tri[:], tri_d, (), [Bc])
        kb.ld(ng[:], ng_d.rearrange("o e -> e o"), (), [Bc])
        ones = kb.sb(es, [128, 128], BF16, "ones")
        rmask = kb.sb(es, [128, 2048], F32, "rmask")
        Bon = P.buf("ones")
        kb.memset("dve", ones[:], 1.0, [Bon])
        kb.memset("dve", rmask[:], 1.0, [Bon])
        kb.memset("dve", rmask[:].rearrange("p (c t) -> p c t", t=64)[:, :, 0:1], 0.0, [Bon])
        A = kb.sb(es, [128, 2048], F32, "A")
        Bb = kb.sb(es, [128, 2048], F32, "B")
        C = kb.sb(es, [128, 2048], F32, "C")
        Dd = kb.sb(es, [128, 2048], F32, "Dd")
        E = kb.sb(es, [128, 2048], F32, "E")
        Fk = kb.sb(es, [128, 2048], F32, "F")
        BA, BB, BC, BD, BE, BF_ = P.buf("A"), P.buf("B"), P.buf("C"), P.buf("D"), P.buf("E"), P.buf("F")
        U = kb.sb(es, [128, 128 * 33], F32, "U")
        Dk = kb.sb(es, [128, 128 * 33], F32, "Dk")
        St = kb.sb(es, [128, 128 * 33], F32, "St")
        BU, BDk, BSt = P.buf("U"), P.buf("Dk"), P.buf("St")
        U3 = U[:].rearrange("p (v c) -> p v c", c=33)
        Dk3 = Dk[:].rearrange("p (v c) -> p v c", c=33)
        St3 = St[:].rearrange("p (v c) -> p v c", c=33)
        Stc = U[:, 0:4096].rearrange("p (c v) -> p c v", v=128)
        kb.memset("pool", Dk[:], 0.0, [BDk])
        kb.memset("pool", U[:], 0.0, [BU])
        vtk = kb.sb(es, [64, 32, 128], F32, "vtk")
        Bvtk = P.buf("vtk")
        kendT = kb.sb(es, [64, 32, 128], F32, "kendT")
        BkT = P.buf("kendT")
        attm = kb.sb(es, [64, 2048], F32, "attm")
        Batt = P.buf("attm")
        o_h = kb.sb(es, [128, 2048], F32, "o_h")
        Boh = P.buf("o_h")
        gs = kb.sb(es, [128, 2048], BF16, "gs")
        Bgs = P.buf("gs")
        oTh = kb.sb(es, [128, 2048], BF16, "oTh")
        BoT = P.buf("oTh")
        ebend = kb.sb(es, [128, 32], F32, "ebend")
        Beb = P.buf("ebend")
        sq = kb.sb(es, [128, 512], BF16, "sq")
        rs = kb.sb(es, [128, 512], F32, "rs")
        tmp = kb.sb(es, [128, 512], F32, "tmp")
        Bsq, Brs, Btmp = P.buf("sq"), P.buf("rs"), P.buf("tmp")
        ptr = [kb.ps(es, [128, 512], F32, "ptr") for _ in range(2)]
        Bptr = P.bufs(2, "ptr")
        patt = kb.ps(es, [128, 512], F32, "patt")
        Bpatt = P.buf("patt")
        pupd = [kb.ps(es, [128, 512], F32, "pupd") for _ in range(2)]
        Bpupd = P.bufs(2, "pupd")
        po = [kb.ps(es, [128, 512], F32, "po") for _ in range(2)]
        Bpo = P.bufs(2, "po")
        psq = kb.ps(es, [128, 512], F32, "psq")
        Bpsq = P.buf("psq")
        B3 = Bb[:].rearrange("p (c t) -> p c t", t=64)
        A3 = A[:].rearrange("p (c t) -> p c t", t=64)
        for h in range(16):
            kb.ld(C[:], hq_d[h], (), [BC])
            kb.ld(A[:], hf_d[h], (), [BA])
            kb.ld(Dd[:], hk_d[h], (), [BD])
            kb.ld(vtk[:], hv_d[:, h * 128:(h + 1) * 128].rearrange("(c s) v -> s c v", s=64), (), [Bvtk])
            kb.ld(gs[:], qkv_d[h], (), [Bgs])
            P.op("dve", lambda: nc.vector.tensor_tensor_scan(out=Bb[:], data0=rmask[:], data1=A[:], initial=0.0, op0=ALU.mult, op1=ALU.add),
                 [Bon, BA], [BB])
            kb.act(A[:], Bb[:], AF.Exp, [BB], [BA])
            kb.tt("pool", C[:], C[:], A[:], ALU.mult, [BC, BA], [BC])
            kb.act(ebend[:], B3[:, :, 63], AF.Exp, [BB], [Beb])
            kb.ts("dve", A[:], Bb[:], -1.0, ALU.mult, [BB, BC], [BA], s2=80.0, op1=ALU.min)
            kb.act(A[:], A[:], AF.Exp, [BA], [BA])
            kb.tt("pool", E[:], Dd[:], A[:], ALU.mult, [BD, BA], [BE])
            kb.tt("dve", A3, B3[:, :, 63:64].to_broadcast([128, 32, 64]), B3, ALU.subtract, [BB, BE], [BA])
            kb.act(A[:], A[:], AF.Exp, [BA], [BA])
            kb.tt("pool", Fk[:], Dd[:], A[:], ALU.mult, [BD, BA], [BF_])
            for c in range(32):
                b = (c // 4) % 2
                kb.tr(ptr[b][0:64, (c % 4) * 128:(c % 4 + 1) * 128], Fk[:, c * 64:(c + 1) * 64], identf[:], [BF_, Bc], [Bptr[b]])
                if c % 4 == 3:
                    kb.cp("act", kendT[:, c - 3:c + 1, :], ptr[b][0:64, :].rearrange("p (c k) -> p c k", c=4), [Bptr[b]], [BkT])
            for grp in range(4):
                for i in range(8):
                    c = grp * 8 + i
                    kb.mm(patt[0:64, i * 64:(i + 1) * 64], E[:, c * 64:(c + 1) * 64], C[:, c * 64:(c + 1) * 64], True, True, [BE, BC], [Bpatt])
                kb.tt("dve", attm[:, grp * 512:(grp + 1) * 512], patt[0:64, :], tri[:], ALU.mult, [Bpatt, Bc], [Batt])
            kb.memset("pool", U3[:, :, 0:1], 0.0, [BU])
            for grp in range(8):
                b = grp % 2
                for i in range(4):
                    c = grp * 4 + i
                    kb.mm(pupd[b][:, i * 128:(i + 1) * 128], kendT[:, c, :], vtk[:, c, :], True, True, [BkT, Bvtk], [Bpupd[b]])
                kb.cp("act" if b == 0 else "dve", U3[:, :, grp * 4 + 1:grp * 4 + 5].rearrange("p v c -> p c v"),
                      pupd[b][:].rearrange("p (c v) -> p c v", c=4), [Bpupd[b]], [BU])
            kb.cp("pool", Dk3[:, :, 1:33], ebend[:].unsqueeze(1).to_broadcast([128, 128, 32]), [Beb], [BDk])
            P.op("dve", lambda: nc.vector.tensor_tensor_scan(out=St[:], data0=Dk[:], data1=U[:], initial=0.0, op0=ALU.mult, op1=ALU.add),
                 [BDk, BU], [BSt])
            kb.cp("pool", Stc, St3[:, :, 0:32].rearrange("p v c -> p c v"), [BSt], [BU])
            for grp in range(4):
                b = grp % 2
                for i in range(8):
                    c = grp * 8 + i
                    kb.mm(po[b][:, i * 64:(i + 1) * 64], vtk[:, c, :], attm[:, c * 64:(c + 1) * 64], True, False, [Bvtk, Batt], [Bpo[b]])
                    kb.mm(po[b][:, i * 64:(i + 1) * 64], Stc[:, c, :], C[:, c * 64:(c + 1) * 64], False, True, [BU, BC], [Bpo[b]])
                kb.cp("act", o_h[:, grp * 512:(grp + 1) * 512], po[b][:], [Bpo[b]], [Boh])
            for cc in range(4):
                cs = slice(cc * 512, (cc + 1) * 512)
                kb.act(sq[:], o_h[:, cs], AF.Square, [Boh], [Bsq])
                kb.mm(psq[:], ones[:], sq[:], True, True, [Bon, Bsq], [Bpsq])
                kb.ts("dve", rs[:], psq[:], 1.0 / 128, ALU.mult, [Bpsq], [Brs], s2=EPS, op1=ALU.add)
                kb.act(rs[:], rs[:], AF.Sqrt, [Brs], [Brs])
                kb.recip(rs[:], rs[:], [Brs], [Brs])
                kb.stt(tmp[:], o_h[:, cs], ng[:, 0:1], rs[:], ALU.mult, ALU.mult, [Boh, Bc, Brs], [Btmp])
                kb.tt("pool", oTh[:, cs], tmp[:], gs[:, cs], ALU.mult, [Btmp, Bgs], [BoT])
            kb.ld(act_d[h], oTh[:], [BoT], (), owner=BoT)
        P.end_phase()

LAYER_NIN = {0: 3072, 1: 6144, 2: 8192, 3: 4176}
MIX_NAMES = {0: "swa", 1: "diff", 2: "hgrn", 3: "dsa"}


def build_program(layers, stop_after=None):
    nc = bass.Bass("TRN2", target_bir_lowering=False)
    dr = {}

    def ext(name, shape, dt=F32):
        dr[name] = nc.dram_tensor(name, list(shape), dt, kind="ExternalInput").ap()
        return dr[name]

    x_in = ext("x", [S, D])
    y_out = nc.dram_tensor("y", [S, D], F32, kind="ExternalOutput").ap()
    normg = ext("norm_g", [16, D])
    ident_d = ext("ident", [128, 128])
    i8_d = ext("i8", [128, 128])
    for l in layers:
        ext("w_in%d" % l, [D, LAYER_NIN[l]])
        ext("w_out%d" % l, [D, D])
        ext("w_up%d" % l, [D, 2 * DFF])
        ext("w_down%d" % l, [DFF, D])
        ext("conv%d" % l, [128, 88, 3])
    if 0 in layers:
        ext("bias_sw", [128, 32 * 256])
        ext("sinkrep", [1, 4096])
    if 1 in layers or 3 in layers:
        ext("biasn", [128, 32 * 256])
        ext("table", [32, 32])
    if 1 in layers:
        ext("diff_lambda", [4, 64])
        ext("diff_subg", [1, 128])
    xres = nc.dram_tensor("xres", [S, D], F32).ap()
    m_d = nc.dram_tensor("m_d", [S, D], F32).ap()
    act_d = nc.dram_tensor("act_d", [KFF, 128, S], BF16).ap()
    qkv_d = nc.dram_tensor("qkv_d", [64, 128, S], BF16).ap()
    vtok_d = nc.dram_tensor("vtok_d", [S, 2048], BF16).ap()
    if 2 in layers:
        ext("lbl", [128, 64])
        ext("hgrn_ng", [1, 128])
        ext("tri", [64, 512])
        hq_d = nc.dram_tensor("hq_d", [16, 128, S], F32).ap()
        hf_d = nc.dram_tensor("hf_d", [16, 128, S], F32).ap()
        hk_d = nc.dram_tensor("hk_d", [16, 128, S], F32).ap()
        hv_d = nc.dram_tensor("hv_d", [S, 2048], F32).ap()
    if 3 in layers:
        ext("cmask", [128, 128])
        wi_d = nc.dram_tensor("wi_d", [S, 16], F32).ap()
        mask_d = nc.dram_tensor("mask_d", [16, 128, S], BF16).ap()

    with ExitStack() as top:
        P = Prog(nc, top)
        kb = KB(nc, P)
        x_cur = x_in
        pend_m = None
        for li, l in enumerate(layers):
            with ExitStack() as hs:
                hT = kb.sb(hs, [128, 16, S], BF16, "hT")
                BhT = Buf("hT")
                phase_rn(kb, x_cur, xres, m_d if pend_m is not None else None,
                         normg[pend_m] if pend_m is not None else None, normg[4 * l + 0], hT, BhT, ident_d)
                if pend_m is not None:
                    x_cur = xres
                w_in = dr["w_in%d" % l]
                if l == 0:
                    tiles = [(i * 128, 128) for i in range(20)]
                    phase_proj_fm(kb, hT, BhT, w_in, tiles, epi_store_fm(kb, qkv_d, lambda ti: ti), epi_store_setup(kb))
                    phase_proj_tok(kb, hT, BhT, w_in, [(2560, 512)], epi_store_tok(kb, vtok_d, lambda gi: 0), epi_tok_setup(kb))
                elif l == 1:
                    tiles = [(i * 128, 128) for i in range(32)]
                    phase_proj_fm(kb, hT, BhT, w_in, tiles, epi_store_fm(kb, qkv_d, lambda ti: ti), epi_store_setup(kb))
                    phase_proj_tok(kb, hT, BhT, w_in, [(4096 + i * 512, 512) for i in range(4)],
                                   epi_store_tok(kb, vtok_d, lambda gi: gi * 512), epi_tok_setup(kb))
                elif l == 2:
                    hgrn_inproj(kb, hT, BhT, w_in, dr["lbl"], l, hq_d, hf_d, hk_d, hv_d, qkv_d)
                elif l == 3:
                    tiles = [(i * 128, 128) for i in range(20)] + [(3072 + i * 128, 128) for i in range(8)] + [(4096, 64)]
                    phase_proj_fm(kb, hT, BhT, w_in, tiles, epi_store_fm(kb, qkv_d, lambda ti: ti), epi_store_setup(kb))

                    def setup3(es):
                        c_ = epi_tok_setup(kb)(es)
                        c_["tf"] = [kb.sb(es, [128, 16], F32, "etf") for _ in range(2)]
                        c_["Bf"] = kb.P.bufs(2, "etf")
                        return c_

                    def epi3(ctx, gi, t, c0, w, pb, Bpb):
                        if gi == 0:
                            epi_store_tok(kb, vtok_d, lambda gi_: 0)(ctx, gi, t, c0, w, pb, Bpb)
                        else:
                            tl, B = ctx["tf"][t % 2], ctx["Bf"][t % 2]
                            kb.cp("dve", tl[:, 0:16], pb[:, 0:16], [Bpb], [B])
                            kb.ld(wi_d[t * 128:(t + 1) * 128, :], tl[:, 0:16], [B], (), owner=B)
                    phase_proj_tok(kb, hT, BhT, w_in, [(2560, 512), (4160, 16)], epi3, setup3)
            if l == 0:
                phase_swa(kb, qkv_d, vtok_d, act_d, dr["bias_sw"], i8_d, dr["sinkrep"])
            elif l == 2:
                phase_hgrn(kb, hq_d, hf_d, hk_d, hv_d, qkv_d, act_d, dr["hgrn_ng"], ident_d, dr["tri"])
            elif l == 3:
                phase_dsa_idx(kb, qkv_d, wi_d, mask_d, dr["cmask"], ident_d)
                phase_dsa_attn(kb, qkv_d, vtok_d, act_d, mask_d, dr["biasn"], i8_d, dr["table"])
            elif l == 1:
                phase_diff(kb, qkv_d, vtok_d, act_d, dr["biasn"], i8_d, dr["table"], dr["diff_lambda"], dr["diff_subg"], l)
            phase_down(kb, dr["w_out%d" % l], 16, act_d, m_d)
            with ExitStack() as hs:
                hT = kb.sb(hs, [128, 16, S], BF16, "hT")
                BhT = Buf("hT")
                phase_rn(kb, x_cur, xres, m_d, normg[4 * l + 1], normg[4 * l + 2], hT, BhT, ident_d)
                x_cur = xres
                phase_up(kb, hT, BhT, dr["w_up%d" % l], dr["conv%d" % l], act_d)
            phase_down(kb, dr["w_down%d" % l], KFF, act_d, m_d)
            pend_m = 4 * l + 3
        phase_rn(kb, x_cur, y_out, m_d, normg[pend_m], None, None, None, ident_d)
    return nc


def _bucket(n):
    n = np.maximum(n, 0)
    max_exact = 16
    lr = np.log(np.maximum(n, 1).astype(np.float32) / max_exact) / math.log(128 / max_exact)
    large = np.minimum(max_exact + (lr * (32 - max_exact)).astype(np.int32), 31)
    return np.where(n < max_exact, n, large)


def _host_consts(inputs, layers):
    c = {}
    c["ident"] = np.eye(128, dtype=np.float32)
    c["i8"] = (8.0 * np.eye(128)).astype(np.float32)
    table = np.asarray(inputs["rel_bias_table"], np.float32)
    if 0 in layers:
        s_ = np.arange(128)[:, None]
        q_ = np.arange(256)[None, :]
        dist = q_ - s_
        valid = (dist >= 0) & (dist < 128)
        bk = _bucket(dist)
        t = table[bk]
        t = np.where(valid[:, :, None], t, np.float32(MASKV))
        c["bias_sw"] = np.ascontiguousarray(t.transpose(0, 2, 1)).reshape(128, 32 * 256).astype(np.float32)
        c["sinkrep"] = np.ascontiguousarray(np.repeat(np.asarray(inputs["swa_sinks"], np.float32)[0], 128)[None, :])
    if 1 in layers or 3 in layers:
        s_ = np.arange(128)[:, None]
        q_ = np.arange(256)[None, :]
        dist = q_ - s_
        t = table[_bucket(dist)]
        t = np.where((dist >= 0)[:, :, None], t, np.float32(MASKV))
        c["biasn"] = np.ascontiguousarray(t.transpose(0, 2, 1)).reshape(128, 32 * 256).astype(np.float32)
        c["table"] = np.ascontiguousarray(table)
    if 2 in layers:
        lg = np.asarray(inputs["hgrn_lb_logits"], np.float32)
        c["lbl"] = np.ascontiguousarray(lg.reshape(4, 16, 128).transpose(2, 1, 0)).reshape(128, 64)
        c["hgrn_ng"] = np.ascontiguousarray(np.asarray(inputs["hgrn_norm_g"], np.float32)[0][None, :])
        tr_ = (np.arange(64)[:, None] <= np.arange(64)[None, :]).astype(np.float32)
        c["tri"] = np.ascontiguousarray(np.tile(tr_, (1, 8)))
    if 3 in layers:
        c["cmask"] = np.where(np.arange(128)[None, :] > np.arange(128)[:, None], np.float32(-1.0e30), np.float32(0.0)).astype(np.float32)
    if 1 in layers:
        c["diff_lambda"] = np.ascontiguousarray(np.asarray(inputs["diff_lambda"], np.float32)[0])
        c["diff_subg"] = np.ascontiguousarray(np.asarray(inputs["diff_subln_g"], np.float32)[0][None, :])
    return c


_W_KEYS = {0: ("swa_w_in", "swa_w_out"), 1: ("diff_w_in", "diff_w_out"), 2: ("hgrn_w_in", "hgrn_w_out"), 3: ("dsa_w_in", "dsa_w_out")}
LAUNCHES = [[0, 1, 2, 3]]
N_CORES = 4


def _layer_inputs(inputs, layers):
    m = {}
    m["norm_g"] = np.ascontiguousarray(np.asarray(inputs["norm_g"], np.float32).reshape(16, D))
    for l in layers:
        kin, kout = _W_KEYS[l]
        m["w_in%d" % l] = np.ascontiguousarray(np.asarray(inputs[kin], np.float32)[0])
        m["w_out%d" % l] = np.ascontiguousarray(np.asarray(inputs[kout], np.float32)[0])
        m["w_up%d" % l] = np.ascontiguousarray(np.asarray(inputs["ffn_w_up"], np.float32)[l])
        m["w_down%d" % l] = np.ascontiguousarray(np.asarray(inputs["ffn_w_down"], np.float32)[l])
        cv = np.asarray(inputs["ffn_conv"], np.float32)[l]
        m["conv%d" % l] = np.ascontiguousarray(cv.reshape(3, 88, 128).transpose(2, 1, 0))
    m.update(_host_consts(inputs, layers))
    return m


def run_launch(inputs, x, layers, cores=None):
    nc = build_program(layers)
    shared = _layer_inputs(inputs, layers)
    B = x.shape[0]
    cores = cores if cores is not None else N_CORES
    in_maps = []
    for c in range(cores):
        d = dict(shared)
        d["x"] = np.ascontiguousarray(x[c % B])
        in_maps.append(d)
    res = run_bass_kernel_spmd(nc, in_maps, core_ids=list(range(cores)))
    return np.stack([res.results[b]["y"] for b in range(B)], axis=0)


def kernel(**inputs):
    x = np.asarray(inputs["x"], np.float32)
    for layers in LAUNCHES:
        x = run_launch(inputs, x, layers)
    return x.astype(np.float32)
```

```python
import math
from contextlib import ExitStack

import numpy as np
import concourse.bass as bass
import concourse.mybir as mybir
from concourse.bass_utils import run_bass_kernel_spmd

F32 = mybir.dt.float32
BF16 = mybir.dt.bfloat16
AF = mybir.ActivationFunctionType
ALU = mybir.AluOpType
AX = mybir.AxisListType

ENGS = ("pe", "act", "dve", "pool", "sp")
CENGS = ("pe", "act", "dve", "pool")

S = 2048
D = 2048
NT = 16
DFF = 5632
KFF = 44
EPS = 1e-6
MASKV = -30000.0
N_DSEM = 56


class Buf:
    __slots__ = ("name", "w", "r", "dsem")

    def __init__(self, name):
        self.name = name
        self.w = None
        self.r = {}
        self.dsem = None


class Prog:
    def __init__(self, nc, es):
        self.nc = nc
        self.ops = {e: [] for e in ENGS}
        self.sems = {}
        self.tot = {}
        self.seen = {e: {} for e in ENGS}
        for e in CENGS:
            self.sems[e] = es.enter_context(nc.semaphore("c_" + e))
            self.tot[e] = 0
        self.dfree = []
        for i in range(N_DSEM):
            k = "d%d" % i
            self.sems[k] = es.enter_context(nc.semaphore(k))
            self.tot[k] = 0
            self.dfree.append(k)
        self.dused = []
        self.phase_bufs = []
        self.nphase = 0

    def buf(self, name="b"):
        b = Buf(name)
        self.phase_bufs.append(b)
        return b

    def bufs(self, n, name="b"):
        return [self.buf(name + str(i)) for i in range(n)]

    def _dsem(self, b):
        if b.dsem is None:
            b.dsem = self.dfree.pop()
            self.dused.append(b.dsem)
        return b.dsem

    def _deps(self, eng, reads, writes):
        deps = {}

        def add(ev):
            if ev is None:
                return
            k, v = ev
            if v > deps.get(k, 0):
                deps[k] = v
        for b in reads:
            add(b.w)
        for b in writes:
            add(b.w)
            for k, v in b.r.items():
                add((k, v))
        waits = []
        for k, v in deps.items():
            if k == eng and eng == "pe":
                continue
            if k[0] == "d":
                v = self.tot[k]
            if v > self.seen[eng].get(k, 0):
                self.seen[eng][k] = v
                waits.append((k, v))
        return waits

    def op(self, eng, fn, reads=(), writes=()):
        waits = self._deps(eng, reads, writes)
        self.tot[eng] += 1
        seq = self.tot[eng]
        self.ops[eng].append((waits, fn, eng, 1))
        for b in reads:
            if b.r.get(eng, 0) < seq:
                b.r[eng] = seq
        for b in writes:
            b.w = (eng, seq)
            b.r = {}

    def dma(self, q, fn, reads=(), writes=(), owner=None):
        waits = self._deps(q, reads, writes)
        if owner is None:
            owner = writes[0] if writes else reads[0]
        k = self._dsem(owner)
        self.tot[k] += 16
        val = self.tot[k]
        self.ops[q].append((waits, fn, k, 16))
        for b in reads:
            b.r[k] = val
        for b in writes:
            b.w = (k, val)
            b.r = {}

    def end_phase(self):
        nc = self.nc
        keys = list(CENGS) + list(self.dused)
        for e in ENGS:
            waits = []
            for k in keys:
                if k == e:
                    continue
                v = self.tot[k]
                if v > self.seen[e].get(k, 0):
                    self.seen[e][k] = v
                    waits.append((k, v))
            self.ops[e].append((waits, None, None, 0))
        with nc.Block() as block:
            def mk(ename, attr):
                lst = self.ops[ename]

                def body(e):
                    for waits, fn, k, inc in lst:
                        for wk, wv in waits:
                            e.wait_ge(self.sems[wk], wv)
                        if fn is not None:
                            fn().then_inc(self.sems[k], inc)
                getattr(block, attr)(body)
            mk("sp", "sync")
            mk("pe", "tensor")
            mk("act", "scalar")
            mk("dve", "vector")
            mk("pool", "gpsimd")
        self.ops = {e: [] for e in ENGS}
        for b in self.phase_bufs:
            b.dsem = None
            b.w = None
            b.r = {}
        self.phase_bufs = []
        self.dfree.extend(self.dused)
        self.dused = []
        self.nphase += 1


class KB:
    def __init__(self, nc, P):
        self.nc = nc
        self.P = P
        self.uid = 0

    def sb(self, es, shape, dt, name="t"):
        self.uid += 1
        return es.enter_context(self.nc.sbuf_tensor("%s_%d" % (name, self.uid), list(shape), dt))

    def ps(self, es, shape, dt, name="p"):
        self.uid += 1
        return es.enter_context(self.nc.psum_tensor("%s_%d" % (name, self.uid), list(shape), dt))

    def mm(self, out, lhsT, rhs, start, stop, r, w):
        nc = self.nc
        self.P.op("pe", lambda: nc.tensor.matmul(out, lhsT=lhsT, rhs=rhs, start=start, stop=stop), r, w)

    def tr(self, out, in_, ident, r, w):
        nc = self.nc
        self.P.op("pe", lambda: nc.tensor.transpose(out, in_, ident), r, w)

    def act(self, out, in_, func, r, w, bias=None, scale=None, accum=None):
        nc = self.nc
        kw = {}
        if bias is not None:
            kw["bias"] = bias
        if scale is not None:
            kw["scale"] = scale
        if accum is not None:
            kw["accum_out"] = accum
        self.P.op("act", lambda: nc.scalar.activation(out=out, in_=in_, func=func, **kw), r, w)

    def ts(self, eng, out, in0, s1, op0, r, w, s2=None, op1=None):
        e = self.nc.vector if eng == "dve" else self.nc.gpsimd
        if op1 is None:
            self.P.op(eng, lambda: e.tensor_scalar(out=out, in0=in0, scalar1=s1, scalar2=None, op0=op0), r, w)
        else:
            self.P.op(eng, lambda: e.tensor_scalar(out=out, in0=in0, scalar1=s1, scalar2=s2, op0=op0, op1=op1), r, w)

    def tt(self, eng, out, in0, in1, op, r, w):
        e = self.nc.vector if eng == "dve" else self.nc.gpsimd
        self.P.op(eng, lambda: e.tensor_tensor(out=out, in0=in0, in1=in1, op=op), r, w)

    def stt(self, out, in0, scalar, in1, op0, op1, r, w):
        nc = self.nc
        self.P.op("dve", lambda: nc.vector.scalar_tensor_tensor(out=out, in0=in0, scalar=scalar, in1=in1, op0=op0, op1=op1), r, w)

    def cp(self, eng, out, in_, r, w):
        nc = self.nc
        if eng == "act":
            self.P.op("act", lambda: nc.scalar.copy(out=out, in_=in_), r, w)
        elif eng == "dve":
            self.P.op("dve", lambda: nc.vector.tensor_copy(out=out, in_=in_), r, w)
        else:
            self.P.op("pool", lambda: nc.gpsimd.tensor_copy(out=out, in_=in_), r, w)

    def recip(self, out, in_, r, w):
        nc = self.nc
        self.P.op("dve", lambda: nc.vector.reciprocal(out=out, in_=in_), r, w)

    def memset(self, eng, ap, val, w):
        e = self.nc.vector if eng == "dve" else self.nc.gpsimd
        self.P.op(eng, lambda: e.memset(ap, val), (), w)

    def ld(self, out, in_, r, w, q="sp", owner=None):
        nc = self.nc
        if q == "sp":
            self.P.dma("sp", lambda: nc.sync.dma_start(out=out, in_=in_), r, w, owner)
        else:
            self.P.dma("pool", lambda: nc.gpsimd.dma_start(out=out, in_=in_), r, w, owner)

    def rstd(self, es_tiles, ss, r, w):
        self.ts("dve", ss, ss, 1.0 / D, ALU.mult, r, w, s2=EPS, op1=ALU.add)
        self.act(ss, ss, AF.Sqrt, r, w)
        self.recip(ss, ss, r, w)


def phase_rn(kb, x_src, x_dst, m_src, gpost, gpre, hT, BhT, ident_d):
    nc, P = kb.nc, kb.P
    with ExitStack() as es:
        xt = [kb.sb(es, [128, D], F32, "xt") for _ in range(2)]
        Bxt = P.bufs(2, "xt")
        junk = kb.sb(es, [128, D], BF16, "junk")
        Bjunk = P.buf("junk")
        st = [kb.sb(es, [128, 2], F32, "st") for _ in range(2)]
        Bst = P.bufs(2, "st")
        if m_src is not None:
            mt = [kb.sb(es, [128, D], F32, "mt") for _ in range(2)]
            Bmt = P.bufs(2, "mt")
            gp = kb.sb(es, [128, D], F32, "gp")
            Bgp = P.buf("gp")
            kb.ld(gp[:], gpost.partition_broadcast(128), (), [Bgp])
        if gpre is not None:
            gq = kb.sb(es, [128, D], F32, "gq")
            Bgq = P.buf("gq")
            kb.ld(gq[:], gpre.partition_broadcast(128), (), [Bgq])
            hb = [kb.sb(es, [128, D], BF16, "hb") for _ in range(2)]
            Bhb = P.bufs(2, "hb")
            ident = kb.sb(es, [128, 128], BF16, "ident")
            Bid = P.buf("ident")
            kb.ld(ident[:], ident_d, (), [Bid], q="pool")
            pT = [kb.ps(es, [128, 1024], BF16, "pT") for _ in range(2)]
            BpT = P.bufs(2, "pT")
        Bxd = P.buf("xdst")
        def load_t(t_):
            if t_ >= NT:
                return
            rows_ = slice(t_ * 128, (t_ + 1) * 128)
            kb.ld(xt[t_ % 2][:], x_src[rows_, :], (), [Bxt[t_ % 2]])
            if m_src is not None:
                kb.ld(mt[t_ % 2][:], m_src[rows_, :], (), [Bmt[t_ % 2]])
        load_t(0)
        for t in range(NT):
            s = t % 2
            rows = slice(t * 128, (t + 1) * 128)
            load_t(t + 1)
            if m_src is not None:
                kb.act(junk[:], mt[s][:], AF.Square, [Bmt[s]], [Bjunk, Bst[s]], accum=st[s][:, 0:1])
                kb.rstd(None, st[s][:, 0:1], [Bst[s]], [Bst[s]])
                kb.stt(mt[s][:], mt[s][:], st[s][:, 0:1], gp[:], ALU.mult, ALU.mult, [Bmt[s], Bst[s], Bgp], [Bmt[s]])
                kb.tt("dve", xt[s][:], xt[s][:], mt[s][:], ALU.add, [Bxt[s], Bmt[s]], [Bxt[s]])
                kb.ld(x_dst[rows, :], xt[s][:], [Bxt[s]], (), owner=Bxt[s])
            if gpre is not None:
                kb.act(junk[:], xt[s][:], AF.Square, [Bxt[s]], [Bjunk, Bst[s]], accum=st[s][:, 1:2])
                kb.rstd(None, st[s][:, 1:2], [Bst[s]], [Bst[s]])
                kb.stt(hb[s][:], xt[s][:], st[s][:, 1:2], gq[:], ALU.mult, ALU.mult, [Bxt[s], Bst[s], Bgq], [Bhb[s]])
                for half in range(2):
                    for j in range(8):
                        i = half * 8 + j
                        kb.tr(pT[half][:, j * 128:(j + 1) * 128], hb[s][:, i * 128:(i + 1) * 128], ident[:], [Bhb[s], Bid], [BpT[half]])
                    src = pT[half][:].rearrange("p (j q) -> p j q", j=8)
                    dst = hT[:, half * 8:(half + 1) * 8, t * 128:(t + 1) * 128]
                    kb.cp("act" if half == 0 else "dve", dst, src, [BpT[half]], [BhT])
        P.end_phase()


def phase_proj_fm(kb, hT, BhT, w_d, coltiles, epilogue, epi_setup=None):
    nc, P = kb.nc, kb.P
    wv = w_d.rearrange("(kc p) n -> p kc n", p=128)
    with ExitStack() as es:
        NSL = 3
        wsl = [kb.sb(es, [128, 16, 256], BF16, "wsl") for _ in range(NSL)]
        Bw = P.bufs(NSL, "wsl")
        pb = [kb.ps(es, [128, 2048], F32, "pb") for _ in range(2)]
        Bpb = P.bufs(2, "pb")
        ctx = epi_setup(es) if epi_setup is not None else None
        slabs = []
        i = 0
        while i < len(coltiles):
            c0, w0 = coltiles[i]
            if i + 1 < len(coltiles) and coltiles[i + 1][0] == c0 + w0 and w0 == 128:
                slabs.append([coltiles[i], coltiles[i + 1]])
                i += 2
            else:
                slabs.append([coltiles[i]])
                i += 1
        ti = 0

        def load_slab(si):
            if si >= len(slabs):
                return
            s_ = si % NSL
            c0_ = slabs[si][0][0]
            wtot = sum(w for _, w in slabs[si])
            kb.ld(wsl[s_][:, :, 0:wtot], wv[:, :, c0_:c0_ + wtot], (), [Bw[s_]], q="pool")
        load_slab(0)
        load_slab(1)
        for si, sl in enumerate(slabs):
            s = si % NSL
            load_slab(si + 2)
            off = 0
            for (cc, w) in sl:
                pi = ti % 2
                for g in range(4):
                    for k in range(16):
                        kb.mm(pb[pi][0:w, g * 512:(g + 1) * 512], wsl[s][:, k, off:off + w], hT[:, k, g * 512:(g + 1) * 512],
                              k == 0, k == 15, [Bw[s], BhT], [Bpb[pi]])
                epilogue(ctx, ti, cc, w, pb[pi], Bpb[pi])
                off += w
                ti += 1
        P.end_phase()


def phase_proj_tok(kb, hT, BhT, w_d, colgroups, epilogue, epi_setup=None):
    nc, P = kb.nc, kb.P
    wv = w_d.rearrange("(kc p) n -> p kc n", p=128)
    with ExitStack() as es:
        wg = [kb.sb(es, [128, 16, 512], BF16, "wg") for _ in range(2)]
        Bwg = P.bufs(2, "wg")
        pb = [kb.ps(es, [128, 512], F32, "pk") for _ in range(4)]
        Bpb = P.bufs(4, "pk")
        ctx = epi_setup(es) if epi_setup is not None else None
        n = 0

        def load_g(gi):
            if gi >= len(colgroups):
                return
            c0_, w_ = colgroups[gi]
            kb.ld(wg[gi % 2][:, :, 0:w_], wv[:, :, c0_:c0_ + w_], (), [Bwg[gi % 2]], q="pool")
        load_g(0)
        for gi, (c0, w) in enumerate(colgroups):
            s = gi % 2
            load_g(gi + 1)
            for t in range(NT):
                pi = n % 4
                n += 1
                for k in range(16):
                    kb.mm(pb[pi][:, 0:w], hT[:, k, t * 128:(t + 1) * 128], wg[s][:, k, 0:w], k == 0, k == 15, [BhT, Bwg[s]], [Bpb[pi]])
                epilogue(ctx, gi, t, c0, w, pb[pi], Bpb[pi])
        P.end_phase()


def phase_up(kb, hT, BhT, w_up, conv_d, act_d):
    nc, P = kb.nc, kb.P
    wv = w_up.rearrange("(kc p) n -> p kc n", p=128)
    with ExitStack() as es:
        NSL = 3
        wsl = [kb.sb(es, [128, 16, 512], BF16, "wup") for _ in range(NSL)]
        Bw = P.bufs(NSL, "wup")
        cw = kb.sb(es, [128, 88, 3], F32, "cw")
        Bcw = P.buf("cw")
        kb.ld(cw[:], conv_d, (), [Bcw])
        psg = kb.ps(es, [128, 2048], F32, "psg")
        psv = kb.ps(es, [128, 2048], F32, "psv")
        Bpsg, Bpsv = P.buf("psg"), P.buf("psv")
        ub = [kb.sb(es, [128, 2050], F32, "ub") for _ in range(4)]
        Bub = P.bufs(4, "ub")
        cb = [kb.sb(es, [128, 2048], F32, "cb") for _ in range(4)]
        Bcb = P.bufs(4, "cb")
        ob = [kb.sb(es, [128, 2048], BF16, "ob") for _ in range(2)]
        Bob = P.bufs(2, "ob")
        Bact = P.buf("actd")
        for i in range(4):
            kb.memset("pool", ub[i][:, 0:2], 0.0, [Bub[i]])
        def load_up(sl_):
            if sl_ >= KFF // 2:
                return
            s_ = sl_ % NSL
            c_ = sl_ * 2
            kb.ld(wsl[s_][:, :, 0:256], wv[:, :, c_ * 128:c_ * 128 + 256], (), [Bw[s_]], q="pool")
            kb.ld(wsl[s_][:, :, 256:512], wv[:, :, DFF + c_ * 128:DFF + c_ * 128 + 256], (), [Bw[s_]], q="pool")
        load_up(0)
        load_up(1)
        for c in range(KFF):
            sl = c // 2
            s = sl % NSL
            if c % 2 == 0:
                load_up(sl + 2)
            par = c % 2
            for part, (pp, Bpp) in enumerate(((psg, Bpsg), (psv, Bpsv))):
                off = part * 256 + par * 128
                for g in range(4):
                    for k in range(16):
                        kb.mm(pp[:, g * 512:(g + 1) * 512], wsl[s][:, k, off:off + 128], hT[:, k, g * 512:(g + 1) * 512],
                              k == 0, k == 15, [Bw[s], BhT], [Bpp])
                u = par * 2 + part
                fi = part * KFF + c
                kb.cp("act", ub[u][:, 2:2050], pp[:], [Bpp], [Bub[u]])
                kb.act(cb[u][:], ub[u][:, 0:2048], AF.Copy, [Bub[u], Bcw], [Bcb[u]], scale=cw[:, fi, 0:1])
                kb.stt(cb[u][:], ub[u][:, 1:2049], cw[:, fi, 1:2], cb[u][:], ALU.mult, ALU.add, [Bub[u], Bcw, Bcb[u]], [Bcb[u]])
                kb.stt(cb[u][:], ub[u][:, 2:2050], cw[:, fi, 2:3], cb[u][:], ALU.mult, ALU.add, [Bub[u], Bcw, Bcb[u]], [Bcb[u]])
                if part == 0:
                    kb.act(cb[u][:], cb[u][:], AF.Silu, [Bcb[u]], [Bcb[u]])
            ug, uv = par * 2, par * 2 + 1
            kb.tt("dve", ob[par][:], cb[ug][:], cb[uv][:], ALU.mult, [Bcb[ug], Bcb[uv]], [Bob[par]])
            kb.ld(act_d[c], ob[par][:], [Bob[par]], (), owner=Bob[par])
        P.end_phase()


def phase_down(kb, w_d, KC, act_d, m_d):
    nc, P = kb.nc, kb.P
    wv = w_d.rearrange("(kc p) n -> p kc n", p=128)
    av = act_d.rearrange("c p s -> p c s")
    TG = 256
    with ExitStack() as es:
        wres = [kb.sb(es, [128, KC, 512], BF16, "wres") for _ in range(2)]
        Bwr = [P.bufs(4, "wr%d_" % i) for i in range(2)]
        slab = [kb.sb(es, [128, KC, TG], BF16, "slab") for _ in range(2)]
        Bsl = P.bufs(2, "slab")
        pb = [kb.ps(es, [128, 512], F32, "pd") for _ in range(4)]
        Bpb = P.bufs(4, "pd")
        ot = [kb.sb(es, [128, 512], F32, "ot") for _ in range(3)]
        Bot = P.bufs(3, "ot")
        Bm = P.buf("md")
        kq = [(i * KC) // 4 for i in range(5)]
        n = 0
        ns = 0
        NTG = S // TG

        def load_w(cg_):
            if cg_ >= 4:
                return
            for pq_ in range(4):
                kb.ld(wres[cg_ % 2][:, kq[pq_]:kq[pq_ + 1], :], wv[:, kq[pq_]:kq[pq_ + 1], cg_ * 512:(cg_ + 1) * 512], (),
                      [Bwr[cg_ % 2][pq_]], q="pool")

        def load_slab(i_):
            if i_ >= 4 * NTG:
                return
            tg_ = i_ % NTG
            kb.ld(slab[i_ % 2][:], av[:, 0:KC, tg_ * TG:(tg_ + 1) * TG], (), [Bsl[i_ % 2]])
        load_w(0)
        load_slab(0)
        for cg in range(4):
            ws = cg % 2
            load_w(cg + 1)
            for tg in range(NTG):
                ss = ns % 2
                ns += 1
                load_slab(ns)
                for tt_ in range(TG // 128):
                    pi = n % 4
                    oi = n % 3
                    n += 1
                    for c in range(KC):
                        pq = 0
                        while c >= kq[pq + 1]:
                            pq += 1
                        kb.mm(pb[pi][:], slab[ss][:, c, tt_ * 128:(tt_ + 1) * 128], wres[ws][:, c, :], c == 0, c == KC - 1,
                              [Bsl[ss], Bwr[ws][pq]], [Bpb[pi]])
                    kb.cp("act" if n % 2 == 0 else "dve", ot[oi][:], pb[pi][:], [Bpb[pi]], [Bot[oi]])
                    r0 = tg * TG + tt_ * 128
                    kb.ld(m_d[r0:r0 + 128, cg * 512:(cg + 1) * 512], ot[oi][:], [Bot[oi]], (), owner=Bot[oi])
        P.end_phase()


def epi_store_setup(kb, n=2):
    def setup(es):
        P = kb.P
        return {"t": [kb.sb(es, [128, 2048], BF16, "eo") for _ in range(n)], "B": P.bufs(n, "eo"), "Bd": P.buf("qkvd"), "n": 0}
    return setup


def epi_store_fm(kb, qkv_d, tile_of):
    def epi(ctx, ti, c0, w, pb, Bpb):
        i = ctx["n"] % len(ctx["t"])
        ctx["n"] += 1
        t, B = ctx["t"][i], ctx["B"][i]
        kb.cp("act" if ti % 2 == 0 else "dve", t[0:w, :], pb[0:w, :], [Bpb], [B])
        kb.ld(qkv_d[tile_of(ti), 0:w, :], t[0:w, :], [B], (), owner=B)
    return epi


def epi_tok_setup(kb, dt=BF16):
    def setup(es):
        P = kb.P
        return {"t": [kb.sb(es, [128, 512], dt, "et") for _ in range(3)], "B": P.bufs(3, "et"), "Bd": P.buf("vtokd"), "n": 0}
    return setup


def epi_store_tok(kb, vtok_d, col_of):
    def epi(ctx, gi, t, c0, w, pb, Bpb):
        i = ctx["n"] % 3
        ctx["n"] += 1
        tl, B = ctx["t"][i], ctx["B"][i]
        kb.cp("act" if ctx["n"] % 2 == 0 else "dve", tl[:, 0:w], pb[:, 0:w], [Bpb], [B])
        d0 = col_of(gi)
        kb.ld(vtok_d[t * 128:(t + 1) * 128, d0:d0 + w], tl[:, 0:w], [B], (), owner=B)
    return epi


def phase_swa(kb, qkv_d, vtok_d, act_d, bias_sw_d, i8_d, sink_d):
    nc, P = kb.nc, kb.P
    with ExitStack() as es:
        vt = kb.sb(es, [128, 16, 512], BF16, "vt")
        Bvt = P.buf("vt")
        kb.ld(vt[:], vtok_d[:, 0:512].rearrange("(t p) c -> p t c", p=128), (), [Bvt])
        bsw = kb.sb(es, [128, 32, 256], BF16, "bsw")
        Bbsw = P.buf("bsw")
        kb.ld(bsw[:], bias_sw_d.rearrange("p (h q) -> p h q", h=32), (), [Bbsw], q="pool")
        i8 = kb.sb(es, [128, 128], BF16, "i8")
        Bi8 = P.buf("i8")
        kb.ld(i8[:], i8_d, (), [Bi8], q="pool")
        ones64 = kb.sb(es, [128, 64], BF16, "ones64")
        Bon = P.buf("ones")
        kb.memset("dve", ones64[:], 1.0, [Bon])
        skf = kb.sb(es, [1, 4096], F32, "skf")
        skb = kb.sb(es, [1, 4096], BF16, "skb")
        Bsk = P.buf("sk")
        kb.ld(skf[:], sink_d, (), [Bsk])
        kb.act(skb[:], skf[:], AF.Exp, [Bsk], [Bsk])
        kh = [kb.sb(es, [64, 2048], BF16, "kh") for _ in range(2)]
        qh = [kb.sb(es, [64, 4, 2048], BF16, "qh") for _ in range(2)]
        Bkq = P.bufs(2, "kq")
        PT = [kb.sb(es, [128, 4, 256], BF16, "PT") for _ in range(3)]
        BPT = P.bufs(3, "PT")
        oT = [kb.sb(es, [64, 4, 2048], BF16, "oT") for _ in range(2)]
        BoT = P.bufs(2, "oT")
        rec = [kb.sb(es, [64, 512], F32, "rec") for _ in range(2)]
        Brec = P.bufs(2, "rec")
        pss = [kb.ps(es, [128, 1024], F32, "pss") for _ in range(2)]
        Bpss = P.bufs(2, "pss")
        pnum = [kb.ps(es, [64, 512], F32, "pnum") for _ in range(2)]
        Bpn = P.bufs(2, "pnum")
        pden = [kb.ps(es, [64, 512], F32, "pden") for _ in range(2)]
        Bpd = P.bufs(2, "pden")
        Bact = P.buf("actd")
        nj = 0
        nb = 0

        def load_kv(kv_):
            if kv_ >= 8:
                return
            s_ = kv_ % 2
            kb.ld(kh[s_][:], qkv_d[16 + kv_ // 2, (kv_ % 2) * 64:(kv_ % 2) * 64 + 64, :], (), [Bkq[s_]])
            for g_ in range(4):
                hq_ = 4 * kv_ + g_
                kb.ld(qh[s_][:, g_, :], qkv_d[hq_ // 2, (hq_ % 2) * 64:(hq_ % 2) * 64 + 64, :], (), [Bkq[s_]])
        load_kv(0)
        for kv in range(8):
            s = kv % 2
            load_kv(kv + 1)

            def stageA(j, kv=kv, s=s, base=nj):
                nq = 256 if j < 15 else 128
                sl = (base + j) % 2
                pt = (base + j) % 3
                for g in range(4):
                    o = pss[sl][:, g * 256:g * 256 + nq]
                    kb.mm(o, kh[s][:, j * 128:(j + 1) * 128], qh[s][:, g, j * 128:j * 128 + nq], True, False, [Bkq[s]], [Bpss[sl]])
                    kb.mm(o, i8[:], bsw[:, 4 * kv + g, 0:nq], False, True, [Bi8, Bbsw], [Bpss[sl]])
                src = pss[sl][:].rearrange("p (g q) -> p g q", g=4)[:, :, 0:nq]
                kb.act(PT[pt][:, :, 0:nq], src, AF.Exp, [Bpss[sl]], [BPT[pt]], scale=0.125)

            def stageB(j, kv=kv, s=s, base=nj, nb0=nb):
                pt = (base + j) % 3
                prev = (base + j - 1) % 3 if j > 0 else None
                b = (nb0 + j) % 2
                rds = [Bvt, BPT[pt]] + ([BPT[prev]] if prev is not None else [])
                kb.mm(pden[b][:], ones64[0:1, 0:64], skb[0:1, 4 * kv * 128:(4 * kv + 4) * 128], True, False, [Bon, Bsk], [Bpd[b]])
                if prev is not None:
                    kb.mm(pnum[b][:], vt[:, j - 1, kv * 64:(kv + 1) * 64], PT[prev][:, :, 128:256], True, False, rds, [Bpn[b]])
                    kb.mm(pden[b][:], ones64[:], PT[prev][:, :, 128:256], False, False, rds + [Bon], [Bpd[b]])
                kb.mm(pnum[b][:], vt[:, j, kv * 64:(kv + 1) * 64], PT[pt][:, :, 0:128], prev is None, True, rds, [Bpn[b]])
                kb.mm(pden[b][:], ones64[:], PT[pt][:, :, 0:128], False, True, rds + [Bon], [Bpd[b]])
                kb.recip(rec[b][:], pden[b][:], [Bpd[b]], [Brec[b]])
                kb.tt("dve", oT[s][:, :, j * 128:(j + 1) * 128], pnum[b][:].rearrange("p (g q) -> p g q", g=4),
                      rec[b][:].rearrange("p (g q) -> p g q", g=4), ALU.mult, [Bpn[b], Brec[b]], [BoT[s]])
            stageA(0)
            for j in range(16):
                if j + 1 < 16:
                    stageA(j + 1)
                stageB(j)
            nj += 16
            nb += 16
            for g in range(4):
                hq = 4 * kv + g
                kb.ld(act_d[hq // 2, (hq % 2) * 64:(hq % 2) * 64 + 64, :], oT[s][:, g, :], [BoT[s]], (), owner=BoT[s])
        P.end_phase()


def phase_diff(kb, qkv_d, vtok_d, act_d, biasn_d, i8_d, table_d, lam_d, subg_d, layer_idx):
    nc, P = kb.nc, kb.P
    lam_init = 0.8 - 0.6 * math.exp(-0.3 * layer_idx)
    with ExitStack() as es:
        bn = kb.sb(es, [128, 32, 256], BF16, "bn")
        Bbn = P.buf("bn")
        kb.ld(bn[:], biasn_d.rearrange("p (h q) -> p h q", h=32), (), [Bbn], q="pool")
        i8 = kb.sb(es, [128, 128], BF16, "i8")
        Bi8 = P.buf("i8")
        kb.ld(i8[:], i8_d, (), [Bi8], q="pool")
        ones = kb.sb(es, [128, 128], BF16, "ones")
        onesf = kb.sb(es, [1, 128], F32, "onesf")
        Bon = P.buf("ones")
        kb.memset("dve", ones[:], 1.0, [Bon])
        kb.memset("dve", onesf[:], 1.0, [Bon])
        c31 = kb.sb(es, [128, 32], F32, "c31")
        Bc31 = P.buf("c31")
        kb.ld(c31[:], table_d[31].partition_broadcast(128), (), [Bc31])
        lamt = kb.sb(es, [1, 256], F32, "lamt")
        lsm = kb.sb(es, [1, 136], F32, "lsm")
        Blam = P.buf("lam")
        kb.ld(lamt[:], lam_d.rearrange("(o r) c -> o (r c)", o=1), (), [Blam])
        kb.tt("dve", lsm[:, 0:64], lamt[:, 0:64], lamt[:, 64:128], ALU.mult, [Blam], [Blam])
        kb.tt("dve", lsm[:, 64:128], lamt[:, 128:192], lamt[:, 192:256], ALU.mult, [Blam], [Blam])
        P.op("dve", lambda: nc.vector.tensor_reduce(out=lsm[:, 128:130], in_=lsm[:, 0:128].rearrange("p (a b) -> p a b", a=2), axis=AX.X, op=ALU.add), [Blam], [Blam])
        kb.act(lsm[:, 130:132], lsm[:, 128:130], AF.Exp, [Blam], [Blam])
        kb.tt("dve", lsm[:, 132:133], lsm[:, 130:131], lsm[:, 131:132], ALU.subtract, [Blam], [Blam])
        kb.ts("dve", lsm[:, 132:133], lsm[:, 132:133], lam_init, ALU.add, [Blam], [Blam])
        kb.cp("dve", lsm[:, 133:134], lsm[:, 132:133], [Blam], [Blam])
        pl = kb.ps(es, [128, 512], F32, "pl")
        Bpl = P.buf("pl")
        kb.mm(pl[:, 0:2], onesf[0:1, :], lsm[0:1, 132:134], True, True, [Bon, Blam], [Bpl])
        neglam = kb.sb(es, [128, 2], F32, "neglam")
        Bnl = P.buf("neglam")
        kb.ts("dve", neglam[:], pl[:, 0:2], -1.0, ALU.mult, [Bpl], [Bnl])
        gsc = kb.sb(es, [128, 1], F32, "gsc")
        Bgsc = P.buf("gsc")
        kb.ld(gsc[:], subg_d.rearrange("o e -> e o"), (), [Bgsc])
        kb.ts("dve", gsc[:], gsc[:], 1.0 - lam_init, ALU.mult, [Bgsc], [Bgsc])
        qh = [kb.sb(es, [128, 2048], BF16, "qh") for _ in range(2)]
        kh = [kb.sb(es, [128, 2048], BF16, "kh") for _ in range(2)]
        vh = [kb.sb(es, [128, 16, 128], BF16, "vh") for _ in range(2)]
        Bin = P.bufs(2, "qkv")
        PT = [kb.sb(es, [128, 512], BF16, "PT") for _ in range(4)]
        BPT = P.bufs(4, "PT")
        oTh = [kb.sb(es, [128, 2048], BF16, "oTh") for _ in range(2)]
        BoT = P.bufs(2, "oTh")
        r1 = kb.sb(es, [128, 512], F32, "r1")
        r2 = kb.sb(es, [128, 512], F32, "r2")
        t1 = kb.sb(es, [128, 512], F32, "t1")
        t2 = kb.sb(es, [128, 512], F32, "t2")
        sq = kb.sb(es, [128, 512], BF16, "sq")
        rs = kb.sb(es, [128, 512], F32, "rs")
        Bw = P.buf("work")
        Bsq = P.buf("sq")
        Brs = P.buf("rs")
        pss = [pl, kb.ps(es, [128, 512], F32, "pss"), kb.ps(es, [128, 512], F32, "pss")]
        Bpss = [Bpl, P.buf("pss"), P.buf("pss")]
        pnum = [kb.ps(es, [128, 512], F32, "pnum") for _ in range(2)]
        pden = [kb.ps(es, [128, 512], F32, "pden") for _ in range(2)]
        Bpn = P.bufs(2, "pn")
        Bpd = P.bufs(2, "pd")
        psq = kb.ps(es, [128, 512], F32, "psq")
        Bpsq = P.buf("psq")

        def load_h(h_):
            if h_ >= 16:
                return
            s_ = h_ % 2
            kb.ld(qh[s_][:], qkv_d[h_], (), [Bin[s_]])
            kb.ld(kh[s_][:], qkv_d[16 + h_], (), [Bin[s_]])
            kb.ld(vh[s_][:], vtok_d[:, h_ * 128:(h_ + 1) * 128].rearrange("(t p) e -> p t e", p=128), (), [Bin[s_]])
        load_h(0)
        base = 0
        NPS = 3
        for h in range(16):
            s = h % 2
            load_h(h + 1)
            its = [(c, m, j) for c in range(4) for m in range(2) for j in range(4 * c + 4)]

            def geom(c, j):
                lo = max(j, 4 * c)
                N = (4 * c + 4 - lo) * 128
                if j >= 4 * c:
                    nn, boff = min(N, 256), 0
                elif j == 4 * c - 1:
                    nn, boff = 128, 128
                else:
                    nn, boff = 0, 0
                return lo, N, nn, boff

            def stageA(idx):
                c, m, j = its[idx]
                lo, N, nn, boff = geom(c, j)
                pr = slice(m * 64, (m + 1) * 64)
                mp = 2 * h + m
                q0 = lo * 128
                sl = (base + idx) % NPS
                pt = (base + idx) % 4
                kb.mm(pss[sl][:, 0:N], kh[s][pr, j * 128:(j + 1) * 128], qh[s][pr, q0:q0 + N], True, nn == 0, [Bin[s]], [Bpss[sl]])
                if nn:
                    kb.mm(pss[sl][:, 0:nn], i8[:], bn[:, mp, boff:boff + nn], False, True, [Bi8, Bbn], [Bpss[sl]])
                    kb.act(PT[pt][:, 0:nn], pss[sl][:, 0:nn], AF.Exp, [Bpss[sl]], [BPT[pt]], scale=0.125)
                if N > nn:
                    kb.act(PT[pt][:, nn:N], pss[sl][:, nn:N], AF.Exp, [Bpss[sl], Bc31], [BPT[pt]], scale=0.125, bias=c31[:, mp:mp + 1])

            def stageB(idx):
                c, m, j = its[idx]
                lo, N, nn, boff = geom(c, j)
                off = (lo - 4 * c) * 128
                jl = 4 * c + 3
                pt = (base + idx) % 4
                kb.mm(pnum[m][:, off:512], vh[s][:, j, :], PT[pt][:, 0:N], j == 0, j == jl, [Bin[s], BPT[pt]], [Bpn[m]])
                kb.mm(pden[m][:, off:512], ones[:], PT[pt][:, 0:N], j == 0, j == jl, [Bon, BPT[pt]], [Bpd[m]])
                if m == 1 and j == jl:
                    kb.recip(r1[:], pden[0][:], [Bpd[0]], [Bw])
                    kb.recip(r2[:], pden[1][:], [Bpd[1]], [Bw])
                    kb.tt("dve", t1[:], pnum[0][:], r1[:], ALU.mult, [Bpn[0], Bw], [Bw])
                    kb.tt("dve", t2[:], pnum[1][:], r2[:], ALU.mult, [Bpn[1], Bw], [Bw])
                    kb.stt(t1[:], t2[:], neglam[:, 0:1], t1[:], ALU.mult, ALU.add, [Bw, Bnl], [Bw])
                    kb.act(sq[:], t1[:], AF.Square, [Bw], [Bsq])
                    kb.mm(psq[:], ones[:], sq[:], True, True, [Bon, Bsq], [Bpsq])
                    kb.ts("dve", rs[:], psq[:], 1.0 / 128, ALU.mult, [Bpsq], [Brs], s2=EPS, op1=ALU.add)
                    kb.act(rs[:], rs[:], AF.Sqrt, [Brs], [Brs])
                    kb.recip(rs[:], rs[:], [Brs], [Brs])
                    kb.stt(oTh[s][:, c * 512:(c + 1) * 512], t1[:], gsc[:, 0:1], rs[:], ALU.mult, ALU.mult, [Bw, Bgsc, Brs], [BoT[s]])
            LA = 2
            for i0 in range(min(LA, len(its))):
                stageA(i0)
            for idx in range(len(its)):
                if idx + LA < len(its):
                    stageA(idx + LA)
                stageB(idx)
            base += len(its)
            kb.ld(act_d[h], oTh[s][:], [BoT[s]], (), owner=BoT[s])
        P.end_phase()


def phase_dsa_idx(kb, qkv_d, wi_d, mask_d, cmask_d, ident_d):
    nc, P = kb.nc, kb.P
    BIG = 1.0e30
    with ExitStack() as es:
        kiT = kb.sb(es, [64, 2048], BF16, "kiT")
        qi = kb.sb(es, [64, 16, 2048], BF16, "qi")
        Bqk = P.buf("qiki")
        kb.ld(kiT[:], qkv_d[28, 0:64, :], (), [Bqk])
        for h in range(16):
            kb.ld(qi[:, h, :], qkv_d[20 + h // 2, (h % 2) * 64:(h % 2) * 64 + 64, :], (), [Bqk])
        cm = kb.sb(es, [128, 128], F32, "cm")
        Bcm = P.buf("cm")
        kb.ld(cm[:], cmask_d, (), [Bcm])
        ident = kb.sb(es, [128, 128], BF16, "ident")
        Bid = P.buf("ident")
        kb.ld(ident[:], ident_d, (), [Bid], q="pool")
        zt = kb.sb(es, [128, 256], BF16, "zt")
        Bzt = P.buf("zt")
        kb.memset("pool", zt[:], 0.0, [Bzt])
        kb.ld(mask_d[0, :, 0:256], zt[:], [Bzt], (), owner=Bzt)
        kb.ld(mask_d[1, :, 0:256], zt[:], [Bzt], (), owner=Bzt)
        wi = [kb.sb(es, [128, 16], F32, "wi") for _ in range(2)]
        Bwi = P.bufs(2, "wi")
        acc = [kb.sb(es, [128, 2048], F32, "acc") for _ in range(2)]
        Bacc = P.bufs(2, "acc")
        Rb = [kb.sb(es, [128, 1024], F32, "Rb") for _ in range(2)]
        BRb = P.bufs(2, "Rb")
        work = kb.sb(es, [128, 2048], F32, "work")
        Bwork = P.buf("work")
        m8 = kb.sb(es, [128, 8], F32, "m8")
        Bm8 = P.buf("m8")
        Mb = [kb.sb(es, [128, 2048], BF16, "Mb") for _ in range(2)]
        BMb = P.bufs(2, "Mb")
        mT = [kb.sb(es, [128, 8, 128], BF16, "mT") for _ in range(2)]
        BmT = P.bufs(2, "mT")
        pl = [kb.ps(es, [128, 1024], F32, "pl") for _ in range(2)]
        Bpl = P.bufs(2, "pl")
        pT = [kb.ps(es, [128, 1024], BF16, "pT") for _ in range(2)]
        BpT = P.bufs(2, "pT")
        nl = 0
        nT = 0
        for qt in range(2, 16):
            a = qt % 2
            ns = (qt + 1) * 128
            kb.ld(wi[a][:], wi_d[qt * 128:(qt + 1) * 128, :], (), [Bwi[a]])
            for h in range(16):
                for half in range((ns + 1023) // 1024):
                    n = min(1024, ns - half * 1024)
                    sl = nl % 2
                    nl += 1
                    for sc in range((n + 511) // 512):
                        n2 = min(512, n - sc * 512)
                        c0 = half * 1024 + sc * 512
                        kb.mm(pl[sl][:, sc * 512:sc * 512 + n2], qi[:, h, qt * 128:(qt + 1) * 128], kiT[:, c0:c0 + n2], True, True, [Bqk], [Bpl[sl]])
                    kb.act(Rb[sl][:, 0:n], pl[sl][:, 0:n], AF.Relu, [Bpl[sl]], [BRb[sl]])
                    av = acc[a][:, half * 1024:half * 1024 + n]
                    if h == 0:
                        kb.ts("dve", av, Rb[sl][:, 0:n], wi[a][:, 0:1], ALU.mult, [BRb[sl], Bwi[a]], [Bacc[a]])
                    else:
                        kb.stt(av, Rb[sl][:, 0:n], wi[a][:, h:h + 1], av, ALU.mult, ALU.add, [BRb[sl], Bwi[a], Bacc[a]], [Bacc[a]])
            dg = acc[a][:, qt * 128:(qt + 1) * 128]
            kb.tt("dve", dg, dg, cm[:], ALU.add, [Bacc[a], Bcm], [Bacc[a]])
            src, Bsrc = acc[a], Bacc[a]
            for r in range(32):
                sv = src[:, 0:ns]
                P.op("dve", (lambda sv=sv: nc.vector.max(out=m8[:], in_=sv)), [Bsrc], [Bm8])
                if r < 31:
                    wv_ = work[:, 0:ns]
                    P.op("dve", (lambda sv=sv, wv_=wv_: nc.vector.match_replace(out=wv_, in_to_replace=m8[:], in_values=sv, imm_value=-BIG)), [Bsrc, Bm8], [Bwork])
                    src, Bsrc = work, Bwork
            kb.ts("dve", Mb[a][:, 0:ns], acc[a][:, 0:ns], m8[:, 7:8], ALU.is_ge, [Bacc[a], Bm8], [BMb[a]])
            for j0 in range(0, qt + 1, 8):
                njj = min(8, qt + 1 - j0)
                b = nT % 2
                nT += 1
                for i in range(njj):
                    j = j0 + i
                    kb.tr(pT[b][:, i * 128:(i + 1) * 128], Mb[a][:, j * 128:(j + 1) * 128], ident[:], [BMb[a], Bid], [BpT[b]])
                srcv = pT[b][:].rearrange("p (j q) -> p j q", j=8)[:, 0:njj, :]
                kb.ts("dve" if b == 0 else "dve", mT[b][:, 0:njj, :], srcv, -1.0, ALU.add, [BpT[b]], [BmT[b]], s2=-MASKV, op1=ALU.mult)
                kb.ld(mask_d[j0:j0 + njj, :, qt * 128:(qt + 1) * 128].rearrange("j p q -> p j q"), mT[b][:, 0:njj, :], [BmT[b]], (), owner=BmT[b])
        P.end_phase()


def phase_dsa_attn(kb, qkv_d, vtok_d, act_d, mask_d, biasn_d, i8_d, table_d):
    nc, P = kb.nc, kb.P
    with ExitStack() as es:
        bn = kb.sb(es, [128, 32, 256], BF16, "bn")
        Bbn = P.buf("bn")
        kb.ld(bn[:], biasn_d.rearrange("p (h q) -> p h q", h=32), (), [Bbn], q="pool")
        i8 = kb.sb(es, [128, 128], BF16, "i8")
        Bi8 = P.buf("i8")
        kb.ld(i8[:], i8_d, (), [Bi8], q="pool")
        ones = kb.sb(es, [128, 64], BF16, "ones")
        Bon = P.buf("ones")
        kb.memset("dve", ones[:], 1.0, [Bon])
        c31 = kb.sb(es, [128, 32], F32, "c31")
        Bc31 = P.buf("c31")
        kb.ld(c31[:], table_d[31].partition_broadcast(128), (), [Bc31])
        vt = kb.sb(es, [128, 16, 512], BF16, "vt")
        Bvt = P.buf("vt")
        kb.ld(vt[:], vtok_d[:, 0:512].rearrange("(t p) c -> p t c", p=128), (), [Bvt])
        mk = kb.sb(es, [128, 16, 2048], BF16, "mk")
        Bmk = P.buf("mk")
        for j in range(16):
            kb.ld(mk[:, j, j * 128:2048], mask_d[j, :, j * 128:2048], (), [Bmk])
        kh = [kb.sb(es, [64, 2048], BF16, "kh") for _ in range(2)]
        Bkh = P.bufs(2, "kh")
        qh = [kb.sb(es, [64, 2048], BF16, "qh") for _ in range(2)]
        Bqh = P.bufs(2, "qh")
        PT = [kb.sb(es, [128, 512], BF16, "PT") for _ in range(4)]
        BPT = P.bufs(4, "PT")
        oTh = [kb.sb(es, [64, 2048], BF16, "oTh") for _ in range(2)]
        BoT = P.bufs(2, "oTh")
        r1 = [kb.sb(es, [64, 512], F32, "r1") for _ in range(2)]
        Br1 = P.bufs(2, "r1")
        pss = [kb.ps(es, [128, 512], F32, "pss") for _ in range(4)]
        Bpss = P.bufs(4, "pss")
        pnum = [kb.ps(es, [64, 512], F32, "pnum") for _ in range(2)]
        pden = [kb.ps(es, [64, 512], F32, "pden") for _ in range(2)]
        Bpn = P.bufs(2, "pn")
        Bpd = P.bufs(2, "pd")

        def load_q(hq_):
            if hq_ >= 32:
                return
            kb.ld(qh[hq_ % 2][:], qkv_d[hq_ // 2, (hq_ % 2) * 64:(hq_ % 2) * 64 + 64, :], (), [Bqh[hq_ % 2]])

        def load_k(kv_):
            if kv_ >= 8:
                return
            kb.ld(kh[kv_ % 2][:], qkv_d[16 + kv_ // 2, (kv_ % 2) * 64:(kv_ % 2) * 64 + 64, :], (), [Bkh[kv_ % 2]])
        load_k(0)
        load_q(0)
        nj = 0
        nb = 0
        for kv in range(8):
            ks = kv % 2
            load_k(kv + 1)
            for g in range(4):
                hq = 4 * kv + g
                s = hq % 2
                load_q(hq + 1)
                its = [(c, j) for c in range(4) for j in range(4 * c + 4)]

                def geom(c, j):
                    lo = max(j, 4 * c)
                    N = (4 * c + 4 - lo) * 128
                    if j >= 4 * c:
                        nn, boff = min(N, 256), 0
                    elif j == 4 * c - 1:
                        nn, boff = 128, 128
                    else:
                        nn, boff = 0, 0
                    return lo, N, nn, boff

                def stageA(idx, hq=hq, ks=ks, s=s, base=nj):
                    c, j = its[idx]
                    lo, N, nn, boff = geom(c, j)
                    q0 = lo * 128
                    sl = (base + idx) % 4
                    pt = (base + idx) % 4
                    kb.mm(pss[sl][:, 0:N], kh[ks][:, j * 128:(j + 1) * 128], qh[s][:, q0:q0 + N], True, False, [Bkh[ks], Bqh[s]], [Bpss[sl]])
                    if nn:
                        kb.mm(pss[sl][:, 0:nn], i8[:], bn[:, hq, boff:boff + nn], False, False, [Bi8, Bbn], [Bpss[sl]])
                    kb.mm(pss[sl][:, 0:N], i8[:], mk[:, j, q0:q0 + N], False, True, [Bi8, Bmk], [Bpss[sl]])
                    if nn:
                        kb.act(PT[pt][:, 0:nn], pss[sl][:, 0:nn], AF.Exp, [Bpss[sl]], [BPT[pt]], scale=0.125)
                    if N > nn:
                        kb.act(PT[pt][:, nn:N], pss[sl][:, nn:N], AF.Exp, [Bpss[sl], Bc31], [BPT[pt]], scale=0.125, bias=c31[:, hq:hq + 1])

                def stageB(idx, hq=hq, kv=kv, s=s, base=nj, nb0=nb):
                    c, j = its[idx]
                    lo, N, nn, boff = geom(c, j)
                    off = (lo - 4 * c) * 128
                    jl = 4 * c + 3
                    pt = (base + idx) % 4
                    b = (nb0 + c) % 2
                    kb.mm(pnum[b][:, off:512], vt[:, j, kv * 64:(kv + 1) * 64], PT[pt][:, 0:N], j == 0, j == jl, [Bvt, BPT[pt]], [Bpn[b]])
                    kb.mm(pden[b][:, off:512], ones[:], PT[pt][:, 0:N], j == 0, j == jl, [Bon, BPT[pt]], [Bpd[b]])
                    if j == jl:
                        kb.recip(r1[b][:], pden[b][:], [Bpd[b]], [Br1[b]])
                        kb.tt("dve", oTh[s][:, c * 512:(c + 1) * 512], pnum[b][:], r1[b][:], ALU.mult, [Bpn[b], Br1[b]], [BoT[s]])
                LA = 2
                for i0 in range(min(LA, len(its))):
                    stageA(i0)
                for idx in range(len(its)):
                    if idx + LA < len(its):
                        stageA(idx + LA)
                    stageB(idx)
                nj += len(its)
                nb += 4
                kb.ld(act_d[hq // 2, (hq % 2) * 64:(hq % 2) * 64 + 64, :], oTh[s][:], [BoT[s]], (), owner=BoT[s])
        P.end_phase()


def hgrn_inproj(kb, hT, BhT, w_in, lbl_d, layer_idx, hq_d, hf_d, hk_d, hv_d, qkv_d):
    nc, P = kb.nc, kb.P
    tiles = [(i * 128, 128) for i in range(16)] + [(2048 + i * 128, 128) for i in range(16)] + [(6144 + i * 128, 128) for i in range(16)]

    def setup(es):
        c = {}
        lbl = kb.sb(es, [128, 16, 4], F32, "lbl")
        Bl = P.buf("lbl")
        kb.ld(lbl[:], lbl_d.rearrange("p (t l) -> p t l", l=4), (), [Bl])
        kb.act(lbl[:], lbl[:], AF.Exp, [Bl], [Bl])
        ssum = kb.sb(es, [128, 16], F32, "ssum")
        lb = kb.sb(es, [128, 16], F32, "lb")
        oml = kb.sb(es, [128, 16], F32, "oml")
        Blb = P.buf("lb")
        P.op("dve", lambda: nc.vector.tensor_reduce(out=ssum[:], in_=lbl[:], axis=AX.X, op=ALU.add), [Bl], [Blb])
        kb.recip(ssum[:], ssum[:], [Blb], [Blb])
        kb.cp("dve", lb[:], lbl[:, :, 1], [Bl, Blb], [Blb])
        for j in range(2, layer_idx + 1):
            kb.tt("dve", lb[:], lb[:], lbl[:, :, j], ALU.add, [Bl, Blb], [Blb])
        kb.tt("dve", lb[:], lb[:], ssum[:], ALU.mult, [Blb], [Blb])
        kb.ts("dve", oml[:], lb[:], -1.0, ALU.mult, [Blb], [Blb], s2=1.0, op1=ALU.add)
        c["lb"], c["oml"], c["Blb"] = lb, oml, Blb
        c["fo"] = [kb.sb(es, [128, 2048], F32, "fo") for _ in range(2)]
        c["Bfo"] = P.bufs(2, "fo")
        c["fk"] = [kb.sb(es, [128, 2048], F32, "fk") for _ in range(2)]
        c["Bfk"] = P.bufs(2, "fk")
        c["bo"] = [kb.sb(es, [128, 2048], BF16, "bo") for _ in range(2)]
        c["Bbo"] = P.bufs(2, "bo")
        return c

    def epi(c, ti, c0, w, pb, Bpb):
        kind, h = ti // 16, ti % 16
        a = ti % 2
        if kind == 0:
            kb.act(c["fo"][a][:], pb[:], AF.Silu, [Bpb], [c["Bfo"][a]])
            kb.ld(hq_d[h], c["fo"][a][:], [c["Bfo"][a]], (), owner=c["Bfo"][a])
        elif kind == 1:
            fo, Bfo, fk, Bfk = c["fo"][a], c["Bfo"][a], c["fk"][a], c["Bfk"][a]
            kb.act(fo[:], pb[:], AF.Sigmoid, [Bpb], [Bfo])
            kb.ts("dve", fo[:], fo[:], c["oml"][:, h:h + 1], ALU.mult, [Bfo, c["Blb"]], [Bfo], s2=c["lb"][:, h:h + 1], op1=ALU.add)
            kb.ts("dve", fk[:], fo[:], -1.0, ALU.mult, [Bfo], [Bfk], s2=1.0, op1=ALU.add)
            kb.ld(hk_d[h], fk[:], [Bfk], (), owner=Bfk)
            kb.act(fo[:], fo[:], AF.Ln, [Bfo], [Bfo])
            kb.ld(hf_d[h], fo[:], [Bfo], (), owner=Bfo)
        else:
            kb.act(c["bo"][a][:], pb[:], AF.Silu, [Bpb], [c["Bbo"][a]])
            kb.ld(qkv_d[h], c["bo"][a][:], [c["Bbo"][a]], (), owner=c["Bbo"][a])
    phase_proj_fm(kb, hT, BhT, w_in, tiles, epi, setup)
    phase_proj_tok(kb, hT, BhT, w_in, [(4096 + i * 512, 512) for i in range(4)],
                   epi_store_tok(kb, hv_d, lambda gi: gi * 512), epi_tok_setup(kb, F32))


def phase_hgrn(kb, hq_d, hf_d, hk_d, hv_d, qkv_d, act_d, ng_d, ident_d, tri_d):
    nc, P = kb.nc, kb.P
    with ExitStack() as es:
        identf = kb.sb(es, [128, 128], F32, "identf")
        tri = kb.sb(es, [64, 512], F32, "tri")
        ng = kb.sb(es, [128, 1], F32, "ng")
        Bc = P.buf("consts")
        kb.ld(identf[:], ident_d, (), [Bc])
        kb.ld(tri[:], tri_d, (), [Bc])
        kb.ld(ng[:], ng_d.rearrange("o e -> e o"), (), [Bc])
        ones = kb.sb(es, [128, 128], BF16, "ones")
        rmask = kb.sb(es, [128, 2048], F32, "rmask")
        Bon = P.buf("ones")
        kb.memset("dve", ones[:], 1.0, [Bon])
        kb.memset("dve", rmask[:], 1.0, [Bon])
        kb.memset("dve", rmask[:].rearrange("p (c t) -> p c t", t=64)[:, :, 0:1], 0.0, [Bon])
        A = kb.sb(es, [128, 2048], F32, "A")
        Bb = kb.sb(es, [128, 2048], F32, "B")
        C = kb.sb(es, [128, 2048], F32, "C")
        Dd = kb.sb(es, [128, 2048], F32, "Dd")
        E = kb.sb(es, [128, 2048], F32, "E")
        Fk = kb.sb(es, [128, 2048], F32, "F")
        BA, BB, BC, BD, BE, BF_ = P.buf("A"), P.buf("B"), P.buf("C"), P.buf("D"), P.buf("E"), P.buf("F")
        U = kb.sb(es, [128, 128 * 33], F32, "U")
        Dk = kb.sb(es, [128, 128 * 33], F32, "Dk")
        St = kb.sb(es, [128, 128 * 33], F32, "St")
        BU, BDk, BSt = P.buf("U"), P.buf("Dk"), P.buf("St")
        U3 = U[:].rearrange("p (v c) -> p v c", c=33)
        Dk3 = Dk[:].rearrange("p (v c) -> p v c", c=33)
        St3 = St[:].rearrange("p (v c) -> p v c", c=33)
        Stc = U[:, 0:4096].rearrange("p (c v) -> p c v", v=128)
        kb.memset("pool", Dk[:], 0.0, [BDk])
        kb.memset("pool", U[:], 0.0, [BU])
        vtk = kb.sb(es, [64, 32, 128], F32, "vtk")
        Bvtk = P.buf("vtk")
        kendT = kb.sb(es, [64, 32, 128], F32, "kendT")
        BkT = P.buf("kendT")
        attm = kb.sb(es, [64, 2048], F32, "attm")
        Batt = P.buf("attm")
        o_h = kb.sb(es, [128, 2048], F32, "o_h")
        Boh = P.buf("o_h")
        gs = kb.sb(es, [128, 2048], BF16, "gs")
        Bgs = P.buf("gs")
        oTh = kb.sb(es, [128, 2048], BF16, "oTh")
        BoT = P.buf("oTh")
        ebend = kb.sb(es, [128, 32], F32, "ebend")
        Beb = P.buf("ebend")
        sq = kb.sb(es, [128, 512], BF16, "sq")
        rs = kb.sb(es, [128, 512], F32, "rs")
        tmp = kb.sb(es, [128, 512], F32, "tmp")
        Bsq, Brs, Btmp = P.buf("sq"), P.buf("rs"), P.buf("tmp")
        ptr = [kb.ps(es, [128, 512], F32, "ptr") for _ in range(2)]
        Bptr = P.bufs(2, "ptr")
        patt = kb.ps(es, [128, 512], F32, "patt")
        Bpatt = P.buf("patt")
        pupd = [kb.ps(es, [128, 512], F32, "pupd") for _ in range(2)]
        Bpupd = P.bufs(2, "pupd")
        po = [kb.ps(es, [128, 512], F32, "po") for _ in range(2)]
        Bpo = P.bufs(2, "po")
        psq = kb.ps(es, [128, 512], F32, "psq")
        Bpsq = P.buf("psq")
        B3 = Bb[:].rearrange("p (c t) -> p c t", t=64)
        A3 = A[:].rearrange("p (c t) -> p c t", t=64)
        for h in range(16):
            kb.ld(C[:], hq_d[h], (), [BC])
            kb.ld(A[:], hf_d[h], (), [BA])
            kb.ld(Dd[:], hk_d[h], (), [BD])
            kb.ld(vtk[:], hv_d[:, h * 128:(h + 1) * 128].rearrange("(c s) v -> s c v", s=64), (), [Bvtk])
            kb.ld(gs[:], qkv_d[h], (), [Bgs])
            P.op("dve", lambda: nc.vector.tensor_tensor_scan(out=Bb[:], data0=rmask[:], data1=A[:], initial=0.0, op0=ALU.mult, op1=ALU.add),
                 [Bon, BA], [BB])
            kb.act(A[:], Bb[:], AF.Exp, [BB], [BA])
            kb.tt("dve", C[:], C[:], A[:], ALU.mult, [BC, BA], [BC])
            kb.act(ebend[:], B3[:, :, 63], AF.Exp, [BB], [Beb])
            kb.ts("dve", A[:], Bb[:], -1.0, ALU.mult, [BB, BC], [BA], s2=80.0, op1=ALU.min)
            kb.act(A[:], A[:], AF.Exp, [BA], [BA])
            kb.tt("dve", E[:], Dd[:], A[:], ALU.mult, [BD, BA], [BE])
            kb.tt("dve", A3, B3[:, :, 63:64].to_broadcast([128, 32, 64]), B3, ALU.subtract, [BB, BE], [BA])
            kb.act(A[:], A[:], AF.Exp, [BA], [BA])
            kb.tt("dve", Fk[:], Dd[:], A[:], ALU.mult, [BD, BA], [BF_])
            for c in range(32):
                b = (c // 4) % 2
                kb.tr(ptr[b][0:64, (c % 4) * 128:(c % 4 + 1) * 128], Fk[:, c * 64:(c + 1) * 64], identf[:], [BF_, Bc], [Bptr[b]])
                if c % 4 == 3:
                    kb.cp("act", kendT[:, c - 3:c + 1, :], ptr[b][0:64, :].rearrange("p (c k) -> p c k", c=4), [Bptr[b]], [BkT])
            for grp in range(4):
                for i in range(8):
                    c = grp * 8 + i
                    kb.mm(patt[0:64, i * 64:(i + 1) * 64], E[:, c * 64:(c + 1) * 64], C[:, c * 64:(c + 1) * 64], True, True, [BE, BC], [Bpatt])
                kb.tt("dve", attm[:, grp * 512:(grp + 1) * 512], patt[0:64, :], tri[:], ALU.mult, [Bpatt, Bc], [Batt])
            kb.memset("dve", U3[:, :, 0:1], 0.0, [BU])
            for grp in range(8):
                b = grp % 2
                for i in range(4):
                    c = grp * 4 + i
                    kb.mm(pupd[b][:, i * 128:(i + 1) * 128], kendT[:, c, :], vtk[:, c, :], True, True, [BkT, Bvtk], [Bpupd[b]])
                kb.cp("act" if b == 0 else "dve", U3[:, :, grp * 4 + 1:grp * 4 + 5].rearrange("p v c -> p c v"),
                      pupd[b][:].rearrange("p (c v) -> p c v", c=4), [Bpupd[b]], [BU])
            kb.cp("act", Dk3[:, :, 1:33], ebend[:].unsqueeze(1).to_broadcast([128, 128, 32]), [Beb], [BDk])
            P.op("dve", lambda: nc.vector.tensor_tensor_scan(out=St[:], data0=Dk[:], data1=U[:], initial=0.0, op0=ALU.mult, op1=ALU.add),
                 [BDk, BU], [BSt])
            kb.cp("act", Stc, St3[:, :, 0:32].rearrange("p v c -> p c v"), [BSt], [BU])
            for grp in range(4):
                b = grp % 2
                for i in range(8):
                    c = grp * 8 + i
                    kb.mm(po[b][:, i * 64:(i + 1) * 64], vtk[:, c, :], attm[:, c * 64:(c + 1) * 64], True, False, [Bvtk, Batt], [Bpo[b]])
                    kb.mm(po[b][:, i * 64:(i + 1) * 64], Stc[:, c, :], C[:, c * 64:(c + 1) * 64], False, True, [BU, BC], [Bpo[b]])
                kb.cp("act", o_h[:, grp * 512:(grp + 1) * 512], po[b][:], [Bpo[b]], [Boh])
            for cc in range(4):
                cs = slice(cc * 512, (cc + 1) * 512)
                kb.act(sq[:], o_h[:, cs], AF.Square, [Boh], [Bsq])
                kb.mm(psq[:], ones[:], sq[:], True, True, [Bon, Bsq], [Bpsq])
                kb.ts("dve", rs[:], psq[:], 1.0 / 128, ALU.mult, [Bpsq], [Brs], s2=EPS, op1=ALU.add)
                kb.act(rs[:], rs[:], AF.Sqrt, [Brs], [Brs])
                kb.recip(rs[:], rs[:], [Brs], [Brs])
                kb.stt(tmp[:], o_h[:, cs], ng[:, 0:1], rs[:], ALU.mult, ALU.mult, [Boh, Bc, Brs], [Btmp])
                kb.tt("dve", oTh[:, cs], tmp[:], gs[:, cs], ALU.mult, [Btmp, Bgs], [BoT])
            kb.ld(act_d[h], oTh[:], [BoT], (), owner=BoT)
        P.end_phase()

LAYER_NIN = {0: 3072, 1: 6144, 2: 8192, 3: 4176}
MIX_NAMES = {0: "swa", 1: "diff", 2: "hgrn", 3: "dsa"}


def build_program(layers, stop_after=None):
    nc = bass.Bass("TRN2", target_bir_lowering=False)
    dr = {}

    def ext(name, shape, dt=F32):
        dr[name] = nc.dram_tensor(name, list(shape), dt, kind="ExternalInput").ap()
        return dr[name]

    x_in = ext("x", [S, D])
    y_out = nc.dram_tensor("y", [S, D], F32, kind="ExternalOutput").ap()
    normg = ext("norm_g", [16, D])
    ident_d = ext("ident", [128, 128])
    i8_d = ext("i8", [128, 128])
    for l in layers:
        ext("w_in%d" % l, [D, LAYER_NIN[l]])
        ext("w_out%d" % l, [D, D])
        ext("w_up%d" % l, [D, 2 * DFF])
        ext("w_down%d" % l, [DFF, D])
        ext("conv%d" % l, [128, 88, 3])
    if 0 in layers:
        ext("bias_sw", [128, 32 * 256])
        ext("sinkrep", [1, 4096])
    if 1 in layers or 3 in layers:
        ext("biasn", [128, 32 * 256])
        ext("table", [32, 32])
    if 1 in layers:
        ext("diff_lambda", [4, 64])
        ext("diff_subg", [1, 128])
    xres = nc.dram_tensor("xres", [S, D], F32).ap()
    m_d = nc.dram_tensor("m_d", [S, D], F32).ap()
    act_d = nc.dram_tensor("act_d", [KFF, 128, S], BF16).ap()
    qkv_d = nc.dram_tensor("qkv_d", [64, 128, S], BF16).ap()
    vtok_d = nc.dram_tensor("vtok_d", [S, 2048], BF16).ap()
    if 2 in layers:
        ext("lbl", [128, 64])
        ext("hgrn_ng", [1, 128])
        ext("tri", [64, 512])
        hq_d = nc.dram_tensor("hq_d", [16, 128, S], F32).ap()
        hf_d = nc.dram_tensor("hf_d", [16, 128, S], F32).ap()
        hk_d = nc.dram_tensor("hk_d", [16, 128, S], F32).ap()
        hv_d = nc.dram_tensor("hv_d", [S, 2048], F32).ap()
    if 3 in layers:
        ext("cmask", [128, 128])
        wi_d = nc.dram_tensor("wi_d", [S, 16], F32).ap()
        mask_d = nc.dram_tensor("mask_d", [16, 128, S], BF16).ap()

    with ExitStack() as top:
        P = Prog(nc, top)
        kb = KB(nc, P)
        x_cur = x_in
        pend_m = None
        for li, l in enumerate(layers):
            with ExitStack() as hs:
                hT = kb.sb(hs, [128, 16, S], BF16, "hT")
                BhT = Buf("hT")
                phase_rn(kb, x_cur, xres, m_d if pend_m is not None else None,
                         normg[pend_m] if pend_m is not None else None, normg[4 * l + 0], hT, BhT, ident_d)
                if pend_m is not None:
                    x_cur = xres
                w_in = dr["w_in%d" % l]
                if l == 0:
                    tiles = [(i * 128, 128) for i in range(20)]
                    phase_proj_fm(kb, hT, BhT, w_in, tiles, epi_store_fm(kb, qkv_d, lambda ti: ti), epi_store_setup(kb))
                    phase_proj_tok(kb, hT, BhT, w_in, [(2560, 512)], epi_store_tok(kb, vtok_d, lambda gi: 0), epi_tok_setup(kb))
                elif l == 1:
                    tiles = [(i * 128, 128) for i in range(32)]
                    phase_proj_fm(kb, hT, BhT, w_in, tiles, epi_store_fm(kb, qkv_d, lambda ti: ti), epi_store_setup(kb))
                    phase_proj_tok(kb, hT, BhT, w_in, [(4096 + i * 512, 512) for i in range(4)],
                                   epi_store_tok(kb, vtok_d, lambda gi: gi * 512), epi_tok_setup(kb))
                elif l == 2:
                    hgrn_inproj(kb, hT, BhT, w_in, dr["lbl"], l, hq_d, hf_d, hk_d, hv_d, qkv_d)
                elif l == 3:
                    tiles = [(i * 128, 128) for i in range(20)] + [(3072 + i * 128, 128) for i in range(8)] + [(4096, 64)]
                    phase_proj_fm(kb, hT, BhT, w_in, tiles, epi_store_fm(kb, qkv_d, lambda ti: ti), epi_store_setup(kb))

                    def setup3(es):
                        c_ = epi_tok_setup(kb)(es)
                        c_["tf"] = [kb.sb(es, [128, 16], F32, "etf") for _ in range(2)]
                        c_["Bf"] = kb.P.bufs(2, "etf")
                        return c_

                    def epi3(ctx, gi, t, c0, w, pb, Bpb):
                        if gi == 0:
                            epi_store_tok(kb, vtok_d, lambda gi_: 0)(ctx, gi, t, c0, w, pb, Bpb)
                        else:
                            tl, B = ctx["tf"][t % 2], ctx["Bf"][t % 2]
                            kb.cp("dve", tl[:, 0:16], pb[:, 0:16], [Bpb], [B])
                            kb.ld(wi_d[t * 128:(t + 1) * 128, :], tl[:, 0:16], [B], (), owner=B)
                    phase_proj_tok(kb, hT, BhT, w_in, [(2560, 512), (4160, 16)], epi3, setup3)
            if l == 0:
                phase_swa(kb, qkv_d, vtok_d, act_d, dr["bias_sw"], i8_d, dr["sinkrep"])
            elif l == 2:
                phase_hgrn(kb, hq_d, hf_d, hk_d, hv_d, qkv_d, act_d, dr["hgrn_ng"], ident_d, dr["tri"])
            elif l == 3:
                phase_dsa_idx(kb, qkv_d, wi_d, mask_d, dr["cmask"], ident_d)
                phase_dsa_attn(kb, qkv_d, vtok_d, act_d, mask_d, dr["biasn"], i8_d, dr["table"])
            elif l == 1:
                phase_diff(kb, qkv_d, vtok_d, act_d, dr["biasn"], i8_d, dr["table"], dr["diff_lambda"], dr["diff_subg"], l)
            phase_down(kb, dr["w_out%d" % l], 16, act_d, m_d)
            with ExitStack() as hs:
                hT = kb.sb(hs, [128, 16, S], BF16, "hT")
                BhT = Buf("hT")
                phase_rn(kb, x_cur, xres, m_d, normg[4 * l + 1], normg[4 * l + 2], hT, BhT, ident_d)
                x_cur = xres
                phase_up(kb, hT, BhT, dr["w_up%d" % l], dr["conv%d" % l], act_d)
            phase_down(kb, dr["w_down%d" % l], KFF, act_d, m_d)
            pend_m = 4 * l + 3
        phase_rn(kb, x_cur, y_out, m_d, normg[pend_m], None, None, None, ident_d)
    return nc


def _bucket(n):
    n = np.maximum(n, 0)
    max_exact = 16
    lr = np.log(np.maximum(n, 1).astype(np.float32) / max_exact) / math.log(128 / max_exact)
    large = np.minimum(max_exact + (lr * (32 - max_exact)).astype(np.int32), 31)
    return np.where(n < max_exact, n, large)


def _host_consts(inputs, layers):
    c = {}
    c["ident"] = np.eye(128, dtype=np.float32)
    c["i8"] = (8.0 * np.eye(128)).astype(np.float32)
    table = np.asarray(inputs["rel_bias_table"], np.float32)
    if 0 in layers:
        s_ = np.arange(128)[:, None]
        q_ = np.arange(256)[None, :]
        dist = q_ - s_
        valid = (dist >= 0) & (dist < 128)
        bk = _bucket(dist)
        t = table[bk]
        t = np.where(valid[:, :, None], t, np.float32(MASKV))
        c["bias_sw"] = np.ascontiguousarray(t.transpose(0, 2, 1)).reshape(128, 32 * 256).astype(np.float32)
        c["sinkrep"] = np.ascontiguousarray(np.repeat(np.asarray(inputs["swa_sinks"], np.float32)[0], 128)[None, :])
    if 1 in layers or 3 in layers:
        s_ = np.arange(128)[:, None]
        q_ = np.arange(256)[None, :]
        dist = q_ - s_
        t = table[_bucket(dist)]
        t = np.where((dist >= 0)[:, :, None], t, np.float32(MASKV))
        c["biasn"] = np.ascontiguousarray(t.transpose(0, 2, 1)).reshape(128, 32 * 256).astype(np.float32)
        c["table"] = np.ascontiguousarray(table)
    if 2 in layers:
        lg = np.asarray(inputs["hgrn_lb_logits"], np.float32)
        c["lbl"] = np.ascontiguousarray(lg.reshape(4, 16, 128).transpose(2, 1, 0)).reshape(128, 64)
        c["hgrn_ng"] = np.ascontiguousarray(np.asarray(inputs["hgrn_norm_g"], np.float32)[0][None, :])
        tr_ = (np.arange(64)[:, None] <= np.arange(64)[None, :]).astype(np.float32)
        c["tri"] = np.ascontiguousarray(np.tile(tr_, (1, 8)))
    if 3 in layers:
        c["cmask"] = np.where(np.arange(128)[None, :] > np.arange(128)[:, None], np.float32(-1.0e30), np.float32(0.0)).astype(np.float32)
    if 1 in layers:
        c["diff_lambda"] = np.ascontiguousarray(np.asarray(inputs["diff_lambda"], np.float32)[0])
        c["diff_subg"] = np.ascontiguousarray(np.asarray(inputs["diff_subln_g"], np.float32)[0][None, :])
    return c


_W_KEYS = {0: ("swa_w_in", "swa_w_out"), 1: ("diff_w_in", "diff_w_out"), 2: ("hgrn_w_in", "hgrn_w_out"), 3: ("dsa_w_in", "dsa_w_out")}
LAUNCHES = [[0, 1, 2, 3]]
N_CORES = 4


def _layer_inputs(inputs, layers):
    m = {}
    m["norm_g"] = np.ascontiguousarray(np.asarray(inputs["norm_g"], np.float32).reshape(16, D))
    for l in layers:
        kin, kout = _W_KEYS[l]
        m["w_in%d" % l] = np.ascontiguousarray(np.asarray(inputs[kin], np.float32)[0])
        m["w_out%d" % l] = np.ascontiguousarray(np.asarray(inputs[kout], np.float32)[0])
        m["w_up%d" % l] = np.ascontiguousarray(np.asarray(inputs["ffn_w_up"], np.float32)[l])
        m["w_down%d" % l] = np.ascontiguousarray(np.asarray(inputs["ffn_w_down"], np.float32)[l])
        cv = np.asarray(inputs["ffn_conv"], np.float32)[l]
        m["conv%d" % l] = np.ascontiguousarray(cv.reshape(3, 88, 128).transpose(2, 1, 0))
    m.update(_host_consts(inputs, layers))
    return m


def run_launch(inputs, x, layers, cores=None):
    nc = build_program(layers)
    shared = _layer_inputs(inputs, layers)
    B = x.shape[0]
    cores = cores if cores is not None else N_CORES
    in_maps = []
    for c in range(cores):
        d = dict(shared)
        d["x"] = np.ascontiguousarray(x[c % B])
        in_maps.append(d)
    res = run_bass_kernel_spmd(nc, in_maps, core_ids=list(range(cores)))
    return np.stack([res.results[b]["y"] for b in range(B)], axis=0)


def kernel(**inputs):
    x = np.asarray(inputs["x"], np.float32)
    for layers in LAUNCHES:
        x = run_launch(inputs, x, layers)
    return x.astype(np.float32)
```

```python
import math
from contextlib import ExitStack

import numpy as np
import concourse.bass as bass
import concourse.mybir as mybir
from concourse.bass_utils import run_bass_kernel_spmd

F32 = mybir.dt.float32
BF16 = mybir.dt.bfloat16
AF = mybir.ActivationFunctionType
ALU = mybir.AluOpType
AX = mybir.AxisListType

ENGS = ("pe", "act", "dve", "pool", "sp")
CENGS = ("pe", "act", "dve", "pool")

S = 2048
D = 2048
NT = 16
DFF = 5632
KFF = 44
EPS = 1e-6
MASKV = -30000.0
N_DSEM = 56


class Buf:
    __slots__ = ("name", "w", "r", "dsem")

    def __init__(self, name):
        self.name = name
        self.w = None
        self.r = {}
        self.dsem = None


class Prog:
    def __init__(self, nc, es):
        self.nc = nc
        self.ops = {e: [] for e in ENGS}
        self.sems = {}
        self.tot = {}
        self.seen = {e: {} for e in ENGS}
        for e in CENGS:
            self.sems[e] = es.enter_context(nc.semaphore("c_" + e))
            self.tot[e] = 0
        self.dfree = []
        for i in range(N_DSEM):
            k = "d%d" % i
            self.sems[k] = es.enter_context(nc.semaphore(k))
            self.tot[k] = 0
            self.dfree.append(k)
        self.dused = []
        self.phase_bufs = []
        self.nphase = 0

    def buf(self, name="b"):
        b = Buf(name)
        self.phase_bufs.append(b)
        return b

    def bufs(self, n, name="b"):
        return [self.buf(name + str(i)) for i in range(n)]

    def _dsem(self, b):
        if b.dsem is None:
            b.dsem = self.dfree.pop()
            self.dused.append(b.dsem)
        return b.dsem

    def _deps(self, eng, reads, writes):
        deps = {}

        def add(ev):
            if ev is None:
                return
            k, v = ev
            if v > deps.get(k, 0):
                deps[k] = v
        for b in reads:
            add(b.w)
        for b in writes:
            add(b.w)
            for k, v in b.r.items():
                add((k, v))
        waits = []
        for k, v in deps.items():
            if k == eng and eng == "pe":
                continue
            if k not in CENGS:
                v = self.tot[k]
            if v > self.seen[eng].get(k, 0):
                self.seen[eng][k] = v
                waits.append((k, v))
        return waits

    def op(self, eng, fn, reads=(), writes=()):
        waits = self._deps(eng, reads, writes)
        self.tot[eng] += 1
        seq = self.tot[eng]
        self.ops[eng].append((waits, fn, eng, 1))
        for b in reads:
            if b.r.get(eng, 0) < seq:
                b.r[eng] = seq
        for b in writes:
            b.w = (eng, seq)
            b.r = {}

    def dma(self, q, fn, reads=(), writes=(), owner=None):
        waits = self._deps(q, reads, writes)
        if owner is None:
            owner = writes[0] if writes else reads[0]
        k = self._dsem(owner)
        self.tot[k] += 16
        val = self.tot[k]
        self.ops[q].append((waits, fn, k, 16))
        for b in reads:
            b.r[k] = val
        for b in writes:
            b.w = (k, val)
            b.r = {}

    def end_phase(self):
        nc = self.nc
        keys = list(CENGS) + list(self.dused)
        for e in ENGS:
            waits = []
            for k in keys:
                if k == e:
                    continue
                v = self.tot[k]
                if v > self.seen[e].get(k, 0):
                    self.seen[e][k] = v
                    waits.append((k, v))
            self.ops[e].append((waits, None, None, 0))
        with nc.Block() as block:
            def mk(ename, attr):
                lst = self.ops[ename]

                def body(e):
                    for waits, fn, k, inc in lst:
                        for wk, wv in waits:
                            e.wait_ge(self.sems[wk], wv)
                        if fn is not None:
                            fn().then_inc(self.sems[k], inc)
                getattr(block, attr)(body)
            mk("sp", "sync")
            mk("pe", "tensor")
            mk("act", "scalar")
            mk("dve", "vector")
            mk("pool", "gpsimd")
        self.ops = {e: [] for e in ENGS}
        for b in self.phase_bufs:
            b.dsem = None
            b.w = None
            b.r = {}
        self.phase_bufs = []
        self.dfree.extend(self.dused)
        self.dused = []
        self.nphase += 1


class KB:
    def __init__(self, nc, P):
        self.nc = nc
        self.P = P
        self.uid = 0

    def sb(self, es, shape, dt, name="t"):
        self.uid += 1
        return es.enter_context(self.nc.sbuf_tensor("%s_%d" % (name, self.uid), list(shape), dt))

    def ps(self, es, shape, dt, name="p"):
        self.uid += 1
        return es.enter_context(self.nc.psum_tensor("%s_%d" % (name, self.uid), list(shape), dt))

    def mm(self, out, lhsT, rhs, start, stop, r, w):
        nc = self.nc
        self.P.op("pe", lambda: nc.tensor.matmul(out, lhsT=lhsT, rhs=rhs, start=start, stop=stop), r, w)

    def tr(self, out, in_, ident, r, w):
        nc = self.nc
        self.P.op("pe", lambda: nc.tensor.transpose(out, in_, ident), r, w)

    def act(self, out, in_, func, r, w, bias=None, scale=None, accum=None):
        nc = self.nc
        kw = {}
        if bias is not None:
            kw["bias"] = bias
        if scale is not None:
            kw["scale"] = scale
        if accum is not None:
            kw["accum_out"] = accum
        self.P.op("act", lambda: nc.scalar.activation(out=out, in_=in_, func=func, **kw), r, w)

    def ts(self, eng, out, in0, s1, op0, r, w, s2=None, op1=None):
        e = self.nc.vector if eng == "dve" else self.nc.gpsimd
        if op1 is None:
            self.P.op(eng, lambda: e.tensor_scalar(out=out, in0=in0, scalar1=s1, scalar2=None, op0=op0), r, w)
        else:
            self.P.op(eng, lambda: e.tensor_scalar(out=out, in0=in0, scalar1=s1, scalar2=s2, op0=op0, op1=op1), r, w)

    def tt(self, eng, out, in0, in1, op, r, w):
        e = self.nc.vector if eng == "dve" else self.nc.gpsimd
        self.P.op(eng, lambda: e.tensor_tensor(out=out, in0=in0, in1=in1, op=op), r, w)

    def stt(self, out, in0, scalar, in1, op0, op1, r, w):
        nc = self.nc
        self.P.op("dve", lambda: nc.vector.scalar_tensor_tensor(out=out, in0=in0, scalar=scalar, in1=in1, op0=op0, op1=op1), r, w)

    def cp(self, eng, out, in_, r, w):
        nc = self.nc
        if eng == "act":
            self.P.op("act", lambda: nc.scalar.copy(out=out, in_=in_), r, w)
        elif eng == "dve":
            self.P.op("dve", lambda: nc.vector.tensor_copy(out=out, in_=in_), r, w)
        else:
            self.P.op("pool", lambda: nc.gpsimd.tensor_copy(out=out, in_=in_), r, w)

    def recip(self, out, in_, r, w):
        nc = self.nc
        self.P.op("dve", lambda: nc.vector.reciprocal(out=out, in_=in_), r, w)

    def memset(self, eng, ap, val, w):
        e = self.nc.vector if eng == "dve" else self.nc.gpsimd
        self.P.op(eng, lambda: e.memset(ap, val), (), w)

    def ld(self, out, in_, r, w, q="sp", owner=None):
        nc = self.nc
        if q == "sp":
            self.P.dma("sp", lambda: nc.sync.dma_start(out=out, in_=in_), r, w, owner)
        else:
            self.P.dma("pool", lambda: nc.gpsimd.dma_start(out=out, in_=in_), r, w, owner)

    def rstd(self, es_tiles, ss, r, w):
        self.ts("dve", ss, ss, 1.0 / D, ALU.mult, r, w, s2=EPS, op1=ALU.add)
        self.act(ss, ss, AF.Sqrt, r, w)
        self.recip(ss, ss, r, w)


def phase_rn(kb, x_src, x_dst, m_src, gpost, gpre, hT, BhT, ident_d):
    nc, P = kb.nc, kb.P
    with ExitStack() as es:
        xt = [kb.sb(es, [128, D], F32, "xt") for _ in range(2)]
        Bxt = P.bufs(2, "xt")
        junk = kb.sb(es, [128, D], BF16, "junk")
        Bjunk = P.buf("junk")
        st = [kb.sb(es, [128, 2], F32, "st") for _ in range(2)]
        Bst = P.bufs(2, "st")
        if m_src is not None:
            mt = [kb.sb(es, [128, D], F32, "mt") for _ in range(2)]
            Bmt = P.bufs(2, "mt")
            gp = kb.sb(es, [128, D], F32, "gp")
            Bgp = P.buf("gp")
            kb.ld(gp[:], gpost.partition_broadcast(128), (), [Bgp])
        if gpre is not None:
            gq = kb.sb(es, [128, D], F32, "gq")
            Bgq = P.buf("gq")
            kb.ld(gq[:], gpre.partition_broadcast(128), (), [Bgq])
            hb = [kb.sb(es, [128, D], BF16, "hb") for _ in range(2)]
            Bhb = P.bufs(2, "hb")
            ident = kb.sb(es, [128, 128], BF16, "ident")
            Bid = P.buf("ident")
            kb.ld(ident[:], ident_d, (), [Bid], q="pool")
            pT = [kb.ps(es, [128, 1024], BF16, "pT") for _ in range(2)]
            BpT = P.bufs(2, "pT")
        Bxd = P.buf("xdst")
        def load_t(t_):
            if t_ >= NT:
                return
            rows_ = slice(t_ * 128, (t_ + 1) * 128)
            kb.ld(xt[t_ % 2][:], x_src[rows_, :], (), [Bxt[t_ % 2]])
            if m_src is not None:
                kb.ld(mt[t_ % 2][:], m_src[rows_, :], (), [Bmt[t_ % 2]])
        load_t(0)
        for t in range(NT):
            s = t % 2
            rows = slice(t * 128, (t + 1) * 128)
            load_t(t + 1)
            if m_src is not None:
                kb.act(junk[:], mt[s][:], AF.Square, [Bmt[s]], [Bjunk, Bst[s]], accum=st[s][:, 0:1])
                kb.rstd(None, st[s][:, 0:1], [Bst[s]], [Bst[s]])
                kb.stt(mt[s][:], mt[s][:], st[s][:, 0:1], gp[:], ALU.mult, ALU.mult, [Bmt[s], Bst[s], Bgp], [Bmt[s]])
                kb.tt("dve", xt[s][:], xt[s][:], mt[s][:], ALU.add, [Bxt[s], Bmt[s]], [Bxt[s]])
                kb.ld(x_dst[rows, :], xt[s][:], [Bxt[s]], (), owner=Bxt[s])
            if gpre is not None:
                kb.act(junk[:], xt[s][:], AF.Square, [Bxt[s]], [Bjunk, Bst[s]], accum=st[s][:, 1:2])
                kb.rstd(None, st[s][:, 1:2], [Bst[s]], [Bst[s]])
                kb.stt(hb[s][:], xt[s][:], st[s][:, 1:2], gq[:], ALU.mult, ALU.mult, [Bxt[s], Bst[s], Bgq], [Bhb[s]])
                for half in range(2):
                    for j in range(8):
                        i = half * 8 + j
                        kb.tr(pT[half][:, j * 128:(j + 1) * 128], hb[s][:, i * 128:(i + 1) * 128], ident[:], [Bhb[s], Bid], [BpT[half]])
                    src = pT[half][:].rearrange("p (j q) -> p j q", j=8)
                    dst = hT[:, half * 8:(half + 1) * 8, t * 128:(t + 1) * 128]
                    kb.cp("act" if half == 0 else "dve", dst, src, [BpT[half]], [BhT])
        P.end_phase()


def phase_proj_fm(kb, hT, BhT, w_d, coltiles, epilogue, epi_setup=None):
    nc, P = kb.nc, kb.P
    wv = w_d.rearrange("(kc p) n -> p kc n", p=128)
    with ExitStack() as es:
        NSL = 3
        wsl = [kb.sb(es, [128, 16, 256], BF16, "wsl") for _ in range(NSL)]
        Bw = P.bufs(NSL, "wsl")
        pb = [kb.ps(es, [128, 2048], F32, "pb") for _ in range(2)]
        Bpb = P.bufs(2, "pb")
        ctx = epi_setup(es) if epi_setup is not None else None
        slabs = []
        i = 0
        while i < len(coltiles):
            c0, w0 = coltiles[i]
            if i + 1 < len(coltiles) and coltiles[i + 1][0] == c0 + w0 and w0 == 128:
                slabs.append([coltiles[i], coltiles[i + 1]])
                i += 2
            else:
                slabs.append([coltiles[i]])
                i += 1
        ti = 0

        def load_slab(si):
            if si >= len(slabs):
                return
            s_ = si % NSL
            c0_ = slabs[si][0][0]
            wtot = sum(w for _, w in slabs[si])
            kb.ld(wsl[s_][:, :, 0:wtot], wv[:, :, c0_:c0_ + wtot], (), [Bw[s_]], q="pool")
        load_slab(0)
        load_slab(1)
        for si, sl in enumerate(slabs):
            s = si % NSL
            load_slab(si + 2)
            off = 0
            for (cc, w) in sl:
                pi = ti % 2
                for g in range(4):
                    for k in range(16):
                        kb.mm(pb[pi][0:w, g * 512:(g + 1) * 512], wsl[s][:, k, off:off + w], hT[:, k, g * 512:(g + 1) * 512],
                              k == 0, k == 15, [Bw[s], BhT], [Bpb[pi]])
                epilogue(ctx, ti, cc, w, pb[pi], Bpb[pi])
                off += w
                ti += 1
        P.end_phase()


def phase_proj_tok(kb, hT, BhT, w_d, colgroups, epilogue, epi_setup=None):
    nc, P = kb.nc, kb.P
    wv = w_d.rearrange("(kc p) n -> p kc n", p=128)
    with ExitStack() as es:
        wg = [kb.sb(es, [128, 16, 512], BF16, "wg") for _ in range(2)]
        Bwg = P.bufs(2, "wg")
        pb = [kb.ps(es, [128, 512], F32, "pk") for _ in range(4)]
        Bpb = P.bufs(4, "pk")
        ctx = epi_setup(es) if epi_setup is not None else None
        n = 0

        def load_g(gi):
            if gi >= len(colgroups):
                return
            c0_, w_ = colgroups[gi]
            kb.ld(wg[gi % 2][:, :, 0:w_], wv[:, :, c0_:c0_ + w_], (), [Bwg[gi % 2]], q="pool")
        load_g(0)
        for gi, (c0, w) in enumerate(colgroups):
            s = gi % 2
            load_g(gi + 1)
            for t in range(NT):
                pi = n % 4
                n += 1
                for k in range(16):
                    kb.mm(pb[pi][:, 0:w], hT[:, k, t * 128:(t + 1) * 128], wg[s][:, k, 0:w], k == 0, k == 15, [BhT, Bwg[s]], [Bpb[pi]])
                epilogue(ctx, gi, t, c0, w, pb[pi], Bpb[pi])
        P.end_phase()


def phase_up(kb, hT, BhT, w_up, conv_d, act_d):
    nc, P = kb.nc, kb.P
    wv = w_up.rearrange("(kc p) n -> p kc n", p=128)
    with ExitStack() as es:
        NSL = 3
        wsl = [kb.sb(es, [128, 16, 512], BF16, "wup") for _ in range(NSL)]
        Bw = P.bufs(NSL, "wup")
        cw = kb.sb(es, [128, 88, 3], F32, "cw")
        Bcw = P.buf("cw")
        kb.ld(cw[:], conv_d, (), [Bcw])
        psg = kb.ps(es, [128, 2048], F32, "psg")
        psv = kb.ps(es, [128, 2048], F32, "psv")
        Bpsg, Bpsv = P.buf("psg"), P.buf("psv")
        ub = [kb.sb(es, [128, 2050], F32, "ub") for _ in range(4)]
        Bub = P.bufs(4, "ub")
        cb = [kb.sb(es, [128, 2048], F32, "cb") for _ in range(4)]
        Bcb = P.bufs(4, "cb")
        ob = [kb.sb(es, [128, 2048], BF16, "ob") for _ in range(2)]
        Bob = P.bufs(2, "ob")
        Bact = P.buf("actd")
        for i in range(4):
            kb.memset("pool", ub[i][:, 0:2], 0.0, [Bub[i]])
        def load_up(sl_):
            if sl_ >= KFF // 2:
                return
            s_ = sl_ % NSL
            c_ = sl_ * 2
            kb.ld(wsl[s_][:, :, 0:256], wv[:, :, c_ * 128:c_ * 128 + 256], (), [Bw[s_]], q="pool")
            kb.ld(wsl[s_][:, :, 256:512], wv[:, :, DFF + c_ * 128:DFF + c_ * 128 + 256], (), [Bw[s_]], q="pool")
        load_up(0)
        load_up(1)
        for c in range(KFF):
            sl = c // 2
            s = sl % NSL
            if c % 2 == 0:
                load_up(sl + 2)
            par = c % 2
            for part, (pp, Bpp) in enumerate(((psg, Bpsg), (psv, Bpsv))):
                off = part * 256 + par * 128
                for g in range(4):
                    for k in range(16):
                        kb.mm(pp[:, g * 512:(g + 1) * 512], wsl[s][:, k, off:off + 128], hT[:, k, g * 512:(g + 1) * 512],
                              k == 0, k == 15, [Bw[s], BhT], [Bpp])
                u = par * 2 + part
                fi = part * KFF + c
                kb.cp("act", ub[u][:, 2:2050], pp[:], [Bpp], [Bub[u]])
                kb.act(cb[u][:], ub[u][:, 0:2048], AF.Copy, [Bub[u], Bcw], [Bcb[u]], scale=cw[:, fi, 0:1])
                kb.stt(cb[u][:], ub[u][:, 1:2049], cw[:, fi, 1:2], cb[u][:], ALU.mult, ALU.add, [Bub[u], Bcw, Bcb[u]], [Bcb[u]])
                kb.stt(cb[u][:], ub[u][:, 2:2050], cw[:, fi, 2:3], cb[u][:], ALU.mult, ALU.add, [Bub[u], Bcw, Bcb[u]], [Bcb[u]])
                if part == 0:
                    kb.act(cb[u][:], cb[u][:], AF.Silu, [Bcb[u]], [Bcb[u]])
            ug, uv = par * 2, par * 2 + 1
            kb.tt("dve", ob[par][:], cb[ug][:], cb[uv][:], ALU.mult, [Bcb[ug], Bcb[uv]], [Bob[par]])
            kb.ld(act_d[c], ob[par][:], [Bob[par]], (), owner=Bob[par])
        P.end_phase()


def phase_down(kb, w_d, KC, act_d, m_d):
    nc, P = kb.nc, kb.P
    wv = w_d.rearrange("(kc p) n -> p kc n", p=128)
    av = act_d.rearrange("c p s -> p c s")
    TG = 256
    with ExitStack() as es:
        wres = [kb.sb(es, [128, KC, 512], BF16, "wres") for _ in range(2)]
        Bwr = [P.bufs(4, "wr%d_" % i) for i in range(2)]
        slab = [kb.sb(es, [128, KC, TG], BF16, "slab") for _ in range(2)]
        Bsl = P.bufs(2, "slab")
        pb = [kb.ps(es, [128, 512], F32, "pd") for _ in range(4)]
        Bpb = P.bufs(4, "pd")
        ot = [kb.sb(es, [128, 512], F32, "ot") for _ in range(3)]
        Bot = P.bufs(3, "ot")
        Bm = P.buf("md")
        kq = [(i * KC) // 4 for i in range(5)]
        n = 0
        ns = 0
        NTG = S // TG

        def load_w(cg_):
            if cg_ >= 4:
                return
            for pq_ in range(4):
                kb.ld(wres[cg_ % 2][:, kq[pq_]:kq[pq_ + 1], :], wv[:, kq[pq_]:kq[pq_ + 1], cg_ * 512:(cg_ + 1) * 512], (),
                      [Bwr[cg_ % 2][pq_]], q="pool")

        def load_slab(i_):
            if i_ >= 4 * NTG:
                return
            tg_ = i_ % NTG
            kb.ld(slab[i_ % 2][:], av[:, 0:KC, tg_ * TG:(tg_ + 1) * TG], (), [Bsl[i_ % 2]])
        load_w(0)
        load_slab(0)
        for cg in range(4):
            ws = cg % 2
            load_w(cg + 1)
            for tg in range(NTG):
                ss = ns % 2
                ns += 1
                load_slab(ns)
                for tt_ in range(TG // 128):
                    pi = n % 4
                    oi = n % 3
                    n += 1
                    for c in range(KC):
                        pq = 0
                        while c >= kq[pq + 1]:
                            pq += 1
                        kb.mm(pb[pi][:], slab[ss][:, c, tt_ * 128:(tt_ + 1) * 128], wres[ws][:, c, :], c == 0, c == KC - 1,
                              [Bsl[ss], Bwr[ws][pq]], [Bpb[pi]])
                    kb.cp("act" if n % 2 == 0 else "dve", ot[oi][:], pb[pi][:], [Bpb[pi]], [Bot[oi]])
                    r0 = tg * TG + tt_ * 128
                    kb.ld(m_d[r0:r0 + 128, cg * 512:(cg + 1) * 512], ot[oi][:], [Bot[oi]], (), owner=Bot[oi])
        P.end_phase()


def epi_store_setup(kb, n=2):
    def setup(es):
        P = kb.P
        return {"t": [kb.sb(es, [128, 2048], BF16, "eo") for _ in range(n)], "B": P.bufs(n, "eo"), "Bd": P.buf("qkvd"), "n": 0}
    return setup


def epi_store_fm(kb, qkv_d, tile_of):
    def epi(ctx, ti, c0, w, pb, Bpb):
        i = ctx["n"] % len(ctx["t"])
        ctx["n"] += 1
        t, B = ctx["t"][i], ctx["B"][i]
        kb.cp("act" if ti % 2 == 0 else "dve", t[0:w, :], pb[0:w, :], [Bpb], [B])
        kb.ld(qkv_d[tile_of(ti), 0:w, :], t[0:w, :], [B], (), owner=B)
    return epi


def epi_tok_setup(kb, dt=BF16):
    def setup(es):
        P = kb.P
        return {"t": [kb.sb(es, [128, 512], dt, "et") for _ in range(3)], "B": P.bufs(3, "et"), "Bd": P.buf("vtokd"), "n": 0}
    return setup


def epi_store_tok(kb, vtok_d, col_of):
    def epi(ctx, gi, t, c0, w, pb, Bpb):
        i = ctx["n"] % 3
        ctx["n"] += 1
        tl, B = ctx["t"][i], ctx["B"][i]
        kb.cp("act" if ctx["n"] % 2 == 0 else "dve", tl[:, 0:w], pb[:, 0:w], [Bpb], [B])
        d0 = col_of(gi)
        kb.ld(vtok_d[t * 128:(t + 1) * 128, d0:d0 + w], tl[:, 0:w], [B], (), owner=B)
    return epi


def phase_swa(kb, qkv_d, vtok_d, act_d, bias_sw_d, i8_d, sink_d):
    nc, P = kb.nc, kb.P
    with ExitStack() as es:
        vt = kb.sb(es, [128, 16, 512], BF16, "vt")
        Bvt = P.buf("vt")
        kb.ld(vt[:], vtok_d[:, 0:512].rearrange("(t p) c -> p t c", p=128), (), [Bvt])
        bsw = kb.sb(es, [128, 32, 256], BF16, "bsw")
        Bbsw = P.buf("bsw")
        kb.ld(bsw[:], bias_sw_d.rearrange("p (h q) -> p h q", h=32), (), [Bbsw], q="pool")
        i8 = kb.sb(es, [128, 128], BF16, "i8")
        Bi8 = P.buf("i8")
        kb.ld(i8[:], i8_d, (), [Bi8], q="pool")
        ones64 = kb.sb(es, [128, 64], BF16, "ones64")
        Bon = P.buf("ones")
        kb.memset("dve", ones64[:], 1.0, [Bon])
        skf = kb.sb(es, [1, 4096], F32, "skf")
        skb = kb.sb(es, [1, 4096], BF16, "skb")
        Bsk = P.buf("sk")
        kb.ld(skf[:], sink_d, (), [Bsk])
        kb.act(skb[:], skf[:], AF.Exp, [Bsk], [Bsk])
        kh = [kb.sb(es, [64, 2048], BF16, "kh") for _ in range(2)]
        qh = [kb.sb(es, [64, 4, 2048], BF16, "qh") for _ in range(2)]
        Bkq = P.bufs(2, "kq")
        PT = [kb.sb(es, [128, 4, 256], BF16, "PT") for _ in range(3)]
        BPT = P.bufs(3, "PT")
        oT = [kb.sb(es, [64, 4, 2048], BF16, "oT") for _ in range(2)]
        BoT = P.bufs(2, "oT")
        rec = [kb.sb(es, [64, 512], F32, "rec") for _ in range(2)]
        Brec = P.bufs(2, "rec")
        pss = [kb.ps(es, [128, 1024], F32, "pss") for _ in range(2)]
        Bpss = P.bufs(2, "pss")
        pnum = [kb.ps(es, [64, 512], F32, "pnum") for _ in range(2)]
        Bpn = P.bufs(2, "pnum")
        pden = [kb.ps(es, [64, 512], F32, "pden") for _ in range(2)]
        Bpd = P.bufs(2, "pden")
        Bact = P.buf("actd")
        nj = 0
        nb = 0

        def load_kv(kv_):
            if kv_ >= 8:
                return
            s_ = kv_ % 2
            kb.ld(kh[s_][:], qkv_d[16 + kv_ // 2, (kv_ % 2) * 64:(kv_ % 2) * 64 + 64, :], (), [Bkq[s_]])
            for g_ in range(4):
                hq_ = 4 * kv_ + g_
                kb.ld(qh[s_][:, g_, :], qkv_d[hq_ // 2, (hq_ % 2) * 64:(hq_ % 2) * 64 + 64, :], (), [Bkq[s_]])
        load_kv(0)
        for kv in range(8):
            s = kv % 2
            load_kv(kv + 1)

            def stageA(j, kv=kv, s=s, base=nj):
                nq = 256 if j < 15 else 128
                sl = (base + j) % 2
                pt = (base + j) % 3
                for g in range(4):
                    o = pss[sl][:, g * 256:g * 256 + nq]
                    kb.mm(o, kh[s][:, j * 128:(j + 1) * 128], qh[s][:, g, j * 128:j * 128 + nq], True, False, [Bkq[s]], [Bpss[sl]])
                    kb.mm(o, i8[:], bsw[:, 4 * kv + g, 0:nq], False, True, [Bi8, Bbsw], [Bpss[sl]])
                src = pss[sl][:].rearrange("p (g q) -> p g q", g=4)[:, :, 0:nq]
                kb.act(PT[pt][:, :, 0:nq], src, AF.Exp, [Bpss[sl]], [BPT[pt]], scale=0.125)

            def stageB(j, kv=kv, s=s, base=nj, nb0=nb):
                pt = (base + j) % 3
                prev = (base + j - 1) % 3 if j > 0 else None
                b = (nb0 + j) % 2
                rds = [Bvt, BPT[pt]] + ([BPT[prev]] if prev is not None else [])
                kb.mm(pden[b][:], ones64[0:1, 0:64], skb[0:1, 4 * kv * 128:(4 * kv + 4) * 128], True, False, [Bon, Bsk], [Bpd[b]])
                if prev is not None:
                    kb.mm(pnum[b][:], vt[:, j - 1, kv * 64:(kv + 1) * 64], PT[prev][:, :, 128:256], True, False, rds, [Bpn[b]])
                    kb.mm(pden[b][:], ones64[:], PT[prev][:, :, 128:256], False, False, rds + [Bon], [Bpd[b]])
                kb.mm(pnum[b][:], vt[:, j, kv * 64:(kv + 1) * 64], PT[pt][:, :, 0:128], prev is None, True, rds, [Bpn[b]])
                kb.mm(pden[b][:], ones64[:], PT[pt][:, :, 0:128], False, True, rds + [Bon], [Bpd[b]])
                kb.recip(rec[b][:], pden[b][:], [Bpd[b]], [Brec[b]])
                kb.tt("dve", oT[s][:, :, j * 128:(j + 1) * 128], pnum[b][:].rearrange("p (g q) -> p g q", g=4),
                      rec[b][:].rearrange("p (g q) -> p g q", g=4), ALU.mult, [Bpn[b], Brec[b]], [BoT[s]])
            stageA(0)
            for j in range(16):
                if j + 1 < 16:
                    stageA(j + 1)
                stageB(j)
            nj += 16
            nb += 16
            for g in range(4):
                hq = 4 * kv + g
                kb.ld(act_d[hq // 2, (hq % 2) * 64:(hq % 2) * 64 + 64, :], oT[s][:, g, :], [BoT[s]], (), owner=BoT[s])
        P.end_phase()


def phase_diff(kb, qkv_d, vtok_d, act_d, biasn_d, i8_d, table_d, lam_d, subg_d, layer_idx):
    nc, P = kb.nc, kb.P
    lam_init = 0.8 - 0.6 * math.exp(-0.3 * layer_idx)
    with ExitStack() as es:
        bn = kb.sb(es, [128, 32, 256], BF16, "bn")
        Bbn = P.buf("bn")
        kb.ld(bn[:], biasn_d.rearrange("p (h q) -> p h q", h=32), (), [Bbn], q="pool")
        i8 = kb.sb(es, [128, 128], BF16, "i8")
        Bi8 = P.buf("i8")
        kb.ld(i8[:], i8_d, (), [Bi8], q="pool")
        ones = kb.sb(es, [128, 128], BF16, "ones")
        onesf = kb.sb(es, [1, 128], F32, "onesf")
        Bon = P.buf("ones")
        kb.memset("dve", ones[:], 1.0, [Bon])
        kb.memset("dve", onesf[:], 1.0, [Bon])
        c31 = kb.sb(es, [128, 32], F32, "c31")
        Bc31 = P.buf("c31")
        kb.ld(c31[:], table_d[31].partition_broadcast(128), (), [Bc31])
        lamt = kb.sb(es, [1, 256], F32, "lamt")
        lsm = kb.sb(es, [1, 136], F32, "lsm")
        Blam = P.buf("lam")
        kb.ld(lamt[:], lam_d.rearrange("(o r) c -> o (r c)", o=1), (), [Blam])
        kb.tt("dve", lsm[:, 0:64], lamt[:, 0:64], lamt[:, 64:128], ALU.mult, [Blam], [Blam])
        kb.tt("dve", lsm[:, 64:128], lamt[:, 128:192], lamt[:, 192:256], ALU.mult, [Blam], [Blam])
        P.op("dve", lambda: nc.vector.tensor_reduce(out=lsm[:, 128:130], in_=lsm[:, 0:128].rearrange("p (a b) -> p a b", a=2), axis=AX.X, op=ALU.add), [Blam], [Blam])
        kb.act(lsm[:, 130:132], lsm[:, 128:130], AF.Exp, [Blam], [Blam])
        kb.tt("dve", lsm[:, 132:133], lsm[:, 130:131], lsm[:, 131:132], ALU.subtract, [Blam], [Blam])
        kb.ts("dve", lsm[:, 132:133], lsm[:, 132:133], lam_init, ALU.add, [Blam], [Blam])
        kb.cp("dve", lsm[:, 133:134], lsm[:, 132:133], [Blam], [Blam])
        pl = kb.ps(es, [128, 512], F32, "pl")
        Bpl = P.buf("pl")
        kb.mm(pl[:, 0:2], onesf[0:1, :], lsm[0:1, 132:134], True, True, [Bon, Blam], [Bpl])
        neglam = kb.sb(es, [128, 2], F32, "neglam")
        Bnl = P.buf("neglam")
        kb.ts("dve", neglam[:], pl[:, 0:2], -1.0, ALU.mult, [Bpl], [Bnl])
        gsc = kb.sb(es, [128, 1], F32, "gsc")
        Bgsc = P.buf("gsc")
        kb.ld(gsc[:], subg_d.rearrange("o e -> e o"), (), [Bgsc])
        kb.ts("dve", gsc[:], gsc[:], 1.0 - lam_init, ALU.mult, [Bgsc], [Bgsc])
        qh = [kb.sb(es, [128, 2048], BF16, "qh") for _ in range(2)]
        kh = [kb.sb(es, [128, 2048], BF16, "kh") for _ in range(2)]
        vh = [kb.sb(es, [128, 16, 128], BF16, "vh") for _ in range(2)]
        Bin = P.bufs(2, "qkv")
        PT = [kb.sb(es, [128, 512], BF16, "PT") for _ in range(4)]
        BPT = P.bufs(4, "PT")
        oTh = [kb.sb(es, [128, 2048], BF16, "oTh") for _ in range(2)]
        BoT = P.bufs(2, "oTh")
        r1 = kb.sb(es, [128, 512], F32, "r1")
        r2 = kb.sb(es, [128, 512], F32, "r2")
        t1 = kb.sb(es, [128, 512], F32, "t1")
        t2 = kb.sb(es, [128, 512], F32, "t2")
        sq = kb.sb(es, [128, 512], BF16, "sq")
        rs = kb.sb(es, [128, 512], F32, "rs")
        Bw = P.buf("work")
        Bsq = P.buf("sq")
        Brs = P.buf("rs")
        pss = [pl, kb.ps(es, [128, 512], F32, "pss"), kb.ps(es, [128, 512], F32, "pss")]
        Bpss = [Bpl, P.buf("pss"), P.buf("pss")]
        pnum = [kb.ps(es, [128, 512], F32, "pnum") for _ in range(2)]
        pden = [kb.ps(es, [128, 512], F32, "pden") for _ in range(2)]
        Bpn = P.bufs(2, "pn")
        Bpd = P.bufs(2, "pd")
        psq = kb.ps(es, [128, 512], F32, "psq")
        Bpsq = P.buf("psq")

        def load_h(h_):
            if h_ >= 16:
                return
            s_ = h_ % 2
            kb.ld(qh[s_][:], qkv_d[h_], (), [Bin[s_]])
            kb.ld(kh[s_][:], qkv_d[16 + h_], (), [Bin[s_]])
            kb.ld(vh[s_][:], vtok_d[:, h_ * 128:(h_ + 1) * 128].rearrange("(t p) e -> p t e", p=128), (), [Bin[s_]])
        load_h(0)
        base = 0
        NPS = 3
        for h in range(16):
            s = h % 2
            load_h(h + 1)
            its = [(c, m, j) for c in range(4) for m in range(2) for j in range(4 * c + 4)]

            def geom(c, j):
                lo = max(j, 4 * c)
                N = (4 * c + 4 - lo) * 128
                if j >= 4 * c:
                    nn, boff = min(N, 256), 0
                elif j == 4 * c - 1:
                    nn, boff = 128, 128
                else:
                    nn, boff = 0, 0
                return lo, N, nn, boff

            def stageA(idx):
                c, m, j = its[idx]
                lo, N, nn, boff = geom(c, j)
                pr = slice(m * 64, (m + 1) * 64)
                mp = 2 * h + m
                q0 = lo * 128
                sl = (base + idx) % NPS
                pt = (base + idx) % 4
                kb.mm(pss[sl][:, 0:N], kh[s][pr, j * 128:(j + 1) * 128], qh[s][pr, q0:q0 + N], True, nn == 0, [Bin[s]], [Bpss[sl]])
                if nn:
                    kb.mm(pss[sl][:, 0:nn], i8[:], bn[:, mp, boff:boff + nn], False, True, [Bi8, Bbn], [Bpss[sl]])
                    kb.act(PT[pt][:, 0:nn], pss[sl][:, 0:nn], AF.Exp, [Bpss[sl]], [BPT[pt]], scale=0.125)
                if N > nn:
                    kb.act(PT[pt][:, nn:N], pss[sl][:, nn:N], AF.Exp, [Bpss[sl], Bc31], [BPT[pt]], scale=0.125, bias=c31[:, mp:mp + 1])

            def stageB(idx):
                c, m, j = its[idx]
                lo, N, nn, boff = geom(c, j)
                off = (lo - 4 * c) * 128
                jl = 4 * c + 3
                pt = (base + idx) % 4
                kb.mm(pnum[m][:, off:512], vh[s][:, j, :], PT[pt][:, 0:N], j == 0, j == jl, [Bin[s], BPT[pt]], [Bpn[m]])
                kb.mm(pden[m][:, off:512], ones[:], PT[pt][:, 0:N], j == 0, j == jl, [Bon, BPT[pt]], [Bpd[m]])
                if m == 1 and j == jl:
                    kb.recip(r1[:], pden[0][:], [Bpd[0]], [Bw])
                    kb.recip(r2[:], pden[1][:], [Bpd[1]], [Bw])
                    kb.tt("dve", t1[:], pnum[0][:], r1[:], ALU.mult, [Bpn[0], Bw], [Bw])
                    kb.tt("dve", t2[:], pnum[1][:], r2[:], ALU.mult, [Bpn[1], Bw], [Bw])
                    kb.stt(t1[:], t2[:], neglam[:, 0:1], t1[:], ALU.mult, ALU.add, [Bw, Bnl], [Bw])
                    kb.act(sq[:], t1[:], AF.Square, [Bw], [Bsq])
                    kb.mm(psq[:], ones[:], sq[:], True, True, [Bon, Bsq], [Bpsq])
                    kb.ts("dve", rs[:], psq[:], 1.0 / 128, ALU.mult, [Bpsq], [Brs], s2=EPS, op1=ALU.add)
                    kb.act(rs[:], rs[:], AF.Sqrt, [Brs], [Brs])
                    kb.recip(rs[:], rs[:], [Brs], [Brs])
                    kb.stt(oTh[s][:, c * 512:(c + 1) * 512], t1[:], gsc[:, 0:1], rs[:], ALU.mult, ALU.mult, [Bw, Bgsc, Brs], [BoT[s]])
            LA = 2
            for i0 in range(min(LA, len(its))):
                stageA(i0)
            for idx in range(len(its)):
                if idx + LA < len(its):
                    stageA(idx + LA)
                stageB(idx)
            base += len(its)
            kb.ld(act_d[h], oTh[s][:], [BoT[s]], (), owner=BoT[s])
        P.end_phase()


def phase_dsa_idx(kb, qkv_d, wi_d, mask_d, cmask_d, ident_d):
    nc, P = kb.nc, kb.P
    BIG = 1.0e30
    with ExitStack() as es:
        kiT = kb.sb(es, [64, 2048], BF16, "kiT")
        qi = kb.sb(es, [64, 16, 2048], BF16, "qi")
        Bqk = P.buf("qiki")
        kb.ld(kiT[:], qkv_d[28, 0:64, :], (), [Bqk])
        for h in range(16):
            kb.ld(qi[:, h, :], qkv_d[20 + h // 2, (h % 2) * 64:(h % 2) * 64 + 64, :], (), [Bqk])
        cm = kb.sb(es, [128, 128], F32, "cm")
        Bcm = P.buf("cm")
        kb.ld(cm[:], cmask_d, (), [Bcm])
        ident = kb.sb(es, [128, 128], BF16, "ident")
        Bid = P.buf("ident")
        kb.ld(ident[:], ident_d, (), [Bid], q="pool")
        zt = kb.sb(es, [128, 256], BF16, "zt")
        Bzt = P.buf("zt")
        kb.memset("pool", zt[:], 0.0, [Bzt])
        kb.ld(mask_d[0, :, 0:256], zt[:], [Bzt], (), owner=Bzt)
        kb.ld(mask_d[1, :, 0:256], zt[:], [Bzt], (), owner=Bzt)
        wi = [kb.sb(es, [128, 16], F32, "wi") for _ in range(2)]
        Bwi = P.bufs(2, "wi")
        acc = [kb.sb(es, [128, 2048], F32, "acc") for _ in range(2)]
        Bacc = P.bufs(2, "acc")
        Rb = [kb.sb(es, [128, 1024], F32, "Rb") for _ in range(2)]
        BRb = P.bufs(2, "Rb")
        work = kb.sb(es, [128, 2048], F32, "work")
        Bwork = P.buf("work")
        m8 = kb.sb(es, [128, 8], F32, "m8")
        Bm8 = P.buf("m8")
        Mb = [kb.sb(es, [128, 2048], BF16, "Mb") for _ in range(2)]
        BMb = P.bufs(2, "Mb")
        mT = [kb.sb(es, [128, 8, 128], BF16, "mT") for _ in range(2)]
        BmT = P.bufs(2, "mT")
        pl = [kb.ps(es, [128, 1024], F32, "pl") for _ in range(2)]
        Bpl = P.bufs(2, "pl")
        pT = [kb.ps(es, [128, 1024], BF16, "pT") for _ in range(2)]
        BpT = P.bufs(2, "pT")
        nl = 0
        nT = 0
        for qt in range(2, 16):
            a = qt % 2
            ns = (qt + 1) * 128
            kb.ld(wi[a][:], wi_d[qt * 128:(qt + 1) * 128, :], (), [Bwi[a]])
            for h in range(16):
                for half in range((ns + 1023) // 1024):
                    n = min(1024, ns - half * 1024)
                    sl = nl % 2
                    nl += 1
                    for sc in range((n + 511) // 512):
                        n2 = min(512, n - sc * 512)
                        c0 = half * 1024 + sc * 512
                        kb.mm(pl[sl][:, sc * 512:sc * 512 + n2], qi[:, h, qt * 128:(qt + 1) * 128], kiT[:, c0:c0 + n2], True, True, [Bqk], [Bpl[sl]])
                    kb.act(Rb[sl][:, 0:n], pl[sl][:, 0:n], AF.Relu, [Bpl[sl]], [BRb[sl]])
                    av = acc[a][:, half * 1024:half * 1024 + n]
                    if h == 0:
                        kb.ts("dve", av, Rb[sl][:, 0:n], wi[a][:, 0:1], ALU.mult, [BRb[sl], Bwi[a]], [Bacc[a]])
                    else:
                        kb.stt(av, Rb[sl][:, 0:n], wi[a][:, h:h + 1], av, ALU.mult, ALU.add, [BRb[sl], Bwi[a], Bacc[a]], [Bacc[a]])
            dg = acc[a][:, qt * 128:(qt + 1) * 128]
            kb.tt("dve", dg, dg, cm[:], ALU.add, [Bacc[a], Bcm], [Bacc[a]])
            src, Bsrc = acc[a], Bacc[a]
            for r in range(32):
                sv = src[:, 0:ns]
                P.op("dve", (lambda sv=sv: nc.vector.max(out=m8[:], in_=sv)), [Bsrc], [Bm8])
                if r < 31:
                    wv_ = work[:, 0:ns]
                    P.op("dve", (lambda sv=sv, wv_=wv_: nc.vector.match_replace(out=wv_, in_to_replace=m8[:], in_values=sv, imm_value=-BIG)), [Bsrc, Bm8], [Bwork])
                    src, Bsrc = work, Bwork
            kb.ts("dve", Mb[a][:, 0:ns], acc[a][:, 0:ns], m8[:, 7:8], ALU.is_ge, [Bacc[a], Bm8], [BMb[a]])
            for j0 in range(0, qt + 1, 8):
                njj = min(8, qt + 1 - j0)
                b = nT % 2
                nT += 1
                for i in range(njj):
                    j = j0 + i
                    kb.tr(pT[b][:, i * 128:(i + 1) * 128], Mb[a][:, j * 128:(j + 1) * 128], ident[:], [BMb[a], Bid], [BpT[b]])
                srcv = pT[b][:].rearrange("p (j q) -> p j q", j=8)[:, 0:njj, :]
                kb.ts("dve" if b == 0 else "dve", mT[b][:, 0:njj, :], srcv, -1.0, ALU.add, [BpT[b]], [BmT[b]], s2=-MASKV, op1=ALU.mult)
                kb.ld(mask_d[j0:j0 + njj, :, qt * 128:(qt + 1) * 128].rearrange("j p q -> p j q"), mT[b][:, 0:njj, :], [BmT[b]], (), owner=BmT[b])
        P.end_phase()


def phase_dsa_attn(kb, qkv_d, vtok_d, act_d, mask_d, biasn_d, i8_d, table_d):
    nc, P = kb.nc, kb.P
    with ExitStack() as es:
        bn = kb.sb(es, [128, 32, 256], BF16, "bn")
        Bbn = P.buf("bn")
        kb.ld(bn[:], biasn_d.rearrange("p (h q) -> p h q", h=32), (), [Bbn], q="pool")
        i8 = kb.sb(es, [128, 128], BF16, "i8")
        Bi8 = P.buf("i8")
        kb.ld(i8[:], i8_d, (), [Bi8], q="pool")
        ones = kb.sb(es, [128, 64], BF16, "ones")
        Bon = P.buf("ones")
        kb.memset("dve", ones[:], 1.0, [Bon])
        c31 = kb.sb(es, [128, 32], F32, "c31")
        Bc31 = P.buf("c31")
        kb.ld(c31[:], table_d[31].partition_broadcast(128), (), [Bc31])
        vt = kb.sb(es, [128, 16, 512], BF16, "vt")
        Bvt = P.buf("vt")
        kb.ld(vt[:], vtok_d[:, 0:512].rearrange("(t p) c -> p t c", p=128), (), [Bvt])
        mk = kb.sb(es, [128, 16, 2048], BF16, "mk")
        Bmk = P.buf("mk")
        for j in range(16):
            kb.ld(mk[:, j, j * 128:2048], mask_d[j, :, j * 128:2048], (), [Bmk])
        kh = [kb.sb(es, [64, 2048], BF16, "kh") for _ in range(2)]
        Bkh = P.bufs(2, "kh")
        qh = [kb.sb(es, [64, 2048], BF16, "qh") for _ in range(2)]
        Bqh = P.bufs(2, "qh")
        PT = [kb.sb(es, [128, 512], BF16, "PT") for _ in range(4)]
        BPT = P.bufs(4, "PT")
        oTh = [kb.sb(es, [64, 2048], BF16, "oTh") for _ in range(2)]
        BoT = P.bufs(2, "oTh")
        r1 = [kb.sb(es, [64, 512], F32, "r1") for _ in range(2)]
        Br1 = P.bufs(2, "r1")
        pss = [kb.ps(es, [128, 512], F32, "pss") for _ in range(4)]
        Bpss = P.bufs(4, "pss")
        pnum = [kb.ps(es, [64, 512], F32, "pnum") for _ in range(2)]
        pden = [kb.ps(es, [64, 512], F32, "pden") for _ in range(2)]
        Bpn = P.bufs(2, "pn")
        Bpd = P.bufs(2, "pd")

        def load_q(hq_):
            if hq_ >= 32:
                return
            kb.ld(qh[hq_ % 2][:], qkv_d[hq_ // 2, (hq_ % 2) * 64:(hq_ % 2) * 64 + 64, :], (), [Bqh[hq_ % 2]])

        def load_k(kv_):
            if kv_ >= 8:
                return
            kb.ld(kh[kv_ % 2][:], qkv_d[16 + kv_ // 2, (kv_ % 2) * 64:(kv_ % 2) * 64 + 64, :], (), [Bkh[kv_ % 2]])
        load_k(0)
        load_q(0)
        nj = 0
        nb = 0
        for kv in range(8):
            ks = kv % 2
            load_k(kv + 1)
            for g in range(4):
                hq = 4 * kv + g
                s = hq % 2
                load_q(hq + 1)
                its = [(c, j) for c in range(4) for j in range(4 * c + 4)]

                def geom(c, j):
                    lo = max(j, 4 * c)
                    N = (4 * c + 4 - lo) * 128
                    if j >= 4 * c:
                        nn, boff = min(N, 256), 0
                    elif j == 4 * c - 1:
                        nn, boff = 128, 128
                    else:
                        nn, boff = 0, 0
                    return lo, N, nn, boff

                def stageA(idx, hq=hq, ks=ks, s=s, base=nj):
                    c, j = its[idx]
                    lo, N, nn, boff = geom(c, j)
                    q0 = lo * 128
                    sl = (base + idx) % 4
                    pt = (base + idx) % 4
                    kb.mm(pss[sl][:, 0:N], kh[ks][:, j * 128:(j + 1) * 128], qh[s][:, q0:q0 + N], True, False, [Bkh[ks], Bqh[s]], [Bpss[sl]])
                    if nn:
                        kb.mm(pss[sl][:, 0:nn], i8[:], bn[:, hq, boff:boff + nn], False, False, [Bi8, Bbn], [Bpss[sl]])
                    kb.mm(pss[sl][:, 0:N], i8[:], mk[:, j, q0:q0 + N], False, True, [Bi8, Bmk], [Bpss[sl]])
                    if nn:
                        kb.act(PT[pt][:, 0:nn], pss[sl][:, 0:nn], AF.Exp, [Bpss[sl]], [BPT[pt]], scale=0.125)
                    if N > nn:
                        kb.act(PT[pt][:, nn:N], pss[sl][:, nn:N], AF.Exp, [Bpss[sl], Bc31], [BPT[pt]], scale=0.125, bias=c31[:, hq:hq + 1])

                def stageB(idx, hq=hq, kv=kv, s=s, base=nj, nb0=nb):
                    c, j = its[idx]
                    lo, N, nn, boff = geom(c, j)
                    off = (lo - 4 * c) * 128
                    jl = 4 * c + 3
                    pt = (base + idx) % 4
                    b = (nb0 + c) % 2
                    kb.mm(pnum[b][:, off:512], vt[:, j, kv * 64:(kv + 1) * 64], PT[pt][:, 0:N], j == 0, j == jl, [Bvt, BPT[pt]], [Bpn[b]])
                    kb.mm(pden[b][:, off:512], ones[:], PT[pt][:, 0:N], j == 0, j == jl, [Bon, BPT[pt]], [Bpd[b]])
                    if j == jl:
                        kb.recip(r1[b][:], pden[b][:], [Bpd[b]], [Br1[b]])
                        kb.tt("dve", oTh[s][:, c * 512:(c + 1) * 512], pnum[b][:], r1[b][:], ALU.mult, [Bpn[b], Br1[b]], [BoT[s]])
                LA = 2
                for i0 in range(min(LA, len(its))):
                    stageA(i0)
                for idx in range(len(its)):
                    if idx + LA < len(its):
                        stageA(idx + LA)
                    stageB(idx)
                nj += len(its)
                nb += 4
                kb.ld(act_d[hq // 2, (hq % 2) * 64:(hq % 2) * 64 + 64, :], oTh[s][:], [BoT[s]], (), owner=BoT[s])
        P.end_phase()


def hgrn_inproj(kb, hT, BhT, w_in, lbl_d, layer_idx, hq_d, hf_d, hk_d, hv_d, qkv_d):
    nc, P = kb.nc, kb.P
    tiles = [(i * 128, 128) for i in range(16)] + [(2048 + i * 128, 128) for i in range(16)] + [(6144 + i * 128, 128) for i in range(16)]

    def setup(es):
        c = {}
        lbl = kb.sb(es, [128, 16, 4], F32, "lbl")
        Bl = P.buf("lbl")
        kb.ld(lbl[:], lbl_d.rearrange("p (t l) -> p t l", l=4), (), [Bl])
        kb.act(lbl[:], lbl[:], AF.Exp, [Bl], [Bl])
        ssum = kb.sb(es, [128, 16], F32, "ssum")
        lb = kb.sb(es, [128, 16], F32, "lb")
        oml = kb.sb(es, [128, 16], F32, "oml")
        Blb = P.buf("lb")
        P.op("dve", lambda: nc.vector.tensor_reduce(out=ssum[:], in_=lbl[:], axis=AX.X, op=ALU.add), [Bl], [Blb])
        kb.recip(ssum[:], ssum[:], [Blb], [Blb])
        kb.cp("dve", lb[:], lbl[:, :, 1], [Bl, Blb], [Blb])
        for j in range(2, layer_idx + 1):
            kb.tt("dve", lb[:], lb[:], lbl[:, :, j], ALU.add, [Bl, Blb], [Blb])
        kb.tt("dve", lb[:], lb[:], ssum[:], ALU.mult, [Blb], [Blb])
        kb.ts("dve", oml[:], lb[:], -1.0, ALU.mult, [Blb], [Blb], s2=1.0, op1=ALU.add)
        c["lb"], c["oml"], c["Blb"] = lb, oml, Blb
        c["fo"] = [kb.sb(es, [128, 2048], F32, "fo") for _ in range(2)]
        c["Bfo"] = P.bufs(2, "fo")
        c["fk"] = [kb.sb(es, [128, 2048], F32, "fk") for _ in range(2)]
        c["Bfk"] = P.bufs(2, "fk")
        c["bo"] = [kb.sb(es, [128, 2048], BF16, "bo") for _ in range(2)]
        c["Bbo"] = P.bufs(2, "bo")
        return c

    def epi(c, ti, c0, w, pb, Bpb):
        kind, h = ti // 16, ti % 16
        a = ti % 2
        if kind == 0:
            kb.act(c["fo"][a][:], pb[:], AF.Silu, [Bpb], [c["Bfo"][a]])
            kb.ld(hq_d[h], c["fo"][a][:], [c["Bfo"][a]], (), owner=c["Bfo"][a])
        elif kind == 1:
            fo, Bfo, fk, Bfk = c["fo"][a], c["Bfo"][a], c["fk"][a], c["Bfk"][a]
            kb.act(fo[:], pb[:], AF.Sigmoid, [Bpb], [Bfo])
            kb.ts("dve", fo[:], fo[:], c["oml"][:, h:h + 1], ALU.mult, [Bfo, c["Blb"]], [Bfo], s2=c["lb"][:, h:h + 1], op1=ALU.add)
            kb.ts("dve", fk[:], fo[:], -1.0, ALU.mult, [Bfo], [Bfk], s2=1.0, op1=ALU.add)
            kb.ld(hk_d[h], fk[:], [Bfk], (), owner=Bfk)
            kb.act(fo[:], fo[:], AF.Ln, [Bfo], [Bfo])
            kb.ld(hf_d[h], fo[:], [Bfo], (), owner=Bfo)
        else:
            kb.act(c["bo"][a][:], pb[:], AF.Silu, [Bpb], [c["Bbo"][a]])
            kb.ld(qkv_d[h], c["bo"][a][:], [c["Bbo"][a]], (), owner=c["Bbo"][a])
    phase_proj_fm(kb, hT, BhT, w_in, tiles, epi, setup)
    phase_proj_tok(kb, hT, BhT, w_in, [(4096 + i * 512, 512) for i in range(4)],
                   epi_store_tok(kb, hv_d, lambda gi: gi * 512), epi_tok_setup(kb, F32))


def phase_hgrn(kb, hq_d, hf_d, hk_d, hv_d, qkv_d, act_d, ng_d, ident_d, tri_d):
    nc, P = kb.nc, kb.P
    with ExitStack() as es:
        identf = kb.sb(es, [128, 128], F32, "identf")
        tri = kb.sb(es, [64, 512], F32, "tri")
        ng = kb.sb(es, [128, 1], F32, "ng")
        Bc = P.buf("consts")
        kb.ld(identf[:], ident_d, (), [Bc])
        kb.ld(tri[:], tri_d, (), [Bc])
        kb.ld(ng[:], ng_d.rearrange("o e -> e o"), (), [Bc])
        ones = kb.sb(es, [128, 128], BF16, "ones")
        rmask = kb.sb(es, [128, 2048], F32, "rmask")
        Bon = P.buf("ones")
        kb.memset("dve", ones[:], 1.0, [Bon])
        kb.memset("dve", rmask[:], 1.0, [Bon])
        kb.memset("dve", rmask[:].rearrange("p (c t) -> p c t", t=64)[:, :, 0:1], 0.0, [Bon])
        A = kb.sb(es, [128, 2048], F32, "A")
        Bb = kb.sb(es, [128, 2048], F32, "B")
        C = kb.sb(es, [128, 2048], F32, "C")
        Dd = kb.sb(es, [128, 2048], F32, "Dd")
        E = kb.sb(es, [128, 2048], F32, "E")
        Fk = kb.sb(es, [128, 2048], F32, "F")
        BA, BB, BC, BD, BE, BF_ = P.buf("A"), P.buf("B"), P.buf("C"), P.buf("D"), P.buf("E"), P.buf("F")
        U = kb.sb(es, [128, 128 * 33], F32, "U")
        Dk = kb.sb(es, [128, 128 * 33], F32, "Dk")
        St = kb.sb(es, [128, 128 * 33], F32, "St")
        BU, BDk, BSt = P.buf("U"), P.buf("Dk"), P.buf("St")
        U3 = U[:].rearrange("p (v c) -> p v c", c=33)
        Dk3 = Dk[:].rearrange("p (v c) -> p v c", c=33)
        St3 = St[:].rearrange("p (v c) -> p v c", c=33)
        Stc = U[:, 0:4096].rearrange("p (c v) -> p c v", v=128)
        kb.memset("pool", Dk[:], 0.0, [BDk])
        kb.memset("pool", U[:], 0.0, [BU])
        vtk = kb.sb(es, [64, 32, 128], F32, "vtk")
        Bvtk = P.buf("vtk")
        kendT = kb.sb(es, [64, 32, 128], F32, "kendT")
        BkT = P.buf("kendT")
        attm = kb.sb(es, [64, 2048], F32, "attm")
        Batt = P.buf("attm")
        o_h = kb.sb(es, [128, 2048], F32, "o_h")
        Boh = P.buf("o_h")
        gs = kb.sb(es, [128, 2048], BF16, "gs")
        Bgs = P.buf("gs")
        oTh = kb.sb(es, [128, 2048], BF16, "oTh")
        BoT = P.buf("oTh")
        ebend = kb.sb(es, [128, 32], F32, "ebend")
        Beb = P.buf("ebend")
        sq = kb.sb(es, [128, 512], BF16, "sq")
        rs = kb.sb(es, [128, 512], F32, "rs")
        tmp = kb.sb(es, [128, 512], F32, "tmp")
        Bsq, Brs, Btmp = P.buf("sq"), P.buf("rs"), P.buf("tmp")
        ptr = [kb.ps(es, [128, 512], F32, "ptr") for _ in range(2)]
        Bptr = P.bufs(2, "ptr")
        patt = kb.ps(es, [128, 512], F32, "patt")
        Bpatt = P.buf("patt")
        pupd = [kb.ps(es, [128, 512], F32, "pupd") for _ in range(2)]
        Bpupd = P.bufs(2, "pupd")
        po = [kb.ps(es, [128, 512], F32, "po") for _ in range(2)]
        Bpo = P.bufs(2, "po")
        psq = kb.ps(es, [128, 512], F32, "psq")
        Bpsq = P.buf("psq")
        B3 = Bb[:].rearrange("p (c t) -> p c t", t=64)
        A3 = A[:].rearrange("p (c t) -> p c t", t=64)
        for h in range(16):
            kb.ld(C[:], hq_d[h], (), [BC])
            kb.ld(A[:], hf_d[h], (), [BA])
            kb.ld(Dd[:], hk_d[h], (), [BD])
            kb.ld(vtk[:], hv_d[:, h * 128:(h + 1) * 128].rearrange("(c s) v -> s c v", s=64), (), [Bvtk])
            kb.ld(gs[:], qkv_d[h], (), [Bgs])
            P.op("dve", lambda: nc.vector.tensor_tensor_scan(out=Bb[:], data0=rmask[:], data1=A[:], initial=0.0, op0=ALU.mult, op1=ALU.add),
                 [Bon, BA], [BB])
            kb.act(A[:], Bb[:], AF.Exp, [BB], [BA])
            kb.tt("dve", C[:], C[:], A[:], ALU.mult, [BC, BA], [BC])
            kb.act(ebend[:], B3[:, :, 63], AF.Exp, [BB], [Beb])
            kb.ts("dve", A[:], Bb[:], -1.0, ALU.mult, [BB, BC], [BA], s2=80.0, op1=ALU.min)
            kb.act(A[:], A[:], AF.Exp, [BA], [BA])
            kb.tt("dve", E[:], Dd[:], A[:], ALU.mult, [BD, BA], [BE])
            kb.tt("dve", A3, B3[:, :, 63:64].to_broadcast([128, 32, 64]), B3, ALU.subtract, [BB, BE], [BA])
            kb.act(A[:], A[:], AF.Exp, [BA], [BA])
            kb.tt("dve", Fk[:], Dd[:], A[:], ALU.mult, [BD, BA], [BF_])
            for c in range(32):
                b = (c // 4) % 2
                kb.tr(ptr[b][0:64, (c % 4) * 128:(c % 4 + 1) * 128], Fk[:, c * 64:(c + 1) * 64], identf[:], [BF_, Bc], [Bptr[b]])
                if c % 4 == 3:
                    kb.cp("act", kendT[:, c - 3:c + 1, :], ptr[b][0:64, :].rearrange("p (c k) -> p c k", c=4), [Bptr[b]], [BkT])
            for grp in range(4):
                for i in range(8):
                    c = grp * 8 + i
                    kb.mm(patt[0:64, i * 64:(i + 1) * 64], E[:, c * 64:(c + 1) * 64], C[:, c * 64:(c + 1) * 64], True, True, [BE, BC], [Bpatt])
                kb.tt("dve", attm[:, grp * 512:(grp + 1) * 512], patt[0:64, :], tri[:], ALU.mult, [Bpatt, Bc], [Batt])
            kb.memset("dve", U3[:, :, 0:1], 0.0, [BU])
            for grp in range(8):
                b = grp % 2
                for i in range(4):
                    c = grp * 4 + i
                    kb.mm(pupd[b][:, i * 128:(i + 1) * 128], kendT[:, c, :], vtk[:, c, :], True, True, [BkT, Bvtk], [Bpupd[b]])
                kb.cp("act" if b == 0 else "dve", U3[:, :, grp * 4 + 1:grp * 4 + 5].rearrange("p v c -> p c v"),
                      pupd[b][:].rearrange("p (c v) -> p c v", c=4), [Bpupd[b]], [BU])
            kb.cp("act", Dk3[:, :, 1:33], ebend[:].unsqueeze(1).to_broadcast([128, 128, 32]), [Beb], [BDk])
            P.op("dve", lambda: nc.vector.tensor_tensor_scan(out=St[:], data0=Dk[:], data1=U[:], initial=0.0, op0=ALU.mult, op1=ALU.add),
                 [BDk, BU], [BSt])
            kb.cp("act", Stc, St3[:, :, 0:32].rearrange("p v c -> p c v"), [BSt], [BU])
            for grp in range(4):
                b = grp % 2
                for i in range(8):
                    c = grp * 8 + i
                    kb.mm(po[b][:, i * 64:(i + 1) * 64], vtk[:, c, :], attm[:, c * 64:(c + 1) * 64], True, False, [Bvtk, Batt], [Bpo[b]])
                    kb.mm(po[b][:, i * 64:(i + 1) * 64], Stc[:, c, :], C[:, c * 64:(c + 1) * 64], False, True, [BU, BC], [Bpo[b]])
                kb.cp("act", o_h[:, grp * 512:(grp + 1) * 512], po[b][:], [Bpo[b]], [Boh])
            for cc in range(4):
                cs = slice(cc * 512, (cc + 1) * 512)
                kb.act(sq[:], o_h[:, cs], AF.Square, [Boh], [Bsq])
                kb.mm(psq[:], ones[:], sq[:], True, True, [Bon, Bsq], [Bpsq])
                kb.ts("dve", rs[:], psq[:], 1.0 / 128, ALU.mult, [Bpsq], [Brs], s2=EPS, op1=ALU.add)
                kb.act(rs[:], rs[:], AF.Sqrt, [Brs], [Brs])
                kb.recip(rs[:], rs[:], [Brs], [Brs])
                kb.stt(tmp[:], o_h[:, cs], ng[:, 0:1], rs[:], ALU.mult, ALU.mult, [Boh, Bc, Brs], [Btmp])
                kb.tt("dve", oTh[:, cs], tmp[:], gs[:, cs], ALU.mult, [Btmp, Bgs], [BoT])
            kb.ld(act_d[h], oTh[:], [BoT], (), owner=BoT)
        P.end_phase()

LAYER_NIN = {0: 3072, 1: 6144, 2: 8192, 3: 4176}
MIX_NAMES = {0: "swa", 1: "diff", 2: "hgrn", 3: "dsa"}


def build_program(layers, stop_after=None):
    nc = bass.Bass("TRN2", target_bir_lowering=False)
    dr = {}

    def ext(name, shape, dt=F32):
        dr[name] = nc.dram_tensor(name, list(shape), dt, kind="ExternalInput").ap()
        return dr[name]

    x_in = ext("x", [S, D])
    y_out = nc.dram_tensor("y", [S, D], F32, kind="ExternalOutput").ap()
    normg = ext("norm_g", [16, D])
    ident_d = ext("ident", [128, 128])
    i8_d = ext("i8", [128, 128])
    for l in layers:
        ext("w_in%d" % l, [D, LAYER_NIN[l]])
        ext("w_out%d" % l, [D, D])
        ext("w_up%d" % l, [D, 2 * DFF])
        ext("w_down%d" % l, [DFF, D])
        ext("conv%d" % l, [128, 88, 3])
    if 0 in layers:
        ext("bias_sw", [128, 32 * 256])
        ext("sinkrep", [1, 4096])
    if 1 in layers or 3 in layers:
        ext("biasn", [128, 32 * 256])
        ext("table", [32, 32])
    if 1 in layers:
        ext("diff_lambda", [4, 64])
        ext("diff_subg", [1, 128])
    xres = nc.dram_tensor("xres", [S, D], F32).ap()
    m_d = nc.dram_tensor("m_d", [S, D], F32).ap()
    act_d = nc.dram_tensor("act_d", [KFF, 128, S], BF16).ap()
    qkv_d = nc.dram_tensor("qkv_d", [64, 128, S], BF16).ap()
    vtok_d = nc.dram_tensor("vtok_d", [S, 2048], BF16).ap()
    if 2 in layers:
        ext("lbl", [128, 64])
        ext("hgrn_ng", [1, 128])
        ext("tri", [64, 512])
        hq_d = nc.dram_tensor("hq_d", [16, 128, S], F32).ap()
        hf_d = nc.dram_tensor("hf_d", [16, 128, S], F32).ap()
        hk_d = nc.dram_tensor("hk_d", [16, 128, S], F32).ap()
        hv_d = nc.dram_tensor("hv_d", [S, 2048], F32).ap()
    if 3 in layers:
        ext("cmask", [128, 128])
        wi_d = nc.dram_tensor("wi_d", [S, 16], F32).ap()
        mask_d = nc.dram_tensor("mask_d", [16, 128, S], BF16).ap()

    with ExitStack() as top:
        P = Prog(nc, top)
        kb = KB(nc, P)
        x_cur = x_in
        pend_m = None
        for li, l in enumerate(layers):
            with ExitStack() as hs:
                hT = kb.sb(hs, [128, 16, S], BF16, "hT")
                BhT = Buf("hT")
                phase_rn(kb, x_cur, xres, m_d if pend_m is not None else None,
                         normg[pend_m] if pend_m is not None else None, normg[4 * l + 0], hT, BhT, ident_d)
                if pend_m is not None:
                    x_cur = xres
                w_in = dr["w_in%d" % l]
                if l == 0:
                    tiles = [(i * 128, 128) for i in range(20)]
                    phase_proj_fm(kb, hT, BhT, w_in, tiles, epi_store_fm(kb, qkv_d, lambda ti: ti), epi_store_setup(kb))
                    phase_proj_tok(kb, hT, BhT, w_in, [(2560, 512)], epi_store_tok(kb, vtok_d, lambda gi: 0), epi_tok_setup(kb))
                elif l == 1:
                    tiles = [(i * 128, 128) for i in range(32)]
                    phase_proj_fm(kb, hT, BhT, w_in, tiles, epi_store_fm(kb, qkv_d, lambda ti: ti), epi_store_setup(kb))
                    phase_proj_tok(kb, hT, BhT, w_in, [(4096 + i * 512, 512) for i in range(4)],
                                   epi_store_tok(kb, vtok_d, lambda gi: gi * 512), epi_tok_setup(kb))
                elif l == 2:
                    hgrn_inproj(kb, hT, BhT, w_in, dr["lbl"], l, hq_d, hf_d, hk_d, hv_d, qkv_d)
                elif l == 3:
                    tiles = [(i * 128, 128) for i in range(20)] + [(3072 + i * 128, 128) for i in range(8)] + [(4096, 64)]
                    phase_proj_fm(kb, hT, BhT, w_in, tiles, epi_store_fm(kb, qkv_d, lambda ti: ti), epi_store_setup(kb))

                    def setup3(es):
                        c_ = epi_tok_setup(kb)(es)
                        c_["tf"] = [kb.sb(es, [128, 16], F32, "etf") for _ in range(2)]
                        c_["Bf"] = kb.P.bufs(2, "etf")
                        return c_

                    def epi3(ctx, gi, t, c0, w, pb, Bpb):
                        if gi == 0:
                            epi_store_tok(kb, vtok_d, lambda gi_: 0)(ctx, gi, t, c0, w, pb, Bpb)
                        else:
                            tl, B = ctx["tf"][t % 2], ctx["Bf"][t % 2]
                            kb.cp("dve", tl[:, 0:16], pb[:, 0:16], [Bpb], [B])
                            kb.ld(wi_d[t * 128:(t + 1) * 128, :], tl[:, 0:16], [B], (), owner=B)
                    phase_proj_tok(kb, hT, BhT, w_in, [(2560, 512), (4160, 16)], epi3, setup3)
            if l == 0:
                phase_swa(kb, qkv_d, vtok_d, act_d, dr["bias_sw"], i8_d, dr["sinkrep"])
            elif l == 2:
                phase_hgrn(kb, hq_d, hf_d, hk_d, hv_d, qkv_d, act_d, dr["hgrn_ng"], ident_d, dr["tri"])
            elif l == 3:
                phase_dsa_idx(kb, qkv_d, wi_d, mask_d, dr["cmask"], ident_d)
                phase_dsa_attn(kb, qkv_d, vtok_d, act_d, mask_d, dr["biasn"], i8_d, dr["table"])
            elif l == 1:
                phase_diff(kb, qkv_d, vtok_d, act_d, dr["biasn"], i8_d, dr["table"], dr["diff_lambda"], dr["diff_subg"], l)
            phase_down(kb, dr["w_out%d" % l], 16, act_d, m_d)
            with ExitStack() as hs:
                hT = kb.sb(hs, [128, 16, S], BF16, "hT")
                BhT = Buf("hT")
                phase_rn(kb, x_cur, xres, m_d, normg[4 * l + 1], normg[4 * l + 2], hT, BhT, ident_d)
                x_cur = xres
                phase_up(kb, hT, BhT, dr["w_up%d" % l], dr["conv%d" % l], act_d)
            phase_down(kb, dr["w_down%d" % l], KFF, act_d, m_d)
            pend_m = 4 * l + 3
        phase_rn(kb, x_cur, y_out, m_d, normg[pend_m], None, None, None, ident_d)
    return nc


def _bucket(n):
    n = np.maximum(n, 0)
    max_exact = 16
    lr = np.log(np.maximum(n, 1).astype(np.float32) / max_exact) / math.log(128 / max_exact)
    large = np.minimum(max_exact + (lr * (32 - max_exact)).astype(np.int32), 31)
    return np.where(n < max_exact, n, large)


def _host_consts(inputs, layers):
    c = {}
    c["ident"] = np.eye(128, dtype=np.float32)
    c["i8"] = (8.0 * np.eye(128)).astype(np.float32)
    table = np.asarray(inputs["rel_bias_table"], np.float32)
    if 0 in layers:
        s_ = np.arange(128)[:, None]
        q_ = np.arange(256)[None, :]
        dist = q_ - s_
        valid = (dist >= 0) & (dist < 128)
        bk = _bucket(dist)
        t = table[bk]
        t = np.where(valid[:, :, None], t, np.float32(MASKV))
        c["bias_sw"] = np.ascontiguousarray(t.transpose(0, 2, 1)).reshape(128, 32 * 256).astype(np.float32)
        c["sinkrep"] = np.ascontiguousarray(np.repeat(np.asarray(inputs["swa_sinks"], np.float32)[0], 128)[None, :])
    if 1 in layers or 3 in layers:
        s_ = np.arange(128)[:, None]
        q_ = np.arange(256)[None, :]
        dist = q_ - s_
        t = table[_bucket(dist)]
        t = np.where((dist >= 0)[:, :, None], t, np.float32(MASKV))
        c["biasn"] = np.ascontiguousarray(t.transpose(0, 2, 1)).reshape(128, 32 * 256).astype(np.float32)
        c["table"] = np.ascontiguousarray(table)
    if 2 in layers:
        lg = np.asarray(inputs["hgrn_lb_logits"], np.float32)
        c["lbl"] = np.ascontiguousarray(lg.reshape(4, 16, 128).transpose(2, 1, 0)).reshape(128, 64)
        c["hgrn_ng"] = np.ascontiguousarray(np.asarray(inputs["hgrn_norm_g"], np.float32)[0][None, :])
        tr_ = (np.arange(64)[:, None] <= np.arange(64)[None, :]).astype(np.float32)
        c["tri"] = np.ascontiguousarray(np.tile(tr_, (1, 8)))
    if 3 in layers:
        c["cmask"] = np.where(np.arange(128)[None, :] > np.arange(128)[:, None], np.float32(-1.0e30), np.float32(0.0)).astype(np.float32)
    if 1 in layers:
        c["diff_lambda"] = np.ascontiguousarray(np.asarray(inputs["diff_lambda"], np.float32)[0])
        c["diff_subg"] = np.ascontiguousarray(np.asarray(inputs["diff_subln_g"], np.float32)[0][None, :])
    return c


_W_KEYS = {0: ("swa_w_in", "swa_w_out"), 1: ("diff_w_in", "diff_w_out"), 2: ("hgrn_w_in", "hgrn_w_out"), 3: ("dsa_w_in", "dsa_w_out")}
LAUNCHES = [[0, 1, 2, 3]]
N_CORES = 4


def _layer_inputs(inputs, layers):
    m = {}
    m["norm_g"] = np.ascontiguousarray(np.asarray(inputs["norm_g"], np.float32).reshape(16, D))
    for l in layers:
        kin, kout = _W_KEYS[l]
        m["w_in%d" % l] = np.ascontiguousarray(np.asarray(inputs[kin], np.float32)[0])
        m["w_out%d" % l] = np.ascontiguousarray(np.asarray(inputs[kout], np.float32)[0])
        m["w_up%d" % l] = np.ascontiguousarray(np.asarray(inputs["ffn_w_up"], np.float32)[l])
        m["w_down%d" % l] = np.ascontiguousarray(np.asarray(inputs["ffn_w_down"], np.float32)[l])
        cv = np.asarray(inputs["ffn_conv"], np.float32)[l]
        m["conv%d" % l] = np.ascontiguousarray(cv.reshape(3, 88, 128).transpose(2, 1, 0))
    m.update(_host_consts(inputs, layers))
    return m


def run_launch(inputs, x, layers, cores=None):
    nc = build_program(layers)
    shared = _layer_inputs(inputs, layers)
    B = x.shape[0]
    cores = cores if cores is not None else N_CORES
    in_maps = []
    for c in range(cores):
        d = dict(shared)
        d["x"] = np.ascontiguousarray(x[c % B])
        in_maps.append(d)
    res = run_bass_kernel_spmd(nc, in_maps, core_ids=list(range(cores)))
    return np.stack([res.results[b]["y"] for b in range(B)], axis=0)


def kernel(**inputs):
    x = np.asarray(inputs["x"], np.float32)
    for layers in LAUNCHES:
        x = run_launch(inputs, x, layers)
    return x.astype(np.float32)
```

```python
import math
from contextlib import ExitStack

import numpy as np
import concourse.bass as bass
import concourse.mybir as mybir
from concourse.bass_utils import run_bass_kernel_spmd

F32 = mybir.dt.float32
BF16 = mybir.dt.bfloat16
AF = mybir.ActivationFunctionType
ALU = mybir.AluOpType
AX = mybir.AxisListType

ENGS = ("pe", "act", "dve", "pool", "sp")
CENGS = ("pe", "act", "dve", "pool")

S = 2048
D = 2048
NT = 16
DFF = 5632
KFF = 44
EPS = 1e-6
MASKV = -30000.0
N_DSEM = 56


class Buf:
    __slots__ = ("name", "w", "r", "dsem")

    def __init__(self, name):
        self.name = name
        self.w = None
        self.r = {}
        self.dsem = None


class Prog:
    def __init__(self, nc, es):
        self.nc = nc
        self.ops = {e: [] for e in ENGS}
        self.sems = {}
        self.tot = {}
        self.seen = {e: {} for e in ENGS}
        for e in CENGS:
            self.sems[e] = es.enter_context(nc.semaphore("c_" + e))
            self.tot[e] = 0
        self.dfree = []
        for i in range(N_DSEM):
            k = "d%d" % i
            self.sems[k] = es.enter_context(nc.semaphore(k))
            self.tot[k] = 0
            self.dfree.append(k)
        self.dused = []
        self.phase_bufs = []
        self.nphase = 0

    def buf(self, name="b"):
        b = Buf(name)
        self.phase_bufs.append(b)
        return b

    def bufs(self, n, name="b"):
        return [self.buf(name + str(i)) for i in range(n)]

    def _dsem(self, b):
        if b.dsem is None:
            b.dsem = self.dfree.pop()
            self.dused.append(b.dsem)
        return b.dsem

    def _deps(self, eng, reads, writes):
        deps = {}

        def add(ev):
            if ev is None:
                return
            k, v = ev
            if v > deps.get(k, 0):
                deps[k] = v
        for b in reads:
            add(b.w)
        for b in writes:
            add(b.w)
            for k, v in b.r.items():
                add((k, v))
        waits = []
        for k, v in deps.items():
            if k == eng and eng == "pe":
                continue
            if k not in CENGS:
                v = self.tot[k]
            if v > self.seen[eng].get(k, 0):
                self.seen[eng][k] = v
                waits.append((k, v))
        return waits

    def op(self, eng, fn, reads=(), writes=()):
        waits = self._deps(eng, reads, writes)
        self.tot[eng] += 1
        seq = self.tot[eng]
        self.ops[eng].append((waits, fn, eng, 1))
        for b in reads:
            if b.r.get(eng, 0) < seq:
                b.r[eng] = seq
        for b in writes:
            b.w = (eng, seq)
            b.r = {}

    def dma(self, q, fn, reads=(), writes=(), owner=None):
        waits = self._deps(q, reads, writes)
        if owner is None:
            owner = writes[0] if writes else reads[0]
        k = self._dsem(owner)
        self.tot[k] += 16
        val = self.tot[k]
        self.ops[q].append((waits, fn, k, 16))
        for b in reads:
            b.r[k] = val
        for b in writes:
            b.w = (k, val)
            b.r = {}

    def end_phase(self):
        nc = self.nc
        keys = list(CENGS) + list(self.dused)
        for e in ENGS:
            waits = []
            for k in keys:
                if k == e:
                    continue
                v = self.tot[k]
                if v > self.seen[e].get(k, 0):
                    self.seen[e][k] = v
                    waits.append((k, v))
            self.ops[e].append((waits, None, None, 0))
        with nc.Block() as block:
            def mk(ename, attr):
                lst = self.ops[ename]

                def body(e):
                    for waits, fn, k, inc in lst:
                        for wk, wv in waits:
                            e.wait_ge(self.sems[wk], wv)
                        if fn is not None:
                            fn().then_inc(self.sems[k], inc)
                getattr(block, attr)(body)
            mk("sp", "sync")
            mk("pe", "tensor")
            mk("act", "scalar")
            mk("dve", "vector")
            mk("pool", "gpsimd")
        self.ops = {e: [] for e in ENGS}
        for b in self.phase_bufs:
            b.dsem = None
            b.w = None
            b.r = {}
        self.phase_bufs = []
        self.dfree.extend(self.dused)
        self.dused = []
        self.nphase += 1


class KB:
    def __init__(self, nc, P):
        self.nc = nc
        self.P = P
        self.uid = 0

    def sb(self, es, shape, dt, name="t"):
        self.uid += 1
        return es.enter_context(self.nc.sbuf_tensor("%s_%d" % (name, self.uid), list(shape), dt))

    def ps(self, es, shape, dt, name="p"):
        self.uid += 1
        return es.enter_context(self.nc.psum_tensor("%s_%d" % (name, self.uid), list(shape), dt))

    def mm(self, out, lhsT, rhs, start, stop, r, w):
        nc = self.nc
        self.P.op("pe", lambda: nc.tensor.matmul(out, lhsT=lhsT, rhs=rhs, start=start, stop=stop), r, w)

    def tr(self, out, in_, ident, r, w):
        nc = self.nc
        self.P.op("pe", lambda: nc.tensor.transpose(out, in_, ident), r, w)

    def act(self, out, in_, func, r, w, bias=None, scale=None, accum=None):
        nc = self.nc
        kw = {}
        if bias is not None:
            kw["bias"] = bias
        if scale is not None:
            kw["scale"] = scale
        if accum is not None:
            kw["accum_out"] = accum
        self.P.op("act", lambda: nc.scalar.activation(out=out, in_=in_, func=func, **kw), r, w)

    def ts(self, eng, out, in0, s1, op0, r, w, s2=None, op1=None):
        e = self.nc.vector if eng == "dve" else self.nc.gpsimd
        if op1 is None:
            self.P.op(eng, lambda: e.tensor_scalar(out=out, in0=in0, scalar1=s1, scalar2=None, op0=op0), r, w)
        else:
            self.P.op(eng, lambda: e.tensor_scalar(out=out, in0=in0, scalar1=s1, scalar2=s2, op0=op0, op1=op1), r, w)

    def tt(self, eng, out, in0, in1, op, r, w):
        e = self.nc.vector if eng == "dve" else self.nc.gpsimd
        self.P.op(eng, lambda: e.tensor_tensor(out=out, in0=in0, in1=in1, op=op), r, w)

    def stt(self, out, in0, scalar, in1, op0, op1, r, w):
        nc = self.nc
        self.P.op("dve", lambda: nc.vector.scalar_tensor_tensor(out=out, in0=in0, scalar=scalar, in1=in1, op0=op0, op1=op1), r, w)

    def cp(self, eng, out, in_, r, w):
        nc = self.nc
        if eng == "act":
            self.P.op("act", lambda: nc.scalar.copy(out=out, in_=in_), r, w)
        elif eng == "dve":
            self.P.op("dve", lambda: nc.vector.tensor_copy(out=out, in_=in_), r, w)
        else:
            self.P.op("pool", lambda: nc.gpsimd.tensor_copy(out=out, in_=in_), r, w)

    def recip(self, out, in_, r, w):
        nc = self.nc
        self.P.op("dve", lambda: nc.vector.reciprocal(out=out, in_=in_), r, w)

    def memset(self, eng, ap, val, w):
        e = self.nc.vector if eng == "dve" else self.nc.gpsimd
        self.P.op(eng, lambda: e.memset(ap, val), (), w)

    def ld(self, out, in_, r, w, q="sp", owner=None):
        nc = self.nc
        if q == "sp":
            self.P.dma("sp", lambda: nc.sync.dma_start(out=out, in_=in_), r, w, owner)
        else:
            self.P.dma("pool", lambda: nc.gpsimd.dma_start(out=out, in_=in_), r, w, owner)

    def rstd(self, es_tiles, ss, r, w):
        self.ts("dve", ss, ss, 1.0 / D, ALU.mult, r, w, s2=EPS, op1=ALU.add)
        self.act(ss, ss, AF.Sqrt, r, w)
        self.recip(ss, ss, r, w)


def phase_rn(kb, x_src, x_dst, m_src, gpost, gpre, hT, BhT, ident_d):
    nc, P = kb.nc, kb.P
    NS = 3
    with ExitStack() as es:
        xt = [kb.sb(es, [128, D], F32, "xt") for _ in range(NS)]
        Bxt = P.bufs(NS, "xt")
        junk = [kb.sb(es, [128, D], BF16, "junk") for _ in range(2)]
        Bjunk = P.bufs(2, "junk")
        st = [kb.sb(es, [128, 2], F32, "st") for _ in range(NS)]
        Bst0 = P.bufs(NS, "st0")
        Bst1 = P.bufs(NS, "st1")
        if m_src is not None:
            mt = [kb.sb(es, [128, D], F32, "mt") for _ in range(NS)]
            Bmt = P.bufs(NS, "mt")
            gp = kb.sb(es, [128, D], F32, "gp")
            Bgp = P.buf("gp")
            kb.ld(gp[:], gpost.partition_broadcast(128), (), [Bgp])
        if gpre is not None:
            gq = kb.sb(es, [128, D], F32, "gq")
            Bgq = P.buf("gq")
            kb.ld(gq[:], gpre.partition_broadcast(128), (), [Bgq])
            hb = [kb.sb(es, [128, D], BF16, "hb") for _ in range(2)]
            Bhb = P.bufs(2, "hb")
            ident = kb.sb(es, [128, 128], BF16, "ident")
            Bid = P.buf("ident")
            kb.ld(ident[:], ident_d, (), [Bid], q="pool")
            pT = [kb.ps(es, [128, 1024], BF16, "pT") for _ in range(4)]
            BpT = P.bufs(4, "pT")

        def load_t(t_):
            if t_ >= NT:
                return
            rows_ = slice(t_ * 128, (t_ + 1) * 128)
            kb.ld(xt[t_ % NS][:], x_src[rows_, :], (), [Bxt[t_ % NS]])
            if m_src is not None:
                kb.ld(mt[t_ % NS][:], m_src[rows_, :], (), [Bmt[t_ % NS]])

        def stage1(t):
            if t >= NT or m_src is None:
                return
            s = t % NS
            rows = slice(t * 128, (t + 1) * 128)
            kb.act(junk[0][:], mt[s][:], AF.Square, [Bmt[s]], [Bjunk[0], Bst0[s]], accum=st[s][:, 0:1])
            kb.rstd(None, st[s][:, 0:1], [Bst0[s]], [Bst0[s]])
            kb.stt(mt[s][:], mt[s][:], st[s][:, 0:1], gp[:], ALU.mult, ALU.mult, [Bmt[s], Bst0[s], Bgp], [Bmt[s]])
            kb.tt("dve", xt[s][:], xt[s][:], mt[s][:], ALU.add, [Bxt[s], Bmt[s]], [Bxt[s]])
            kb.ld(x_dst[rows, :], xt[s][:], [Bxt[s]], (), owner=Bxt[s])

        def stage2(t):
            if gpre is None:
                return
            s = t % NS
            hs_ = t % 2
            kb.act(junk[1][:], xt[s][:], AF.Square, [Bxt[s]], [Bjunk[1], Bst1[s]], accum=st[s][:, 1:2])
            kb.rstd(None, st[s][:, 1:2], [Bst1[s]], [Bst1[s]])
            kb.stt(hb[hs_][:], xt[s][:], st[s][:, 1:2], gq[:], ALU.mult, ALU.mult, [Bxt[s], Bst1[s], Bgq], [Bhb[hs_]])
            for half in range(2):
                pi = (2 * t + half) % 4
                for j in range(8):
                    i = half * 8 + j
                    kb.tr(pT[pi][:, j * 128:(j + 1) * 128], hb[hs_][:, i * 128:(i + 1) * 128], ident[:], [Bhb[hs_], Bid], [BpT[pi]])
                src = pT[pi][:].rearrange("p (j q) -> p j q", j=8)
                dst = hT[:, half * 8:(half + 1) * 8, t * 128:(t + 1) * 128]
                kb.cp("act" if half == 0 else "dve", dst, src, [BpT[pi]], [P.buf("hTw")])
        load_t(0)
        load_t(1)
        stage1(0)
        for t in range(NT):
            load_t(t + 2)
            stage1(t + 1)
            stage2(t)
        P.end_phase()


def phase_proj_fm(kb, hT, BhT, w_d, coltiles, epilogue, epi_setup=None):
    nc, P = kb.nc, kb.P
    wv = w_d.rearrange("(kc p) n -> p kc n", p=128)
    with ExitStack() as es:
        NSL = 3
        wsl = [kb.sb(es, [128, 16, 256], BF16, "wsl") for _ in range(NSL)]
        Bw = P.bufs(NSL, "wsl")
        pb = [kb.ps(es, [128, 2048], F32, "pb") for _ in range(2)]
        Bpb = P.bufs(2, "pb")
        ctx = epi_setup(es) if epi_setup is not None else None
        slabs = []
        i = 0
        while i < len(coltiles):
            c0, w0 = coltiles[i]
            if i + 1 < len(coltiles) and coltiles[i + 1][0] == c0 + w0 and w0 == 128:
                slabs.append([coltiles[i], coltiles[i + 1]])
                i += 2
            else:
                slabs.append([coltiles[i]])
                i += 1
        ti = 0

        def load_slab(si):
            if si >= len(slabs):
                return
            s_ = si % NSL
            c0_ = slabs[si][0][0]
            wtot = sum(w for _, w in slabs[si])
            kb.ld(wsl[s_][:, :, 0:wtot], wv[:, :, c0_:c0_ + wtot], (), [Bw[s_]], q="pool")
        load_slab(0)
        load_slab(1)
        for si, sl in enumerate(slabs):
            s = si % NSL
            load_slab(si + 2)
            off = 0
            for (cc, w) in sl:
                pi = ti % 2
                for g in range(4):
                    for k in range(16):
                        kb.mm(pb[pi][0:w, g * 512:(g + 1) * 512], wsl[s][:, k, off:off + w], hT[:, k, g * 512:(g + 1) * 512],
                              k == 0, k == 15, [Bw[s], BhT], [Bpb[pi]])
                epilogue(ctx, ti, cc, w, pb[pi], Bpb[pi])
                off += w
                ti += 1
        P.end_phase()


def phase_proj_tok(kb, hT, BhT, w_d, colgroups, epilogue, epi_setup=None):
    nc, P = kb.nc, kb.P
    wv = w_d.rearrange("(kc p) n -> p kc n", p=128)
    with ExitStack() as es:
        wg = [kb.sb(es, [128, 16, 512], BF16, "wg") for _ in range(2)]
        Bwg = P.bufs(2, "wg")
        pb = [kb.ps(es, [128, 512], F32, "pk") for _ in range(4)]
        Bpb = P.bufs(4, "pk")
        ctx = epi_setup(es) if epi_setup is not None else None
        n = 0

        def load_g(gi):
            if gi >= len(colgroups):
                return
            c0_, w_ = colgroups[gi]
            kb.ld(wg[gi % 2][:, :, 0:w_], wv[:, :, c0_:c0_ + w_], (), [Bwg[gi % 2]], q="pool")
        load_g(0)
        for gi, (c0, w) in enumerate(colgroups):
            s = gi % 2
            load_g(gi + 1)
            for t in range(NT):
                pi = n % 4
                n += 1
                for k in range(16):
                    kb.mm(pb[pi][:, 0:w], hT[:, k, t * 128:(t + 1) * 128], wg[s][:, k, 0:w], k == 0, k == 15, [BhT, Bwg[s]], [Bpb[pi]])
                epilogue(ctx, gi, t, c0, w, pb[pi], Bpb[pi])
        P.end_phase()


def phase_up(kb, hT, BhT, w_up, conv_d, act_d):
    nc, P = kb.nc, kb.P
    wv = w_up.rearrange("(kc p) n -> p kc n", p=128)
    with ExitStack() as es:
        NSL = 3
        wsl = [kb.sb(es, [128, 16, 512], BF16, "wup") for _ in range(NSL)]
        Bw = P.bufs(NSL, "wup")
        cw = kb.sb(es, [128, 88, 3], F32, "cw")
        Bcw = P.buf("cw")
        kb.ld(cw[:], conv_d, (), [Bcw])
        psg = kb.ps(es, [128, 2048], F32, "psg")
        psv = kb.ps(es, [128, 2048], F32, "psv")
        Bpsg, Bpsv = P.buf("psg"), P.buf("psv")
        ub = [kb.sb(es, [128, 2050], F32, "ub") for _ in range(4)]
        Bub = P.bufs(4, "ub")
        cb = [kb.sb(es, [128, 2048], F32, "cb") for _ in range(4)]
        Bcb = P.bufs(4, "cb")
        ob = [kb.sb(es, [128, 2048], BF16, "ob") for _ in range(2)]
        Bob = P.bufs(2, "ob")
        Bact = P.buf("actd")
        for i in range(4):
            kb.memset("pool", ub[i][:, 0:2], 0.0, [Bub[i]])
        def load_up(sl_):
            if sl_ >= KFF // 2:
                return
            s_ = sl_ % NSL
            c_ = sl_ * 2
            kb.ld(wsl[s_][:, :, 0:256], wv[:, :, c_ * 128:c_ * 128 + 256], (), [Bw[s_]], q="pool")
            kb.ld(wsl[s_][:, :, 256:512], wv[:, :, DFF + c_ * 128:DFF + c_ * 128 + 256], (), [Bw[s_]], q="pool")
        load_up(0)
        load_up(1)
        for c in range(KFF):
            sl = c // 2
            s = sl % NSL
            if c % 2 == 0:
                load_up(sl + 2)
            par = c % 2
            for part, (pp, Bpp) in enumerate(((psg, Bpsg), (psv, Bpsv))):
                off = part * 256 + par * 128
                for g in range(4):
                    for k in range(16):
                        kb.mm(pp[:, g * 512:(g + 1) * 512], wsl[s][:, k, off:off + 128], hT[:, k, g * 512:(g + 1) * 512],
                              k == 0, k == 15, [Bw[s], BhT], [Bpp])
                u = par * 2 + part
                fi = part * KFF + c
                kb.cp("act", ub[u][:, 2:2050], pp[:], [Bpp], [Bub[u]])
                kb.act(cb[u][:], ub[u][:, 0:2048], AF.Copy, [Bub[u], Bcw], [Bcb[u]], scale=cw[:, fi, 0:1])
                kb.stt(cb[u][:], ub[u][:, 1:2049], cw[:, fi, 1:2], cb[u][:], ALU.mult, ALU.add, [Bub[u], Bcw, Bcb[u]], [Bcb[u]])
                kb.stt(cb[u][:], ub[u][:, 2:2050], cw[:, fi, 2:3], cb[u][:], ALU.mult, ALU.add, [Bub[u], Bcw, Bcb[u]], [Bcb[u]])
                if part == 0:
                    kb.act(cb[u][:], cb[u][:], AF.Silu, [Bcb[u]], [Bcb[u]])
            ug, uv = par * 2, par * 2 + 1
            kb.tt("dve", ob[par][:], cb[ug][:], cb[uv][:], ALU.mult, [Bcb[ug], Bcb[uv]], [Bob[par]])
            kb.ld(act_d[c], ob[par][:], [Bob[par]], (), owner=Bob[par])
        P.end_phase()


def phase_down(kb, w_d, KC, act_d, m_d):
    nc, P = kb.nc, kb.P
    wv = w_d.rearrange("(kc p) n -> p kc n", p=128)
    av = act_d.rearrange("c p s -> p c s")
    TG = 256
    with ExitStack() as es:
        wres = [kb.sb(es, [128, KC, 512], BF16, "wres") for _ in range(2)]
        Bwr = [P.bufs(4, "wr%d_" % i) for i in range(2)]
        slab = [kb.sb(es, [128, KC, TG], BF16, "slab") for _ in range(2)]
        Bsl = P.bufs(2, "slab")
        pb = [kb.ps(es, [128, 512], F32, "pd") for _ in range(4)]
        Bpb = P.bufs(4, "pd")
        ot = [kb.sb(es, [128, 512], F32, "ot") for _ in range(3)]
        Bot = P.bufs(3, "ot")
        Bm = P.buf("md")
        kq = [(i * KC) // 4 for i in range(5)]
        n = 0
        ns = 0
        NTG = S // TG

        def load_w(cg_):
            if cg_ >= 4:
                return
            for pq_ in range(4):
                kb.ld(wres[cg_ % 2][:, kq[pq_]:kq[pq_ + 1], :], wv[:, kq[pq_]:kq[pq_ + 1], cg_ * 512:(cg_ + 1) * 512], (),
                      [Bwr[cg_ % 2][pq_]], q="pool")

        def load_slab(i_):
            if i_ >= 4 * NTG:
                return
            tg_ = i_ % NTG
            kb.ld(slab[i_ % 2][:], av[:, 0:KC, tg_ * TG:(tg_ + 1) * TG], (), [Bsl[i_ % 2]])
        load_slab(0)
        load_w(0)
        for cg in range(4):
            ws = cg % 2
            load_w(cg + 1)
            for tg in range(NTG):
                ss = ns % 2
                ns += 1
                load_slab(ns)
                for tt_ in range(TG // 128):
                    pi = n % 4
                    oi = n % 3
                    n += 1
                    for c in range(KC):
                        pq = 0
                        while c >= kq[pq + 1]:
                            pq += 1
                        kb.mm(pb[pi][:], slab[ss][:, c, tt_ * 128:(tt_ + 1) * 128], wres[ws][:, c, :], c == 0, c == KC - 1,
                              [Bsl[ss], Bwr[ws][pq]], [Bpb[pi]])
                    kb.cp("act" if n % 2 == 0 else "dve", ot[oi][:], pb[pi][:], [Bpb[pi]], [Bot[oi]])
                    r0 = tg * TG + tt_ * 128
                    kb.ld(m_d[r0:r0 + 128, cg * 512:(cg + 1) * 512], ot[oi][:], [Bot[oi]], (), owner=Bot[oi])
        P.end_phase()


def epi_store_setup(kb, n=2):
    def setup(es):
        P = kb.P
        return {"t": [kb.sb(es, [128, 2048], BF16, "eo") for _ in range(n)], "B": P.bufs(n, "eo"), "Bd": P.buf("qkvd"), "n": 0}
    return setup


def epi_store_fm(kb, qkv_d, tile_of):
    def epi(ctx, ti, c0, w, pb, Bpb):
        i = ctx["n"] % len(ctx["t"])
        ctx["n"] += 1
        t, B = ctx["t"][i], ctx["B"][i]
        kb.cp("act" if ti % 2 == 0 else "dve", t[0:w, :], pb[0:w, :], [Bpb], [B])
        kb.ld(qkv_d[tile_of(ti), 0:w, :], t[0:w, :], [B], (), owner=B)
    return epi


def epi_tok_setup(kb, dt=BF16):
    def setup(es):
        P = kb.P
        return {"t": [kb.sb(es, [128, 512], dt, "et") for _ in range(3)], "B": P.bufs(3, "et"), "Bd": P.buf("vtokd"), "n": 0}
    return setup


def epi_store_tok(kb, vtok_d, col_of):
    def epi(ctx, gi, t, c0, w, pb, Bpb):
        i = ctx["n"] % 3
        ctx["n"] += 1
        tl, B = ctx["t"][i], ctx["B"][i]
        kb.cp("act" if ctx["n"] % 2 == 0 else "dve", tl[:, 0:w], pb[:, 0:w], [Bpb], [B])
        d0 = col_of(gi)
        kb.ld(vtok_d[t * 128:(t + 1) * 128, d0:d0 + w], tl[:, 0:w], [B], (), owner=B)
    return epi


def phase_swa(kb, qkv_d, vtok_d, act_d, bias_sw_d, i8_d, sink_d):
    nc, P = kb.nc, kb.P
    with ExitStack() as es:
        vt = kb.sb(es, [128, 16, 512], BF16, "vt")
        Bvt = P.buf("vt")
        kb.ld(vt[:], vtok_d[:, 0:512].rearrange("(t p) c -> p t c", p=128), (), [Bvt])
        bsw = kb.sb(es, [128, 32, 256], BF16, "bsw")
        Bbsw = P.buf("bsw")
        kb.ld(bsw[:], bias_sw_d.rearrange("p (h q) -> p h q", h=32), (), [Bbsw], q="pool")
        i8 = kb.sb(es, [128, 128], BF16, "i8")
        Bi8 = P.buf("i8")
        kb.ld(i8[:], i8_d, (), [Bi8], q="pool")
        ones64 = kb.sb(es, [128, 64], BF16, "ones64")
        Bon = P.buf("ones")
        kb.memset("dve", ones64[:], 1.0, [Bon])
        skf = kb.sb(es, [1, 4096], F32, "skf")
        skb = kb.sb(es, [1, 4096], BF16, "skb")
        Bsk = P.buf("sk")
        kb.ld(skf[:], sink_d, (), [Bsk])
        kb.act(skb[:], skf[:], AF.Exp, [Bsk], [Bsk])
        kh = [kb.sb(es, [64, 2048], BF16, "kh") for _ in range(2)]
        qh = [kb.sb(es, [64, 4, 2048], BF16, "qh") for _ in range(2)]
        Bkq = P.bufs(2, "kq")
        PT = [kb.sb(es, [128, 4, 256], BF16, "PT") for _ in range(3)]
        BPT = P.bufs(3, "PT")
        oT = [kb.sb(es, [64, 4, 2048], BF16, "oT") for _ in range(2)]
        BoT = P.bufs(2, "oT")
        rec = [kb.sb(es, [64, 512], F32, "rec") for _ in range(2)]
        Brec = P.bufs(2, "rec")
        pss = [kb.ps(es, [128, 1024], F32, "pss") for _ in range(2)]
        Bpss = P.bufs(2, "pss")
        pnum = [kb.ps(es, [64, 512], F32, "pnum") for _ in range(2)]
        Bpn = P.bufs(2, "pnum")
        pden = [kb.ps(es, [64, 512], F32, "pden") for _ in range(2)]
        Bpd = P.bufs(2, "pden")
        Bact = P.buf("actd")
        nj = 0
        nb = 0

        def load_kv(kv_):
            if kv_ >= 8:
                return
            s_ = kv_ % 2
            kb.ld(kh[s_][:], qkv_d[16 + kv_ // 2, (kv_ % 2) * 64:(kv_ % 2) * 64 + 64, :], (), [Bkq[s_]])
            for g_ in range(4):
                hq_ = 4 * kv_ + g_
                kb.ld(qh[s_][:, g_, :], qkv_d[hq_ // 2, (hq_ % 2) * 64:(hq_ % 2) * 64 + 64, :], (), [Bkq[s_]])
        load_kv(0)
        for kv in range(8):
            s = kv % 2
            load_kv(kv + 1)

            def stageA(j, kv=kv, s=s, base=nj):
                nq = 256 if j < 15 else 128
                sl = (base + j) % 2
                pt = (base + j) % 3
                for g in range(4):
                    o = pss[sl][:, g * 256:g * 256 + nq]
                    kb.mm(o, kh[s][:, j * 128:(j + 1) * 128], qh[s][:, g, j * 128:j * 128 + nq], True, False, [Bkq[s]], [Bpss[sl]])
                    kb.mm(o, i8[:], bsw[:, 4 * kv + g, 0:nq], False, True, [Bi8, Bbsw], [Bpss[sl]])
                src = pss[sl][:].rearrange("p (g q) -> p g q", g=4)[:, :, 0:nq]
                kb.act(PT[pt][:, :, 0:nq], src, AF.Exp, [Bpss[sl]], [BPT[pt]], scale=0.125)

            def stageB(j, kv=kv, s=s, base=nj, nb0=nb):
                pt = (base + j) % 3
                prev = (base + j - 1) % 3 if j > 0 else None
                b = (nb0 + j) % 2
                rds = [Bvt, BPT[pt]] + ([BPT[prev]] if prev is not None else [])
                kb.mm(pden[b][:], ones64[0:1, 0:64], skb[0:1, 4 * kv * 128:(4 * kv + 4) * 128], True, False, [Bon, Bsk], [Bpd[b]])
                if prev is not None:
                    kb.mm(pnum[b][:], vt[:, j - 1, kv * 64:(kv + 1) * 64], PT[prev][:, :, 128:256], True, False, rds, [Bpn[b]])
                    kb.mm(pden[b][:], ones64[:], PT[prev][:, :, 128:256], False, False, rds + [Bon], [Bpd[b]])
                kb.mm(pnum[b][:], vt[:, j, kv * 64:(kv + 1) * 64], PT[pt][:, :, 0:128], prev is None, True, rds, [Bpn[b]])
                kb.mm(pden[b][:], ones64[:], PT[pt][:, :, 0:128], False, True, rds + [Bon], [Bpd[b]])
                kb.recip(rec[b][:], pden[b][:], [Bpd[b]], [Brec[b]])
                kb.tt("dve", oT[s][:, :, j * 128:(j + 1) * 128], pnum[b][:].rearrange("p (g q) -> p g q", g=4),
                      rec[b][:].rearrange("p (g q) -> p g q", g=4), ALU.mult, [Bpn[b], Brec[b]], [BoT[s]])
            stageA(0)
            for j in range(16):
                if j + 1 < 16:
                    stageA(j + 1)
                stageB(j)
            nj += 16
            nb += 16
            for g in range(4):
                hq = 4 * kv + g
                kb.ld(act_d[hq // 2, (hq % 2) * 64:(hq % 2) * 64 + 64, :], oT[s][:, g, :], [BoT[s]], (), owner=BoT[s])
        P.end_phase()


def phase_diff(kb, qkv_d, vtok_d, act_d, biasn_d, i8_d, table_d, lam_d, subg_d, layer_idx):
    nc, P = kb.nc, kb.P
    lam_init = 0.8 - 0.6 * math.exp(-0.3 * layer_idx)
    with ExitStack() as es:
        bn = kb.sb(es, [128, 32, 256], BF16, "bn")
        Bbn = P.buf("bn")
        kb.ld(bn[:], biasn_d.rearrange("p (h q) -> p h q", h=32), (), [Bbn], q="pool")
        i8 = kb.sb(es, [128, 128], BF16, "i8")
        Bi8 = P.buf("i8")
        kb.ld(i8[:], i8_d, (), [Bi8], q="pool")
        ones = kb.sb(es, [128, 128], BF16, "ones")
        onesf = kb.sb(es, [1, 128], F32, "onesf")
        Bon = P.buf("ones")
        kb.memset("dve", ones[:], 1.0, [Bon])
        kb.memset("dve", onesf[:], 1.0, [Bon])
        c31 = kb.sb(es, [128, 32], F32, "c31")
        Bc31 = P.buf("c31")
        kb.ld(c31[:], table_d[31].partition_broadcast(128), (), [Bc31])
        lamt = kb.sb(es, [1, 256], F32, "lamt")
        lsm = kb.sb(es, [1, 136], F32, "lsm")
        Blam = P.buf("lam")
        kb.ld(lamt[:], lam_d.rearrange("(o r) c -> o (r c)", o=1), (), [Blam])
        kb.tt("dve", lsm[:, 0:64], lamt[:, 0:64], lamt[:, 64:128], ALU.mult, [Blam], [Blam])
        kb.tt("dve", lsm[:, 64:128], lamt[:, 128:192], lamt[:, 192:256], ALU.mult, [Blam], [Blam])
        P.op("dve", lambda: nc.vector.tensor_reduce(out=lsm[:, 128:130], in_=lsm[:, 0:128].rearrange("p (a b) -> p a b", a=2), axis=AX.X, op=ALU.add), [Blam], [Blam])
        kb.act(lsm[:, 130:132], lsm[:, 128:130], AF.Exp, [Blam], [Blam])
        kb.tt("dve", lsm[:, 132:133], lsm[:, 130:131], lsm[:, 131:132], ALU.subtract, [Blam], [Blam])
        kb.ts("dve", lsm[:, 132:133], lsm[:, 132:133], lam_init, ALU.add, [Blam], [Blam])
        kb.cp("dve", lsm[:, 133:134], lsm[:, 132:133], [Blam], [Blam])
        pl = kb.ps(es, [128, 512], F32, "pl")
        Bpl = P.buf("pl")
        kb.mm(pl[:, 0:2], onesf[0:1, :], lsm[0:1, 132:134], True, True, [Bon, Blam], [Bpl])
        neglam = kb.sb(es, [128, 2], F32, "neglam")
        Bnl = P.buf("neglam")
        kb.ts("dve", neglam[:], pl[:, 0:2], -1.0, ALU.mult, [Bpl], [Bnl])
        gsc = kb.sb(es, [128, 1], F32, "gsc")
        Bgsc = P.buf("gsc")
        kb.ld(gsc[:], subg_d.rearrange("o e -> e o"), (), [Bgsc])
        kb.ts("dve", gsc[:], gsc[:], 1.0 - lam_init, ALU.mult, [Bgsc], [Bgsc])
        qh = [kb.sb(es, [128, 2048], BF16, "qh") for _ in range(2)]
        kh = [kb.sb(es, [128, 2048], BF16, "kh") for _ in range(2)]
        vh = [kb.sb(es, [128, 16, 128], BF16, "vh") for _ in range(2)]
        Bin = P.bufs(2, "qkv")
        PT = [kb.sb(es, [128, 512], BF16, "PT") for _ in range(4)]
        BPT = P.bufs(4, "PT")
        oTh = [kb.sb(es, [128, 2048], BF16, "oTh") for _ in range(2)]
        BoT = P.bufs(2, "oTh")
        r1 = kb.sb(es, [128, 512], F32, "r1")
        r2 = kb.sb(es, [128, 512], F32, "r2")
        t1 = kb.sb(es, [128, 512], F32, "t1")
        t2 = kb.sb(es, [128, 512], F32, "t2")
        sq = kb.sb(es, [128, 512], BF16, "sq")
        rs = kb.sb(es, [128, 512], F32, "rs")
        Bw = P.buf("work")
        Bsq = P.buf("sq")
        Brs = P.buf("rs")
        pss = [pl, kb.ps(es, [128, 512], F32, "pss"), kb.ps(es, [128, 512], F32, "pss")]
        Bpss = [Bpl, P.buf("pss"), P.buf("pss")]
        pnum = [kb.ps(es, [128, 512], F32, "pnum") for _ in range(2)]
        pden = [kb.ps(es, [128, 512], F32, "pden") for _ in range(2)]
        Bpn = P.bufs(2, "pn")
        Bpd = P.bufs(2, "pd")
        psq = kb.ps(es, [128, 512], F32, "psq")
        Bpsq = P.buf("psq")

        def load_h(h_):
            if h_ >= 16:
                return
            s_ = h_ % 2
            kb.ld(qh[s_][:], qkv_d[h_], (), [Bin[s_]])
            kb.ld(kh[s_][:], qkv_d[16 + h_], (), [Bin[s_]])
            kb.ld(vh[s_][:], vtok_d[:, h_ * 128:(h_ + 1) * 128].rearrange("(t p) e -> p t e", p=128), (), [Bin[s_]])
        load_h(0)
        base = 0
        NPS = 3
        for h in range(16):
            s = h % 2
            load_h(h + 1)
            its = [(c, m, j) for c in range(4) for m in range(2) for j in range(4 * c + 4)]

            def geom(c, j):
                lo = max(j, 4 * c)
                N = (4 * c + 4 - lo) * 128
                if j >= 4 * c:
                    nn, boff = min(N, 256), 0
                elif j == 4 * c - 1:
                    nn, boff = 128, 128
                else:
                    nn, boff = 0, 0
                return lo, N, nn, boff

            def stageA(idx):
                c, m, j = its[idx]
                lo, N, nn, boff = geom(c, j)
                pr = slice(m * 64, (m + 1) * 64)
                mp = 2 * h + m
                q0 = lo * 128
                sl = (base + idx) % NPS
                pt = (base + idx) % 4
                kb.mm(pss[sl][:, 0:N], kh[s][pr, j * 128:(j + 1) * 128], qh[s][pr, q0:q0 + N], True, nn == 0, [Bin[s]], [Bpss[sl]])
                if nn:
                    kb.mm(pss[sl][:, 0:nn], i8[:], bn[:, mp, boff:boff + nn], False, True, [Bi8, Bbn], [Bpss[sl]])
                    kb.act(PT[pt][:, 0:nn], pss[sl][:, 0:nn], AF.Exp, [Bpss[sl]], [BPT[pt]], scale=0.125)
                if N > nn:
                    kb.act(PT[pt][:, nn:N], pss[sl][:, nn:N], AF.Exp, [Bpss[sl], Bc31], [BPT[pt]], scale=0.125, bias=c31[:, mp:mp + 1])

            def stageB(idx):
                c, m, j = its[idx]
                lo, N, nn, boff = geom(c, j)
                off = (lo - 4 * c) * 128
                jl = 4 * c + 3
                pt = (base + idx) % 4
                kb.mm(pnum[m][:, off:512], vh[s][:, j, :], PT[pt][:, 0:N], j == 0, j == jl, [Bin[s], BPT[pt]], [Bpn[m]])
                kb.mm(pden[m][:, off:512], ones[:], PT[pt][:, 0:N], j == 0, j == jl, [Bon, BPT[pt]], [Bpd[m]])
                if m == 1 and j == jl:
                    kb.recip(r1[:], pden[0][:], [Bpd[0]], [Bw])
                    kb.recip(r2[:], pden[1][:], [Bpd[1]], [Bw])
                    kb.tt("dve", t1[:], pnum[0][:], r1[:], ALU.mult, [Bpn[0], Bw], [Bw])
                    kb.tt("dve", t2[:], pnum[1][:], r2[:], ALU.mult, [Bpn[1], Bw], [Bw])
                    kb.stt(t1[:], t2[:], neglam[:, 0:1], t1[:], ALU.mult, ALU.add, [Bw, Bnl], [Bw])
                    kb.act(sq[:], t1[:], AF.Square, [Bw], [Bsq])
                    kb.mm(psq[:], ones[:], sq[:], True, True, [Bon, Bsq], [Bpsq])
                    kb.ts("dve", rs[:], psq[:], 1.0 / 128, ALU.mult, [Bpsq], [Brs], s2=EPS, op1=ALU.add)
                    kb.act(rs[:], rs[:], AF.Sqrt, [Brs], [Brs])
                    kb.recip(rs[:], rs[:], [Brs], [Brs])
                    kb.stt(oTh[s][:, c * 512:(c + 1) * 512], t1[:], gsc[:, 0:1], rs[:], ALU.mult, ALU.mult, [Bw, Bgsc, Brs], [BoT[s]])
            LA = 2
            for i0 in range(min(LA, len(its))):
                stageA(i0)
            for idx in range(len(its)):
                if idx + LA < len(its):
                    stageA(idx + LA)
                stageB(idx)
            base += len(its)
            kb.ld(act_d[h], oTh[s][:], [BoT[s]], (), owner=BoT[s])
        P.end_phase()


def phase_dsa_idx(kb, qkv_d, wi_d, mask_d, cmask_d, ident_d):
    nc, P = kb.nc, kb.P
    BIG = 1.0e30
    with ExitStack() as es:
        kiT = kb.sb(es, [64, 2048], BF16, "kiT")
        qi = kb.sb(es, [64, 16, 2048], BF16, "qi")
        Bqk = P.buf("qiki")
        kb.ld(kiT[:], qkv_d[28, 0:64, :], (), [Bqk])
        for h in range(16):
            kb.ld(qi[:, h, :], qkv_d[20 + h // 2, (h % 2) * 64:(h % 2) * 64 + 64, :], (), [Bqk])
        cm = kb.sb(es, [128, 128], F32, "cm")
        Bcm = P.buf("cm")
        kb.ld(cm[:], cmask_d, (), [Bcm])
        ident = kb.sb(es, [128, 128], BF16, "ident")
        Bid = P.buf("ident")
        kb.ld(ident[:], ident_d, (), [Bid], q="pool")
        zt = kb.sb(es, [128, 256], BF16, "zt")
        Bzt = P.buf("zt")
        kb.memset("pool", zt[:], 0.0, [Bzt])
        kb.ld(mask_d[0, :, 0:256], zt[:], [Bzt], (), owner=Bzt)
        kb.ld(mask_d[1, :, 0:256], zt[:], [Bzt], (), owner=Bzt)
        wi = [kb.sb(es, [128, 16], F32, "wi") for _ in range(2)]
        Bwi = P.bufs(2, "wi")
        dgw = [kb.sb(es, [128, 16, 128], BF16, "dgw") for _ in range(2)]
        Bdg = P.bufs(2, "dgw")
        acc = [kb.sb(es, [128, 2048], F32, "acc") for _ in range(2)]
        Bacc = P.bufs(2, "acc")
        Rb = [kb.sb(es, [128, 512], BF16, "Rb") for _ in range(3)]
        BRb = P.bufs(3, "Rb")
        work = kb.sb(es, [128, 2048], F32, "work")
        Bwork = P.buf("work")
        m8 = kb.sb(es, [128, 8], F32, "m8")
        Bm8 = P.buf("m8")
        Mb = [kb.sb(es, [128, 2048], BF16, "Mb") for _ in range(2)]
        BMb = P.bufs(2, "Mb")
        mT = [kb.sb(es, [128, 8, 128], BF16, "mT") for _ in range(2)]
        BmT = P.bufs(2, "mT")
        pl = [kb.ps(es, [128, 512], F32, "pl") for _ in range(2)]
        Bpl = P.bufs(2, "pl")
        pacc = kb.ps(es, [128, 2048], F32, "pacc")
        Bpacc = P.bufs(4, "pacc")
        pT = [kb.ps(es, [128, 1024], BF16, "pT") for _ in range(2)]
        BpT = P.bufs(2, "pT")
        nl = 0
        nT = 0
        for qt in range(2, 16):
            a = qt % 2
            ns = (qt + 1) * 128
            kb.ld(wi[a][:], wi_d[qt * 128:(qt + 1) * 128, :], (), [Bwi[a]])
            for h in range(16):
                kb.ts("dve", dgw[a][:, h, :], ident[:], wi[a][:, h:h + 1], ALU.mult, [Bid, Bwi[a]], [Bdg[a]])
            its = [(sc, h) for sc in range((ns + 511) // 512) for h in range(16)]

            def stA(idx, qt=qt, ns=ns, base=nl):
                sc, h = its[idx]
                n2 = min(512, ns - sc * 512)
                sl = (base + idx) % 2
                rb = (base + idx) % 3
                kb.mm(pl[sl][:, 0:n2], qi[:, h, qt * 128:(qt + 1) * 128], kiT[:, sc * 512:sc * 512 + n2], True, True, [Bqk], [Bpl[sl]])
                kb.act(Rb[rb][:, 0:n2], pl[sl][:, 0:n2], AF.Relu, [Bpl[sl]], [BRb[rb]])

            def stB(idx, ns=ns, base=nl, a=a):
                sc, h = its[idx]
                n2 = min(512, ns - sc * 512)
                rb = (base + idx) % 3
                kb.mm(pacc[:, sc * 512:sc * 512 + n2], dgw[a][:, h, :], Rb[rb][:, 0:n2], h == 0, h == 15, [Bdg[a], BRb[rb]], [Bpacc[sc]])
            stA(0)
            for idx in range(len(its)):
                if idx + 1 < len(its):
                    stA(idx + 1)
                stB(idx)
            nl += len(its)
            nsc = (ns + 511) // 512
            kb.cp("act", acc[a][:, 0:ns], pacc[:, 0:ns], [Bpacc[i] for i in range(nsc)], [Bacc[a]])
            dg = acc[a][:, qt * 128:(qt + 1) * 128]
            kb.tt("dve", dg, dg, cm[:], ALU.add, [Bacc[a], Bcm], [Bacc[a]])
            src, Bsrc = acc[a], Bacc[a]
            for r in range(32):
                sv = src[:, 0:ns]
                P.op("dve", (lambda sv=sv: nc.vector.max(out=m8[:], in_=sv)), [Bsrc], [Bm8])
                if r < 31:
                    wv_ = work[:, 0:ns]
                    P.op("dve", (lambda sv=sv, wv_=wv_: nc.vector.match_replace(out=wv_, in_to_replace=m8[:], in_values=sv, imm_value=-BIG)), [Bsrc, Bm8], [Bwork])
                    src, Bsrc = work, Bwork
            kb.ts("dve", Mb[a][:, 0:ns], acc[a][:, 0:ns], m8[:, 7:8], ALU.is_ge, [Bacc[a], Bm8], [BMb[a]])
            for j0 in range(0, qt + 1, 8):
                njj = min(8, qt + 1 - j0)
                b = nT % 2
                nT += 1
                for i in range(njj):
                    j = j0 + i
                    kb.tr(pT[b][:, i * 128:(i + 1) * 128], Mb[a][:, j * 128:(j + 1) * 128], ident[:], [BMb[a], Bid], [BpT[b]])
                srcv = pT[b][:].rearrange("p (j q) -> p j q", j=8)[:, 0:njj, :]
                kb.ts("dve" if b == 0 else "dve", mT[b][:, 0:njj, :], srcv, -1.0, ALU.add, [BpT[b]], [BmT[b]], s2=-MASKV, op1=ALU.mult)
                kb.ld(mask_d[j0:j0 + njj, :, qt * 128:(qt + 1) * 128].rearrange("j p q -> p j q"), mT[b][:, 0:njj, :], [BmT[b]], (), owner=BmT[b])
        P.end_phase()


def phase_dsa_attn(kb, qkv_d, vtok_d, act_d, mask_d, biasn_d, i8_d, table_d):
    nc, P = kb.nc, kb.P
    with ExitStack() as es:
        bn = kb.sb(es, [128, 32, 256], BF16, "bn")
        Bbn = P.buf("bn")
        kb.ld(bn[:], biasn_d.rearrange("p (h q) -> p h q", h=32), (), [Bbn], q="pool")
        i8 = kb.sb(es, [128, 128], BF16, "i8")
        Bi8 = P.buf("i8")
        kb.ld(i8[:], i8_d, (), [Bi8], q="pool")
        ones = kb.sb(es, [128, 64], BF16, "ones")
        Bon = P.buf("ones")
        kb.memset("dve", ones[:], 1.0, [Bon])
        c31 = kb.sb(es, [128, 32], F32, "c31")
        Bc31 = P.buf("c31")
        kb.ld(c31[:], table_d[31].partition_broadcast(128), (), [Bc31])
        vt = kb.sb(es, [128, 16, 512], BF16, "vt")
        Bvt = P.buf("vt")
        kb.ld(vt[:], vtok_d[:, 0:512].rearrange("(t p) c -> p t c", p=128), (), [Bvt])
        mk = kb.sb(es, [128, 16, 2048], BF16, "mk")
        Bmk = P.buf("mk")
        for j in range(16):
            kb.ld(mk[:, j, j * 128:2048], mask_d[j, :, j * 128:2048], (), [Bmk])
        kh = [kb.sb(es, [64, 2048], BF16, "kh") for _ in range(2)]
        Bkh = P.bufs(2, "kh")
        qh = [kb.sb(es, [64, 2048], BF16, "qh") for _ in range(2)]
        Bqh = P.bufs(2, "qh")
        PT = [kb.sb(es, [128, 512], BF16, "PT") for _ in range(4)]
        BPT = P.bufs(4, "PT")
        oTh = [kb.sb(es, [64, 2048], BF16, "oTh") for _ in range(2)]
        BoT = P.bufs(2, "oTh")
        r1 = [kb.sb(es, [64, 512], F32, "r1") for _ in range(2)]
        Br1 = P.bufs(2, "r1")
        pss = [kb.ps(es, [128, 512], F32, "pss") for _ in range(4)]
        Bpss = P.bufs(4, "pss")
        pnum = [kb.ps(es, [64, 512], F32, "pnum") for _ in range(2)]
        pden = [kb.ps(es, [64, 512], F32, "pden") for _ in range(2)]
        Bpn = P.bufs(2, "pn")
        Bpd = P.bufs(2, "pd")

        def load_q(hq_):
            if hq_ >= 32:
                return
            kb.ld(qh[hq_ % 2][:], qkv_d[hq_ // 2, (hq_ % 2) * 64:(hq_ % 2) * 64 + 64, :], (), [Bqh[hq_ % 2]])

        def load_k(kv_):
            if kv_ >= 8:
                return
            kb.ld(kh[kv_ % 2][:], qkv_d[16 + kv_ // 2, (kv_ % 2) * 64:(kv_ % 2) * 64 + 64, :], (), [Bkh[kv_ % 2]])
        load_k(0)
        load_q(0)
        nj = 0
        nb = 0
        for kv in range(8):
            ks = kv % 2
            load_k(kv + 1)
            for g in range(4):
                hq = 4 * kv + g
                s = hq % 2
                load_q(hq + 1)
                its = [(c, j) for c in range(4) for j in range(4 * c + 4)]

                def geom(c, j):
                    lo = max(j, 4 * c)
                    N = (4 * c + 4 - lo) * 128
                    if j >= 4 * c:
                        nn, boff = min(N, 256), 0
                    elif j == 4 * c - 1:
                        nn, boff = 128, 128
                    else:
                        nn, boff = 0, 0
                    return lo, N, nn, boff

                def stageA(idx, hq=hq, ks=ks, s=s, base=nj):
                    c, j = its[idx]
                    lo, N, nn, boff = geom(c, j)
                    q0 = lo * 128
                    sl = (base + idx) % 4
                    pt = (base + idx) % 4
                    kb.mm(pss[sl][:, 0:N], kh[ks][:, j * 128:(j + 1) * 128], qh[s][:, q0:q0 + N], True, False, [Bkh[ks], Bqh[s]], [Bpss[sl]])
                    if nn:
                        kb.mm(pss[sl][:, 0:nn], i8[:], bn[:, hq, boff:boff + nn], False, False, [Bi8, Bbn], [Bpss[sl]])
                    kb.mm(pss[sl][:, 0:N], i8[:], mk[:, j, q0:q0 + N], False, True, [Bi8, Bmk], [Bpss[sl]])
                    if nn:
                        kb.act(PT[pt][:, 0:nn], pss[sl][:, 0:nn], AF.Exp, [Bpss[sl]], [BPT[pt]], scale=0.125)
                    if N > nn:
                        kb.act(PT[pt][:, nn:N], pss[sl][:, nn:N], AF.Exp, [Bpss[sl], Bc31], [BPT[pt]], scale=0.125, bias=c31[:, hq:hq + 1])

                def stageB(idx, hq=hq, kv=kv, s=s, base=nj, nb0=nb):
                    c, j = its[idx]
                    lo, N, nn, boff = geom(c, j)
                    off = (lo - 4 * c) * 128
                    jl = 4 * c + 3
                    pt = (base + idx) % 4
                    b = (nb0 + c) % 2
                    kb.mm(pnum[b][:, off:512], vt[:, j, kv * 64:(kv + 1) * 64], PT[pt][:, 0:N], j == 0, j == jl, [Bvt, BPT[pt]], [Bpn[b]])
                    kb.mm(pden[b][:, off:512], ones[:], PT[pt][:, 0:N], j == 0, j == jl, [Bon, BPT[pt]], [Bpd[b]])
                    if j == jl:
                        kb.recip(r1[b][:], pden[b][:], [Bpd[b]], [Br1[b]])
                        kb.tt("dve", oTh[s][:, c * 512:(c + 1) * 512], pnum[b][:], r1[b][:], ALU.mult, [Bpn[b], Br1[b]], [BoT[s]])
                LA = 2
                for i0 in range(min(LA, len(its))):
                    stageA(i0)
                for idx in range(len(its)):
                    if idx + LA < len(its):
                        stageA(idx + LA)
                    stageB(idx)
                nj += len(its)
                nb += 4
                kb.ld(act_d[hq // 2, (hq % 2) * 64:(hq % 2) * 64 + 64, :], oTh[s][:], [BoT[s]], (), owner=BoT[s])
        P.end_phase()


def hgrn_inproj(kb, hT, BhT, w_in, lbl_d, layer_idx, hq_d, hf_d, hk_d, hv_d, qkv_d):
    nc, P = kb.nc, kb.P
    tiles = [(i * 128, 128) for i in range(16)] + [(2048 + i * 128, 128) for i in range(16)] + [(6144 + i * 128, 128) for i in range(16)]

    def setup(es):
        c = {}
        lbl = kb.sb(es, [128, 16, 4], F32, "lbl")
        Bl = P.buf("lbl")
        kb.ld(lbl[:], lbl_d.rearrange("p (t l) -> p t l", l=4), (), [Bl])
        kb.act(lbl[:], lbl[:], AF.Exp, [Bl], [Bl])
        ssum = kb.sb(es, [128, 16], F32, "ssum")
        lb = kb.sb(es, [128, 16], F32, "lb")
        oml = kb.sb(es, [128, 16], F32, "oml")
        Blb = P.buf("lb")
        P.op("dve", lambda: nc.vector.tensor_reduce(out=ssum[:], in_=lbl[:], axis=AX.X, op=ALU.add), [Bl], [Blb])
        kb.recip(ssum[:], ssum[:], [Blb], [Blb])
        kb.cp("dve", lb[:], lbl[:, :, 1], [Bl, Blb], [Blb])
        for j in range(2, layer_idx + 1):
            kb.tt("dve", lb[:], lb[:], lbl[:, :, j], ALU.add, [Bl, Blb], [Blb])
        kb.tt("dve", lb[:], lb[:], ssum[:], ALU.mult, [Blb], [Blb])
        kb.ts("dve", oml[:], lb[:], -1.0, ALU.mult, [Blb], [Blb], s2=1.0, op1=ALU.add)
        c["lb"], c["oml"], c["Blb"] = lb, oml, Blb
        c["fo"] = [kb.sb(es, [128, 2048], F32, "fo") for _ in range(2)]
        c["Bfo"] = P.bufs(2, "fo")
        c["fk"] = [kb.sb(es, [128, 2048], F32, "fk") for _ in range(2)]
        c["Bfk"] = P.bufs(2, "fk")
        c["bo"] = [kb.sb(es, [128, 2048], BF16, "bo") for _ in range(2)]
        c["Bbo"] = P.bufs(2, "bo")
        return c

    def epi(c, ti, c0, w, pb, Bpb):
        kind, h = ti // 16, ti % 16
        a = ti % 2
        if kind == 0:
            kb.act(c["fo"][a][:], pb[:], AF.Silu, [Bpb], [c["Bfo"][a]])
            kb.ld(hq_d[h], c["fo"][a][:], [c["Bfo"][a]], (), owner=c["Bfo"][a])
        elif kind == 1:
            fo, Bfo, fk, Bfk = c["fo"][a], c["Bfo"][a], c["fk"][a], c["Bfk"][a]
            kb.act(fo[:], pb[:], AF.Sigmoid, [Bpb], [Bfo])
            kb.ts("dve", fo[:], fo[:], c["oml"][:, h:h + 1], ALU.mult, [Bfo, c["Blb"]], [Bfo], s2=c["lb"][:, h:h + 1], op1=ALU.add)
            kb.ts("dve", fk[:], fo[:], -1.0, ALU.mult, [Bfo], [Bfk], s2=1.0, op1=ALU.add)
            kb.ld(hk_d[h], fk[:], [Bfk], (), owner=Bfk)
            kb.act(fo[:], fo[:], AF.Ln, [Bfo], [Bfo])
            kb.ld(hf_d[h], fo[:], [Bfo], (), owner=Bfo)
        else:
            kb.act(c["bo"][a][:], pb[:], AF.Silu, [Bpb], [c["Bbo"][a]])
            kb.ld(qkv_d[h], c["bo"][a][:], [c["Bbo"][a]], (), owner=c["Bbo"][a])
    phase_proj_fm(kb, hT, BhT, w_in, tiles, epi, setup)
    phase_proj_tok(kb, hT, BhT, w_in, [(4096 + i * 512, 512) for i in range(4)],
                   epi_store_tok(kb, hv_d, lambda gi: gi * 512), epi_tok_setup(kb, F32))


def phase_hgrn(kb, hq_d, hf_d, hk_d, hv_d, qkv_d, act_d, ng_d, ident_d, tri_d):
    nc, P = kb.nc, kb.P
    with ExitStack() as es:
        identf = kb.sb(es, [128, 128], F32, "identf")
        tri = kb.sb(es, [64, 512], F32, "tri")
        ng = kb.sb(es, [128, 1], F32, "ng")
        Bc = P.buf("consts")
        kb.ld(identf[:], ident_d, (), [Bc])
        kb.ld(tri[:], tri_d, (), [Bc])
        kb.ld(ng[:], ng_d.rearrange("o e -> e o"), (), [Bc])
        ones = kb.sb(es, [128, 128], BF16, "ones")
        rmask = kb.sb(es, [128, 2048], F32, "rmask")
        Bon = P.buf("ones")
        kb.memset("dve", ones[:], 1.0, [Bon])
        kb.memset("dve", rmask[:], 1.0, [Bon])
        kb.memset("dve", rmask[:].rearrange("p (c t) -> p c t", t=64)[:, :, 0:1], 0.0, [Bon])
        A = kb.sb(es, [128, 2048], F32, "A")
        Bb = kb.sb(es, [128, 2048], F32, "B")
        C = kb.sb(es, [128, 2048], F32, "C")
        Dd = kb.sb(es, [128, 2048], F32, "Dd")
        E = kb.sb(es, [128, 2048], F32, "E")
        Fk = kb.sb(es, [128, 2048], F32, "F")
        BA, BB, BC, BD, BE, BF_ = P.buf("A"), P.buf("B"), P.buf("C"), P.buf("D"), P.buf("E"), P.buf("F")
        U = kb.sb(es, [128, 128 * 33], F32, "U")
        Dk = kb.sb(es, [128, 128 * 33], F32, "Dk")
        St = kb.sb(es, [128, 128 * 33], F32, "St")
        BU, BDk, BSt = P.buf("U"), P.buf("Dk"), P.buf("St")
        U3 = U[:].rearrange("p (v c) -> p v c", c=33)
        Dk3 = Dk[:].rearrange("p (v c) -> p v c", c=33)
        St3 = St[:].rearrange("p (v c) -> p v c", c=33)
        Stc = U[:, 0:4096].rearrange("p (c v) -> p c v", v=128)
        kb.memset("pool", Dk[:], 0.0, [BDk])
        kb.memset("pool", U[:], 0.0, [BU])
        vtk = kb.sb(es, [64, 32, 128], F32, "vtk")
        Bvtk = P.buf("vtk")
        kendT = kb.sb(es, [64, 32, 128], F32, "kendT")
        BkT = P.buf("kendT")
        attm = kb.sb(es, [64, 2048], F32, "attm")
        Batt = P.buf("attm")
        o_h = kb.sb(es, [128, 2048], F32, "o_h")
        Boh = P.buf("o_h")
        gs = kb.sb(es, [128, 2048], BF16, "gs")
        Bgs = P.buf("gs")
        oTh = kb.sb(es, [128, 2048], BF16, "oTh")
        BoT = P.buf("oTh")
        ebend = kb.sb(es, [128, 32], F32, "ebend")
        Beb = P.buf("ebend")
        sq = kb.sb(es, [128, 512], BF16, "sq")
        rs = kb.sb(es, [128, 512], F32, "rs")
        tmp = kb.sb(es, [128, 512], F32, "tmp")
        Bsq, Brs, Btmp = P.buf("sq"), P.buf("rs"), P.buf("tmp")
        ptr = [kb.ps(es, [128, 512], F32, "ptr") for _ in range(2)]
        Bptr = P.bufs(2, "ptr")
        patt = kb.ps(es, [128, 512], F32, "patt")
        Bpatt = P.buf("patt")
        pupd = [kb.ps(es, [128, 512], F32, "pupd") for _ in range(2)]
        Bpupd = P.bufs(2, "pupd")
        po = [kb.ps(es, [128, 512], F32, "po") for _ in range(2)]
        Bpo = P.bufs(2, "po")
        psq = kb.ps(es, [128, 512], F32, "psq")
        Bpsq = P.buf("psq")
        B3 = Bb[:].rearrange("p (c t) -> p c t", t=64)
        A3 = A[:].rearrange("p (c t) -> p c t", t=64)
        for h in range(16):
            kb.ld(C[:], hq_d[h], (), [BC])
            kb.ld(A[:], hf_d[h], (), [BA])
            kb.ld(Dd[:], hk_d[h], (), [BD])
            kb.ld(vtk[:], hv_d[:, h * 128:(h + 1) * 128].rearrange("(c s) v -> s c v", s=64), (), [Bvtk])
            kb.ld(gs[:], qkv_d[h], (), [Bgs])
            P.op("dve", lambda: nc.vector.tensor_tensor_scan(out=Bb[:], data0=rmask[:], data1=A[:], initial=0.0, op0=ALU.mult, op1=ALU.add),
                 [Bon, BA], [BB])
            kb.act(A[:], Bb[:], AF.Exp, [BB], [BA])
            kb.tt("dve", C[:], C[:], A[:], ALU.mult, [BC, BA], [BC])
            kb.act(ebend[:], B3[:, :, 63], AF.Exp, [BB], [Beb])
            kb.ts("dve", A[:], Bb[:], -1.0, ALU.mult, [BB, BC], [BA], s2=80.0, op1=ALU.min)
            kb.act(A[:], A[:], AF.Exp, [BA], [BA])
            kb.tt("dve", E[:], Dd[:], A[:], ALU.mult, [BD, BA], [BE])
            kb.tt("dve", A3, B3[:, :, 63:64].to_broadcast([128, 32, 64]), B3, ALU.subtract, [BB, BE], [BA])
            kb.act(A[:], A[:], AF.Exp, [BA], [BA])
            kb.tt("dve", Fk[:], Dd[:], A[:], ALU.mult, [BD, BA], [BF_])
            for c in range(32):
                b = (c // 4) % 2
                kb.tr(ptr[b][0:64, (c % 4) * 128:(c % 4 + 1) * 128], Fk[:, c * 64:(c + 1) * 64], identf[:], [BF_, Bc], [Bptr[b]])
                if c % 4 == 3:
                    kb.cp("act", kendT[:, c - 3:c + 1, :], ptr[b][0:64, :].rearrange("p (c k) -> p c k", c=4), [Bptr[b]], [BkT])
            for grp in range(4):
                for i in range(8):
                    c = grp * 8 + i
                    kb.mm(patt[0:64, i * 64:(i + 1) * 64], E[:, c * 64:(c + 1) * 64], C[:, c * 64:(c + 1) * 64], True, True, [BE, BC], [Bpatt])
                kb.tt("dve", attm[:, grp * 512:(grp + 1) * 512], patt[0:64, :], tri[:], ALU.mult, [Bpatt, Bc], [Batt])
            kb.memset("dve", U3[:, :, 0:1], 0.0, [BU])
            for grp in range(8):
                b = grp % 2
                for i in range(4):
                    c = grp * 4 + i
                    kb.mm(pupd[b][:, i * 128:(i + 1) * 128], kendT[:, c, :], vtk[:, c, :], True, True, [BkT, Bvtk], [Bpupd[b]])
                kb.cp("act" if b == 0 else "dve", U3[:, :, grp * 4 + 1:grp * 4 + 5].rearrange("p v c -> p c v"),
                      pupd[b][:].rearrange("p (c v) -> p c v", c=4), [Bpupd[b]], [BU])
            kb.cp("act", Dk3[:, :, 1:33], ebend[:].unsqueeze(1).to_broadcast([128, 128, 32]), [Beb], [BDk])
            P.op("dve", lambda: nc.vector.tensor_tensor_scan(out=St[:], data0=Dk[:], data1=U[:], initial=0.0, op0=ALU.mult, op1=ALU.add),
                 [BDk, BU], [BSt])
            kb.cp("act", Stc, St3[:, :, 0:32].rearrange("p v c -> p c v"), [BSt], [BU])
            for grp in range(4):
                b = grp % 2
                for i in range(8):
                    c = grp * 8 + i
                    kb.mm(po[b][:, i * 64:(i + 1) * 64], vtk[:, c, :], attm[:, c * 64:(c + 1) * 64], True, False, [Bvtk, Batt], [Bpo[b]])
                    kb.mm(po[b][:, i * 64:(i + 1) * 64], Stc[:, c, :], C[:, c * 64:(c + 1) * 64], False, True, [BU, BC], [Bpo[b]])
                kb.cp("act", o_h[:, grp * 512:(grp + 1) * 512], po[b][:], [Bpo[b]], [Boh])
            for cc in range(4):
                cs = slice(cc * 512, (cc + 1) * 512)
                kb.act(sq[:], o_h[:, cs], AF.Square, [Boh], [Bsq])
                kb.mm(psq[:], ones[:], sq[:], True, True, [Bon, Bsq], [Bpsq])
                kb.ts("dve", rs[:], psq[:], 1.0 / 128, ALU.mult, [Bpsq], [Brs], s2=EPS, op1=ALU.add)
                kb.act(rs[:], rs[:], AF.Sqrt, [Brs], [Brs])
                kb.recip(rs[:], rs[:], [Brs], [Brs])
                kb.stt(tmp[:], o_h[:, cs], ng[:, 0:1], rs[:], ALU.mult, ALU.mult, [Boh, Bc, Brs], [Btmp])
                kb.tt("dve", oTh[:, cs], tmp[:], gs[:, cs], ALU.mult, [Btmp, Bgs], [BoT])
            kb.ld(act_d[h], oTh[:], [BoT], (), owner=BoT)
        P.end_phase()

LAYER_NIN = {0: 3072, 1: 6144, 2: 8192, 3: 4176}
MIX_NAMES = {0: "swa", 1: "diff", 2: "hgrn", 3: "dsa"}


def build_program(layers, stop_after=None):
    nc = bass.Bass("TRN2", target_bir_lowering=False)
    dr = {}

    def ext(name, shape, dt=F32):
        dr[name] = nc.dram_tensor(name, list(shape), dt, kind="ExternalInput").ap()
        return dr[name]

    x_in = ext("x", [S, D])
    y_out = nc.dram_tensor("y", [S, D], F32, kind="ExternalOutput").ap()
    normg = ext("norm_g", [16, D])
    ident_d = ext("ident", [128, 128])
    i8_d = ext("i8", [128, 128])
    for l in layers:
        ext("w_in%d" % l, [D, LAYER_NIN[l]])
        ext("w_out%d" % l, [D, D])
        ext("w_up%d" % l, [D, 2 * DFF])
        ext("w_down%d" % l, [DFF, D])
        ext("conv%d" % l, [128, 88, 3])
    if 0 in layers:
        ext("bias_sw", [128, 32 * 256])
        ext("sinkrep", [1, 4096])
    if 1 in layers or 3 in layers:
        ext("biasn", [128, 32 * 256])
        ext("table", [32, 32])
    if 1 in layers:
        ext("diff_lambda", [4, 64])
        ext("diff_subg", [1, 128])
    xres = nc.dram_tensor("xres", [S, D], F32).ap()
    m_d = nc.dram_tensor("m_d", [S, D], F32).ap()
    act_d = nc.dram_tensor("act_d", [KFF, 128, S], BF16).ap()
    qkv_d = nc.dram_tensor("qkv_d", [64, 128, S], BF16).ap()
    vtok_d = nc.dram_tensor("vtok_d", [S, 2048], BF16).ap()
    if 2 in layers:
        ext("lbl", [128, 64])
        ext("hgrn_ng", [1, 128])
        ext("tri", [64, 512])
        hq_d = nc.dram_tensor("hq_d", [16, 128, S], F32).ap()
        hf_d = nc.dram_tensor("hf_d", [16, 128, S], F32).ap()
        hk_d = nc.dram_tensor("hk_d", [16, 128, S], F32).ap()
        hv_d = nc.dram_tensor("hv_d", [S, 2048], F32).ap()
    if 3 in layers:
        ext("cmask", [128, 128])
        wi_d = nc.dram_tensor("wi_d", [S, 16], F32).ap()
        mask_d = nc.dram_tensor("mask_d", [16, 128, S], BF16).ap()

    with ExitStack() as top:
        P = Prog(nc, top)
        kb = KB(nc, P)
        x_cur = x_in
        pend_m = None
        for li, l in enumerate(layers):
            with ExitStack() as hs:
                hT = kb.sb(hs, [128, 16, S], BF16, "hT")
                BhT = Buf("hT")
                phase_rn(kb, x_cur, xres, m_d if pend_m is not None else None,
                         normg[pend_m] if pend_m is not None else None, normg[4 * l + 0], hT, BhT, ident_d)
                if pend_m is not None:
                    x_cur = xres
                w_in = dr["w_in%d" % l]
                if l == 0:
                    tiles = [(i * 128, 128) for i in range(20)]
                    phase_proj_fm(kb, hT, BhT, w_in, tiles, epi_store_fm(kb, qkv_d, lambda ti: ti), epi_store_setup(kb))
                    phase_proj_tok(kb, hT, BhT, w_in, [(2560, 512)], epi_store_tok(kb, vtok_d, lambda gi: 0), epi_tok_setup(kb))
                elif l == 1:
                    tiles = [(i * 128, 128) for i in range(32)]
                    phase_proj_fm(kb, hT, BhT, w_in, tiles, epi_store_fm(kb, qkv_d, lambda ti: ti), epi_store_setup(kb))
                    phase_proj_tok(kb, hT, BhT, w_in, [(4096 + i * 512, 512) for i in range(4)],
                                   epi_store_tok(kb, vtok_d, lambda gi: gi * 512), epi_tok_setup(kb))
                elif l == 2:
                    hgrn_inproj(kb, hT, BhT, w_in, dr["lbl"], l, hq_d, hf_d, hk_d, hv_d, qkv_d)
                elif l == 3:
                    tiles = [(i * 128, 128) for i in range(20)] + [(3072 + i * 128, 128) for i in range(8)] + [(4096, 64)]
                    phase_proj_fm(kb, hT, BhT, w_in, tiles, epi_store_fm(kb, qkv_d, lambda ti: ti), epi_store_setup(kb))

                    def setup3(es):
                        c_ = epi_tok_setup(kb)(es)
                        c_["tf"] = [kb.sb(es, [128, 16], F32, "etf") for _ in range(2)]
                        c_["Bf"] = kb.P.bufs(2, "etf")
                        return c_

                    def epi3(ctx, gi, t, c0, w, pb, Bpb):
                        if gi == 0:
                            epi_store_tok(kb, vtok_d, lambda gi_: 0)(ctx, gi, t, c0, w, pb, Bpb)
                        else:
                            tl, B = ctx["tf"][t % 2], ctx["Bf"][t % 2]
                            kb.cp("dve", tl[:, 0:16], pb[:, 0:16], [Bpb], [B])
                            kb.ld(wi_d[t * 128:(t + 1) * 128, :], tl[:, 0:16], [B], (), owner=B)
                    phase_proj_tok(kb, hT, BhT, w_in, [(2560, 512), (4160, 16)], epi3, setup3)
            if l == 0:
                phase_swa(kb, qkv_d, vtok_d, act_d, dr["bias_sw"], i8_d, dr["sinkrep"])
            elif l == 2:
                phase_hgrn(kb, hq_d, hf_d, hk_d, hv_d, qkv_d, act_d, dr["hgrn_ng"], ident_d, dr["tri"])
            elif l == 3:
                phase_dsa_idx(kb, qkv_d, wi_d, mask_d, dr["cmask"], ident_d)
                phase_dsa_attn(kb, qkv_d, vtok_d, act_d, mask_d, dr["biasn"], i8_d, dr["table"])
            elif l == 1:
                phase_diff(kb, qkv_d, vtok_d, act_d, dr["biasn"], i8_d, dr["table"], dr["diff_lambda"], dr["diff_subg"], l)
            phase_down(kb, dr["w_out%d" % l], 16, act_d, m_d)
            with ExitStack() as hs:
                hT = kb.sb(hs, [128, 16, S], BF16, "hT")
                BhT = Buf("hT")
                phase_rn(kb, x_cur, xres, m_d, normg[4 * l + 1], normg[4 * l + 2], hT, BhT, ident_d)
                x_cur = xres
                phase_up(kb, hT, BhT, dr["w_up%d" % l], dr["conv%d" % l], act_d)
            phase_down(kb, dr["w_down%d" % l], KFF, act_d, m_d)
            pend_m = 4 * l + 3
        phase_rn(kb, x_cur, y_out, m_d, normg[pend_m], None, None, None, ident_d)
    return nc


def _bucket(n):
    n = np.maximum(n, 0)
    max_exact = 16
    lr = np.log(np.maximum(n, 1).astype(np.float32) / max_exact) / math.log(128 / max_exact)
    large = np.minimum(max_exact + (lr * (32 - max_exact)).astype(np.int32), 31)
    return np.where(n < max_exact, n, large)


def _host_consts(inputs, layers):
    c = {}
    c["ident"] = np.eye(128, dtype=np.float32)
    c["i8"] = (8.0 * np.eye(128)).astype(np.float32)
    table = np.asarray(inputs["rel_bias_table"], np.float32)
    if 0 in layers:
        s_ = np.arange(128)[:, None]
        q_ = np.arange(256)[None, :]
        dist = q_ - s_
        valid = (dist >= 0) & (dist < 128)
        bk = _bucket(dist)
        t = table[bk]
        t = np.where(valid[:, :, None], t, np.float32(MASKV))
        c["bias_sw"] = np.ascontiguousarray(t.transpose(0, 2, 1)).reshape(128, 32 * 256).astype(np.float32)
        c["sinkrep"] = np.ascontiguousarray(np.repeat(np.asarray(inputs["swa_sinks"], np.float32)[0], 128)[None, :])
    if 1 in layers or 3 in layers:
        s_ = np.arange(128)[:, None]
        q_ = np.arange(256)[None, :]
        dist = q_ - s_
        t = table[_bucket(dist)]
        t = np.where((dist >= 0)[:, :, None], t, np.float32(MASKV))
        c["biasn"] = np.ascontiguousarray(t.transpose(0, 2, 1)).reshape(128, 32 * 256).astype(np.float32)
        c["table"] = np.ascontiguousarray(table)
    if 2 in layers:
        lg = np.asarray(inputs["hgrn_lb_logits"], np.float32)
        c["lbl"] = np.ascontiguousarray(lg.reshape(4, 16, 128).transpose(2, 1, 0)).reshape(128, 64)
        c["hgrn_ng"] = np.ascontiguousarray(np.asarray(inputs["hgrn_norm_g"], np.float32)[0][None, :])
        tr_ = (np.arange(64)[:, None] <= np.arange(64)[None, :]).astype(np.float32)
        c["tri"] = np.ascontiguousarray(np.tile(tr_, (1, 8)))
    if 3 in layers:
        c["cmask"] = np.where(np.arange(128)[None, :] > np.arange(128)[:, None], np.float32(-1.0e30), np.float32(0.0)).astype(np.float32)
    if 1 in layers:
        c["diff_lambda"] = np.ascontiguousarray(np.asarray(inputs["diff_lambda"], np.float32)[0])
        c["diff_subg"] = np.ascontiguousarray(np.asarray(inputs["diff_subln_g"], np.float32)[0][None, :])
    return c


_W_KEYS = {0: ("swa_w_in", "swa_w_out"), 1: ("diff_w_in", "diff_w_out"), 2: ("hgrn_w_in", "hgrn_w_out"), 3: ("dsa_w_in", "dsa_w_out")}
LAUNCHES = [[0, 1, 2, 3]]
N_CORES = 4


def _layer_inputs(inputs, layers):
    m = {}
    m["norm_g"] = np.ascontiguousarray(np.asarray(inputs["norm_g"], np.float32).reshape(16, D))
    for l in layers:
        kin, kout = _W_KEYS[l]
        m["w_in%d" % l] = np.ascontiguousarray(np.asarray(inputs[kin], np.float32)[0])
        m["w_out%d" % l] = np.ascontiguousarray(np.asarray(inputs[kout], np.float32)[0])
        m["w_up%d" % l] = np.ascontiguousarray(np.asarray(inputs["ffn_w_up"], np.float32)[l])
        m["w_down%d" % l] = np.ascontiguousarray(np.asarray(inputs["ffn_w_down"], np.float32)[l])
        cv = np.asarray(inputs["ffn_conv"], np.float32)[l]
        m["conv%d" % l] = np.ascontiguousarray(cv.reshape(3, 88, 128).transpose(2, 1, 0))
    m.update(_host_consts(inputs, layers))
    return m


def run_launch(inputs, x, layers, cores=None):
    nc = build_program(layers)
    shared = _layer_inputs(inputs, layers)
    B = x.shape[0]
    cores = cores if cores is not None else N_CORES
    in_maps = []
    for c in range(cores):
        d = dict(shared)
        d["x"] = np.ascontiguousarray(x[c % B])
        in_maps.append(d)
    res = run_bass_kernel_spmd(nc, in_maps, core_ids=list(range(cores)))
    return np.stack([res.results[b]["y"] for b in range(B)], axis=0)


def kernel(**inputs):
    x = np.asarray(inputs["x"], np.float32)
    for layers in LAUNCHES:
        x = run_launch(inputs, x, layers)
    return x.astype(np.float32)
```

```python
import math
from contextlib import ExitStack

import numpy as np
import concourse.bass as bass
import concourse.mybir as mybir
from concourse.bass_utils import run_bass_kernel_spmd

F32 = mybir.dt.float32
BF16 = mybir.dt.bfloat16
AF = mybir.ActivationFunctionType
ALU = mybir.AluOpType
AX = mybir.AxisListType

ENGS = ("pe", "act", "dve", "pool", "sp")
CENGS = ("pe", "act", "dve", "pool")

S = 2048
D = 2048
NT = 16
DFF = 5632
KFF = 44
EPS = 1e-6
MASKV = -30000.0
N_DSEM = 56


class Buf:
    __slots__ = ("name", "w", "r", "dsem")

    def __init__(self, name):
        self.name = name
        self.w = None
        self.r = {}
        self.dsem = None


class Prog:
    def __init__(self, nc, es):
        self.nc = nc
        self.ops = {e: [] for e in ENGS}
        self.sems = {}
        self.tot = {}
        self.seen = {e: {} for e in ENGS}
        for e in CENGS:
            self.sems[e] = es.enter_context(nc.semaphore("c_" + e))
            self.tot[e] = 0
        self.dfree = []
        for i in range(N_DSEM):
            k = "d%d" % i
            self.sems[k] = es.enter_context(nc.semaphore(k))
            self.tot[k] = 0
            self.dfree.append(k)
        self.dused = []
        self.phase_bufs = []
        self.nphase = 0

    def buf(self, name="b"):
        b = Buf(name)
        self.phase_bufs.append(b)
        return b

    def bufs(self, n, name="b"):
        return [self.buf(name + str(i)) for i in range(n)]

    def _dsem(self, b):
        if b.dsem is None:
            b.dsem = self.dfree.pop()
            self.dused.append(b.dsem)
        return b.dsem

    def _deps(self, eng, reads, writes):
        deps = {}

        def add(ev):
            if ev is None:
                return
            k, v = ev
            if v > deps.get(k, 0):
                deps[k] = v
        for b in reads:
            add(b.w)
        for b in writes:
            add(b.w)
            for k, v in b.r.items():
                add((k, v))
        waits = []
        for k, v in deps.items():
            if k == eng and eng == "pe":
                continue
            if k not in CENGS:
                v = self.tot[k]
            if v > self.seen[eng].get(k, 0):
                self.seen[eng][k] = v
                waits.append((k, v))
        return waits

    def op(self, eng, fn, reads=(), writes=()):
        waits = self._deps(eng, reads, writes)
        self.tot[eng] += 1
        seq = self.tot[eng]
        self.ops[eng].append((waits, fn, eng, 1))
        for b in reads:
            if b.r.get(eng, 0) < seq:
                b.r[eng] = seq
        for b in writes:
            b.w = (eng, seq)
            b.r = {}

    def dma(self, q, fn, reads=(), writes=(), owner=None):
        waits = self._deps(q, reads, writes)
        if owner is None:
            owner = writes[0] if writes else reads[0]
        k = self._dsem(owner)
        self.tot[k] += 16
        val = self.tot[k]
        self.ops[q].append((waits, fn, k, 16))
        for b in reads:
            b.r[k] = val
        for b in writes:
            b.w = (k, val)
            b.r = {}

    def end_phase(self):
        nc = self.nc
        keys = list(CENGS) + list(self.dused)
        for e in ENGS:
            waits = []
            for k in keys:
                if k == e:
                    continue
                v = self.tot[k]
                if v > self.seen[e].get(k, 0):
                    self.seen[e][k] = v
                    waits.append((k, v))
            self.ops[e].append((waits, None, None, 0))
        with nc.Block() as block:
            def mk(ename, attr):
                lst = self.ops[ename]

                def body(e):
                    for waits, fn, k, inc in lst:
                        for wk, wv in waits:
                            e.wait_ge(self.sems[wk], wv)
                        if fn is not None:
                            fn().then_inc(self.sems[k], inc)
                getattr(block, attr)(body)
            mk("sp", "sync")
            mk("pe", "tensor")
            mk("act", "scalar")
            mk("dve", "vector")
            mk("pool", "gpsimd")
        self.ops = {e: [] for e in ENGS}
        for b in self.phase_bufs:
            b.dsem = None
            b.w = None
            b.r = {}
        self.phase_bufs = []
        self.dfree.extend(self.dused)
        self.dused = []
        self.nphase += 1


class KB:
    def __init__(self, nc, P):
        self.nc = nc
        self.P = P
        self.uid = 0

    def sb(self, es, shape, dt, name="t"):
        self.uid += 1
        return es.enter_context(self.nc.sbuf_tensor("%s_%d" % (name, self.uid), list(shape), dt))

    def ps(self, es, shape, dt, name="p"):
        self.uid += 1
        return es.enter_context(self.nc.psum_tensor("%s_%d" % (name, self.uid), list(shape), dt))

    def mm(self, out, lhsT, rhs, start, stop, r, w):
        nc = self.nc
        self.P.op("pe", lambda: nc.tensor.matmul(out, lhsT=lhsT, rhs=rhs, start=start, stop=stop), r, w)

    def tr(self, out, in_, ident, r, w):
        nc = self.nc
        self.P.op("pe", lambda: nc.tensor.transpose(out, in_, ident), r, w)

    def act(self, out, in_, func, r, w, bias=None, scale=None, accum=None):
        nc = self.nc
        kw = {}
        if bias is not None:
            kw["bias"] = bias
        if scale is not None:
            kw["scale"] = scale
        if accum is not None:
            kw["accum_out"] = accum
        self.P.op("act", lambda: nc.scalar.activation(out=out, in_=in_, func=func, **kw), r, w)

    def ts(self, eng, out, in0, s1, op0, r, w, s2=None, op1=None):
        e = self.nc.vector if eng == "dve" else self.nc.gpsimd
        if op1 is None:
            self.P.op(eng, lambda: e.tensor_scalar(out=out, in0=in0, scalar1=s1, scalar2=None, op0=op0), r, w)
        else:
            self.P.op(eng, lambda: e.tensor_scalar(out=out, in0=in0, scalar1=s1, scalar2=s2, op0=op0, op1=op1), r, w)

    def tt(self, eng, out, in0, in1, op, r, w):
        e = self.nc.vector if eng == "dve" else self.nc.gpsimd
        self.P.op(eng, lambda: e.tensor_tensor(out=out, in0=in0, in1=in1, op=op), r, w)

    def stt(self, out, in0, scalar, in1, op0, op1, r, w):
        nc = self.nc
        self.P.op("dve", lambda: nc.vector.scalar_tensor_tensor(out=out, in0=in0, scalar=scalar, in1=in1, op0=op0, op1=op1), r, w)

    def cp(self, eng, out, in_, r, w):
        nc = self.nc
        if eng == "act":
            self.P.op("act", lambda: nc.scalar.copy(out=out, in_=in_), r, w)
        elif eng == "dve":
            self.P.op("dve", lambda: nc.vector.tensor_copy(out=out, in_=in_), r, w)
        else:
            self.P.op("pool", lambda: nc.gpsimd.tensor_copy(out=out, in_=in_), r, w)

    def recip(self, out, in_, r, w):
        nc = self.nc
        self.P.op("dve", lambda: nc.vector.reciprocal(out=out, in_=in_), r, w)

    def memset(self, eng, ap, val, w):
        e = self.nc.vector if eng == "dve" else self.nc.gpsimd
        self.P.op(eng, lambda: e.memset(ap, val), (), w)

    def ld(self, out, in_, r, w, q="sp", owner=None):
        nc = self.nc
        if q == "sp":
            self.P.dma("sp", lambda: nc.sync.dma_start(out=out, in_=in_), r, w, owner)
        else:
            self.P.dma("pool", lambda: nc.gpsimd.dma_start(out=out, in_=in_), r, w, owner)

    def rstd(self, es_tiles, ss, r, w):
        self.ts("dve", ss, ss, 1.0 / D, ALU.mult, r, w, s2=EPS, op1=ALU.add)
        self.act(ss, ss, AF.Sqrt, r, w)
        self.recip(ss, ss, r, w)


def phase_rn(kb, x_src, x_dst, m_src, gpost, gpre, hT, BhT, ident_d):
    nc, P = kb.nc, kb.P
    NS = 3
    with ExitStack() as es:
        xt = [kb.sb(es, [128, D], F32, "xt") for _ in range(NS)]
        Bxt = P.bufs(NS, "xt")
        junk = [kb.sb(es, [128, D], BF16, "junk") for _ in range(2)]
        Bjunk = P.bufs(2, "junk")
        st = [kb.sb(es, [128, 2], F32, "st") for _ in range(NS)]
        Bst0 = P.bufs(NS, "st0")
        Bst1 = P.bufs(NS, "st1")
        if m_src is not None:
            mt = [kb.sb(es, [128, D], F32, "mt") for _ in range(NS)]
            Bmt = P.bufs(NS, "mt")
            gp = kb.sb(es, [128, D], F32, "gp")
            Bgp = P.buf("gp")
            kb.ld(gp[:], gpost.partition_broadcast(128), (), [Bgp])
        if gpre is not None:
            gq = kb.sb(es, [128, D], F32, "gq")
            Bgq = P.buf("gq")
            kb.ld(gq[:], gpre.partition_broadcast(128), (), [Bgq])
            hb = [kb.sb(es, [128, D], BF16, "hb") for _ in range(2)]
            Bhb = P.bufs(2, "hb")
            ident = kb.sb(es, [128, 128], BF16, "ident")
            Bid = P.buf("ident")
            kb.ld(ident[:], ident_d, (), [Bid], q="pool")
            pT = [kb.ps(es, [128, 1024], BF16, "pT") for _ in range(4)]
            BpT = P.bufs(4, "pT")

        def load_t(t_):
            if t_ >= NT:
                return
            rows_ = slice(t_ * 128, (t_ + 1) * 128)
            kb.ld(xt[t_ % NS][:], x_src[rows_, :], (), [Bxt[t_ % NS]])
            if m_src is not None:
                kb.ld(mt[t_ % NS][:], m_src[rows_, :], (), [Bmt[t_ % NS]])

        def stage1(t):
            if t >= NT or m_src is None:
                return
            s = t % NS
            rows = slice(t * 128, (t + 1) * 128)
            kb.act(junk[0][:], mt[s][:], AF.Square, [Bmt[s]], [Bjunk[0], Bst0[s]], accum=st[s][:, 0:1])
            kb.rstd(None, st[s][:, 0:1], [Bst0[s]], [Bst0[s]])
            kb.stt(mt[s][:], mt[s][:], st[s][:, 0:1], gp[:], ALU.mult, ALU.mult, [Bmt[s], Bst0[s], Bgp], [Bmt[s]])
            kb.tt("dve", xt[s][:], xt[s][:], mt[s][:], ALU.add, [Bxt[s], Bmt[s]], [Bxt[s]])
            kb.ld(x_dst[rows, :], xt[s][:], [Bxt[s]], (), owner=Bxt[s])

        def stage2(t):
            if gpre is None:
                return
            s = t % NS
            hs_ = t % 2
            kb.act(junk[1][:], xt[s][:], AF.Square, [Bxt[s]], [Bjunk[1], Bst1[s]], accum=st[s][:, 1:2])
            kb.rstd(None, st[s][:, 1:2], [Bst1[s]], [Bst1[s]])
            kb.stt(hb[hs_][:], xt[s][:], st[s][:, 1:2], gq[:], ALU.mult, ALU.mult, [Bxt[s], Bst1[s], Bgq], [Bhb[hs_]])
            for half in range(2):
                pi = (2 * t + half) % 4
                for j in range(8):
                    i = half * 8 + j
                    kb.tr(pT[pi][:, j * 128:(j + 1) * 128], hb[hs_][:, i * 128:(i + 1) * 128], ident[:], [Bhb[hs_], Bid], [BpT[pi]])
                src = pT[pi][:].rearrange("p (j q) -> p j q", j=8)
                dst = hT[:, half * 8:(half + 1) * 8, t * 128:(t + 1) * 128]
                kb.cp("act" if half == 0 else "dve", dst, src, [BpT[pi]], [P.buf("hTw")])
        load_t(0)
        load_t(1)
        stage1(0)
        for t in range(NT):
            load_t(t + 2)
            stage1(t + 1)
            stage2(t)
        P.end_phase()


def phase_proj_fm(kb, hT, BhT, w_d, coltiles, epilogue, epi_setup=None):
    nc, P = kb.nc, kb.P
    wv = w_d.rearrange("(kc p) n -> p kc n", p=128)
    with ExitStack() as es:
        NSL = 3
        wsl = [kb.sb(es, [128, 16, 256], BF16, "wsl") for _ in range(NSL)]
        Bw = P.bufs(NSL, "wsl")
        pb = [kb.ps(es, [128, 2048], F32, "pb") for _ in range(2)]
        Bpb = P.bufs(2, "pb")
        ctx = epi_setup(es) if epi_setup is not None else None
        slabs = []
        i = 0
        while i < len(coltiles):
            c0, w0 = coltiles[i]
            if i + 1 < len(coltiles) and coltiles[i + 1][0] == c0 + w0 and w0 == 128:
                slabs.append([coltiles[i], coltiles[i + 1]])
                i += 2
            else:
                slabs.append([coltiles[i]])
                i += 1
        ti = 0

        def load_slab(si):
            if si >= len(slabs):
                return
            s_ = si % NSL
            c0_ = slabs[si][0][0]
            wtot = sum(w for _, w in slabs[si])
            kb.ld(wsl[s_][:, :, 0:wtot], wv[:, :, c0_:c0_ + wtot], (), [Bw[s_]], q="pool")
        load_slab(0)
        load_slab(1)
        for si, sl in enumerate(slabs):
            s = si % NSL
            load_slab(si + 2)
            off = 0
            for (cc, w) in sl:
                pi = ti % 2
                for g in range(4):
                    for k in range(16):
                        kb.mm(pb[pi][0:w, g * 512:(g + 1) * 512], wsl[s][:, k, off:off + w], hT[:, k, g * 512:(g + 1) * 512],
                              k == 0, k == 15, [Bw[s], BhT], [Bpb[pi]])
                epilogue(ctx, ti, cc, w, pb[pi], Bpb[pi])
                off += w
                ti += 1
        P.end_phase()


def phase_proj_tok(kb, hT, BhT, w_d, colgroups, epilogue, epi_setup=None):
    nc, P = kb.nc, kb.P
    wv = w_d.rearrange("(kc p) n -> p kc n", p=128)
    with ExitStack() as es:
        wg = [kb.sb(es, [128, 16, 512], BF16, "wg") for _ in range(2)]
        Bwg = P.bufs(2, "wg")
        pb = [kb.ps(es, [128, 512], F32, "pk") for _ in range(4)]
        Bpb = P.bufs(4, "pk")
        ctx = epi_setup(es) if epi_setup is not None else None
        n = 0

        def load_g(gi):
            if gi >= len(colgroups):
                return
            c0_, w_ = colgroups[gi]
            kb.ld(wg[gi % 2][:, :, 0:w_], wv[:, :, c0_:c0_ + w_], (), [Bwg[gi % 2]], q="pool")
        load_g(0)
        for gi, (c0, w) in enumerate(colgroups):
            s = gi % 2
            load_g(gi + 1)
            for t in range(NT):
                pi = n % 4
                n += 1
                for k in range(16):
                    kb.mm(pb[pi][:, 0:w], hT[:, k, t * 128:(t + 1) * 128], wg[s][:, k, 0:w], k == 0, k == 15, [BhT, Bwg[s]], [Bpb[pi]])
                epilogue(ctx, gi, t, c0, w, pb[pi], Bpb[pi])
        P.end_phase()


def phase_up(kb, hT, BhT, w_up, conv_d, act_d):
    nc, P = kb.nc, kb.P
    wv = w_up.rearrange("(kc p) n -> p kc n", p=128)
    with ExitStack() as es:
        NSL = 3
        wsl = [kb.sb(es, [128, 16, 512], BF16, "wup") for _ in range(NSL)]
        Bw = P.bufs(NSL, "wup")
        cw = kb.sb(es, [128, 88, 3], F32, "cw")
        Bcw = P.buf("cw")
        kb.ld(cw[:], conv_d, (), [Bcw])
        psg = kb.ps(es, [128, 2048], F32, "psg")
        psv = kb.ps(es, [128, 2048], F32, "psv")
        Bpsg, Bpsv = P.buf("psg"), P.buf("psv")
        ub = [kb.sb(es, [128, 2050], F32, "ub") for _ in range(4)]
        Bub = P.bufs(4, "ub")
        cb = [kb.sb(es, [128, 2048], F32, "cb") for _ in range(4)]
        Bcb = P.bufs(4, "cb")
        ob = [kb.sb(es, [128, 2048], BF16, "ob") for _ in range(2)]
        Bob = P.bufs(2, "ob")
        Bact = P.buf("actd")
        for i in range(4):
            kb.memset("pool", ub[i][:, 0:2], 0.0, [Bub[i]])
        def load_up(sl_):
            if sl_ >= KFF // 2:
                return
            s_ = sl_ % NSL
            c_ = sl_ * 2
            kb.ld(wsl[s_][:, :, 0:256], wv[:, :, c_ * 128:c_ * 128 + 256], (), [Bw[s_]], q="pool")
            kb.ld(wsl[s_][:, :, 256:512], wv[:, :, DFF + c_ * 128:DFF + c_ * 128 + 256], (), [Bw[s_]], q="pool")
        load_up(0)
        load_up(1)
        for c in range(KFF):
            sl = c // 2
            s = sl % NSL
            if c % 2 == 0:
                load_up(sl + 2)
            par = c % 2
            for part, (pp, Bpp) in enumerate(((psg, Bpsg), (psv, Bpsv))):
                off = part * 256 + par * 128
                for g in range(4):
                    for k in range(16):
                        kb.mm(pp[:, g * 512:(g + 1) * 512], wsl[s][:, k, off:off + 128], hT[:, k, g * 512:(g + 1) * 512],
                              k == 0, k == 15, [Bw[s], BhT], [Bpp])
                u = par * 2 + part
                fi = part * KFF + c
                kb.cp("act", ub[u][:, 2:2050], pp[:], [Bpp], [Bub[u]])
                kb.act(cb[u][:], ub[u][:, 0:2048], AF.Copy, [Bub[u], Bcw], [Bcb[u]], scale=cw[:, fi, 0:1])
                kb.stt(cb[u][:], ub[u][:, 1:2049], cw[:, fi, 1:2], cb[u][:], ALU.mult, ALU.add, [Bub[u], Bcw, Bcb[u]], [Bcb[u]])
                kb.stt(cb[u][:], ub[u][:, 2:2050], cw[:, fi, 2:3], cb[u][:], ALU.mult, ALU.add, [Bub[u], Bcw, Bcb[u]], [Bcb[u]])
                if part == 0:
                    kb.act(cb[u][:], cb[u][:], AF.Silu, [Bcb[u]], [Bcb[u]])
            ug, uv = par * 2, par * 2 + 1
            kb.tt("dve", ob[par][:], cb[ug][:], cb[uv][:], ALU.mult, [Bcb[ug], Bcb[uv]], [Bob[par]])
            kb.ld(act_d[c], ob[par][:], [Bob[par]], (), owner=Bob[par])
        P.end_phase()


def phase_down(kb, w_d, KC, act_d, m_d):
    nc, P = kb.nc, kb.P
    wv = w_d.rearrange("(kc p) n -> p kc n", p=128)
    av = act_d.rearrange("c p s -> p c s")
    TG = 256
    with ExitStack() as es:
        wres = [kb.sb(es, [128, KC, 512], BF16, "wres") for _ in range(2)]
        Bwr = [P.bufs(4, "wr%d_" % i) for i in range(2)]
        slab = [kb.sb(es, [128, KC, TG], BF16, "slab") for _ in range(2)]
        Bsl = P.bufs(2, "slab")
        pb = [kb.ps(es, [128, 512], F32, "pd") for _ in range(4)]
        Bpb = P.bufs(4, "pd")
        ot = [kb.sb(es, [128, 512], F32, "ot") for _ in range(3)]
        Bot = P.bufs(3, "ot")
        Bm = P.buf("md")
        kq = [(i * KC) // 4 for i in range(5)]
        n = 0
        ns = 0
        NTG = S // TG

        def load_w(cg_):
            if cg_ >= 4:
                return
            for pq_ in range(4):
                kb.ld(wres[cg_ % 2][:, kq[pq_]:kq[pq_ + 1], :], wv[:, kq[pq_]:kq[pq_ + 1], cg_ * 512:(cg_ + 1) * 512], (),
                      [Bwr[cg_ % 2][pq_]], q="pool")

        def load_slab(i_):
            if i_ >= 4 * NTG:
                return
            tg_ = i_ % NTG
            kb.ld(slab[i_ % 2][:], av[:, 0:KC, tg_ * TG:(tg_ + 1) * TG], (), [Bsl[i_ % 2]])
        load_slab(0)
        load_w(0)
        for cg in range(4):
            ws = cg % 2
            load_w(cg + 1)
            for tg in range(NTG):
                ss = ns % 2
                ns += 1
                load_slab(ns)
                for tt_ in range(TG // 128):
                    pi = n % 4
                    oi = n % 3
                    n += 1
                    for c in range(KC):
                        pq = 0
                        while c >= kq[pq + 1]:
                            pq += 1
                        kb.mm(pb[pi][:], slab[ss][:, c, tt_ * 128:(tt_ + 1) * 128], wres[ws][:, c, :], c == 0, c == KC - 1,
                              [Bsl[ss], Bwr[ws][pq]], [Bpb[pi]])
                    kb.cp("act" if n % 2 == 0 else "dve", ot[oi][:], pb[pi][:], [Bpb[pi]], [Bot[oi]])
                    r0 = tg * TG + tt_ * 128
                    kb.ld(m_d[r0:r0 + 128, cg * 512:(cg + 1) * 512], ot[oi][:], [Bot[oi]], (), owner=Bot[oi])
        P.end_phase()


def epi_store_setup(kb, n=2):
    def setup(es):
        P = kb.P
        return {"t": [kb.sb(es, [128, 2048], BF16, "eo") for _ in range(n)], "B": P.bufs(n, "eo"), "Bd": P.buf("qkvd"), "n": 0}
    return setup


def epi_store_fm(kb, qkv_d, tile_of):
    def epi(ctx, ti, c0, w, pb, Bpb):
        i = ctx["n"] % len(ctx["t"])
        ctx["n"] += 1
        t, B = ctx["t"][i], ctx["B"][i]
        kb.cp("act" if ti % 2 == 0 else "dve", t[0:w, :], pb[0:w, :], [Bpb], [B])
        kb.ld(qkv_d[tile_of(ti), 0:w, :], t[0:w, :], [B], (), owner=B)
    return epi


def epi_tok_setup(kb, dt=BF16):
    def setup(es):
        P = kb.P
        return {"t": [kb.sb(es, [128, 512], dt, "et") for _ in range(3)], "B": P.bufs(3, "et"), "Bd": P.buf("vtokd"), "n": 0}
    return setup


def epi_store_tok(kb, vtok_d, col_of):
    def epi(ctx, gi, t, c0, w, pb, Bpb):
        i = ctx["n"] % 3
        ctx["n"] += 1
        tl, B = ctx["t"][i], ctx["B"][i]
        kb.cp("act" if ctx["n"] % 2 == 0 else "dve", tl[:, 0:w], pb[:, 0:w], [Bpb], [B])
        d0 = col_of(gi)
        kb.ld(vtok_d[t * 128:(t + 1) * 128, d0:d0 + w], tl[:, 0:w], [B], (), owner=B)
    return epi


def phase_swa(kb, qkv_d, vtok_d, act_d, bias_sw_d, i8_d, sink_d):
    nc, P = kb.nc, kb.P
    with ExitStack() as es:
        vt = kb.sb(es, [128, 16, 512], BF16, "vt")
        Bvt = P.buf("vt")
        kb.ld(vt[:], vtok_d[:, 0:512].rearrange("(t p) c -> p t c", p=128), (), [Bvt])
        bsw = kb.sb(es, [128, 32, 256], BF16, "bsw")
        Bbsw = P.buf("bsw")
        kb.ld(bsw[:], bias_sw_d.rearrange("p (h q) -> p h q", h=32), (), [Bbsw], q="pool")
        i8 = kb.sb(es, [128, 128], BF16, "i8")
        Bi8 = P.buf("i8")
        kb.ld(i8[:], i8_d, (), [Bi8], q="pool")
        ones64 = kb.sb(es, [128, 64], BF16, "ones64")
        Bon = P.buf("ones")
        kb.memset("dve", ones64[:], 1.0, [Bon])
        skf = kb.sb(es, [1, 4096], F32, "skf")
        skb = kb.sb(es, [1, 4096], BF16, "skb")
        Bsk = P.buf("sk")
        kb.ld(skf[:], sink_d, (), [Bsk])
        kb.act(skb[:], skf[:], AF.Exp, [Bsk], [Bsk])
        kh = [kb.sb(es, [64, 2048], BF16, "kh") for _ in range(2)]
        qh = [kb.sb(es, [64, 4, 2048], BF16, "qh") for _ in range(2)]
        Bkq = P.bufs(2, "kq")
        PT = [kb.sb(es, [128, 4, 256], BF16, "PT") for _ in range(3)]
        BPT = P.bufs(3, "PT")
        oT = [kb.sb(es, [64, 4, 2048], BF16, "oT") for _ in range(2)]
        BoT = P.bufs(2, "oT")
        rec = [kb.sb(es, [64, 512], F32, "rec") for _ in range(2)]
        Brec = P.bufs(2, "rec")
        pss = [kb.ps(es, [128, 1024], F32, "pss") for _ in range(2)]
        Bpss = P.bufs(2, "pss")
        pnum = [kb.ps(es, [64, 512], F32, "pnum") for _ in range(2)]
        Bpn = P.bufs(2, "pnum")
        pden = [kb.ps(es, [64, 512], F32, "pden") for _ in range(2)]
        Bpd = P.bufs(2, "pden")
        Bact = P.buf("actd")
        nj = 0
        nb = 0

        def load_kv(kv_):
            if kv_ >= 8:
                return
            s_ = kv_ % 2
            kb.ld(kh[s_][:], qkv_d[16 + kv_ // 2, (kv_ % 2) * 64:(kv_ % 2) * 64 + 64, :], (), [Bkq[s_]])
            for g_ in range(4):
                hq_ = 4 * kv_ + g_
                kb.ld(qh[s_][:, g_, :], qkv_d[hq_ // 2, (hq_ % 2) * 64:(hq_ % 2) * 64 + 64, :], (), [Bkq[s_]])
        load_kv(0)
        for kv in range(8):
            s = kv % 2
            load_kv(kv + 1)

            def stageA(j, kv=kv, s=s, base=nj):
                nq = 256 if j < 15 else 128
                sl = (base + j) % 2
                pt = (base + j) % 3
                for g in range(4):
                    o = pss[sl][:, g * 256:g * 256 + nq]
                    kb.mm(o, kh[s][:, j * 128:(j + 1) * 128], qh[s][:, g, j * 128:j * 128 + nq], True, False, [Bkq[s]], [Bpss[sl]])
                    kb.mm(o, i8[:], bsw[:, 4 * kv + g, 0:nq], False, True, [Bi8, Bbsw], [Bpss[sl]])
                src = pss[sl][:].rearrange("p (g q) -> p g q", g=4)[:, :, 0:nq]
                kb.act(PT[pt][:, :, 0:nq], src, AF.Exp, [Bpss[sl]], [BPT[pt]], scale=0.125)

            def stageB(j, kv=kv, s=s, base=nj, nb0=nb):
                pt = (base + j) % 3
                prev = (base + j - 1) % 3 if j > 0 else None
                b = (nb0 + j) % 2
                rds = [Bvt, BPT[pt]] + ([BPT[prev]] if prev is not None else [])
                kb.mm(pden[b][:], ones64[0:1, 0:64], skb[0:1, 4 * kv * 128:(4 * kv + 4) * 128], True, False, [Bon, Bsk], [Bpd[b]])
                if prev is not None:
                    kb.mm(pnum[b][:], vt[:, j - 1, kv * 64:(kv + 1) * 64], PT[prev][:, :, 128:256], True, False, rds, [Bpn[b]])
                    kb.mm(pden[b][:], ones64[:], PT[prev][:, :, 128:256], False, False, rds + [Bon], [Bpd[b]])
                kb.mm(pnum[b][:], vt[:, j, kv * 64:(kv + 1) * 64], PT[pt][:, :, 0:128], prev is None, True, rds, [Bpn[b]])
                kb.mm(pden[b][:], ones64[:], PT[pt][:, :, 0:128], False, True, rds + [Bon], [Bpd[b]])
                kb.recip(rec[b][:], pden[b][:], [Bpd[b]], [Brec[b]])
                kb.tt("dve", oT[s][:, :, j * 128:(j + 1) * 128], pnum[b][:].rearrange("p (g q) -> p g q", g=4),
                      rec[b][:].rearrange("p (g q) -> p g q", g=4), ALU.mult, [Bpn[b], Brec[b]], [BoT[s]])
            stageA(0)
            for j in range(16):
                if j + 1 < 16:
                    stageA(j + 1)
                stageB(j)
            nj += 16
            nb += 16
            for g in range(4):
                hq = 4 * kv + g
                kb.ld(act_d[hq // 2, (hq % 2) * 64:(hq % 2) * 64 + 64, :], oT[s][:, g, :], [BoT[s]], (), owner=BoT[s])
        P.end_phase()


def phase_diff(kb, qkv_d, vtok_d, act_d, biasn_d, i8_d, table_d, lam_d, subg_d, layer_idx):
    nc, P = kb.nc, kb.P
    lam_init = 0.8 - 0.6 * math.exp(-0.3 * layer_idx)
    with ExitStack() as es:
        bn = kb.sb(es, [128, 32, 256], BF16, "bn")
        Bbn = P.buf("bn")
        kb.ld(bn[:], biasn_d.rearrange("p (h q) -> p h q", h=32), (), [Bbn], q="pool")
        i8 = kb.sb(es, [128, 128], BF16, "i8")
        Bi8 = P.buf("i8")
        kb.ld(i8[:], i8_d, (), [Bi8], q="pool")
        ones = kb.sb(es, [128, 128], BF16, "ones")
        onesf = kb.sb(es, [1, 128], F32, "onesf")
        Bon = P.buf("ones")
        kb.memset("dve", ones[:], 1.0, [Bon])
        kb.memset("dve", onesf[:], 1.0, [Bon])
        c31 = kb.sb(es, [128, 32], F32, "c31")
        Bc31 = P.buf("c31")
        kb.ld(c31[:], table_d[31].partition_broadcast(128), (), [Bc31])
        lamt = kb.sb(es, [1, 256], F32, "lamt")
        lsm = kb.sb(es, [1, 136], F32, "lsm")
        Blam = P.buf("lam")
        kb.ld(lamt[:], lam_d.rearrange("(o r) c -> o (r c)", o=1), (), [Blam])
        kb.tt("dve", lsm[:, 0:64], lamt[:, 0:64], lamt[:, 64:128], ALU.mult, [Blam], [Blam])
        kb.tt("dve", lsm[:, 64:128], lamt[:, 128:192], lamt[:, 192:256], ALU.mult, [Blam], [Blam])
        P.op("dve", lambda: nc.vector.tensor_reduce(out=lsm[:, 128:130], in_=lsm[:, 0:128].rearrange("p (a b) -> p a b", a=2), axis=AX.X, op=ALU.add), [Blam], [Blam])
        kb.act(lsm[:, 130:132], lsm[:, 128:130], AF.Exp, [Blam], [Blam])
        kb.tt("dve", lsm[:, 132:133], lsm[:, 130:131], lsm[:, 131:132], ALU.subtract, [Blam], [Blam])
        kb.ts("dve", lsm[:, 132:133], lsm[:, 132:133], lam_init, ALU.add, [Blam], [Blam])
        kb.cp("dve", lsm[:, 133:134], lsm[:, 132:133], [Blam], [Blam])
        pl = kb.ps(es, [128, 512], F32, "pl")
        Bpl = P.buf("pl")
        kb.mm(pl[:, 0:2], onesf[0:1, :], lsm[0:1, 132:134], True, True, [Bon, Blam], [Bpl])
        neglam = kb.sb(es, [128, 2], F32, "neglam")
        Bnl = P.buf("neglam")
        kb.ts("dve", neglam[:], pl[:, 0:2], -1.0, ALU.mult, [Bpl], [Bnl])
        gsc = kb.sb(es, [128, 1], F32, "gsc")
        Bgsc = P.buf("gsc")
        kb.ld(gsc[:], subg_d.rearrange("o e -> e o"), (), [Bgsc])
        kb.ts("dve", gsc[:], gsc[:], 1.0 - lam_init, ALU.mult, [Bgsc], [Bgsc])
        qh = [kb.sb(es, [128, 2048], BF16, "qh") for _ in range(2)]
        kh = [kb.sb(es, [128, 2048], BF16, "kh") for _ in range(2)]
        vh = [kb.sb(es, [128, 16, 128], BF16, "vh") for _ in range(2)]
        Bin = P.bufs(2, "qkv")
        PT = [kb.sb(es, [128, 512], BF16, "PT") for _ in range(4)]
        BPT = P.bufs(4, "PT")
        oTh = [kb.sb(es, [128, 2048], BF16, "oTh") for _ in range(2)]
        BoT = P.bufs(2, "oTh")
        r1 = kb.sb(es, [128, 512], F32, "r1")
        r2 = kb.sb(es, [128, 512], F32, "r2")
        t1 = kb.sb(es, [128, 512], F32, "t1")
        t2 = kb.sb(es, [128, 512], F32, "t2")
        sq = kb.sb(es, [128, 512], BF16, "sq")
        rs = kb.sb(es, [128, 512], F32, "rs")
        Bw = P.buf("work")
        Bsq = P.buf("sq")
        Brs = P.buf("rs")
        pss = [pl, kb.ps(es, [128, 512], F32, "pss"), kb.ps(es, [128, 512], F32, "pss")]
        Bpss = [Bpl, P.buf("pss"), P.buf("pss")]
        pnum = [kb.ps(es, [128, 512], F32, "pnum") for _ in range(2)]
        pden = [kb.ps(es, [128, 512], F32, "pden") for _ in range(2)]
        Bpn = P.bufs(2, "pn")
        Bpd = P.bufs(2, "pd")
        psq = kb.ps(es, [128, 512], F32, "psq")
        Bpsq = P.buf("psq")

        def load_h(h_):
            if h_ >= 16:
                return
            s_ = h_ % 2
            kb.ld(qh[s_][:], qkv_d[h_], (), [Bin[s_]])
            kb.ld(kh[s_][:], qkv_d[16 + h_], (), [Bin[s_]])
            kb.ld(vh[s_][:], vtok_d[:, h_ * 128:(h_ + 1) * 128].rearrange("(t p) e -> p t e", p=128), (), [Bin[s_]])
        load_h(0)
        base = 0
        NPS = 3
        for h in range(16):
            s = h % 2
            load_h(h + 1)
            its = [(c, m, j) for c in range(4) for m in range(2) for j in range(4 * c + 4)]

            def geom(c, j):
                lo = max(j, 4 * c)
                N = (4 * c + 4 - lo) * 128
                if j >= 4 * c:
                    nn, boff = min(N, 256), 0
                elif j == 4 * c - 1:
                    nn, boff = 128, 128
                else:
                    nn, boff = 0, 0
                return lo, N, nn, boff

            def stageA(idx):
                c, m, j = its[idx]
                lo, N, nn, boff = geom(c, j)
                pr = slice(m * 64, (m + 1) * 64)
                mp = 2 * h + m
                q0 = lo * 128
                sl = (base + idx) % NPS
                pt = (base + idx) % 4
                kb.mm(pss[sl][:, 0:N], kh[s][pr, j * 128:(j + 1) * 128], qh[s][pr, q0:q0 + N], True, nn == 0, [Bin[s]], [Bpss[sl]])
                if nn:
                    kb.mm(pss[sl][:, 0:nn], i8[:], bn[:, mp, boff:boff + nn], False, True, [Bi8, Bbn], [Bpss[sl]])
                    kb.act(PT[pt][:, 0:nn], pss[sl][:, 0:nn], AF.Exp, [Bpss[sl]], [BPT[pt]], scale=0.125)
                if N > nn:
                    kb.act(PT[pt][:, nn:N], pss[sl][:, nn:N], AF.Exp, [Bpss[sl], Bc31], [BPT[pt]], scale=0.125, bias=c31[:, mp:mp + 1])

            def stageB(idx):
                c, m, j = its[idx]
                lo, N, nn, boff = geom(c, j)
                off = (lo - 4 * c) * 128
                jl = 4 * c + 3
                pt = (base + idx) % 4
                kb.mm(pnum[m][:, off:512], vh[s][:, j, :], PT[pt][:, 0:N], j == 0, j == jl, [Bin[s], BPT[pt]], [Bpn[m]])
                kb.mm(pden[m][:, off:512], ones[:], PT[pt][:, 0:N], j == 0, j == jl, [Bon, BPT[pt]], [Bpd[m]])
                if m == 1 and j == jl:
                    kb.recip(r1[:], pden[0][:], [Bpd[0]], [Bw])
                    kb.recip(r2[:], pden[1][:], [Bpd[1]], [Bw])
                    kb.tt("dve", t1[:], pnum[0][:], r1[:], ALU.mult, [Bpn[0], Bw], [Bw])
                    kb.tt("dve", t2[:], pnum[1][:], r2[:], ALU.mult, [Bpn[1], Bw], [Bw])
                    kb.stt(t1[:], t2[:], neglam[:, 0:1], t1[:], ALU.mult, ALU.add, [Bw, Bnl], [Bw])
                    kb.act(sq[:], t1[:], AF.Square, [Bw], [Bsq])
                    kb.mm(psq[:], ones[:], sq[:], True, True, [Bon, Bsq], [Bpsq])
                    kb.ts("dve", rs[:], psq[:], 1.0 / 128, ALU.mult, [Bpsq], [Brs], s2=EPS, op1=ALU.add)
                    kb.act(rs[:], rs[:], AF.Sqrt, [Brs], [Brs])
                    kb.recip(rs[:], rs[:], [Brs], [Brs])
                    kb.stt(oTh[s][:, c * 512:(c + 1) * 512], t1[:], gsc[:, 0:1], rs[:], ALU.mult, ALU.mult, [Bw, Bgsc, Brs], [BoT[s]])
            LA = 2
            for i0 in range(min(LA, len(its))):
                stageA(i0)
            for idx in range(len(its)):
                if idx + LA < len(its):
                    stageA(idx + LA)
                stageB(idx)
            base += len(its)
            kb.ld(act_d[h], oTh[s][:], [BoT[s]], (), owner=BoT[s])
        P.end_phase()


def phase_dsa_idx(kb, qkv_d, wi_d, mask_d, cmask_d, ident_d):
    nc, P = kb.nc, kb.P
    BIG = 1.0e30
    with ExitStack() as es:
        kiT = kb.sb(es, [64, 2048], BF16, "kiT")
        qi = kb.sb(es, [64, 16, 2048], BF16, "qi")
        Bqk = P.buf("qiki")
        kb.ld(kiT[:], qkv_d[28, 0:64, :], (), [Bqk])
        for h in range(16):
            kb.ld(qi[:, h, :], qkv_d[20 + h // 2, (h % 2) * 64:(h % 2) * 64 + 64, :], (), [Bqk])
        cm = kb.sb(es, [128, 128], F32, "cm")
        Bcm = P.buf("cm")
        kb.ld(cm[:], cmask_d, (), [Bcm])
        ident = kb.sb(es, [128, 128], BF16, "ident")
        Bid = P.buf("ident")
        kb.ld(ident[:], ident_d, (), [Bid], q="pool")
        zt = kb.sb(es, [128, 256], BF16, "zt")
        Bzt = P.buf("zt")
        kb.memset("pool", zt[:], 0.0, [Bzt])
        kb.ld(mask_d[0, :, 0:256], zt[:], [Bzt], (), owner=Bzt)
        kb.ld(mask_d[1, :, 0:256], zt[:], [Bzt], (), owner=Bzt)
        wi = [kb.sb(es, [128, 16], F32, "wi") for _ in range(2)]
        Bwi = P.bufs(2, "wi")
        dgw = [kb.sb(es, [128, 16, 128], BF16, "dgw") for _ in range(2)]
        Bdg = P.bufs(2, "dgw")
        acc = [kb.sb(es, [128, 2048], F32, "acc") for _ in range(2)]
        Bacc = P.bufs(2, "acc")
        Rb = [kb.sb(es, [128, 512], BF16, "Rb") for _ in range(3)]
        BRb = P.bufs(3, "Rb")
        work = kb.sb(es, [128, 2048], F32, "work")
        Bwork = P.buf("work")
        m8 = kb.sb(es, [128, 8], F32, "m8")
        Bm8 = P.buf("m8")
        Mb = [kb.sb(es, [128, 2048], BF16, "Mb") for _ in range(2)]
        BMb = P.bufs(2, "Mb")
        mT = [kb.sb(es, [128, 8, 128], BF16, "mT") for _ in range(2)]
        BmT = P.bufs(2, "mT")
        pl = [kb.ps(es, [128, 512], F32, "pl") for _ in range(2)]
        Bpl = P.bufs(2, "pl")
        pacc = kb.ps(es, [128, 2048], F32, "pacc")
        Bpacc = P.bufs(4, "pacc")
        pT = [kb.ps(es, [128, 1024], BF16, "pT") for _ in range(2)]
        BpT = P.bufs(2, "pT")
        nl = 0
        nT = 0
        cnt = {"nl": 0, "nT": 0}

        def front(qt):
            if qt >= 16:
                return
            a = qt % 2
            ns = (qt + 1) * 128
            kb.ld(wi[a][:], wi_d[qt * 128:(qt + 1) * 128, :], (), [Bwi[a]])
            for h in range(16):
                kb.ts("dve", dgw[a][:, h, :], ident[:], wi[a][:, h:h + 1], ALU.mult, [Bid, Bwi[a]], [Bdg[a]])
            its = [(sc, h) for sc in range((ns + 511) // 512) for h in range(16)]
            base = cnt["nl"]

            def stA(idx):
                sc, h = its[idx]
                n2 = min(512, ns - sc * 512)
                sl = (base + idx) % 2
                rb = (base + idx) % 3
                kb.mm(pl[sl][:, 0:n2], qi[:, h, qt * 128:(qt + 1) * 128], kiT[:, sc * 512:sc * 512 + n2], True, True, [Bqk], [Bpl[sl]])
                kb.act(Rb[rb][:, 0:n2], pl[sl][:, 0:n2], AF.Relu, [Bpl[sl]], [BRb[rb]])

            def stB(idx):
                sc, h = its[idx]
                n2 = min(512, ns - sc * 512)
                rb = (base + idx) % 3
                kb.mm(pacc[:, sc * 512:sc * 512 + n2], dgw[a][:, h, :], Rb[rb][:, 0:n2], h == 0, h == 15, [Bdg[a], BRb[rb]], [Bpacc[sc]])
            stA(0)
            for idx in range(len(its)):
                if idx + 1 < len(its):
                    stA(idx + 1)
                stB(idx)
            cnt["nl"] += len(its)
            nsc = (ns + 511) // 512
            kb.cp("act", acc[a][:, 0:ns], pacc[:, 0:ns], [Bpacc[i] for i in range(nsc)], [Bacc[a]])

        def back(qt):
            a = qt % 2
            ns = (qt + 1) * 128
            dg = acc[a][:, qt * 128:(qt + 1) * 128]
            kb.tt("dve", dg, dg, cm[:], ALU.add, [Bacc[a], Bcm], [Bacc[a]])
            src, Bsrc = acc[a], Bacc[a]
            for r in range(32):
                sv = src[:, 0:ns]
                P.op("dve", (lambda sv=sv: nc.vector.max(out=m8[:], in_=sv)), [Bsrc], [Bm8])
                if r < 31:
                    wv_ = work[:, 0:ns]
                    P.op("dve", (lambda sv=sv, wv_=wv_: nc.vector.match_replace(out=wv_, in_to_replace=m8[:], in_values=sv, imm_value=-BIG)), [Bsrc, Bm8], [Bwork])
                    src, Bsrc = work, Bwork
            kb.ts("dve", Mb[a][:, 0:ns], acc[a][:, 0:ns], m8[:, 7:8], ALU.is_ge, [Bacc[a], Bm8], [BMb[a]])
            for j0 in range(0, qt + 1, 8):
                njj = min(8, qt + 1 - j0)
                b = cnt["nT"] % 2
                cnt["nT"] += 1
                for i in range(njj):
                    j = j0 + i
                    kb.tr(pT[b][:, i * 128:(i + 1) * 128], Mb[a][:, j * 128:(j + 1) * 128], ident[:], [BMb[a], Bid], [BpT[b]])
                srcv = pT[b][:].rearrange("p (j q) -> p j q", j=8)[:, 0:njj, :]
                kb.ts("dve", mT[b][:, 0:njj, :], srcv, -1.0, ALU.add, [BpT[b]], [BmT[b]], s2=-MASKV, op1=ALU.mult)
                kb.ld(mask_d[j0:j0 + njj, :, qt * 128:(qt + 1) * 128].rearrange("j p q -> p j q"), mT[b][:, 0:njj, :], [BmT[b]], (), owner=BmT[b])
        front(2)
        for qt in range(2, 16):
            front(qt + 1)
            back(qt)
        P.end_phase()


def phase_dsa_attn(kb, qkv_d, vtok_d, act_d, mask_d, biasn_d, i8_d, table_d):
    nc, P = kb.nc, kb.P
    with ExitStack() as es:
        bn = kb.sb(es, [128, 32, 256], BF16, "bn")
        Bbn = P.buf("bn")
        kb.ld(bn[:], biasn_d.rearrange("p (h q) -> p h q", h=32), (), [Bbn], q="pool")
        i8 = kb.sb(es, [128, 128], BF16, "i8")
        Bi8 = P.buf("i8")
        kb.ld(i8[:], i8_d, (), [Bi8], q="pool")
        ones = kb.sb(es, [128, 64], BF16, "ones")
        Bon = P.buf("ones")
        kb.memset("dve", ones[:], 1.0, [Bon])
        c31 = kb.sb(es, [128, 32], F32, "c31")
        Bc31 = P.buf("c31")
        kb.ld(c31[:], table_d[31].partition_broadcast(128), (), [Bc31])
        vt = kb.sb(es, [128, 16, 512], BF16, "vt")
        Bvt = P.buf("vt")
        kb.ld(vt[:], vtok_d[:, 0:512].rearrange("(t p) c -> p t c", p=128), (), [Bvt])
        mk = kb.sb(es, [128, 16, 2048], BF16, "mk")
        Bmk = P.buf("mk")
        for j in range(16):
            kb.ld(mk[:, j, j * 128:2048], mask_d[j, :, j * 128:2048], (), [Bmk])
        kh = [kb.sb(es, [64, 2048], BF16, "kh") for _ in range(2)]
        Bkh = P.bufs(2, "kh")
        qh = [kb.sb(es, [64, 2048], BF16, "qh") for _ in range(2)]
        Bqh = P.bufs(2, "qh")
        PT = [kb.sb(es, [128, 512], BF16, "PT") for _ in range(4)]
        BPT = P.bufs(4, "PT")
        oTh = [kb.sb(es, [64, 2048], BF16, "oTh") for _ in range(2)]
        BoT = P.bufs(2, "oTh")
        r1 = [kb.sb(es, [64, 512], F32, "r1") for _ in range(2)]
        Br1 = P.bufs(2, "r1")
        pss = [kb.ps(es, [128, 512], F32, "pss") for _ in range(4)]
        Bpss = P.bufs(4, "pss")
        pnum = [kb.ps(es, [64, 512], F32, "pnum") for _ in range(2)]
        pden = [kb.ps(es, [64, 512], F32, "pden") for _ in range(2)]
        Bpn = P.bufs(2, "pn")
        Bpd = P.bufs(2, "pd")

        def load_q(hq_):
            if hq_ >= 32:
                return
            kb.ld(qh[hq_ % 2][:], qkv_d[hq_ // 2, (hq_ % 2) * 64:(hq_ % 2) * 64 + 64, :], (), [Bqh[hq_ % 2]])

        def load_k(kv_):
            if kv_ >= 8:
                return
            kb.ld(kh[kv_ % 2][:], qkv_d[16 + kv_ // 2, (kv_ % 2) * 64:(kv_ % 2) * 64 + 64, :], (), [Bkh[kv_ % 2]])
        load_k(0)
        load_q(0)
        nj = 0
        nb = 0
        for kv in range(8):
            ks = kv % 2
            load_k(kv + 1)
            for g in range(4):
                hq = 4 * kv + g
                s = hq % 2
                load_q(hq + 1)
                its = [(c, j) for c in range(4) for j in range(4 * c + 4)]

                def geom(c, j):
                    lo = max(j, 4 * c)
                    N = (4 * c + 4 - lo) * 128
                    if j >= 4 * c:
                        nn, boff = min(N, 256), 0
                    elif j == 4 * c - 1:
                        nn, boff = 128, 128
                    else:
                        nn, boff = 0, 0
                    return lo, N, nn, boff

                def stageA(idx, hq=hq, ks=ks, s=s, base=nj):
                    c, j = its[idx]
                    lo, N, nn, boff = geom(c, j)
                    q0 = lo * 128
                    sl = (base + idx) % 4
                    pt = (base + idx) % 4
                    kb.mm(pss[sl][:, 0:N], kh[ks][:, j * 128:(j + 1) * 128], qh[s][:, q0:q0 + N], True, False, [Bkh[ks], Bqh[s]], [Bpss[sl]])
                    if nn:
                        kb.mm(pss[sl][:, 0:nn], i8[:], bn[:, hq, boff:boff + nn], False, False, [Bi8, Bbn], [Bpss[sl]])
                    kb.mm(pss[sl][:, 0:N], i8[:], mk[:, j, q0:q0 + N], False, True, [Bi8, Bmk], [Bpss[sl]])
                    if nn:
                        kb.act(PT[pt][:, 0:nn], pss[sl][:, 0:nn], AF.Exp, [Bpss[sl]], [BPT[pt]], scale=0.125)
                    if N > nn:
                        kb.act(PT[pt][:, nn:N], pss[sl][:, nn:N], AF.Exp, [Bpss[sl], Bc31], [BPT[pt]], scale=0.125, bias=c31[:, hq:hq + 1])

                def stageB(idx, hq=hq, kv=kv, s=s, base=nj, nb0=nb):
                    c, j = its[idx]
                    lo, N, nn, boff = geom(c, j)
                    off = (lo - 4 * c) * 128
                    jl = 4 * c + 3
                    pt = (base + idx) % 4
                    b = (nb0 + c) % 2
                    kb.mm(pnum[b][:, off:512], vt[:, j, kv * 64:(kv + 1) * 64], PT[pt][:, 0:N], j == 0, j == jl, [Bvt, BPT[pt]], [Bpn[b]])
                    kb.mm(pden[b][:, off:512], ones[:], PT[pt][:, 0:N], j == 0, j == jl, [Bon, BPT[pt]], [Bpd[b]])
                    if j == jl:
                        kb.recip(r1[b][:], pden[b][:], [Bpd[b]], [Br1[b]])
                        kb.tt("dve", oTh[s][:, c * 512:(c + 1) * 512], pnum[b][:], r1[b][:], ALU.mult, [Bpn[b], Br1[b]], [BoT[s]])
                LA = 2
                for i0 in range(min(LA, len(its))):
                    stageA(i0)
                for idx in range(len(its)):
                    if idx + LA < len(its):
                        stageA(idx + LA)
                    stageB(idx)
                nj += len(its)
                nb += 4
                kb.ld(act_d[hq // 2, (hq % 2) * 64:(hq % 2) * 64 + 64, :], oTh[s][:], [BoT[s]], (), owner=BoT[s])
        P.end_phase()


def hgrn_inproj(kb, hT, BhT, w_in, lbl_d, layer_idx, hq_d, hf_d, hk_d, hv_d, qkv_d):
    nc, P = kb.nc, kb.P
    tiles = [(i * 128, 128) for i in range(16)] + [(2048 + i * 128, 128) for i in range(16)] + [(6144 + i * 128, 128) for i in range(16)]

    def setup(es):
        c = {}
        lbl = kb.sb(es, [128, 16, 4], F32, "lbl")
        Bl = P.buf("lbl")
        kb.ld(lbl[:], lbl_d.rearrange("p (t l) -> p t l", l=4), (), [Bl])
        kb.act(lbl[:], lbl[:], AF.Exp, [Bl], [Bl])
        ssum = kb.sb(es, [128, 16], F32, "ssum")
        lb = kb.sb(es, [128, 16], F32, "lb")
        oml = kb.sb(es, [128, 16], F32, "oml")
        Blb = P.buf("lb")
        P.op("dve", lambda: nc.vector.tensor_reduce(out=ssum[:], in_=lbl[:], axis=AX.X, op=ALU.add), [Bl], [Blb])
        kb.recip(ssum[:], ssum[:], [Blb], [Blb])
        kb.cp("dve", lb[:], lbl[:, :, 1], [Bl, Blb], [Blb])
        for j in range(2, layer_idx + 1):
            kb.tt("dve", lb[:], lb[:], lbl[:, :, j], ALU.add, [Bl, Blb], [Blb])
        kb.tt("dve", lb[:], lb[:], ssum[:], ALU.mult, [Blb], [Blb])
        kb.ts("dve", oml[:], lb[:], -1.0, ALU.mult, [Blb], [Blb], s2=1.0, op1=ALU.add)
        c["lb"], c["oml"], c["Blb"] = lb, oml, Blb
        c["fo"] = [kb.sb(es, [128, 2048], F32, "fo") for _ in range(2)]
        c["Bfo"] = P.bufs(2, "fo")
        c["fk"] = [kb.sb(es, [128, 2048], F32, "fk") for _ in range(2)]
        c["Bfk"] = P.bufs(2, "fk")
        c["bo"] = [kb.sb(es, [128, 2048], BF16, "bo") for _ in range(2)]
        c["Bbo"] = P.bufs(2, "bo")
        return c

    def epi(c, ti, c0, w, pb, Bpb):
        kind, h = ti // 16, ti % 16
        a = ti % 2
        if kind == 0:
            kb.act(c["fo"][a][:], pb[:], AF.Silu, [Bpb], [c["Bfo"][a]])
            kb.ld(hq_d[h], c["fo"][a][:], [c["Bfo"][a]], (), owner=c["Bfo"][a])
        elif kind == 1:
            fo, Bfo, fk, Bfk = c["fo"][a], c["Bfo"][a], c["fk"][a], c["Bfk"][a]
            kb.act(fo[:], pb[:], AF.Sigmoid, [Bpb], [Bfo])
            kb.ts("dve", fo[:], fo[:], c["oml"][:, h:h + 1], ALU.mult, [Bfo, c["Blb"]], [Bfo], s2=c["lb"][:, h:h + 1], op1=ALU.add)
            kb.ts("dve", fk[:], fo[:], -1.0, ALU.mult, [Bfo], [Bfk], s2=1.0, op1=ALU.add)
            kb.ld(hk_d[h], fk[:], [Bfk], (), owner=Bfk)
            kb.act(fo[:], fo[:], AF.Ln, [Bfo], [Bfo])
            kb.ld(hf_d[h], fo[:], [Bfo], (), owner=Bfo)
        else:
            kb.act(c["bo"][a][:], pb[:], AF.Silu, [Bpb], [c["Bbo"][a]])
            kb.ld(qkv_d[h], c["bo"][a][:], [c["Bbo"][a]], (), owner=c["Bbo"][a])
    phase_proj_fm(kb, hT, BhT, w_in, tiles, epi, setup)
    phase_proj_tok(kb, hT, BhT, w_in, [(4096 + i * 512, 512) for i in range(4)],
                   epi_store_tok(kb, hv_d, lambda gi: gi * 512), epi_tok_setup(kb, BF16))


def phase_hgrn(kb, hq_d, hf_d, hk_d, hv_d, qkv_d, act_d, ng_d, ident_d, tri_d):
    nc, P = kb.nc, kb.P
    with ExitStack() as es:
        identb = kb.sb(es, [128, 128], BF16, "identb")
        tri = kb.sb(es, [64, 512], F32, "tri")
        ng = kb.sb(es, [128, 1], F32, "ng")
        Bc = P.buf("consts")
        kb.ld(identb[:], ident_d, (), [Bc], q="pool")
        kb.ld(tri[:], tri_d, (), [Bc])
        kb.ld(ng[:], ng_d.rearrange("o e -> e o"), (), [Bc])
        ones = kb.sb(es, [128, 128], BF16, "ones")
        rmask = kb.sb(es, [128, 2048], F32, "rmask")
        Bon = P.buf("ones")
        kb.memset("dve", ones[:], 1.0, [Bon])
        kb.memset("dve", rmask[:], 1.0, [Bon])
        kb.memset("dve", rmask[:].rearrange("p (c t) -> p c t", t=64)[:, :, 0:1], 0.0, [Bon])
        A = kb.sb(es, [128, 2048], F32, "A")
        Bb = kb.sb(es, [128, 2048], F32, "B")
        Cq = kb.sb(es, [128, 2048], F32, "Cq")
        Dd = kb.sb(es, [128, 2048], F32, "Dd")
        BA, BB, BCq, BD = P.buf("A"), P.buf("B"), P.buf("Cq"), P.buf("D")
        Cb = [kb.sb(es, [128, 2048], BF16, "Cb") for _ in range(2)]
        Eb = [kb.sb(es, [128, 2048], BF16, "Eb") for _ in range(2)]
        Fb = [kb.sb(es, [128, 2048], BF16, "Fb") for _ in range(2)]
        vtk = [kb.sb(es, [64, 32, 128], BF16, "vtk") for _ in range(2)]
        gs = [kb.sb(es, [128, 2048], BF16, "gs") for _ in range(2)]
        ebend = [kb.sb(es, [128, 32], F32, "ebend") for _ in range(2)]
        BCb, BEb, BFb, Bvtk, Bgs, Beb = (P.bufs(2, "Cb"), P.bufs(2, "Eb"), P.bufs(2, "Fb"), P.bufs(2, "vtk"), P.bufs(2, "gs"), P.bufs(2, "eb"))
        U = kb.sb(es, [128, 128 * 33], F32, "U")
        Dk = kb.sb(es, [128, 128 * 33], F32, "Dk")
        St = kb.sb(es, [128, 128 * 33], F32, "St")
        BU, BDk, BSt = P.buf("U"), P.buf("Dk"), P.buf("St")
        U3 = U[:].rearrange("p (v c) -> p v c", c=33)
        Dk3 = Dk[:].rearrange("p (v c) -> p v c", c=33)
        St3 = St[:].rearrange("p (v c) -> p v c", c=33)
        Stc = kb.sb(es, [128, 32, 128], BF16, "Stc")
        BStc = P.buf("Stc")
        kb.memset("pool", Dk[:], 0.0, [BDk])
        kb.memset("pool", U[:], 0.0, [BU])
        kendT = kb.sb(es, [64, 32, 128], BF16, "kendT")
        BkT = P.buf("kendT")
        attm = kb.sb(es, [64, 2048], BF16, "attm")
        Batt = P.buf("attm")
        o_h = kb.sb(es, [128, 2048], F32, "o_h")
        Boh = P.buf("o_h")
        oTh = [kb.sb(es, [128, 2048], BF16, "oTh") for _ in range(2)]
        BoT = P.bufs(2, "oTh")
        sq = kb.sb(es, [128, 512], BF16, "sq")
        rs = kb.sb(es, [128, 512], F32, "rs")
        tmp = kb.sb(es, [128, 512], F32, "tmp")
        Bsq, Brs, Btmp = P.buf("sq"), P.buf("rs"), P.buf("tmp")
        ptr = [kb.ps(es, [128, 1024], BF16, "ptr") for _ in range(2)]
        Bptr = P.bufs(2, "ptr")
        patt = kb.ps(es, [128, 512], F32, "patt")
        Bpatt = P.buf("patt")
        pupd = [kb.ps(es, [128, 512], F32, "pupd") for _ in range(2)]
        Bpupd = P.bufs(2, "pupd")
        po = [kb.ps(es, [128, 512], F32, "po") for _ in range(2)]
        Bpo = P.bufs(2, "po")
        psq = kb.ps(es, [128, 512], F32, "psq")
        Bpsq = P.buf("psq")
        B3 = Bb[:].rearrange("p (c t) -> p c t", t=64)
        A3 = A[:].rearrange("p (c t) -> p c t", t=64)

        def load_h(h):
            if h >= 16:
                return
            kb.ld(Cq[:], hq_d[h], (), [BCq])
            kb.ld(A[:], hf_d[h], (), [BA])
            kb.ld(Dd[:], hk_d[h], (), [BD])
            kb.ld(vtk[h % 2][:], hv_d[:, h * 128:(h + 1) * 128].rearrange("(c s) v -> s c v", s=64), (), [Bvtk[h % 2]])
            kb.ld(gs[h % 2][:], qkv_d[h], (), [Bgs[h % 2]])

        def ew(h):
            if h >= 16:
                return
            s = h % 2
            P.op("dve", lambda: nc.vector.tensor_tensor_scan(out=Bb[:], data0=rmask[:], data1=A[:], initial=0.0, op0=ALU.mult, op1=ALU.add),
                 [Bon, BA], [BB])
            kb.act(A[:], Bb[:], AF.Exp, [BB], [BA])
            kb.tt("dve", Cb[s][:], Cq[:], A[:], ALU.mult, [BCq, BA], [BCb[s]])
            kb.act(ebend[s][:], B3[:, :, 63], AF.Exp, [BB], [Beb[s]])
            kb.ts("dve", A[:], Bb[:], -1.0, ALU.mult, [BB, BCb[s]], [BA], s2=80.0, op1=ALU.min)
            kb.act(A[:], A[:], AF.Exp, [BA], [BA])
            kb.tt("dve", Eb[s][:], Dd[:], A[:], ALU.mult, [BD, BA], [BEb[s]])
            kb.tt("dve", A3, B3[:, :, 63:64].to_broadcast([128, 32, 64]), B3, ALU.subtract, [BB, BEb[s]], [BA])
            kb.act(A[:], A[:], AF.Exp, [BA], [BA])
            kb.tt("dve", Fb[s][:], Dd[:], A[:], ALU.mult, [BD, BA], [BFb[s]])

        def pe_front(h):
            s = h % 2
            for c in range(32):
                b = (c // 8) % 2
                kb.tr(ptr[b][0:64, (c % 8) * 128:(c % 8 + 1) * 128], Fb[s][:, c * 64:(c + 1) * 64], identb[:], [BFb[s], Bc], [Bptr[b]])
                if c % 8 == 7:
                    kb.cp("act", kendT[:, c - 7:c + 1, :], ptr[b][0:64, :].rearrange("p (c k) -> p c k", c=8), [Bptr[b]], [BkT])
            for grp in range(4):
                for i in range(8):
                    c = grp * 8 + i
                    kb.mm(patt[0:64, i * 64:(i + 1) * 64], Eb[s][:, c * 64:(c + 1) * 64], Cb[s][:, c * 64:(c + 1) * 64], True, True,
                          [BEb[s], BCb[s]], [Bpatt])
                kb.tt("dve", attm[:, grp * 512:(grp + 1) * 512], patt[0:64, :], tri[:], ALU.mult, [Bpatt, Bc], [Batt])
            kb.memset("dve", U3[:, :, 0:1], 0.0, [BU])
            for grp in range(8):
                b = grp % 2
                for i in range(4):
                    c = grp * 4 + i
                    kb.mm(pupd[b][:, i * 128:(i + 1) * 128], kendT[:, c, :], vtk[s][:, c, :], True, True, [BkT, Bvtk[s]], [Bpupd[b]])
                kb.cp("act" if b == 0 else "dve", U3[:, :, grp * 4 + 1:grp * 4 + 5].rearrange("p v c -> p c v"),
                      pupd[b][:].rearrange("p (c v) -> p c v", c=4), [Bpupd[b]], [BU])
            kb.cp("act", Dk3[:, :, 1:33], ebend[s][:].unsqueeze(1).to_broadcast([128, 128, 32]), [Beb[s]], [BDk])
            P.op("dve", lambda: nc.vector.tensor_tensor_scan(out=St[:], data0=Dk[:], data1=U[:], initial=0.0, op0=ALU.mult, op1=ALU.add),
                 [BDk, BU], [BSt])
            kb.cp("act", Stc[:], St3[:, :, 0:32].rearrange("p v c -> p c v"), [BSt], [BStc])

        def pe_back(h):
            s = h % 2
            for grp in range(4):
                b = grp % 2
                for i in range(8):
                    c = grp * 8 + i
                    kb.mm(po[b][:, i * 64:(i + 1) * 64], vtk[s][:, c, :], attm[:, c * 64:(c + 1) * 64], True, False, [Bvtk[s], Batt], [Bpo[b]])
                    kb.mm(po[b][:, i * 64:(i + 1) * 64], Stc[:, c, :], Cb[s][:, c * 64:(c + 1) * 64], False, True, [BStc, BCb[s]], [Bpo[b]])
                kb.cp("act", o_h[:, grp * 512:(grp + 1) * 512], po[b][:], [Bpo[b]], [Boh])
            for cc in range(4):
                cs = slice(cc * 512, (cc + 1) * 512)
                kb.act(sq[:], o_h[:, cs], AF.Square, [Boh], [Bsq])
                kb.mm(psq[:], ones[:], sq[:], True, True, [Bon, Bsq], [Bpsq])
                kb.ts("dve", rs[:], psq[:], 1.0 / 128, ALU.mult, [Bpsq], [Brs], s2=EPS, op1=ALU.add)
                kb.act(rs[:], rs[:], AF.Sqrt, [Brs], [Brs])
                kb.recip(rs[:], rs[:], [Brs], [Brs])
                kb.stt(tmp[:], o_h[:, cs], ng[:, 0:1], rs[:], ALU.mult, ALU.mult, [Boh, Bc, Brs], [Btmp])
                kb.tt("dve", oTh[s][:, cs], tmp[:], gs[s][:, cs], ALU.mult, [Btmp, Bgs[s]], [BoT[s]])
            kb.ld(act_d[h], oTh[s][:], [BoT[s]], (), owner=BoT[s])
        load_h(0)
        ew(0)
        for h in range(16):
            load_h(h + 1)
            pe_front(h)
            ew(h + 1)
            pe_back(h)
        P.end_phase()


LAYER_NIN = {0: 3072, 1: 6144, 2: 8192, 3: 4176}
MIX_NAMES = {0: "swa", 1: "diff", 2: "hgrn", 3: "dsa"}


def build_program(layers, stop_after=None):
    nc = bass.Bass("TRN2", target_bir_lowering=False)
    dr = {}

    def ext(name, shape, dt=F32):
        dr[name] = nc.dram_tensor(name, list(shape), dt, kind="ExternalInput").ap()
        return dr[name]

    x_in = ext("x", [S, D])
    y_out = nc.dram_tensor("y", [S, D], F32, kind="ExternalOutput").ap()
    normg = ext("norm_g", [16, D])
    ident_d = ext("ident", [128, 128])
    i8_d = ext("i8", [128, 128])
    for l in layers:
        ext("w_in%d" % l, [D, LAYER_NIN[l]])
        ext("w_out%d" % l, [D, D])
        ext("w_up%d" % l, [D, 2 * DFF])
        ext("w_down%d" % l, [DFF, D])
        ext("conv%d" % l, [128, 88, 3])
    if 0 in layers:
        ext("bias_sw", [128, 32 * 256])
        ext("sinkrep", [1, 4096])
    if 1 in layers or 3 in layers:
        ext("biasn", [128, 32 * 256])
        ext("table", [32, 32])
    if 1 in layers:
        ext("diff_lambda", [4, 64])
        ext("diff_subg", [1, 128])
    xres = nc.dram_tensor("xres", [S, D], F32).ap()
    m_d = nc.dram_tensor("m_d", [S, D], F32).ap()
    act_d = nc.dram_tensor("act_d", [KFF, 128, S], BF16).ap()
    qkv_d = nc.dram_tensor("qkv_d", [64, 128, S], BF16).ap()
    vtok_d = nc.dram_tensor("vtok_d", [S, 2048], BF16).ap()
    if 2 in layers:
        ext("lbl", [128, 64])
        ext("hgrn_ng", [1, 128])
        ext("tri", [64, 512])
        hq_d = nc.dram_tensor("hq_d", [16, 128, S], F32).ap()
        hf_d = nc.dram_tensor("hf_d", [16, 128, S], F32).ap()
        hk_d = nc.dram_tensor("hk_d", [16, 128, S], F32).ap()
        hv_d = nc.dram_tensor("hv_d", [S, 2048], BF16).ap()
    if 3 in layers:
        ext("cmask", [128, 128])
        wi_d = nc.dram_tensor("wi_d", [S, 16], F32).ap()
        mask_d = nc.dram_tensor("mask_d", [16, 128, S], BF16).ap()

    with ExitStack() as top:
        P = Prog(nc, top)
        kb = KB(nc, P)
        x_cur = x_in
        pend_m = None
        for li, l in enumerate(layers):
            with ExitStack() as hs:
                hT = kb.sb(hs, [128, 16, S], BF16, "hT")
                BhT = Buf("hT")
                phase_rn(kb, x_cur, xres, m_d if pend_m is not None else None,
                         normg[pend_m] if pend_m is not None else None, normg[4 * l + 0], hT, BhT, ident_d)
                if pend_m is not None:
                    x_cur = xres
                w_in = dr["w_in%d" % l]
                if l == 0:
                    tiles = [(i * 128, 128) for i in range(20)]
                    phase_proj_fm(kb, hT, BhT, w_in, tiles, epi_store_fm(kb, qkv_d, lambda ti: ti), epi_store_setup(kb))
                    phase_proj_tok(kb, hT, BhT, w_in, [(2560, 512)], epi_store_tok(kb, vtok_d, lambda gi: 0), epi_tok_setup(kb))
                elif l == 1:
                    tiles = [(i * 128, 128) for i in range(32)]
                    phase_proj_fm(kb, hT, BhT, w_in, tiles, epi_store_fm(kb, qkv_d, lambda ti: ti), epi_store_setup(kb))
                    phase_proj_tok(kb, hT, BhT, w_in, [(4096 + i * 512, 512) for i in range(4)],
                                   epi_store_tok(kb, vtok_d, lambda gi: gi * 512), epi_tok_setup(kb))
                elif l == 2:
                    hgrn_inproj(kb, hT, BhT, w_in, dr["lbl"], l, hq_d, hf_d, hk_d, hv_d, qkv_d)
                elif l == 3:
                    tiles = [(i * 128, 128) for i in range(20)] + [(3072 + i * 128, 128) for i in range(8)] + [(4096, 64)]
                    phase_proj_fm(kb, hT, BhT, w_in, tiles, epi_store_fm(kb, qkv_d, lambda ti: ti), epi_store_setup(kb))

                    def setup3(es):
                        c_ = epi_tok_setup(kb)(es)
                        c_["tf"] = [kb.sb(es, [128, 16], F32, "etf") for _ in range(2)]
                        c_["Bf"] = kb.P.bufs(2, "etf")
                        return c_

                    def epi3(ctx, gi, t, c0, w, pb, Bpb):
                        if gi == 0:
                            epi_store_tok(kb, vtok_d, lambda gi_: 0)(ctx, gi, t, c0, w, pb, Bpb)
                        else:
                            tl, B = ctx["tf"][t % 2], ctx["Bf"][t % 2]
                            kb.cp("dve", tl[:, 0:16], pb[:, 0:16], [Bpb], [B])
                            kb.ld(wi_d[t * 128:(t + 1) * 128, :], tl[:, 0:16], [B], (), owner=B)
                    phase_proj_tok(kb, hT, BhT, w_in, [(2560, 512), (4160, 16)], epi3, setup3)
            if l == 0:
                phase_swa(kb, qkv_d, vtok_d, act_d, dr["bias_sw"], i8_d, dr["sinkrep"])
            elif l == 2:
                phase_hgrn(kb, hq_d, hf_d, hk_d, hv_d, qkv_d, act_d, dr["hgrn_ng"], ident_d, dr["tri"])
            elif l == 3:
                phase_dsa_idx(kb, qkv_d, wi_d, mask_d, dr["cmask"], ident_d)
                phase_dsa_attn(kb, qkv_d, vtok_d, act_d, mask_d, dr["biasn"], i8_d, dr["table"])
            elif l == 1:
                phase_diff(kb, qkv_d, vtok_d, act_d, dr["biasn"], i8_d, dr["table"], dr["diff_lambda"], dr["diff_subg"], l)
            phase_down(kb, dr["w_out%d" % l], 16, act_d, m_d)
            with ExitStack() as hs:
                hT = kb.sb(hs, [128, 16, S], BF16, "hT")
                BhT = Buf("hT")
                phase_rn(kb, x_cur, xres, m_d, normg[4 * l + 1], normg[4 * l + 2], hT, BhT, ident_d)
                x_cur = xres
                phase_up(kb, hT, BhT, dr["w_up%d" % l], dr["conv%d" % l], act_d)
            phase_down(kb, dr["w_down%d" % l], KFF, act_d, m_d)
            pend_m = 4 * l + 3
        phase_rn(kb, x_cur, y_out, m_d, normg[pend_m], None, None, None, ident_d)
    return nc


def _bucket(n):
    n = np.maximum(n, 0)
    max_exact = 16
    lr = np.log(np.maximum(n, 1).astype(np.float32) / max_exact) / math.log(128 / max_exact)
    large = np.minimum(max_exact + (lr * (32 - max_exact)).astype(np.int32), 31)
    return np.where(n < max_exact, n, large)


def _host_consts(inputs, layers):
    c = {}
    c["ident"] = np.eye(128, dtype=np.float32)
    c["i8"] = (8.0 * np.eye(128)).astype(np.float32)
    table = np.asarray(inputs["rel_bias_table"], np.float32)
    if 0 in layers:
        s_ = np.arange(128)[:, None]
        q_ = np.arange(256)[None, :]
        dist = q_ - s_
        valid = (dist >= 0) & (dist < 128)
        bk = _bucket(dist)
        t = table[bk]
        t = np.where(valid[:, :, None], t, np.float32(MASKV))
        c["bias_sw"] = np.ascontiguousarray(t.transpose(0, 2, 1)).reshape(128, 32 * 256).astype(np.float32)
        c["sinkrep"] = np.ascontiguousarray(np.repeat(np.asarray(inputs["swa_sinks"], np.float32)[0], 128)[None, :])
    if 1 in layers or 3 in layers:
        s_ = np.arange(128)[:, None]
        q_ = np.arange(256)[None, :]
        dist = q_ - s_
        t = table[_bucket(dist)]
        t = np.where((dist >= 0)[:, :, None], t, np.float32(MASKV))
        c["biasn"] = np.ascontiguousarray(t.transpose(0, 2, 1)).reshape(128, 32 * 256).astype(np.float32)
        c["table"] = np.ascontiguousarray(table)
    if 2 in layers:
        lg = np.asarray(inputs["hgrn_lb_logits"], np.float32)
        c["lbl"] = np.ascontiguousarray(lg.reshape(4, 16, 128).transpose(2, 1, 0)).reshape(128, 64)
        c["hgrn_ng"] = np.ascontiguousarray(np.asarray(inputs["hgrn_norm_g"], np.float32)[0][None, :])
        tr_ = (np.arange(64)[:, None] <= np.arange(64)[None, :]).astype(np.float32)
        c["tri"] = np.ascontiguousarray(np.tile(tr_, (1, 8)))
    if 3 in layers:
        c["cmask"] = np.where(np.arange(128)[None, :] > np.arange(128)[:, None], np.float32(-1.0e30), np.float32(0.0)).astype(np.float32)
    if 1 in layers:
        c["diff_lambda"] = np.ascontiguousarray(np.asarray(inputs["diff_lambda"], np.float32)[0])
        c["diff_subg"] = np.ascontiguousarray(np.asarray(inputs["diff_subln_g"], np.float32)[0][None, :])
    return c


_W_KEYS = {0: ("swa_w_in", "swa_w_out"), 1: ("diff_w_in", "diff_w_out"), 2: ("hgrn_w_in", "hgrn_w_out"), 3: ("dsa_w_in", "dsa_w_out")}
LAUNCHES = [[0, 1, 2, 3]]
N_CORES = 4


def _layer_inputs(inputs, layers):
    m = {}
    m["norm_g"] = np.ascontiguousarray(np.asarray(inputs["norm_g"], np.float32).reshape(16, D))
    for l in layers:
        kin, kout = _W_KEYS[l]
        m["w_in%d" % l] = np.ascontiguousarray(np.asarray(inputs[kin], np.float32)[0])
        m["w_out%d" % l] = np.ascontiguousarray(np.asarray(inputs[kout], np.float32)[0])
        m["w_up%d" % l] = np.ascontiguousarray(np.asarray(inputs["ffn_w_up"], np.float32)[l])
        m["w_down%d" % l] = np.ascontiguousarray(np.asarray(inputs["ffn_w_down"], np.float32)[l])
        cv = np.asarray(inputs["ffn_conv"], np.float32)[l]
        m["conv%d" % l] = np.ascontiguousarray(cv.reshape(3, 88, 128).transpose(2, 1, 0))
    m.update(_host_consts(inputs, layers))
    return m


def run_launch(inputs, x, layers, cores=None):
    nc = build_program(layers)
    shared = _layer_inputs(inputs, layers)
    B = x.shape[0]
    cores = cores if cores is not None else N_CORES
    in_maps = []
    for c in range(cores):
        d = dict(shared)
        d["x"] = np.ascontiguousarray(x[c % B])
        in_maps.append(d)
    res = run_bass_kernel_spmd(nc, in_maps, core_ids=list(range(cores)))
    return np.stack([res.results[b]["y"] for b in range(B)], axis=0)


def kernel(**inputs):
    x = np.asarray(inputs["x"], np.float32)
    for layers in LAUNCHES:
        x = run_launch(inputs, x, layers)
    return x.astype(np.float32)
```

```python
import math
from contextlib import ExitStack

import numpy as np
import concourse.bass as bass
import concourse.mybir as mybir
from concourse.bass_utils import run_bass_kernel_spmd

F32 = mybir.dt.float32
BF16 = mybir.dt.bfloat16
AF = mybir.ActivationFunctionType
ALU = mybir.AluOpType
AX = mybir.AxisListType

ENGS = ("pe", "act", "dve", "pool", "sp")
CENGS = ("pe", "act", "dve", "pool")

S = 2048
D = 2048
NT = 16
DFF = 5632
KFF = 44
EPS = 1e-6
MASKV = -30000.0
N_DSEM = 56


class Buf:
    __slots__ = ("name", "w", "r", "dsem")

    def __init__(self, name):
        self.name = name
        self.w = None
        self.r = {}
        self.dsem = None


class Prog:
    def __init__(self, nc, es):
        self.nc = nc
        self.ops = {e: [] for e in ENGS}
        self.sems = {}
        self.tot = {}
        self.seen = {e: {} for e in ENGS}
        for e in CENGS:
            self.sems[e] = es.enter_context(nc.semaphore("c_" + e))
            self.tot[e] = 0
        self.dfree = []
        for i in range(N_DSEM):
            k = "d%d" % i
            self.sems[k] = es.enter_context(nc.semaphore(k))
            self.tot[k] = 0
            self.dfree.append(k)
        self.dused = []
        self.phase_bufs = []
        self.nphase = 0

    def buf(self, name="b"):
        b = Buf(name)
        self.phase_bufs.append(b)
        return b

    def bufs(self, n, name="b"):
        return [self.buf(name + str(i)) for i in range(n)]

    def _dsem(self, b):
        if b.dsem is None:
            b.dsem = self.dfree.pop()
            self.dused.append(b.dsem)
        return b.dsem

    def _deps(self, eng, reads, writes):
        deps = {}

        def add(ev):
            if ev is None:
                return
            k, v = ev
            if v > deps.get(k, 0):
                deps[k] = v
        for b in reads:
            add(b.w)
        for b in writes:
            add(b.w)
            for k, v in b.r.items():
                add((k, v))
        waits = []
        for k, v in deps.items():
            if k == eng and eng == "pe":
                continue
            if k not in CENGS:
                v = self.tot[k]
            if v > self.seen[eng].get(k, 0):
                self.seen[eng][k] = v
                waits.append((k, v))
        return waits

    def op(self, eng, fn, reads=(), writes=()):
        waits = self._deps(eng, reads, writes)
        self.tot[eng] += 1
        seq = self.tot[eng]
        self.ops[eng].append((waits, fn, eng, 1))
        for b in reads:
            if b.r.get(eng, 0) < seq:
                b.r[eng] = seq
        for b in writes:
            b.w = (eng, seq)
            b.r = {}

    def dma(self, q, fn, reads=(), writes=(), owner=None):
        waits = self._deps(q, reads, writes)
        if owner is None:
            owner = writes[0] if writes else reads[0]
        k = self._dsem(owner)
        self.tot[k] += 16
        val = self.tot[k]
        self.ops[q].append((waits, fn, k, 16))
        for b in reads:
            b.r[k] = val
        for b in writes:
            b.w = (k, val)
            b.r = {}

    def end_phase(self):
        nc = self.nc
        keys = list(CENGS) + list(self.dused)
        for e in ENGS:
            waits = []
            for k in keys:
                if k == e:
                    continue
                v = self.tot[k]
                if v > self.seen[e].get(k, 0):
                    self.seen[e][k] = v
                    waits.append((k, v))
            self.ops[e].append((waits, None, None, 0))
        with nc.Block() as block:
            def mk(ename, attr):
                lst = self.ops[ename]

                def body(e):
                    for waits, fn, k, inc in lst:
                        for wk, wv in waits:
                            e.wait_ge(self.sems[wk], wv)
                        if fn is not None:
                            fn().then_inc(self.sems[k], inc)
                getattr(block, attr)(body)
            mk("sp", "sync")
            mk("pe", "tensor")
            mk("act", "scalar")
            mk("dve", "vector")
            mk("pool", "gpsimd")
        self.ops = {e: [] for e in ENGS}
        for b in self.phase_bufs:
            b.dsem = None
            b.w = None
            b.r = {}
        self.phase_bufs = []
        self.dfree.extend(self.dused)
        self.dused = []
        self.nphase += 1


class KB:
    def __init__(self, nc, P):
        self.nc = nc
        self.P = P
        self.uid = 0

    def sb(self, es, shape, dt, name="t"):
        self.uid += 1
        return es.enter_context(self.nc.sbuf_tensor("%s_%d" % (name, self.uid), list(shape), dt))

    def ps(self, es, shape, dt, name="p"):
        self.uid += 1
        return es.enter_context(self.nc.psum_tensor("%s_%d" % (name, self.uid), list(shape), dt))

    def mm(self, out, lhsT, rhs, start, stop, r, w):
        nc = self.nc
        self.P.op("pe", lambda: nc.tensor.matmul(out, lhsT=lhsT, rhs=rhs, start=start, stop=stop), r, w)

    def tr(self, out, in_, ident, r, w):
        nc = self.nc
        self.P.op("pe", lambda: nc.tensor.transpose(out, in_, ident), r, w)

    def act(self, out, in_, func, r, w, bias=None, scale=None, accum=None):
        nc = self.nc
        kw = {}
        if bias is not None:
            kw["bias"] = bias
        if scale is not None:
            kw["scale"] = scale
        if accum is not None:
            kw["accum_out"] = accum
        self.P.op("act", lambda: nc.scalar.activation(out=out, in_=in_, func=func, **kw), r, w)

    def ts(self, eng, out, in0, s1, op0, r, w, s2=None, op1=None):
        e = self.nc.vector if eng == "dve" else self.nc.gpsimd
        if op1 is None:
            self.P.op(eng, lambda: e.tensor_scalar(out=out, in0=in0, scalar1=s1, scalar2=None, op0=op0), r, w)
        else:
            self.P.op(eng, lambda: e.tensor_scalar(out=out, in0=in0, scalar1=s1, scalar2=s2, op0=op0, op1=op1), r, w)

    def tt(self, eng, out, in0, in1, op, r, w):
        e = self.nc.vector if eng == "dve" else self.nc.gpsimd
        self.P.op(eng, lambda: e.tensor_tensor(out=out, in0=in0, in1=in1, op=op), r, w)

    def stt(self, out, in0, scalar, in1, op0, op1, r, w):
        nc = self.nc
        self.P.op("dve", lambda: nc.vector.scalar_tensor_tensor(out=out, in0=in0, scalar=scalar, in1=in1, op0=op0, op1=op1), r, w)

    def cp(self, eng, out, in_, r, w):
        nc = self.nc
        if eng == "act":
            self.P.op("act", lambda: nc.scalar.copy(out=out, in_=in_), r, w)
        elif eng == "dve":
            self.P.op("dve", lambda: nc.vector.tensor_copy(out=out, in_=in_), r, w)
        else:
            self.P.op("pool", lambda: nc.gpsimd.tensor_copy(out=out, in_=in_), r, w)

    def recip(self, out, in_, r, w):
        nc = self.nc
        self.P.op("dve", lambda: nc.vector.reciprocal(out=out, in_=in_), r, w)

    def memset(self, eng, ap, val, w):
        e = self.nc.vector if eng == "dve" else self.nc.gpsimd
        self.P.op(eng, lambda: e.memset(ap, val), (), w)

    def ld(self, out, in_, r, w, q="sp", owner=None):
        nc = self.nc
        if q == "sp":
            self.P.dma("sp", lambda: nc.sync.dma_start(out=out, in_=in_), r, w, owner)
        else:
            self.P.dma("pool", lambda: nc.gpsimd.dma_start(out=out, in_=in_), r, w, owner)

    def rstd(self, es_tiles, ss, r, w):
        self.ts("dve", ss, ss, 1.0 / D, ALU.mult, r, w, s2=EPS, op1=ALU.add)
        self.act(ss, ss, AF.Sqrt, r, w)
        self.recip(ss, ss, r, w)


def act_dst(act_d, c, p0=0, p1=128):
    return act_d[:, p0:p1, c, :].rearrange("g p s -> p g s")


def tg8(ap):
    return ap.rearrange("p (g s) -> p g s", g=8)


def phase_rn(kb, x_src, x_dst, m_src, gpost, gpre, hT, BhT, ident_d):
    nc, P = kb.nc, kb.P
    NS = 3
    with ExitStack() as es:
        xt = [kb.sb(es, [128, D], F32, "xt") for _ in range(NS)]
        Bxt = P.bufs(NS, "xt")
        junk = [kb.sb(es, [128, D], BF16, "junk") for _ in range(2)]
        Bjunk = P.bufs(2, "junk")
        st = [kb.sb(es, [128, 2], F32, "st") for _ in range(NS)]
        Bst0 = P.bufs(NS, "st0")
        Bst1 = P.bufs(NS, "st1")
        if m_src is not None:
            mt = [kb.sb(es, [128, D], F32, "mt") for _ in range(NS)]
            Bmt = P.bufs(NS, "mt")
            gp = kb.sb(es, [128, D], F32, "gp")
            Bgp = P.buf("gp")
            kb.ld(gp[:], gpost.partition_broadcast(128), (), [Bgp])
        if gpre is not None:
            gq = kb.sb(es, [128, D], F32, "gq")
            Bgq = P.buf("gq")
            kb.ld(gq[:], gpre.partition_broadcast(128), (), [Bgq])
            hb = [kb.sb(es, [128, D], BF16, "hb") for _ in range(2)]
            Bhb = P.bufs(2, "hb")
            ident = kb.sb(es, [128, 128], BF16, "ident")
            Bid = P.buf("ident")
            kb.ld(ident[:], ident_d, (), [Bid], q="pool")
            pT = [kb.ps(es, [128, 1024], BF16, "pT") for _ in range(4)]
            BpT = P.bufs(4, "pT")

        def load_t(t_):
            if t_ >= NT:
                return
            rows_ = slice(t_ * 128, (t_ + 1) * 128)
            kb.ld(xt[t_ % NS][:], x_src[rows_, :], (), [Bxt[t_ % NS]])
            if m_src is not None:
                kb.ld(mt[t_ % NS][:], m_src[rows_, :], (), [Bmt[t_ % NS]])

        def stage1(t):
            if t >= NT or m_src is None:
                return
            s = t % NS
            rows = slice(t * 128, (t + 1) * 128)
            kb.act(junk[0][:], mt[s][:], AF.Square, [Bmt[s]], [Bjunk[0], Bst0[s]], accum=st[s][:, 0:1])
            kb.rstd(None, st[s][:, 0:1], [Bst0[s]], [Bst0[s]])
            kb.stt(mt[s][:], mt[s][:], st[s][:, 0:1], gp[:], ALU.mult, ALU.mult, [Bmt[s], Bst0[s], Bgp], [Bmt[s]])
            kb.tt("dve", xt[s][:], xt[s][:], mt[s][:], ALU.add, [Bxt[s], Bmt[s]], [Bxt[s]])
            kb.ld(x_dst[rows, :], xt[s][:], [Bxt[s]], (), owner=Bxt[s])

        def stage2(t):
            if gpre is None:
                return
            s = t % NS
            hs_ = t % 2
            kb.act(junk[1][:], xt[s][:], AF.Square, [Bxt[s]], [Bjunk[1], Bst1[s]], accum=st[s][:, 1:2])
            kb.rstd(None, st[s][:, 1:2], [Bst1[s]], [Bst1[s]])
            kb.stt(hb[hs_][:], xt[s][:], st[s][:, 1:2], gq[:], ALU.mult, ALU.mult, [Bxt[s], Bst1[s], Bgq], [Bhb[hs_]])
            for half in range(2):
                pi = (2 * t + half) % 4
                for j in range(8):
                    i = half * 8 + j
                    kb.tr(pT[pi][:, j * 128:(j + 1) * 128], hb[hs_][:, i * 128:(i + 1) * 128], ident[:], [Bhb[hs_], Bid], [BpT[pi]])
                src = pT[pi][:].rearrange("p (j q) -> p j q", j=8)
                dst = hT[:, half * 8:(half + 1) * 8, t * 128:(t + 1) * 128]
                kb.cp("act" if half == 0 else "dve", dst, src, [BpT[pi]], [P.buf("hTw")])
        load_t(0)
        load_t(1)
        stage1(0)
        for t in range(NT):
            load_t(t + 2)
            stage1(t + 1)
            stage2(t)
        P.end_phase()


def phase_proj_fm(kb, hT, BhT, w_d, coltiles, epilogue, epi_setup=None):
    nc, P = kb.nc, kb.P
    wv = w_d.rearrange("(kc p) n -> p kc n", p=128)
    with ExitStack() as es:
        NSL = 3
        wsl = [kb.sb(es, [128, 16, 256], BF16, "wsl") for _ in range(NSL)]
        Bw = P.bufs(NSL, "wsl")
        pb = [kb.ps(es, [128, 2048], F32, "pb") for _ in range(2)]
        Bpb = P.bufs(2, "pb")
        ctx = epi_setup(es) if epi_setup is not None else None
        slabs = []
        i = 0
        while i < len(coltiles):
            c0, w0 = coltiles[i]
            if i + 1 < len(coltiles) and coltiles[i + 1][0] == c0 + w0 and w0 == 128:
                slabs.append([coltiles[i], coltiles[i + 1]])
                i += 2
            else:
                slabs.append([coltiles[i]])
                i += 1
        ti = 0

        def load_slab(si):
            if si >= len(slabs):
                return
            s_ = si % NSL
            c0_ = slabs[si][0][0]
            wtot = sum(w for _, w in slabs[si])
            kb.ld(wsl[s_][:, :, 0:wtot], wv[:, :, c0_:c0_ + wtot], (), [Bw[s_]], q="pool")
        load_slab(0)
        load_slab(1)
        for si, sl in enumerate(slabs):
            s = si % NSL
            load_slab(si + 2)
            off = 0
            for (cc, w) in sl:
                pi = ti % 2
                for g in range(4):
                    for k in range(16):
                        kb.mm(pb[pi][0:w, g * 512:(g + 1) * 512], wsl[s][:, k, off:off + w], hT[:, k, g * 512:(g + 1) * 512],
                              k == 0, k == 15, [Bw[s], BhT], [Bpb[pi]])
                epilogue(ctx, ti, cc, w, pb[pi], Bpb[pi])
                off += w
                ti += 1
        P.end_phase()


def phase_proj_tok(kb, hT, BhT, w_d, colgroups, epilogue, epi_setup=None):
    nc, P = kb.nc, kb.P
    wv = w_d.rearrange("(kc p) n -> p kc n", p=128)
    with ExitStack() as es:
        wg = [kb.sb(es, [128, 16, 512], BF16, "wg") for _ in range(2)]
        Bwg = P.bufs(2, "wg")
        pb = [kb.ps(es, [128, 512], F32, "pk") for _ in range(4)]
        Bpb = P.bufs(4, "pk")
        ctx = epi_setup(es) if epi_setup is not None else None
        n = 0

        def load_g(gi):
            if gi >= len(colgroups):
                return
            c0_, w_ = colgroups[gi]
            kb.ld(wg[gi % 2][:, :, 0:w_], wv[:, :, c0_:c0_ + w_], (), [Bwg[gi % 2]], q="pool")
        load_g(0)
        for gi, (c0, w) in enumerate(colgroups):
            s = gi % 2
            load_g(gi + 1)
            for t in range(NT):
                pi = n % 4
                n += 1
                for k in range(16):
                    kb.mm(pb[pi][:, 0:w], hT[:, k, t * 128:(t + 1) * 128], wg[s][:, k, 0:w], k == 0, k == 15, [BhT, Bwg[s]], [Bpb[pi]])
                epilogue(ctx, gi, t, c0, w, pb[pi], Bpb[pi])
        P.end_phase()


def phase_up(kb, hT, BhT, w_up, conv_d, act_d):
    nc, P = kb.nc, kb.P
    wv = w_up.rearrange("(kc p) n -> p kc n", p=128)
    with ExitStack() as es:
        NSL = 3
        wsl = [kb.sb(es, [128, 16, 512], BF16, "wup") for _ in range(NSL)]
        Bw = P.bufs(NSL, "wup")
        cw = kb.sb(es, [128, 88, 3], F32, "cw")
        Bcw = P.buf("cw")
        kb.ld(cw[:], conv_d, (), [Bcw])
        psg = kb.ps(es, [128, 2048], F32, "psg")
        psv = kb.ps(es, [128, 2048], F32, "psv")
        Bpsg, Bpsv = P.buf("psg"), P.buf("psv")
        ub = [kb.sb(es, [128, 2050], F32, "ub") for _ in range(4)]
        Bub = P.bufs(4, "ub")
        cb = [kb.sb(es, [128, 2048], F32, "cb") for _ in range(4)]
        Bcb = P.bufs(4, "cb")
        ob = [kb.sb(es, [128, 2048], BF16, "ob") for _ in range(2)]
        Bob = P.bufs(2, "ob")
        Bact = P.buf("actd")
        for i in range(4):
            kb.memset("pool", ub[i][:, 0:2], 0.0, [Bub[i]])
        def load_up(sl_):
            if sl_ >= KFF // 2:
                return
            s_ = sl_ % NSL
            c_ = sl_ * 2
            kb.ld(wsl[s_][:, :, 0:256], wv[:, :, c_ * 128:c_ * 128 + 256], (), [Bw[s_]], q="pool")
            kb.ld(wsl[s_][:, :, 256:512], wv[:, :, DFF + c_ * 128:DFF + c_ * 128 + 256], (), [Bw[s_]], q="pool")
        load_up(0)
        load_up(1)
        for c in range(KFF):
            sl = c // 2
            s = sl % NSL
            if c % 2 == 0:
                load_up(sl + 2)
            par = c % 2
            for part, (pp, Bpp) in enumerate(((psg, Bpsg), (psv, Bpsv))):
                off = part * 256 + par * 128
                for g in range(4):
                    for k in range(16):
                        kb.mm(pp[:, g * 512:(g + 1) * 512], wsl[s][:, k, off:off + 128], hT[:, k, g * 512:(g + 1) * 512],
                              k == 0, k == 15, [Bw[s], BhT], [Bpp])
                u = par * 2 + part
                fi = part * KFF + c
                kb.cp("act", ub[u][:, 2:2050], pp[:], [Bpp], [Bub[u]])
                kb.act(cb[u][:], ub[u][:, 0:2048], AF.Copy, [Bub[u], Bcw], [Bcb[u]], scale=cw[:, fi, 0:1])
                kb.stt(cb[u][:], ub[u][:, 1:2049], cw[:, fi, 1:2], cb[u][:], ALU.mult, ALU.add, [Bub[u], Bcw, Bcb[u]], [Bcb[u]])
                kb.stt(cb[u][:], ub[u][:, 2:2050], cw[:, fi, 2:3], cb[u][:], ALU.mult, ALU.add, [Bub[u], Bcw, Bcb[u]], [Bcb[u]])
                if part == 0:
                    kb.act(cb[u][:], cb[u][:], AF.Silu, [Bcb[u]], [Bcb[u]])
            ug, uv = par * 2, par * 2 + 1
            kb.tt("dve", ob[par][:], cb[ug][:], cb[uv][:], ALU.mult, [Bcb[ug], Bcb[uv]], [Bob[par]])
            kb.ld(act_dst(act_d, c), tg8(ob[par][:]), [Bob[par]], (), owner=Bob[par])
        P.end_phase()


def phase_down(kb, w_d, KC, act_d, m_d):
    nc, P = kb.nc, kb.P
    wv = w_d.rearrange("(kc p) n -> p kc n", p=128)
    TG = 256
    with ExitStack() as es:
        wres = [kb.sb(es, [128, KC, 512], BF16, "wres") for _ in range(2)]
        Bwr = [P.bufs(4, "wr%d_" % i) for i in range(2)]
        slab = [kb.sb(es, [128, KC, TG], BF16, "slab") for _ in range(2)]
        Bsl = P.bufs(2, "slab")
        pb = [kb.ps(es, [128, 512], F32, "pd") for _ in range(4)]
        Bpb = P.bufs(4, "pd")
        ot = [kb.sb(es, [128, 512], F32, "ot") for _ in range(3)]
        Bot = P.bufs(3, "ot")
        Bm = P.buf("md")
        kq = [(i * KC) // 4 for i in range(5)]
        n = 0
        ns = 0
        NTG = S // TG

        def load_w(cg_):
            if cg_ >= 4:
                return
            for pq_ in range(4):
                kb.ld(wres[cg_ % 2][:, kq[pq_]:kq[pq_ + 1], :], wv[:, kq[pq_]:kq[pq_ + 1], cg_ * 512:(cg_ + 1) * 512], (),
                      [Bwr[cg_ % 2][pq_]], q="pool")

        def load_slab(i_):
            if i_ >= 4 * NTG:
                return
            tg_ = i_ % NTG
            kb.ld(slab[i_ % 2][:], act_d[tg_, :, 0:KC, :], (), [Bsl[i_ % 2]])
        load_slab(0)
        load_w(0)
        for cg in range(4):
            ws = cg % 2
            load_w(cg + 1)
            for tg in range(NTG):
                ss = ns % 2
                ns += 1
                load_slab(ns)
                for tt_ in range(TG // 128):
                    pi = n % 4
                    oi = n % 3
                    n += 1
                    for c in range(KC):
                        pq = 0
                        while c >= kq[pq + 1]:
                            pq += 1
                        kb.mm(pb[pi][:], slab[ss][:, c, tt_ * 128:(tt_ + 1) * 128], wres[ws][:, c, :], c == 0, c == KC - 1,
                              [Bsl[ss], Bwr[ws][pq]], [Bpb[pi]])
                    kb.cp("act" if n % 2 == 0 else "dve", ot[oi][:], pb[pi][:], [Bpb[pi]], [Bot[oi]])
                    r0 = tg * TG + tt_ * 128
                    kb.ld(m_d[r0:r0 + 128, cg * 512:(cg + 1) * 512], ot[oi][:], [Bot[oi]], (), owner=Bot[oi])
        P.end_phase()


def epi_store_setup(kb, n=2):
    def setup(es):
        P = kb.P
        return {"t": [kb.sb(es, [128, 2048], BF16, "eo") for _ in range(n)], "B": P.bufs(n, "eo"), "Bd": P.buf("qkvd"), "n": 0}
    return setup


def epi_store_fm(kb, qkv_d, tile_of):
    def epi(ctx, ti, c0, w, pb, Bpb):
        i = ctx["n"] % len(ctx["t"])
        ctx["n"] += 1
        t, B = ctx["t"][i], ctx["B"][i]
        kb.cp("act" if ti % 2 == 0 else "dve", t[0:w, :], pb[0:w, :], [Bpb], [B])
        kb.ld(qkv_d[tile_of(ti), 0:w, :], t[0:w, :], [B], (), owner=B)
    return epi


def epi_tok_setup(kb, dt=BF16):
    def setup(es):
        P = kb.P
        return {"t": [kb.sb(es, [128, 512], dt, "et") for _ in range(3)], "B": P.bufs(3, "et"), "Bd": P.buf("vtokd"), "n": 0}
    return setup


def epi_store_tok(kb, vtok_d, col_of):
    def epi(ctx, gi, t, c0, w, pb, Bpb):
        i = ctx["n"] % 3
        ctx["n"] += 1
        tl, B = ctx["t"][i], ctx["B"][i]
        kb.cp("act" if ctx["n"] % 2 == 0 else "dve", tl[:, 0:w], pb[:, 0:w], [Bpb], [B])
        d0 = col_of(gi)
        kb.ld(vtok_d[t * 128:(t + 1) * 128, d0:d0 + w], tl[:, 0:w], [B], (), owner=B)
    return epi


def phase_swa(kb, qkv_d, vtok_d, act_d, bias_sw_d, i8_d, sink_d):
    nc, P = kb.nc, kb.P
    with ExitStack() as es:
        vt = kb.sb(es, [128, 16, 512], BF16, "vt")
        Bvt = P.buf("vt")
        kb.ld(vt[:], vtok_d[:, 0:512].rearrange("(t p) c -> p t c", p=128), (), [Bvt])
        bsw = kb.sb(es, [128, 32, 256], BF16, "bsw")
        Bbsw = P.buf("bsw")
        kb.ld(bsw[:], bias_sw_d.rearrange("p (h q) -> p h q", h=32), (), [Bbsw], q="pool")
        i8 = kb.sb(es, [128, 128], BF16, "i8")
        Bi8 = P.buf("i8")
        kb.ld(i8[:], i8_d, (), [Bi8], q="pool")
        ones64 = kb.sb(es, [128, 64], BF16, "ones64")
        Bon = P.buf("ones")
        kb.memset("dve", ones64[:], 1.0, [Bon])
        skf = kb.sb(es, [1, 4096], F32, "skf")
        skb = kb.sb(es, [1, 4096], BF16, "skb")
        Bsk = P.buf("sk")
        kb.ld(skf[:], sink_d, (), [Bsk])
        kb.act(skb[:], skf[:], AF.Exp, [Bsk], [Bsk])
        kh = [kb.sb(es, [64, 2048], BF16, "kh") for _ in range(2)]
        qh = [kb.sb(es, [64, 4, 2048], BF16, "qh") for _ in range(2)]
        Bkq = P.bufs(2, "kq")
        PT = [kb.sb(es, [128, 4, 256], BF16, "PT") for _ in range(3)]
        BPT = P.bufs(3, "PT")
        oT = [kb.sb(es, [64, 4, 2048], BF16, "oT") for _ in range(2)]
        BoT = P.bufs(2, "oT")
        rec = [kb.sb(es, [64, 512], F32, "rec") for _ in range(2)]
        Brec = P.bufs(2, "rec")
        pss = [kb.ps(es, [128, 1024], F32, "pss") for _ in range(2)]
        Bpss = P.bufs(2, "pss")
        pnum = [kb.ps(es, [64, 512], F32, "pnum") for _ in range(2)]
        Bpn = P.bufs(2, "pnum")
        pden = [kb.ps(es, [64, 512], F32, "pden") for _ in range(2)]
        Bpd = P.bufs(2, "pden")
        Bact = P.buf("actd")
        nj = 0
        nb = 0

        def load_kv(kv_):
            if kv_ >= 8:
                return
            s_ = kv_ % 2
            kb.ld(kh[s_][:], qkv_d[16 + kv_ // 2, (kv_ % 2) * 64:(kv_ % 2) * 64 + 64, :], (), [Bkq[s_]])
            for g_ in range(4):
                hq_ = 4 * kv_ + g_
                kb.ld(qh[s_][:, g_, :], qkv_d[hq_ // 2, (hq_ % 2) * 64:(hq_ % 2) * 64 + 64, :], (), [Bkq[s_]])
        load_kv(0)
        for kv in range(8):
            s = kv % 2
            load_kv(kv + 1)

            def stageA(j, kv=kv, s=s, base=nj):
                nq = 256 if j < 15 else 128
                sl = (base + j) % 2
                pt = (base + j) % 3
                for g in range(4):
                    o = pss[sl][:, g * 256:g * 256 + nq]
                    kb.mm(o, kh[s][:, j * 128:(j + 1) * 128], qh[s][:, g, j * 128:j * 128 + nq], True, False, [Bkq[s]], [Bpss[sl]])
                    kb.mm(o, i8[:], bsw[:, 4 * kv + g, 0:nq], False, True, [Bi8, Bbsw], [Bpss[sl]])
                src = pss[sl][:].rearrange("p (g q) -> p g q", g=4)[:, :, 0:nq]
                kb.act(PT[pt][:, :, 0:nq], src, AF.Exp, [Bpss[sl]], [BPT[pt]], scale=0.125)

            def stageB(j, kv=kv, s=s, base=nj, nb0=nb):
                pt = (base + j) % 3
                prev = (base + j - 1) % 3 if j > 0 else None
                b = (nb0 + j) % 2
                rds = [Bvt, BPT[pt]] + ([BPT[prev]] if prev is not None else [])
                kb.mm(pden[b][:], ones64[0:1, 0:64], skb[0:1, 4 * kv * 128:(4 * kv + 4) * 128], True, False, [Bon, Bsk], [Bpd[b]])
                if prev is not None:
                    kb.mm(pnum[b][:], vt[:, j - 1, kv * 64:(kv + 1) * 64], PT[prev][:, :, 128:256], True, False, rds, [Bpn[b]])
                    kb.mm(pden[b][:], ones64[:], PT[prev][:, :, 128:256], False, False, rds + [Bon], [Bpd[b]])
                kb.mm(pnum[b][:], vt[:, j, kv * 64:(kv + 1) * 64], PT[pt][:, :, 0:128], prev is None, True, rds, [Bpn[b]])
                kb.mm(pden[b][:], ones64[:], PT[pt][:, :, 0:128], False, True, rds + [Bon], [Bpd[b]])
                kb.recip(rec[b][:], pden[b][:], [Bpd[b]], [Brec[b]])
                kb.tt("dve", oT[s][:, :, j * 128:(j + 1) * 128], pnum[b][:].rearrange("p (g q) -> p g q", g=4),
                      rec[b][:].rearrange("p (g q) -> p g q", g=4), ALU.mult, [Bpn[b], Brec[b]], [BoT[s]])
            stageA(0)
            for j in range(16):
                if j + 1 < 16:
                    stageA(j + 1)
                stageB(j)
            nj += 16
            nb += 16
            for g in range(4):
                hq = 4 * kv + g
                kb.ld(act_dst(act_d, hq // 2, (hq % 2) * 64, (hq % 2) * 64 + 64), tg8(oT[s][:, g, :]), [BoT[s]], (), owner=BoT[s])
        P.end_phase()


def phase_diff(kb, qkv_d, vtok_d, act_d, biasn_d, i8_d, table_d, lam_d, subg_d, layer_idx):
    nc, P = kb.nc, kb.P
    lam_init = 0.8 - 0.6 * math.exp(-0.3 * layer_idx)
    with ExitStack() as es:
        bn = kb.sb(es, [128, 32, 256], BF16, "bn")
        Bbn = P.buf("bn")
        kb.ld(bn[:], biasn_d.rearrange("p (h q) -> p h q", h=32), (), [Bbn], q="pool")
        i8 = kb.sb(es, [128, 128], BF16, "i8")
        Bi8 = P.buf("i8")
        kb.ld(i8[:], i8_d, (), [Bi8], q="pool")
        ones = kb.sb(es, [128, 128], BF16, "ones")
        onesf = kb.sb(es, [1, 128], F32, "onesf")
        Bon = P.buf("ones")
        kb.memset("dve", ones[:], 1.0, [Bon])
        kb.memset("dve", onesf[:], 1.0, [Bon])
        c31 = kb.sb(es, [128, 32], F32, "c31")
        Bc31 = P.buf("c31")
        kb.ld(c31[:], table_d[31].partition_broadcast(128), (), [Bc31])
        lamt = kb.sb(es, [1, 256], F32, "lamt")
        lsm = kb.sb(es, [1, 136], F32, "lsm")
        Blam = P.buf("lam")
        kb.ld(lamt[:], lam_d.rearrange("(o r) c -> o (r c)", o=1), (), [Blam])
        kb.tt("dve", lsm[:, 0:64], lamt[:, 0:64], lamt[:, 64:128], ALU.mult, [Blam], [Blam])
        kb.tt("dve", lsm[:, 64:128], lamt[:, 128:192], lamt[:, 192:256], ALU.mult, [Blam], [Blam])
        P.op("dve", lambda: nc.vector.tensor_reduce(out=lsm[:, 128:130], in_=lsm[:, 0:128].rearrange("p (a b) -> p a b", a=2), axis=AX.X, op=ALU.add), [Blam], [Blam])
        kb.act(lsm[:, 130:132], lsm[:, 128:130], AF.Exp, [Blam], [Blam])
        kb.tt("dve", lsm[:, 132:133], lsm[:, 130:131], lsm[:, 131:132], ALU.subtract, [Blam], [Blam])
        kb.ts("dve", lsm[:, 132:133], lsm[:, 132:133], lam_init, ALU.add, [Blam], [Blam])
        kb.cp("dve", lsm[:, 133:134], lsm[:, 132:133], [Blam], [Blam])
        pl = kb.ps(es, [128, 512], F32, "pl")
        Bpl = P.buf("pl")
        kb.mm(pl[:, 0:2], onesf[0:1, :], lsm[0:1, 132:134], True, True, [Bon, Blam], [Bpl])
        neglam = kb.sb(es, [128, 2], F32, "neglam")
        Bnl = P.buf("neglam")
        kb.ts("dve", neglam[:], pl[:, 0:2], -1.0, ALU.mult, [Bpl], [Bnl])
        gsc = kb.sb(es, [128, 1], F32, "gsc")
        Bgsc = P.buf("gsc")
        kb.ld(gsc[:], subg_d.rearrange("o e -> e o"), (), [Bgsc])
        kb.ts("dve", gsc[:], gsc[:], 1.0 - lam_init, ALU.mult, [Bgsc], [Bgsc])
        qh = [kb.sb(es, [128, 2048], BF16, "qh") for _ in range(2)]
        kh = [kb.sb(es, [128, 2048], BF16, "kh") for _ in range(2)]
        vh = [kb.sb(es, [128, 16, 128], BF16, "vh") for _ in range(2)]
        Bin = P.bufs(2, "qkv")
        PT = [kb.sb(es, [128, 512], BF16, "PT") for _ in range(4)]
        BPT = P.bufs(4, "PT")
        oTh = [kb.sb(es, [128, 2048], BF16, "oTh") for _ in range(2)]
        BoT = P.bufs(2, "oTh")
        r1 = kb.sb(es, [128, 512], F32, "r1")
        r2 = kb.sb(es, [128, 512], F32, "r2")
        t1 = kb.sb(es, [128, 512], F32, "t1")
        t2 = kb.sb(es, [128, 512], F32, "t2")
        sq = kb.sb(es, [128, 512], BF16, "sq")
        rs = kb.sb(es, [128, 512], F32, "rs")
        Bw = P.buf("work")
        Bsq = P.buf("sq")
        Brs = P.buf("rs")
        pss = [pl, kb.ps(es, [128, 512], F32, "pss"), kb.ps(es, [128, 512], F32, "pss")]
        Bpss = [Bpl, P.buf("pss"), P.buf("pss")]
        pnum = [kb.ps(es, [128, 512], F32, "pnum") for _ in range(2)]
        pden = [kb.ps(es, [128, 512], F32, "pden") for _ in range(2)]
        Bpn = P.bufs(2, "pn")
        Bpd = P.bufs(2, "pd")
        psq = kb.ps(es, [128, 512], F32, "psq")
        Bpsq = P.buf("psq")

        def load_h(h_):
            if h_ >= 16:
                return
            s_ = h_ % 2
            kb.ld(qh[s_][:], qkv_d[h_], (), [Bin[s_]])
            kb.ld(kh[s_][:], qkv_d[16 + h_], (), [Bin[s_]])
            kb.ld(vh[s_][:], vtok_d[:, h_ * 128:(h_ + 1) * 128].rearrange("(t p) e -> p t e", p=128), (), [Bin[s_]])
        load_h(0)
        base = 0
        NPS = 3
        for h in range(16):
            s = h % 2
            load_h(h + 1)
            its = [(c, m, j) for c in range(4) for m in range(2) for j in range(4 * c + 4)]

            def geom(c, j):
                lo = max(j, 4 * c)
                N = (4 * c + 4 - lo) * 128
                if j >= 4 * c:
                    nn, boff = min(N, 256), 0
                elif j == 4 * c - 1:
                    nn, boff = 128, 128
                else:
                    nn, boff = 0, 0
                return lo, N, nn, boff

            def stageA(idx):
                c, m, j = its[idx]
                lo, N, nn, boff = geom(c, j)
                pr = slice(m * 64, (m + 1) * 64)
                mp = 2 * h + m
                q0 = lo * 128
                sl = (base + idx) % NPS
                pt = (base + idx) % 4
                kb.mm(pss[sl][:, 0:N], kh[s][pr, j * 128:(j + 1) * 128], qh[s][pr, q0:q0 + N], True, nn == 0, [Bin[s]], [Bpss[sl]])
                if nn:
                    kb.mm(pss[sl][:, 0:nn], i8[:], bn[:, mp, boff:boff + nn], False, True, [Bi8, Bbn], [Bpss[sl]])
                    kb.act(PT[pt][:, 0:nn], pss[sl][:, 0:nn], AF.Exp, [Bpss[sl]], [BPT[pt]], scale=0.125)
                if N > nn:
                    kb.act(PT[pt][:, nn:N], pss[sl][:, nn:N], AF.Exp, [Bpss[sl], Bc31], [BPT[pt]], scale=0.125, bias=c31[:, mp:mp + 1])

            def stageB(idx):
                c, m, j = its[idx]
                lo, N, nn, boff = geom(c, j)
                off = (lo - 4 * c) * 128
                jl = 4 * c + 3
                pt = (base + idx) % 4
                kb.mm(pnum[m][:, off:512], vh[s][:, j, :], PT[pt][:, 0:N], j == 0, j == jl, [Bin[s], BPT[pt]], [Bpn[m]])
                kb.mm(pden[m][:, off:512], ones[:], PT[pt][:, 0:N], j == 0, j == jl, [Bon, BPT[pt]], [Bpd[m]])
                if m == 1 and j == jl:
                    kb.recip(r1[:], pden[0][:], [Bpd[0]], [Bw])
                    kb.recip(r2[:], pden[1][:], [Bpd[1]], [Bw])
                    kb.tt("dve", t1[:], pnum[0][:], r1[:], ALU.mult, [Bpn[0], Bw], [Bw])
                    kb.tt("dve", t2[:], pnum[1][:], r2[:], ALU.mult, [Bpn[1], Bw], [Bw])
                    kb.stt(t1[:], t2[:], neglam[:, 0:1], t1[:], ALU.mult, ALU.add, [Bw, Bnl], [Bw])
                    kb.act(sq[:], t1[:], AF.Square, [Bw], [Bsq])
                    kb.mm(psq[:], ones[:], sq[:], True, True, [Bon, Bsq], [Bpsq])
                    kb.ts("dve", rs[:], psq[:], 1.0 / 128, ALU.mult, [Bpsq], [Brs], s2=EPS, op1=ALU.add)
                    kb.act(rs[:], rs[:], AF.Sqrt, [Brs], [Brs])
                    kb.recip(rs[:], rs[:], [Brs], [Brs])
                    kb.stt(oTh[s][:, c * 512:(c + 1) * 512], t1[:], gsc[:, 0:1], rs[:], ALU.mult, ALU.mult, [Bw, Bgsc, Brs], [BoT[s]])
            LA = 2
            for i0 in range(min(LA, len(its))):
                stageA(i0)
            for idx in range(len(its)):
                if idx + LA < len(its):
                    stageA(idx + LA)
                stageB(idx)
            base += len(its)
            kb.ld(act_dst(act_d, h), tg8(oTh[s][:]), [BoT[s]], (), owner=BoT[s])
        P.end_phase()


def phase_dsa_idx(kb, qkv_d, wi_d, mask_d, cmask_d, ident_d):
    nc, P = kb.nc, kb.P
    BIG = 1.0e30
    with ExitStack() as es:
        kiT = kb.sb(es, [64, 2048], BF16, "kiT")
        qi = kb.sb(es, [64, 16, 2048], BF16, "qi")
        Bqk = P.buf("qiki")
        kb.ld(kiT[:], qkv_d[28, 0:64, :], (), [Bqk])
        for h in range(16):
            kb.ld(qi[:, h, :], qkv_d[20 + h // 2, (h % 2) * 64:(h % 2) * 64 + 64, :], (), [Bqk])
        cm = kb.sb(es, [128, 128], F32, "cm")
        Bcm = P.buf("cm")
        kb.ld(cm[:], cmask_d, (), [Bcm])
        ident = kb.sb(es, [128, 128], BF16, "ident")
        Bid = P.buf("ident")
        kb.ld(ident[:], ident_d, (), [Bid], q="pool")
        zt = kb.sb(es, [128, 256], BF16, "zt")
        Bzt = P.buf("zt")
        kb.memset("pool", zt[:], 0.0, [Bzt])
        kb.ld(mask_d[0, :, 0:256], zt[:], [Bzt], (), owner=Bzt)
        kb.ld(mask_d[1, :, 0:256], zt[:], [Bzt], (), owner=Bzt)
        wi = [kb.sb(es, [128, 16], F32, "wi") for _ in range(2)]
        Bwi = P.bufs(2, "wi")
        dgw = [kb.sb(es, [128, 16, 128], BF16, "dgw") for _ in range(2)]
        Bdg = P.bufs(2, "dgw")
        acc = [kb.sb(es, [128, 2048], F32, "acc") for _ in range(2)]
        Bacc = P.bufs(2, "acc")
        Rb = [kb.sb(es, [128, 512], BF16, "Rb") for _ in range(3)]
        BRb = P.bufs(3, "Rb")
        work = kb.sb(es, [128, 2048], F32, "work")
        Bwork = P.buf("work")
        m8 = kb.sb(es, [128, 8], F32, "m8")
        Bm8 = P.buf("m8")
        Mb = [kb.sb(es, [128, 2048], BF16, "Mb") for _ in range(2)]
        BMb = P.bufs(2, "Mb")
        mT = [kb.sb(es, [128, 8, 128], BF16, "mT") for _ in range(2)]
        BmT = P.bufs(2, "mT")
        pl = [kb.ps(es, [128, 512], F32, "pl") for _ in range(2)]
        Bpl = P.bufs(2, "pl")
        pacc = kb.ps(es, [128, 2048], F32, "pacc")
        Bpacc = P.bufs(4, "pacc")
        pT = [kb.ps(es, [128, 1024], BF16, "pT") for _ in range(2)]
        BpT = P.bufs(2, "pT")
        nl = 0
        nT = 0
        cnt = {"nl": 0, "nT": 0}

        def front(qt):
            if qt >= 16:
                return
            a = qt % 2
            ns = (qt + 1) * 128
            kb.ld(wi[a][:], wi_d[qt * 128:(qt + 1) * 128, :], (), [Bwi[a]])
            for h in range(16):
                kb.ts("dve", dgw[a][:, h, :], ident[:], wi[a][:, h:h + 1], ALU.mult, [Bid, Bwi[a]], [Bdg[a]])
            its = [(sc, h) for sc in range((ns + 511) // 512) for h in range(16)]
            base = cnt["nl"]

            def stA(idx):
                sc, h = its[idx]
                n2 = min(512, ns - sc * 512)
                sl = (base + idx) % 2
                rb = (base + idx) % 3
                kb.mm(pl[sl][:, 0:n2], qi[:, h, qt * 128:(qt + 1) * 128], kiT[:, sc * 512:sc * 512 + n2], True, True, [Bqk], [Bpl[sl]])
                kb.act(Rb[rb][:, 0:n2], pl[sl][:, 0:n2], AF.Relu, [Bpl[sl]], [BRb[rb]])

            def stB(idx):
                sc, h = its[idx]
                n2 = min(512, ns - sc * 512)
                rb = (base + idx) % 3
                kb.mm(pacc[:, sc * 512:sc * 512 + n2], dgw[a][:, h, :], Rb[rb][:, 0:n2], h == 0, h == 15, [Bdg[a], BRb[rb]], [Bpacc[sc]])
            stA(0)
            for idx in range(len(its)):
                if idx + 1 < len(its):
                    stA(idx + 1)
                stB(idx)
            cnt["nl"] += len(its)
            nsc = (ns + 511) // 512
            kb.cp("act", acc[a][:, 0:ns], pacc[:, 0:ns], [Bpacc[i] for i in range(nsc)], [Bacc[a]])

        def back(qt):
            a = qt % 2
            ns = (qt + 1) * 128
            dg = acc[a][:, qt * 128:(qt + 1) * 128]
            kb.tt("dve", dg, dg, cm[:], ALU.add, [Bacc[a], Bcm], [Bacc[a]])
            src, Bsrc = acc[a], Bacc[a]
            for r in range(32):
                sv = src[:, 0:ns]
                P.op("dve", (lambda sv=sv: nc.vector.max(out=m8[:], in_=sv)), [Bsrc], [Bm8])
                if r < 31:
                    wv_ = work[:, 0:ns]
                    P.op("dve", (lambda sv=sv, wv_=wv_: nc.vector.match_replace(out=wv_, in_to_replace=m8[:], in_values=sv, imm_value=-BIG)), [Bsrc, Bm8], [Bwork])
                    src, Bsrc = work, Bwork
            kb.ts("dve", Mb[a][:, 0:ns], acc[a][:, 0:ns], m8[:, 7:8], ALU.is_ge, [Bacc[a], Bm8], [BMb[a]])
            for j0 in range(0, qt + 1, 8):
                njj = min(8, qt + 1 - j0)
                b = cnt["nT"] % 2
                cnt["nT"] += 1
                for i in range(njj):
                    j = j0 + i
                    kb.tr(pT[b][:, i * 128:(i + 1) * 128], Mb[a][:, j * 128:(j + 1) * 128], ident[:], [BMb[a], Bid], [BpT[b]])
                srcv = pT[b][:].rearrange("p (j q) -> p j q", j=8)[:, 0:njj, :]
                kb.ts("dve", mT[b][:, 0:njj, :], srcv, -1.0, ALU.add, [BpT[b]], [BmT[b]], s2=-MASKV, op1=ALU.mult)
                kb.ld(mask_d[j0:j0 + njj, :, qt * 128:(qt + 1) * 128].rearrange("j p q -> p j q"), mT[b][:, 0:njj, :], [BmT[b]], (), owner=BmT[b])
        front(2)
        for qt in range(2, 16):
            front(qt + 1)
            back(qt)
        P.end_phase()


def phase_dsa_attn(kb, qkv_d, vtok_d, act_d, mask_d, biasn_d, i8_d, table_d):
    nc, P = kb.nc, kb.P
    with ExitStack() as es:
        bn = kb.sb(es, [128, 32, 256], BF16, "bn")
        Bbn = P.buf("bn")
        kb.ld(bn[:], biasn_d.rearrange("p (h q) -> p h q", h=32), (), [Bbn], q="pool")
        i8 = kb.sb(es, [128, 128], BF16, "i8")
        Bi8 = P.buf("i8")
        kb.ld(i8[:], i8_d, (), [Bi8], q="pool")
        ones = kb.sb(es, [128, 64], BF16, "ones")
        Bon = P.buf("ones")
        kb.memset("dve", ones[:], 1.0, [Bon])
        c31 = kb.sb(es, [128, 32], F32, "c31")
        Bc31 = P.buf("c31")
        kb.ld(c31[:], table_d[31].partition_broadcast(128), (), [Bc31])
        vt = kb.sb(es, [128, 16, 512], BF16, "vt")
        Bvt = P.buf("vt")
        kb.ld(vt[:], vtok_d[:, 0:512].rearrange("(t p) c -> p t c", p=128), (), [Bvt])
        mk = kb.sb(es, [128, 16, 2048], BF16, "mk")
        Bmk = P.buf("mk")
        for j in range(16):
            kb.ld(mk[:, j, j * 128:2048], mask_d[j, :, j * 128:2048], (), [Bmk])
        kh = [kb.sb(es, [64, 2048], BF16, "kh") for _ in range(2)]
        Bkh = P.bufs(2, "kh")
        qh = [kb.sb(es, [64, 2048], BF16, "qh") for _ in range(2)]
        Bqh = P.bufs(2, "qh")
        PT = [kb.sb(es, [128, 512], BF16, "PT") for _ in range(4)]
        BPT = P.bufs(4, "PT")
        oTh = [kb.sb(es, [64, 2048], BF16, "oTh") for _ in range(2)]
        BoT = P.bufs(2, "oTh")
        r1 = [kb.sb(es, [64, 512], F32, "r1") for _ in range(2)]
        Br1 = P.bufs(2, "r1")
        pss = [kb.ps(es, [128, 512], F32, "pss") for _ in range(4)]
        Bpss = P.bufs(4, "pss")
        pnum = [kb.ps(es, [64, 512], F32, "pnum") for _ in range(2)]
        pden = [kb.ps(es, [64, 512], F32, "pden") for _ in range(2)]
        Bpn = P.bufs(2, "pn")
        Bpd = P.bufs(2, "pd")

        def load_q(hq_):
            if hq_ >= 32:
                return
            kb.ld(qh[hq_ % 2][:], qkv_d[hq_ // 2, (hq_ % 2) * 64:(hq_ % 2) * 64 + 64, :], (), [Bqh[hq_ % 2]])

        def load_k(kv_):
            if kv_ >= 8:
                return
            kb.ld(kh[kv_ % 2][:], qkv_d[16 + kv_ // 2, (kv_ % 2) * 64:(kv_ % 2) * 64 + 64, :], (), [Bkh[kv_ % 2]])
        load_k(0)
        load_q(0)
        nj = 0
        nb = 0
        for kv in range(8):
            ks = kv % 2
            load_k(kv + 1)
            for g in range(4):
                hq = 4 * kv + g
                s = hq % 2
                load_q(hq + 1)
                its = [(c, j) for c in range(4) for j in range(4 * c + 4)]

                def geom(c, j):
                    lo = max(j, 4 * c)
                    N = (4 * c + 4 - lo) * 128
                    if j >= 4 * c:
                        nn, boff = min(N, 256), 0
                    elif j == 4 * c - 1:
                        nn, boff = 128, 128
                    else:
                        nn, boff = 0, 0
                    return lo, N, nn, boff

                def stageA(idx, hq=hq, ks=ks, s=s, base=nj):
                    c, j = its[idx]
                    lo, N, nn, boff = geom(c, j)
                    q0 = lo * 128
                    sl = (base + idx) % 4
                    pt = (base + idx) % 4
                    kb.mm(pss[sl][:, 0:N], kh[ks][:, j * 128:(j + 1) * 128], qh[s][:, q0:q0 + N], True, False, [Bkh[ks], Bqh[s]], [Bpss[sl]])
                    if nn:
                        kb.mm(pss[sl][:, 0:nn], i8[:], bn[:, hq, boff:boff + nn], False, False, [Bi8, Bbn], [Bpss[sl]])
                    kb.mm(pss[sl][:, 0:N], i8[:], mk[:, j, q0:q0 + N], False, True, [Bi8, Bmk], [Bpss[sl]])
                    if nn:
                        kb.act(PT[pt][:, 0:nn], pss[sl][:, 0:nn], AF.Exp, [Bpss[sl]], [BPT[pt]], scale=0.125)
                    if N > nn:
                        kb.act(PT[pt][:, nn:N], pss[sl][:, nn:N], AF.Exp, [Bpss[sl], Bc31], [BPT[pt]], scale=0.125, bias=c31[:, hq:hq + 1])

                def stageB(idx, hq=hq, kv=kv, s=s, base=nj, nb0=nb):
                    c, j = its[idx]
                    lo, N, nn, boff = geom(c, j)
                    off = (lo - 4 * c) * 128
                    jl = 4 * c + 3
                    pt = (base + idx) % 4
                    b = (nb0 + c) % 2
                    kb.mm(pnum[b][:, off:512], vt[:, j, kv * 64:(kv + 1) * 64], PT[pt][:, 0:N], j == 0, j == jl, [Bvt, BPT[pt]], [Bpn[b]])
                    kb.mm(pden[b][:, off:512], ones[:], PT[pt][:, 0:N], j == 0, j == jl, [Bon, BPT[pt]], [Bpd[b]])
                    if j == jl:
                        kb.recip(r1[b][:], pden[b][:], [Bpd[b]], [Br1[b]])
                        kb.tt("dve", oTh[s][:, c * 512:(c + 1) * 512], pnum[b][:], r1[b][:], ALU.mult, [Bpn[b], Br1[b]], [BoT[s]])
                LA = 2
                for i0 in range(min(LA, len(its))):
                    stageA(i0)
                for idx in range(len(its)):
                    if idx + LA < len(its):
                        stageA(idx + LA)
                    stageB(idx)
                nj += len(its)
                nb += 4
                kb.ld(act_dst(act_d, hq // 2, (hq % 2) * 64, (hq % 2) * 64 + 64), tg8(oTh[s][:]), [BoT[s]], (), owner=BoT[s])
        P.end_phase()


def hgrn_inproj(kb, hT, BhT, w_in, lbl_d, layer_idx, hq_d, hf_d, hk_d, hv_d, qkv_d):
    nc, P = kb.nc, kb.P
    tiles = [(i * 128, 128) for i in range(16)] + [(2048 + i * 128, 128) for i in range(16)] + [(6144 + i * 128, 128) for i in range(16)]

    def setup(es):
        c = {}
        lbl = kb.sb(es, [128, 16, 4], F32, "lbl")
        Bl = P.buf("lbl")
        kb.ld(lbl[:], lbl_d.rearrange("p (t l) -> p t l", l=4), (), [Bl])
        kb.act(lbl[:], lbl[:], AF.Exp, [Bl], [Bl])
        ssum = kb.sb(es, [128, 16], F32, "ssum")
        lb = kb.sb(es, [128, 16], F32, "lb")
        oml = kb.sb(es, [128, 16], F32, "oml")
        Blb = P.buf("lb")
        P.op("dve", lambda: nc.vector.tensor_reduce(out=ssum[:], in_=lbl[:], axis=AX.X, op=ALU.add), [Bl], [Blb])
        kb.recip(ssum[:], ssum[:], [Blb], [Blb])
        kb.cp("dve", lb[:], lbl[:, :, 1], [Bl, Blb], [Blb])
        for j in range(2, layer_idx + 1):
            kb.tt("dve", lb[:], lb[:], lbl[:, :, j], ALU.add, [Bl, Blb], [Blb])
        kb.tt("dve", lb[:], lb[:], ssum[:], ALU.mult, [Blb], [Blb])
        kb.ts("dve", oml[:], lb[:], -1.0, ALU.mult, [Blb], [Blb], s2=1.0, op1=ALU.add)
        c["lb"], c["oml"], c["Blb"] = lb, oml, Blb
        c["fo"] = [kb.sb(es, [128, 2048], F32, "fo") for _ in range(2)]
        c["Bfo"] = P.bufs(2, "fo")
        c["fk"] = [kb.sb(es, [128, 2048], F32, "fk") for _ in range(2)]
        c["Bfk"] = P.bufs(2, "fk")
        c["bo"] = [kb.sb(es, [128, 2048], BF16, "bo") for _ in range(2)]
        c["Bbo"] = P.bufs(2, "bo")
        return c

    def epi(c, ti, c0, w, pb, Bpb):
        kind, h = ti // 16, ti % 16
        a = ti % 2
        if kind == 0:
            kb.act(c["fo"][a][:], pb[:], AF.Silu, [Bpb], [c["Bfo"][a]])
            kb.ld(hq_d[h], c["fo"][a][:], [c["Bfo"][a]], (), owner=c["Bfo"][a])
        elif kind == 1:
            fo, Bfo, fk, Bfk = c["fo"][a], c["Bfo"][a], c["fk"][a], c["Bfk"][a]
            kb.act(fo[:], pb[:], AF.Sigmoid, [Bpb], [Bfo])
            kb.ts("dve", fo[:], fo[:], c["oml"][:, h:h + 1], ALU.mult, [Bfo, c["Blb"]], [Bfo], s2=c["lb"][:, h:h + 1], op1=ALU.add)
            kb.ts("dve", fk[:], fo[:], -1.0, ALU.mult, [Bfo], [Bfk], s2=1.0, op1=ALU.add)
            kb.ld(hk_d[h], fk[:], [Bfk], (), owner=Bfk)
            kb.act(fo[:], fo[:], AF.Ln, [Bfo], [Bfo])
            kb.ld(hf_d[h], fo[:], [Bfo], (), owner=Bfo)
        else:
            kb.act(c["bo"][a][:], pb[:], AF.Silu, [Bpb], [c["Bbo"][a]])
            kb.ld(qkv_d[h], c["bo"][a][:], [c["Bbo"][a]], (), owner=c["Bbo"][a])
    phase_proj_fm(kb, hT, BhT, w_in, tiles, epi, setup)
    phase_proj_tok(kb, hT, BhT, w_in, [(4096 + i * 512, 512) for i in range(4)],
                   epi_store_tok(kb, hv_d, lambda gi: gi * 512), epi_tok_setup(kb, BF16))


def phase_hgrn(kb, hq_d, hf_d, hk_d, hv_d, qkv_d, act_d, ng_d, ident_d, tri_d):
    nc, P = kb.nc, kb.P
    with ExitStack() as es:
        identb = kb.sb(es, [128, 128], BF16, "identb")
        tri = kb.sb(es, [64, 512], F32, "tri")
        ng = kb.sb(es, [128, 1], F32, "ng")
        Bc = P.buf("consts")
        kb.ld(identb[:], ident_d, (), [Bc], q="pool")
        kb.ld(tri[:], tri_d, (), [Bc])
        kb.ld(ng[:], ng_d.rearrange("o e -> e o"), (), [Bc])
        ones = kb.sb(es, [128, 128], BF16, "ones")
        rmask = kb.sb(es, [128, 2048], F32, "rmask")
        Bon = P.buf("ones")
        kb.memset("dve", ones[:], 1.0, [Bon])
        kb.memset("dve", rmask[:], 1.0, [Bon])
        kb.memset("dve", rmask[:].rearrange("p (c t) -> p c t", t=64)[:, :, 0:1], 0.0, [Bon])
        A = kb.sb(es, [128, 2048], F32, "A")
        Bb = kb.sb(es, [128, 2048], F32, "B")
        Cq = kb.sb(es, [128, 2048], F32, "Cq")
        Dd = kb.sb(es, [128, 2048], F32, "Dd")
        BA, BB, BCq, BD = P.buf("A"), P.buf("B"), P.buf("Cq"), P.buf("D")
        Cb = [kb.sb(es, [128, 2048], BF16, "Cb") for _ in range(2)]
        Eb = [kb.sb(es, [128, 2048], BF16, "Eb") for _ in range(2)]
        Fb = [kb.sb(es, [128, 2048], BF16, "Fb") for _ in range(2)]
        vtk = [kb.sb(es, [64, 32, 128], BF16, "vtk") for _ in range(2)]
        gs = [kb.sb(es, [128, 2048], BF16, "gs") for _ in range(2)]
        ebend = [kb.sb(es, [128, 32], F32, "ebend") for _ in range(2)]
        BCb, BEb, BFb, Bvtk, Bgs, Beb = (P.bufs(2, "Cb"), P.bufs(2, "Eb"), P.bufs(2, "Fb"), P.bufs(2, "vtk"), P.bufs(2, "gs"), P.bufs(2, "eb"))
        U = kb.sb(es, [128, 128 * 33], F32, "U")
        Dk = kb.sb(es, [128, 128 * 33], F32, "Dk")
        St = kb.sb(es, [128, 128 * 33], F32, "St")
        BU, BDk, BSt = P.buf("U"), P.buf("Dk"), P.buf("St")
        U3 = U[:].rearrange("p (v c) -> p v c", c=33)
        Dk3 = Dk[:].rearrange("p (v c) -> p v c", c=33)
        St3 = St[:].rearrange("p (v c) -> p v c", c=33)
        Stc = kb.sb(es, [128, 32, 128], BF16, "Stc")
        BStc = P.buf("Stc")
        kb.memset("pool", Dk[:], 0.0, [BDk])
        kb.memset("pool", U[:], 0.0, [BU])
        kendT = kb.sb(es, [64, 32, 128], BF16, "kendT")
        BkT = P.buf("kendT")
        attm = kb.sb(es, [64, 2048], BF16, "attm")
        Batt = P.buf("attm")
        o_h = kb.sb(es, [128, 2048], F32, "o_h")
        Boh = P.buf("o_h")
        oTh = [kb.sb(es, [128, 2048], BF16, "oTh") for _ in range(2)]
        BoT = P.bufs(2, "oTh")
        sq = kb.sb(es, [128, 512], BF16, "sq")
        rs = kb.sb(es, [128, 512], F32, "rs")
        tmp = kb.sb(es, [128, 512], F32, "tmp")
        Bsq, Brs, Btmp = P.buf("sq"), P.buf("rs"), P.buf("tmp")
        ptr = [kb.ps(es, [128, 1024], BF16, "ptr") for _ in range(2)]
        Bptr = P.bufs(2, "ptr")
        patt = kb.ps(es, [128, 512], F32, "patt")
        Bpatt = P.buf("patt")
        pupd = [kb.ps(es, [128, 512], F32, "pupd") for _ in range(2)]
        Bpupd = P.bufs(2, "pupd")
        po = [kb.ps(es, [128, 512], F32, "po") for _ in range(2)]
        Bpo = P.bufs(2, "po")
        psq = kb.ps(es, [128, 512], F32, "psq")
        Bpsq = P.buf("psq")
        B3 = Bb[:].rearrange("p (c t) -> p c t", t=64)
        A3 = A[:].rearrange("p (c t) -> p c t", t=64)

        def load_h(h):
            if h >= 16:
                return
            kb.ld(Cq[:], hq_d[h], (), [BCq])
            kb.ld(A[:], hf_d[h], (), [BA])
            kb.ld(Dd[:], hk_d[h], (), [BD])
            kb.ld(vtk[h % 2][:], hv_d[:, h * 128:(h + 1) * 128].rearrange("(c s) v -> s c v", s=64), (), [Bvtk[h % 2]])
            kb.ld(gs[h % 2][:], qkv_d[h], (), [Bgs[h % 2]])

        def ew(h):
            if h >= 16:
                return
            s = h % 2
            P.op("dve", lambda: nc.vector.tensor_tensor_scan(out=Bb[:], data0=rmask[:], data1=A[:], initial=0.0, op0=ALU.mult, op1=ALU.add),
                 [Bon, BA], [BB])
            kb.act(A[:], Bb[:], AF.Exp, [BB], [BA])
            kb.tt("dve", Cb[s][:], Cq[:], A[:], ALU.mult, [BCq, BA], [BCb[s]])
            kb.act(ebend[s][:], B3[:, :, 63], AF.Exp, [BB], [Beb[s]])
            kb.ts("dve", A[:], Bb[:], -1.0, ALU.mult, [BB, BCb[s]], [BA], s2=80.0, op1=ALU.min)
            kb.act(A[:], A[:], AF.Exp, [BA], [BA])
            kb.tt("dve", Eb[s][:], Dd[:], A[:], ALU.mult, [BD, BA], [BEb[s]])
            kb.tt("dve", A3, B3[:, :, 63:64].to_broadcast([128, 32, 64]), B3, ALU.subtract, [BB, BEb[s]], [BA])
            kb.act(A[:], A[:], AF.Exp, [BA], [BA])
            kb.tt("dve", Fb[s][:], Dd[:], A[:], ALU.mult, [BD, BA], [BFb[s]])

        def pe_front(h):
            s = h % 2
            for c in range(32):
                b = (c // 8) % 2
                kb.tr(ptr[b][0:64, (c % 8) * 128:(c % 8 + 1) * 128], Fb[s][:, c * 64:(c + 1) * 64], identb[:], [BFb[s], Bc], [Bptr[b]])
                if c % 8 == 7:
                    kb.cp("act", kendT[:, c - 7:c + 1, :], ptr[b][0:64, :].rearrange("p (c k) -> p c k", c=8), [Bptr[b]], [BkT])
            for grp in range(4):
                for i in range(8):
                    c = grp * 8 + i
                    kb.mm(patt[0:64, i * 64:(i + 1) * 64], Eb[s][:, c * 64:(c + 1) * 64], Cb[s][:, c * 64:(c + 1) * 64], True, True,
                          [BEb[s], BCb[s]], [Bpatt])
                kb.tt("dve", attm[:, grp * 512:(grp + 1) * 512], patt[0:64, :], tri[:], ALU.mult, [Bpatt, Bc], [Batt])
            kb.memset("dve", U3[:, :, 0:1], 0.0, [BU])
            for grp in range(8):
                b = grp % 2
                for i in range(4):
                    c = grp * 4 + i
                    kb.mm(pupd[b][:, i * 128:(i + 1) * 128], kendT[:, c, :], vtk[s][:, c, :], True, True, [BkT, Bvtk[s]], [Bpupd[b]])
                kb.cp("act" if b == 0 else "dve", U3[:, :, grp * 4 + 1:grp * 4 + 5].rearrange("p v c -> p c v"),
                      pupd[b][:].rearrange("p (c v) -> p c v", c=4), [Bpupd[b]], [BU])
            kb.cp("act", Dk3[:, :, 1:33], ebend[s][:].unsqueeze(1).to_broadcast([128, 128, 32]), [Beb[s]], [BDk])
            P.op("dve", lambda: nc.vector.tensor_tensor_scan(out=St[:], data0=Dk[:], data1=U[:], initial=0.0, op0=ALU.mult, op1=ALU.add),
                 [BDk, BU], [BSt])
            kb.cp("act", Stc[:], St3[:, :, 0:32].rearrange("p v c -> p c v"), [BSt], [BStc])

        def pe_back(h):
            s = h % 2
            for grp in range(4):
                b = grp % 2
                for i in range(8):
                    c = grp * 8 + i
                    kb.mm(po[b][:, i * 64:(i + 1) * 64], vtk[s][:, c, :], attm[:, c * 64:(c + 1) * 64], True, False, [Bvtk[s], Batt], [Bpo[b]])
                    kb.mm(po[b][:, i * 64:(i + 1) * 64], Stc[:, c, :], Cb[s][:, c * 64:(c + 1) * 64], False, True, [BStc, BCb[s]], [Bpo[b]])
                kb.cp("act", o_h[:, grp * 512:(grp + 1) * 512], po[b][:], [Bpo[b]], [Boh])
            for cc in range(4):
                cs = slice(cc * 512, (cc + 1) * 512)
                kb.act(sq[:], o_h[:, cs], AF.Square, [Boh], [Bsq])
                kb.mm(psq[:], ones[:], sq[:], True, True, [Bon, Bsq], [Bpsq])
                kb.ts("dve", rs[:], psq[:], 1.0 / 128, ALU.mult, [Bpsq], [Brs], s2=EPS, op1=ALU.add)
                kb.act(rs[:], rs[:], AF.Sqrt, [Brs], [Brs])
                kb.recip(rs[:], rs[:], [Brs], [Brs])
                kb.stt(tmp[:], o_h[:, cs], ng[:, 0:1], rs[:], ALU.mult, ALU.mult, [Boh, Bc, Brs], [Btmp])
                kb.tt("dve", oTh[s][:, cs], tmp[:], gs[s][:, cs], ALU.mult, [Btmp, Bgs[s]], [BoT[s]])
            kb.ld(act_dst(act_d, h), tg8(oTh[s][:]), [BoT[s]], (), owner=BoT[s])
        load_h(0)
        ew(0)
        for h in range(16):
            load_h(h + 1)
            pe_front(h)
            ew(h + 1)
            pe_back(h)
        P.end_phase()


LAYER_NIN = {0: 3072, 1: 6144, 2: 8192, 3: 4176}
MIX_NAMES = {0: "swa", 1: "diff", 2: "hgrn", 3: "dsa"}


def build_program(layers, stop_after=None):
    nc = bass.Bass("TRN2", target_bir_lowering=False)
    dr = {}

    def ext(name, shape, dt=F32):
        dr[name] = nc.dram_tensor(name, list(shape), dt, kind="ExternalInput").ap()
        return dr[name]

    x_in = ext("x", [S, D])
    y_out = nc.dram_tensor("y", [S, D], F32, kind="ExternalOutput").ap()
    normg = ext("norm_g", [16, D])
    ident_d = ext("ident", [128, 128])
    i8_d = ext("i8", [128, 128])
    for l in layers:
        ext("w_in%d" % l, [D, LAYER_NIN[l]])
        ext("w_out%d" % l, [D, D])
        ext("w_up%d" % l, [D, 2 * DFF])
        ext("w_down%d" % l, [DFF, D])
        ext("conv%d" % l, [128, 88, 3])
    if 0 in layers:
        ext("bias_sw", [128, 32 * 256])
        ext("sinkrep", [1, 4096])
    if 1 in layers or 3 in layers:
        ext("biasn", [128, 32 * 256])
        ext("table", [32, 32])
    if 1 in layers:
        ext("diff_lambda", [4, 64])
        ext("diff_subg", [1, 128])
    xres = nc.dram_tensor("xres", [S, D], F32).ap()
    m_d = nc.dram_tensor("m_d", [S, D], F32).ap()
    act_d = nc.dram_tensor("act_d", [8, 128, KFF, 256], BF16).ap()
    qkv_d = nc.dram_tensor("qkv_d", [64, 128, S], BF16).ap()
    vtok_d = nc.dram_tensor("vtok_d", [S, 2048], BF16).ap()
    if 2 in layers:
        ext("lbl", [128, 64])
        ext("hgrn_ng", [1, 128])
        ext("tri", [64, 512])
        hq_d = nc.dram_tensor("hq_d", [16, 128, S], F32).ap()
        hf_d = nc.dram_tensor("hf_d", [16, 128, S], F32).ap()
        hk_d = nc.dram_tensor("hk_d", [16, 128, S], F32).ap()
        hv_d = nc.dram_tensor("hv_d", [S, 2048], BF16).ap()
    if 3 in layers:
        ext("cmask", [128, 128])
        wi_d = nc.dram_tensor("wi_d", [S, 16], F32).ap()
        mask_d = nc.dram_tensor("mask_d", [16, 128, S], BF16).ap()

    with ExitStack() as top:
        P = Prog(nc, top)
        kb = KB(nc, P)
        x_cur = x_in
        pend_m = None
        for li, l in enumerate(layers):
            with ExitStack() as hs:
                hT = kb.sb(hs, [128, 16, S], BF16, "hT")
                BhT = Buf("hT")
                phase_rn(kb, x_cur, xres, m_d if pend_m is not None else None,
                         normg[pend_m] if pend_m is not None else None, normg[4 * l + 0], hT, BhT, ident_d)
                if pend_m is not None:
                    x_cur = xres
                w_in = dr["w_in%d" % l]
                if l == 0:
                    tiles = [(i * 128, 128) for i in range(20)]
                    phase_proj_fm(kb, hT, BhT, w_in, tiles, epi_store_fm(kb, qkv_d, lambda ti: ti), epi_store_setup(kb))
                    phase_proj_tok(kb, hT, BhT, w_in, [(2560, 512)], epi_store_tok(kb, vtok_d, lambda gi: 0), epi_tok_setup(kb))
                elif l == 1:
                    tiles = [(i * 128, 128) for i in range(32)]
                    phase_proj_fm(kb, hT, BhT, w_in, tiles, epi_store_fm(kb, qkv_d, lambda ti: ti), epi_store_setup(kb))
                    phase_proj_tok(kb, hT, BhT, w_in, [(4096 + i * 512, 512) for i in range(4)],
                                   epi_store_tok(kb, vtok_d, lambda gi: gi * 512), epi_tok_setup(kb))
                elif l == 2:
                    hgrn_inproj(kb, hT, BhT, w_in, dr["lbl"], l, hq_d, hf_d, hk_d, hv_d, qkv_d)
                elif l == 3:
                    tiles = [(i * 128, 128) for i in range(20)] + [(3072 + i * 128, 128) for i in range(8)] + [(4096, 64)]
                    phase_proj_fm(kb, hT, BhT, w_in, tiles, epi_store_fm(kb, qkv_d, lambda ti: ti), epi_store_setup(kb))

                    def setup3(es):
                        c_ = epi_tok_setup(kb)(es)
                        c_["tf"] = [kb.sb(es, [128, 16], F32, "etf") for _ in range(2)]
                        c_["Bf"] = kb.P.bufs(2, "etf")
                        return c_

                    def epi3(ctx, gi, t, c0, w, pb, Bpb):
                        if gi == 0:
                            epi_store_tok(kb, vtok_d, lambda gi_: 0)(ctx, gi, t, c0, w, pb, Bpb)
                        else:
                            tl, B = ctx["tf"][t % 2], ctx["Bf"][t % 2]
                            kb.cp("dve", tl[:, 0:16], pb[:, 0:16], [Bpb], [B])
                            kb.ld(wi_d[t * 128:(t + 1) * 128, :], tl[:, 0:16], [B], (), owner=B)
                    phase_proj_tok(kb, hT, BhT, w_in, [(2560, 512), (4160, 16)], epi3, setup3)
            if l == 0:
                phase_swa(kb, qkv_d, vtok_d, act_d, dr["bias_sw"], i8_d, dr["sinkrep"])
            elif l == 2:
                phase_hgrn(kb, hq_d, hf_d, hk_d, hv_d, qkv_d, act_d, dr["hgrn_ng"], ident_d, dr["tri"])
            elif l == 3:
                phase_dsa_idx(kb, qkv_d, wi_d, mask_d, dr["cmask"], ident_d)
                phase_dsa_attn(kb, qkv_d, vtok_d, act_d, mask_d, dr["biasn"], i8_d, dr["table"])
            elif l == 1:
                phase_diff(kb, qkv_d, vtok_d, act_d, dr["biasn"], i8_d, dr["table"], dr["diff_lambda"], dr["diff_subg"], l)
            phase_down(kb, dr["w_out%d" % l], 16, act_d, m_d)
            with ExitStack() as hs:
                hT = kb.sb(hs, [128, 16, S], BF16, "hT")
                BhT = Buf("hT")
                phase_rn(kb, x_cur, xres, m_d, normg[4 * l + 1], normg[4 * l + 2], hT, BhT, ident_d)
                x_cur = xres
                phase_up(kb, hT, BhT, dr["w_up%d" % l], dr["conv%d" % l], act_d)
            phase_down(kb, dr["w_down%d" % l], KFF, act_d, m_d)
            pend_m = 4 * l + 3
        phase_rn(kb, x_cur, y_out, m_d, normg[pend_m], None, None, None, ident_d)
    return nc


def _bucket(n):
    n = np.maximum(n, 0)
    max_exact = 16
    lr = np.log(np.maximum(n, 1).astype(np.float32) / max_exact) / math.log(128 / max_exact)
    large = np.minimum(max_exact + (lr * (32 - max_exact)).astype(np.int32), 31)
    return np.where(n < max_exact, n, large)


def _host_consts(inputs, layers):
    c = {}
    c["ident"] = np.eye(128, dtype=np.float32)
    c["i8"] = (8.0 * np.eye(128)).astype(np.float32)
    table = np.asarray(inputs["rel_bias_table"], np.float32)
    if 0 in layers:
        s_ = np.arange(128)[:, None]
        q_ = np.arange(256)[None, :]
        dist = q_ - s_
        valid = (dist >= 0) & (dist < 128)
        bk = _bucket(dist)
        t = table[bk]
        t = np.where(valid[:, :, None], t, np.float32(MASKV))
        c["bias_sw"] = np.ascontiguousarray(t.transpose(0, 2, 1)).reshape(128, 32 * 256).astype(np.float32)
        c["sinkrep"] = np.ascontiguousarray(np.repeat(np.asarray(inputs["swa_sinks"], np.float32)[0], 128)[None, :])
    if 1 in layers or 3 in layers:
        s_ = np.arange(128)[:, None]
        q_ = np.arange(256)[None, :]
        dist = q_ - s_
        t = table[_bucket(dist)]
        t = np.where((dist >= 0)[:, :, None], t, np.float32(MASKV))
        c["biasn"] = np.ascontiguousarray(t.transpose(0, 2, 1)).reshape(128, 32 * 256).astype(np.float32)
        c["table"] = np.ascontiguousarray(table)
    if 2 in layers:
        lg = np.asarray(inputs["hgrn_lb_logits"], np.float32)
        c["lbl"] = np.ascontiguousarray(lg.reshape(4, 16, 128).transpose(2, 1, 0)).reshape(128, 64)
        c["hgrn_ng"] = np.ascontiguousarray(np.asarray(inputs["hgrn_norm_g"], np.float32)[0][None, :])
        tr_ = (np.arange(64)[:, None] <= np.arange(64)[None, :]).astype(np.float32)
        c["tri"] = np.ascontiguousarray(np.tile(tr_, (1, 8)))
    if 3 in layers:
        c["cmask"] = np.where(np.arange(128)[None, :] > np.arange(128)[:, None], np.float32(-1.0e30), np.float32(0.0)).astype(np.float32)
    if 1 in layers:
        c["diff_lambda"] = np.ascontiguousarray(np.asarray(inputs["diff_lambda"], np.float32)[0])
        c["diff_subg"] = np.ascontiguousarray(np.asarray(inputs["diff_subln_g"], np.float32)[0][None, :])
    return c


_W_KEYS = {0: ("swa_w_in", "swa_w_out"), 1: ("diff_w_in", "diff_w_out"), 2: ("hgrn_w_in", "hgrn_w_out"), 3: ("dsa_w_in", "dsa_w_out")}
LAUNCHES = [[0, 1, 2, 3]]
N_CORES = 4


def _layer_inputs(inputs, layers):
    m = {}
    m["norm_g"] = np.ascontiguousarray(np.asarray(inputs["norm_g"], np.float32).reshape(16, D))
    for l in layers:
        kin, kout = _W_KEYS[l]
        m["w_in%d" % l] = np.ascontiguousarray(np.asarray(inputs[kin], np.float32)[0])
        m["w_out%d" % l] = np.ascontiguousarray(np.asarray(inputs[kout], np.float32)[0])
        m["w_up%d" % l] = np.ascontiguousarray(np.asarray(inputs["ffn_w_up"], np.float32)[l])
        m["w_down%d" % l] = np.ascontiguousarray(np.asarray(inputs["ffn_w_down"], np.float32)[l])
        cv = np.asarray(inputs["ffn_conv"], np.float32)[l]
        m["conv%d" % l] = np.ascontiguousarray(cv.reshape(3, 88, 128).transpose(2, 1, 0))
    m.update(_host_consts(inputs, layers))
    return m


def run_launch(inputs, x, layers, cores=None):
    nc = build_program(layers)
    shared = _layer_inputs(inputs, layers)
    B = x.shape[0]
    cores = cores if cores is not None else N_CORES
    in_maps = []
    for c in range(cores):
        d = dict(shared)
        d["x"] = np.ascontiguousarray(x[c % B])
        in_maps.append(d)
    res = run_bass_kernel_spmd(nc, in_maps, core_ids=list(range(cores)))
    return np.stack([res.results[b]["y"] for b in range(B)], axis=0)


def kernel(**inputs):
    x = np.asarray(inputs["x"], np.float32)
    for layers in LAUNCHES:
        x = run_launch(inputs, x, layers)
    return x.astype(np.float32)
```

```python
import math
from contextlib import ExitStack

import numpy as np
import concourse.bass as bass
import concourse.mybir as mybir
from concourse.bass_utils import run_bass_kernel_spmd

F32 = mybir.dt.float32
BF16 = mybir.dt.bfloat16
AF = mybir.ActivationFunctionType
ALU = mybir.AluOpType
AX = mybir.AxisListType

ENGS = ("pe", "act", "dve", "pool", "sp")
CENGS = ("pe", "act", "dve", "pool")

S = 2048
D = 2048
NT = 16
DFF = 5632
KFF = 44
EPS = 1e-6
MASKV = -30000.0
N_DSEM = 56


class Buf:
    __slots__ = ("name", "w", "r", "dsem")

    def __init__(self, name):
        self.name = name
        self.w = None
        self.r = {}
        self.dsem = None


class Prog:
    def __init__(self, nc, es):
        self.nc = nc
        self.ops = {e: [] for e in ENGS}
        self.sems = {}
        self.tot = {}
        self.seen = {e: {} for e in ENGS}
        for e in CENGS:
            self.sems[e] = es.enter_context(nc.semaphore("c_" + e))
            self.tot[e] = 0
        self.dfree = []
        for i in range(N_DSEM):
            k = "d%d" % i
            self.sems[k] = es.enter_context(nc.semaphore(k))
            self.tot[k] = 0
            self.dfree.append(k)
        self.dused = []
        self.phase_bufs = []
        self.nphase = 0

    def buf(self, name="b"):
        b = Buf(name)
        self.phase_bufs.append(b)
        return b

    def bufs(self, n, name="b"):
        return [self.buf(name + str(i)) for i in range(n)]

    def _dsem(self, b):
        if b.dsem is None:
            b.dsem = self.dfree.pop()
            self.dused.append(b.dsem)
        return b.dsem

    def _deps(self, eng, reads, writes):
        deps = {}

        def add(ev):
            if ev is None:
                return
            k, v = ev
            if v > deps.get(k, 0):
                deps[k] = v
        for b in reads:
            add(b.w)
        for b in writes:
            add(b.w)
            for k, v in b.r.items():
                add((k, v))
        waits = []
        for k, v in deps.items():
            if k == eng and eng == "pe":
                continue
            if k not in CENGS:
                v = self.tot[k]
            if v > self.seen[eng].get(k, 0):
                self.seen[eng][k] = v
                waits.append((k, v))
        return waits

    def op(self, eng, fn, reads=(), writes=()):
        waits = self._deps(eng, reads, writes)
        self.tot[eng] += 1
        seq = self.tot[eng]
        self.ops[eng].append((waits, fn, eng, 1))
        for b in reads:
            if b.r.get(eng, 0) < seq:
                b.r[eng] = seq
        for b in writes:
            b.w = (eng, seq)
            b.r = {}

    def dma(self, q, fn, reads=(), writes=(), owner=None):
        waits = self._deps(q, reads, writes)
        if owner is None:
            owner = writes[0] if writes else reads[0]
        k = self._dsem(owner)
        self.tot[k] += 16
        val = self.tot[k]
        self.ops[q].append((waits, fn, k, 16))
        for b in reads:
            b.r[k] = val
        for b in writes:
            b.w = (k, val)
            b.r = {}

    def end_phase(self):
        nc = self.nc
        keys = list(CENGS) + list(self.dused)
        for e in ENGS:
            waits = []
            for k in keys:
                if k == e:
                    continue
                v = self.tot[k]
                if v > self.seen[e].get(k, 0):
                    self.seen[e][k] = v
                    waits.append((k, v))
            self.ops[e].append((waits, None, None, 0))
        with nc.Block() as block:
            def mk(ename, attr):
                lst = self.ops[ename]

                def body(e):
                    for waits, fn, k, inc in lst:
                        for wk, wv in waits:
                            e.wait_ge(self.sems[wk], wv)
                        if fn is not None:
                            fn().then_inc(self.sems[k], inc)
                getattr(block, attr)(body)
            mk("sp", "sync")
            mk("pe", "tensor")
            mk("act", "scalar")
            mk("dve", "vector")
            mk("pool", "gpsimd")
        self.ops = {e: [] for e in ENGS}
        for b in self.phase_bufs:
            b.dsem = None
            b.w = None
            b.r = {}
        self.phase_bufs = []
        self.dfree.extend(self.dused)
        self.dused = []
        self.nphase += 1


class KB:
    def __init__(self, nc, P):
        self.nc = nc
        self.P = P
        self.uid = 0

    def sb(self, es, shape, dt, name="t"):
        self.uid += 1
        return es.enter_context(self.nc.sbuf_tensor("%s_%d" % (name, self.uid), list(shape), dt))

    def ps(self, es, shape, dt, name="p"):
        self.uid += 1
        return es.enter_context(self.nc.psum_tensor("%s_%d" % (name, self.uid), list(shape), dt))

    def mm(self, out, lhsT, rhs, start, stop, r, w):
        nc = self.nc
        self.P.op("pe", lambda: nc.tensor.matmul(out, lhsT=lhsT, rhs=rhs, start=start, stop=stop), r, w)

    def tr(self, out, in_, ident, r, w):
        nc = self.nc
        self.P.op("pe", lambda: nc.tensor.transpose(out, in_, ident), r, w)

    def act(self, out, in_, func, r, w, bias=None, scale=None, accum=None):
        nc = self.nc
        kw = {}
        if bias is not None:
            kw["bias"] = bias
        if scale is not None:
            kw["scale"] = scale
        if accum is not None:
            kw["accum_out"] = accum
        self.P.op("act", lambda: nc.scalar.activation(out=out, in_=in_, func=func, **kw), r, w)

    def ts(self, eng, out, in0, s1, op0, r, w, s2=None, op1=None):
        e = self.nc.vector if eng == "dve" else self.nc.gpsimd
        if op1 is None:
            self.P.op(eng, lambda: e.tensor_scalar(out=out, in0=in0, scalar1=s1, scalar2=None, op0=op0), r, w)
        else:
            self.P.op(eng, lambda: e.tensor_scalar(out=out, in0=in0, scalar1=s1, scalar2=s2, op0=op0, op1=op1), r, w)

    def tt(self, eng, out, in0, in1, op, r, w):
        e = self.nc.vector if eng == "dve" else self.nc.gpsimd
        self.P.op(eng, lambda: e.tensor_tensor(out=out, in0=in0, in1=in1, op=op), r, w)

    def stt(self, out, in0, scalar, in1, op0, op1, r, w):
        nc = self.nc
        self.P.op("dve", lambda: nc.vector.scalar_tensor_tensor(out=out, in0=in0, scalar=scalar, in1=in1, op0=op0, op1=op1), r, w)

    def cp(self, eng, out, in_, r, w):
        nc = self.nc
        if eng == "act":
            self.P.op("act", lambda: nc.scalar.copy(out=out, in_=in_), r, w)
        elif eng == "dve":
            self.P.op("dve", lambda: nc.vector.tensor_copy(out=out, in_=in_), r, w)
        else:
            self.P.op("pool", lambda: nc.gpsimd.tensor_copy(out=out, in_=in_), r, w)

    def recip(self, out, in_, r, w):
        nc = self.nc
        self.P.op("dve", lambda: nc.vector.reciprocal(out=out, in_=in_), r, w)

    def memset(self, eng, ap, val, w):
        e = self.nc.vector if eng == "dve" else self.nc.gpsimd
        self.P.op(eng, lambda: e.memset(ap, val), (), w)

    def ld(self, out, in_, r, w, q="sp", owner=None):
        nc = self.nc
        if q == "sp":
            self.P.dma("sp", lambda: nc.sync.dma_start(out=out, in_=in_), r, w, owner)
        else:
            self.P.dma("pool", lambda: nc.gpsimd.dma_start(out=out, in_=in_), r, w, owner)

    def rstd(self, es_tiles, ss, r, w):
        self.act(ss, ss, AF.Sqrt, list(r) + [self.Beps], w, bias=self.eps_ap, scale=1.0 / D)
        self.recip(ss, ss, r, w)


def phase_rn(kb, x_src, x_dst, m_src, gpost, gpre, hT, BhT, ident_d):
    nc, P = kb.nc, kb.P
    NS = 3
    with ExitStack() as es:
        xt = [kb.sb(es, [128, D], F32, "xt") for _ in range(NS)]
        Bxt = P.bufs(NS, "xt")
        junk = [kb.sb(es, [128, D], BF16, "junk") for _ in range(2)]
        Bjunk = P.bufs(2, "junk")
        st = [kb.sb(es, [128, 2], F32, "st") for _ in range(NS)]
        epst = kb.sb(es, [128, 1], F32, "epst")
        kb.Beps = P.buf("eps")
        kb.memset("dve", epst[:], EPS, [kb.Beps])
        kb.eps_ap = epst[:, 0:1]
        Bst0 = P.bufs(NS, "st0")
        Bst1 = P.bufs(NS, "st1")
        if m_src is not None:
            mt = [kb.sb(es, [128, D], F32, "mt") for _ in range(NS)]
            Bmt = P.bufs(NS, "mt")
            gp = kb.sb(es, [128, D], F32, "gp")
            Bgp = P.buf("gp")
            kb.ld(gp[:], gpost.partition_broadcast(128), (), [Bgp])
        if gpre is not None:
            gq = kb.sb(es, [128, D], F32, "gq")
            Bgq = P.buf("gq")
            kb.ld(gq[:], gpre.partition_broadcast(128), (), [Bgq])
            hb = [kb.sb(es, [128, D], BF16, "hb") for _ in range(2)]
            Bhb = P.bufs(2, "hb")
            ident = kb.sb(es, [128, 128], BF16, "ident")
            Bid = P.buf("ident")
            kb.ld(ident[:], ident_d, (), [Bid], q="pool")
            pT = [kb.ps(es, [128, 1024], BF16, "pT") for _ in range(4)]
            BpT = P.bufs(4, "pT")

        def load_t(t_):
            if t_ >= NT:
                return
            rows_ = slice(t_ * 128, (t_ + 1) * 128)
            kb.ld(xt[t_ % NS][:], x_src[rows_, :], (), [Bxt[t_ % NS]])
            if m_src is not None:
                kb.ld(mt[t_ % NS][:], m_src[rows_, :], (), [Bmt[t_ % NS]])

        def stage1(t):
            if t >= NT or m_src is None:
                return
            s = t % NS
            rows = slice(t * 128, (t + 1) * 128)
            kb.act(junk[0][:], mt[s][:], AF.Square, [Bmt[s]], [Bjunk[0], Bst0[s]], accum=st[s][:, 0:1])
            kb.rstd(None, st[s][:, 0:1], [Bst0[s]], [Bst0[s]])
            kb.stt(mt[s][:], mt[s][:], st[s][:, 0:1], gp[:], ALU.mult, ALU.mult, [Bmt[s], Bst0[s], Bgp], [Bmt[s]])
            kb.tt("dve", xt[s][:], xt[s][:], mt[s][:], ALU.add, [Bxt[s], Bmt[s]], [Bxt[s]])
            kb.ld(x_dst[rows, :], xt[s][:], [Bxt[s]], (), owner=Bxt[s])

        def stage2(t):
            if gpre is None:
                return
            s = t % NS
            hs_ = t % 2
            kb.act(junk[1][:], xt[s][:], AF.Square, [Bxt[s]], [Bjunk[1], Bst1[s]], accum=st[s][:, 1:2])
            kb.rstd(None, st[s][:, 1:2], [Bst1[s]], [Bst1[s]])
            kb.stt(hb[hs_][:], xt[s][:], st[s][:, 1:2], gq[:], ALU.mult, ALU.mult, [Bxt[s], Bst1[s], Bgq], [Bhb[hs_]])
            for half in range(2):
                pi = (2 * t + half) % 4
                for j in range(8):
                    i = half * 8 + j
                    kb.tr(pT[pi][:, j * 128:(j + 1) * 128], hb[hs_][:, i * 128:(i + 1) * 128], ident[:], [Bhb[hs_], Bid], [BpT[pi]])
                src = pT[pi][:].rearrange("p (j q) -> p j q", j=8)
                dst = hT[:, half * 8:(half + 1) * 8, t * 128:(t + 1) * 128]
                kb.cp("act", dst, src, [BpT[pi]], [P.buf("hTw")])
        load_t(0)
        load_t(1)
        stage1(0)
        for t in range(NT):
            load_t(t + 2)
            stage1(t + 1)
            stage2(t)
        P.end_phase()


def phase_proj_fm(kb, hT, BhT, w_d, coltiles, epilogue, epi_setup=None):
    nc, P = kb.nc, kb.P
    wv = w_d.rearrange("(kc p) n -> p kc n", p=128)
    with ExitStack() as es:
        NSL = 3
        wsl = [kb.sb(es, [128, 16, 256], BF16, "wsl") for _ in range(NSL)]
        Bw = P.bufs(NSL, "wsl")
        pb = [kb.ps(es, [128, 2048], F32, "pb") for _ in range(2)]
        Bpb = P.bufs(2, "pb")
        ctx = epi_setup(es) if epi_setup is not None else None
        slabs = []
        i = 0
        while i < len(coltiles):
            c0, w0 = coltiles[i]
            if i + 1 < len(coltiles) and coltiles[i + 1][0] == c0 + w0 and w0 == 128:
                slabs.append([coltiles[i], coltiles[i + 1]])
                i += 2
            else:
                slabs.append([coltiles[i]])
                i += 1
        ti = 0

        def load_slab(si):
            if si >= len(slabs):
                return
            s_ = si % NSL
            c0_ = slabs[si][0][0]
            wtot = sum(w for _, w in slabs[si])
            kb.ld(wsl[s_][:, :, 0:wtot], wv[:, :, c0_:c0_ + wtot], (), [Bw[s_]], q="pool")
        load_slab(0)
        load_slab(1)
        for si, sl in enumerate(slabs):
            s = si % NSL
            load_slab(si + 2)
            off = 0
            for (cc, w) in sl:
                pi = ti % 2
                for g in range(4):
                    for k in range(16):
                        kb.mm(pb[pi][0:w, g * 512:(g + 1) * 512], wsl[s][:, k, off:off + w], hT[:, k, g * 512:(g + 1) * 512],
                              k == 0, k == 15, [Bw[s], BhT], [Bpb[pi]])
                epilogue(ctx, ti, cc, w, pb[pi], Bpb[pi])
                off += w
                ti += 1
        P.end_phase()


def phase_proj_tok(kb, hT, BhT, w_d, colgroups, epilogue, epi_setup=None):
    nc, P = kb.nc, kb.P
    wv = w_d.rearrange("(kc p) n -> p kc n", p=128)
    with ExitStack() as es:
        wg = [kb.sb(es, [128, 16, 512], BF16, "wg") for _ in range(2)]
        Bwg = P.bufs(2, "wg")
        pb = [kb.ps(es, [128, 512], F32, "pk") for _ in range(4)]
        Bpb = P.bufs(4, "pk")
        ctx = epi_setup(es) if epi_setup is not None else None
        n = 0

        def load_g(gi):
            if gi >= len(colgroups):
                return
            c0_, w_ = colgroups[gi]
            kb.ld(wg[gi % 2][:, :, 0:w_], wv[:, :, c0_:c0_ + w_], (), [Bwg[gi % 2]], q="pool")
        load_g(0)
        for gi, (c0, w) in enumerate(colgroups):
            s = gi % 2
            load_g(gi + 1)
            for t in range(NT):
                pi = n % 4
                n += 1
                for k in range(16):
                    kb.mm(pb[pi][:, 0:w], hT[:, k, t * 128:(t + 1) * 128], wg[s][:, k, 0:w], k == 0, k == 15, [BhT, Bwg[s]], [Bpb[pi]])
                epilogue(ctx, gi, t, c0, w, pb[pi], Bpb[pi])
        P.end_phase()


def phase_up(kb, hT, BhT, w_up, conv_d, act_d):
    nc, P = kb.nc, kb.P
    wv = w_up.rearrange("(kc p) n -> p kc n", p=128)
    with ExitStack() as es:
        NSL = 3
        wsl = [kb.sb(es, [128, 16, 512], BF16, "wup") for _ in range(NSL)]
        Bw = P.bufs(NSL, "wup")
        cw = kb.sb(es, [128, 88, 3], F32, "cw")
        Bcw = P.buf("cw")
        kb.ld(cw[:], conv_d, (), [Bcw])
        psg = kb.ps(es, [128, 2048], F32, "psg")
        psv = kb.ps(es, [128, 2048], F32, "psv")
        Bpsg, Bpsv = P.buf("psg"), P.buf("psv")
        ub = [kb.sb(es, [128, 2050], F32, "ub") for _ in range(4)]
        Bub = P.bufs(4, "ub")
        cb = [kb.sb(es, [128, 2048], F32, "cb") for _ in range(4)]
        Bcb = P.bufs(4, "cb")
        ob = [kb.sb(es, [128, 2048], BF16, "ob") for _ in range(2)]
        Bob = P.bufs(2, "ob")
        Bact = P.buf("actd")
        for i in range(4):
            kb.memset("pool", ub[i][:, 0:2], 0.0, [Bub[i]])
        def load_up(sl_):
            if sl_ >= KFF // 2:
                return
            s_ = sl_ % NSL
            c_ = sl_ * 2
            kb.ld(wsl[s_][:, :, 0:256], wv[:, :, c_ * 128:c_ * 128 + 256], (), [Bw[s_]], q="pool")
            kb.ld(wsl[s_][:, :, 256:512], wv[:, :, DFF + c_ * 128:DFF + c_ * 128 + 256], (), [Bw[s_]], q="pool")
        load_up(0)
        load_up(1)
        for c in range(KFF):
            sl = c // 2
            s = sl % NSL
            if c % 2 == 0:
                load_up(sl + 2)
            par = c % 2
            for part, (pp, Bpp) in enumerate(((psg, Bpsg), (psv, Bpsv))):
                off = part * 256 + par * 128
                for g in range(4):
                    for k in range(16):
                        kb.mm(pp[:, g * 512:(g + 1) * 512], wsl[s][:, k, off:off + 128], hT[:, k, g * 512:(g + 1) * 512],
                              k == 0, k == 15, [Bw[s], BhT], [Bpp])
                u = par * 2 + part
                fi = part * KFF + c
                kb.cp("act", ub[u][:, 2:2050], pp[:], [Bpp], [Bub[u]])
                kb.act(cb[u][:], ub[u][:, 0:2048], AF.Copy, [Bub[u], Bcw], [Bcb[u]], scale=cw[:, fi, 0:1])
                kb.stt(cb[u][:], ub[u][:, 1:2049], cw[:, fi, 1:2], cb[u][:], ALU.mult, ALU.add, [Bub[u], Bcw, Bcb[u]], [Bcb[u]])
                kb.stt(cb[u][:], ub[u][:, 2:2050], cw[:, fi, 2:3], cb[u][:], ALU.mult, ALU.add, [Bub[u], Bcw, Bcb[u]], [Bcb[u]])
                if part == 0:
                    kb.act(cb[u][:], cb[u][:], AF.Silu, [Bcb[u]], [Bcb[u]])
            ug, uv = par * 2, par * 2 + 1
            kb.tt("dve", ob[par][:], cb[ug][:], cb[uv][:], ALU.mult, [Bcb[ug], Bcb[uv]], [Bob[par]])
            kb.ld(act_d[c], ob[par][:], [Bob[par]], (), owner=Bob[par])
        P.end_phase()


def phase_down(kb, w_d, KC, act_d, m_d):
    nc, P = kb.nc, kb.P
    wv = w_d.rearrange("(kc p) n -> p kc n", p=128)
    av = act_d.rearrange("c p s -> p c s")
    TG = 256
    with ExitStack() as es:
        wres = [kb.sb(es, [128, KC, 512], BF16, "wres") for _ in range(2)]
        Bwr = [P.bufs(4, "wr%d_" % i) for i in range(2)]
        slab = [kb.sb(es, [128, KC, TG], BF16, "slab") for _ in range(2)]
        Bsl = P.bufs(2, "slab")
        pb = [kb.ps(es, [128, 512], F32, "pd") for _ in range(4)]
        Bpb = P.bufs(4, "pd")
        ot = [kb.sb(es, [128, 512], F32, "ot") for _ in range(3)]
        Bot = P.bufs(3, "ot")
        Bm = P.buf("md")
        kq = [(i * KC) // 4 for i in range(5)]
        n = 0
        ns = 0
        NTG = S // TG

        def load_w(cg_):
            if cg_ >= 4:
                return
            for pq_ in range(4):
                kb.ld(wres[cg_ % 2][:, kq[pq_]:kq[pq_ + 1], :], wv[:, kq[pq_]:kq[pq_ + 1], cg_ * 512:(cg_ + 1) * 512], (),
                      [Bwr[cg_ % 2][pq_]], q="pool")

        def load_slab(i_):
            if i_ >= 4 * NTG:
                return
            tg_ = i_ % NTG
            kb.ld(slab[i_ % 2][:], av[:, 0:KC, tg_ * TG:(tg_ + 1) * TG], (), [Bsl[i_ % 2]])
        load_slab(0)
        load_w(0)
        for cg in range(4):
            ws = cg % 2
            load_w(cg + 1)
            for tg in range(NTG):
                ss = ns % 2
                ns += 1
                load_slab(ns)
                for tt_ in range(TG // 128):
                    pi = n % 4
                    oi = n % 3
                    n += 1
                    for c in range(KC):
                        pq = 0
                        while c >= kq[pq + 1]:
                            pq += 1
                        kb.mm(pb[pi][:], slab[ss][:, c, tt_ * 128:(tt_ + 1) * 128], wres[ws][:, c, :], c == 0, c == KC - 1,
                              [Bsl[ss], Bwr[ws][pq]], [Bpb[pi]])
                    kb.cp("act" if n % 2 == 0 else "dve", ot[oi][:], pb[pi][:], [Bpb[pi]], [Bot[oi]])
                    r0 = tg * TG + tt_ * 128
                    kb.ld(m_d[r0:r0 + 128, cg * 512:(cg + 1) * 512], ot[oi][:], [Bot[oi]], (), owner=Bot[oi])
        P.end_phase()


def epi_store_setup(kb, n=2):
    def setup(es):
        P = kb.P
        return {"t": [kb.sb(es, [128, 2048], BF16, "eo") for _ in range(n)], "B": P.bufs(n, "eo"), "Bd": P.buf("qkvd"), "n": 0}
    return setup


def epi_store_fm(kb, qkv_d, tile_of):
    def epi(ctx, ti, c0, w, pb, Bpb):
        i = ctx["n"] % len(ctx["t"])
        ctx["n"] += 1
        t, B = ctx["t"][i], ctx["B"][i]
        kb.cp("act" if ti % 2 == 0 else "dve", t[0:w, :], pb[0:w, :], [Bpb], [B])
        kb.ld(qkv_d[tile_of(ti), 0:w, :], t[0:w, :], [B], (), owner=B)
    return epi


def epi_tok_setup(kb, dt=BF16):
    def setup(es):
        P = kb.P
        return {"t": [kb.sb(es, [128, 512], dt, "et") for _ in range(3)], "B": P.bufs(3, "et"), "Bd": P.buf("vtokd"), "n": 0}
    return setup


def epi_store_tok(kb, vtok_d, col_of):
    def epi(ctx, gi, t, c0, w, pb, Bpb):
        i = ctx["n"] % 3
        ctx["n"] += 1
        tl, B = ctx["t"][i], ctx["B"][i]
        kb.cp("act" if ctx["n"] % 2 == 0 else "dve", tl[:, 0:w], pb[:, 0:w], [Bpb], [B])
        d0 = col_of(gi)
        kb.ld(vtok_d[t * 128:(t + 1) * 128, d0:d0 + w], tl[:, 0:w], [B], (), owner=B)
    return epi


def phase_swa(kb, qkv_d, vtok_d, act_d, bias_sw_d, i8_d, sink_d):
    nc, P = kb.nc, kb.P
    with ExitStack() as es:
        vt = kb.sb(es, [128, 16, 512], BF16, "vt")
        Bvt = P.buf("vt")
        kb.ld(vt[:], vtok_d[:, 0:512].rearrange("(t p) c -> p t c", p=128), (), [Bvt])
        bsw = kb.sb(es, [128, 32, 256], BF16, "bsw")
        Bbsw = P.buf("bsw")
        kb.ld(bsw[:], bias_sw_d.rearrange("p (h q) -> p h q", h=32), (), [Bbsw], q="pool")
        i8 = kb.sb(es, [128, 128], BF16, "i8")
        Bi8 = P.buf("i8")
        kb.ld(i8[:], i8_d, (), [Bi8], q="pool")
        ones64 = kb.sb(es, [128, 64], BF16, "ones64")
        Bon = P.buf("ones")
        kb.memset("dve", ones64[:], 1.0, [Bon])
        skf = kb.sb(es, [1, 4096], F32, "skf")
        skb = kb.sb(es, [1, 4096], BF16, "skb")
        Bsk = P.buf("sk")
        kb.ld(skf[:], sink_d, (), [Bsk])
        kb.act(skb[:], skf[:], AF.Exp, [Bsk], [Bsk])
        kh = [kb.sb(es, [64, 2048], BF16, "kh") for _ in range(2)]
        qh = [kb.sb(es, [64, 4, 2048], BF16, "qh") for _ in range(2)]
        Bkq = P.bufs(2, "kq")
        PT = [kb.sb(es, [128, 4, 256], BF16, "PT") for _ in range(3)]
        BPT = P.bufs(3, "PT")
        oT = [kb.sb(es, [64, 4, 2048], BF16, "oT") for _ in range(2)]
        BoT = P.bufs(2, "oT")
        rec = [kb.sb(es, [64, 512], F32, "rec") for _ in range(2)]
        Brec = P.bufs(2, "rec")
        pss = [kb.ps(es, [128, 1024], F32, "pss") for _ in range(2)]
        Bpss = P.bufs(2, "pss")
        pnum = [kb.ps(es, [64, 512], F32, "pnum") for _ in range(2)]
        Bpn = P.bufs(2, "pnum")
        pden = [kb.ps(es, [64, 512], F32, "pden") for _ in range(2)]
        Bpd = P.bufs(2, "pden")
        Bact = P.buf("actd")
        nj = 0
        nb = 0

        def load_kv(kv_):
            if kv_ >= 8:
                return
            s_ = kv_ % 2
            kb.ld(kh[s_][:], qkv_d[16 + kv_ // 2, (kv_ % 2) * 64:(kv_ % 2) * 64 + 64, :], (), [Bkq[s_]])
            for g_ in range(4):
                hq_ = 4 * kv_ + g_
                kb.ld(qh[s_][:, g_, :], qkv_d[hq_ // 2, (hq_ % 2) * 64:(hq_ % 2) * 64 + 64, :], (), [Bkq[s_]])
        load_kv(0)
        for kv in range(8):
            s = kv % 2
            load_kv(kv + 1)

            def stageA(j, kv=kv, s=s, base=nj):
                nq = 256 if j < 15 else 128
                sl = (base + j) % 2
                pt = (base + j) % 3
                for g in range(4):
                    o = pss[sl][:, g * 256:g * 256 + nq]
                    kb.mm(o, kh[s][:, j * 128:(j + 1) * 128], qh[s][:, g, j * 128:j * 128 + nq], True, False, [Bkq[s]], [Bpss[sl]])
                    kb.mm(o, i8[:], bsw[:, 4 * kv + g, 0:nq], False, True, [Bi8, Bbsw], [Bpss[sl]])
                src = pss[sl][:].rearrange("p (g q) -> p g q", g=4)[:, :, 0:nq]
                kb.act(PT[pt][:, :, 0:nq], src, AF.Exp, [Bpss[sl]], [BPT[pt]], scale=0.125)

            def stageB(j, kv=kv, s=s, base=nj, nb0=nb):
                pt = (base + j) % 3
                prev = (base + j - 1) % 3 if j > 0 else None
                b = (nb0 + j) % 2
                rds = [Bvt, BPT[pt]] + ([BPT[prev]] if prev is not None else [])
                kb.mm(pden[b][:], ones64[0:1, 0:64], skb[0:1, 4 * kv * 128:(4 * kv + 4) * 128], True, False, [Bon, Bsk], [Bpd[b]])
                if prev is not None:
                    kb.mm(pnum[b][:], vt[:, j - 1, kv * 64:(kv + 1) * 64], PT[prev][:, :, 128:256], True, False, rds, [Bpn[b]])
                    kb.mm(pden[b][:], ones64[:], PT[prev][:, :, 128:256], False, False, rds + [Bon], [Bpd[b]])
                kb.mm(pnum[b][:], vt[:, j, kv * 64:(kv + 1) * 64], PT[pt][:, :, 0:128], prev is None, True, rds, [Bpn[b]])
                kb.mm(pden[b][:], ones64[:], PT[pt][:, :, 0:128], False, True, rds + [Bon], [Bpd[b]])
                kb.recip(rec[b][:], pden[b][:], [Bpd[b]], [Brec[b]])
                kb.tt("dve", oT[s][:, :, j * 128:(j + 1) * 128], pnum[b][:].rearrange("p (g q) -> p g q", g=4),
                      rec[b][:].rearrange("p (g q) -> p g q", g=4), ALU.mult, [Bpn[b], Brec[b]], [BoT[s]])
            stageA(0)
            for j in range(16):
                if j + 1 < 16:
                    stageA(j + 1)
                stageB(j)
            nj += 16
            nb += 16
            for g in range(4):
                hq = 4 * kv + g
                kb.ld(act_d[hq // 2, (hq % 2) * 64:(hq % 2) * 64 + 64, :], oT[s][:, g, :], [BoT[s]], (), owner=BoT[s])
        P.end_phase()


def phase_diff(kb, qkv_d, vtok_d, act_d, biasn_d, i8_d, table_d, lam_d, subg_d, layer_idx):
    nc, P = kb.nc, kb.P
    lam_init = 0.8 - 0.6 * math.exp(-0.3 * layer_idx)
    with ExitStack() as es:
        bn = kb.sb(es, [128, 32, 256], BF16, "bn")
        Bbn = P.buf("bn")
        kb.ld(bn[:], biasn_d.rearrange("p (h q) -> p h q", h=32), (), [Bbn], q="pool")
        i8 = kb.sb(es, [128, 128], BF16, "i8")
        Bi8 = P.buf("i8")
        kb.ld(i8[:], i8_d, (), [Bi8], q="pool")
        ones = kb.sb(es, [128, 128], BF16, "ones")
        onesf = kb.sb(es, [1, 128], F32, "onesf")
        Bon = P.buf("ones")
        kb.memset("dve", ones[:], 1.0, [Bon])
        kb.memset("dve", onesf[:], 1.0, [Bon])
        c31 = kb.sb(es, [128, 32], F32, "c31")
        Bc31 = P.buf("c31")
        kb.ld(c31[:], table_d[31].partition_broadcast(128), (), [Bc31])
        lamt = kb.sb(es, [1, 256], F32, "lamt")
        lsm = kb.sb(es, [1, 136], F32, "lsm")
        Blam = P.buf("lam")
        kb.ld(lamt[:], lam_d.rearrange("(o r) c -> o (r c)", o=1), (), [Blam])
        kb.tt("dve", lsm[:, 0:64], lamt[:, 0:64], lamt[:, 64:128], ALU.mult, [Blam], [Blam])
        kb.tt("dve", lsm[:, 64:128], lamt[:, 128:192], lamt[:, 192:256], ALU.mult, [Blam], [Blam])
        P.op("dve", lambda: nc.vector.tensor_reduce(out=lsm[:, 128:130], in_=lsm[:, 0:128].rearrange("p (a b) -> p a b", a=2), axis=AX.X, op=ALU.add), [Blam], [Blam])
        kb.act(lsm[:, 130:132], lsm[:, 128:130], AF.Exp, [Blam], [Blam])
        kb.tt("dve", lsm[:, 132:133], lsm[:, 130:131], lsm[:, 131:132], ALU.subtract, [Blam], [Blam])
        kb.ts("dve", lsm[:, 132:133], lsm[:, 132:133], lam_init, ALU.add, [Blam], [Blam])
        kb.cp("dve", lsm[:, 133:134], lsm[:, 132:133], [Blam], [Blam])
        pl = kb.ps(es, [128, 512], F32, "pl")
        Bpl = P.buf("pl")
        kb.mm(pl[:, 0:2], onesf[0:1, :], lsm[0:1, 132:134], True, True, [Bon, Blam], [Bpl])
        neglam = kb.sb(es, [128, 2], F32, "neglam")
        Bnl = P.buf("neglam")
        kb.ts("dve", neglam[:], pl[:, 0:2], -1.0, ALU.mult, [Bpl], [Bnl])
        gsc = kb.sb(es, [128, 1], F32, "gsc")
        Bgsc = P.buf("gsc")
        kb.ld(gsc[:], subg_d.rearrange("o e -> e o"), (), [Bgsc])
        kb.ts("dve", gsc[:], gsc[:], 1.0 - lam_init, ALU.mult, [Bgsc], [Bgsc])
        qh = [kb.sb(es, [128, 2048], BF16, "qh") for _ in range(2)]
        kh = [kb.sb(es, [128, 2048], BF16, "kh") for _ in range(2)]
        vh = [kb.sb(es, [128, 16, 128], BF16, "vh") for _ in range(2)]
        Bin = P.bufs(2, "qkv")
        PT = [kb.sb(es, [128, 512], BF16, "PT") for _ in range(4)]
        BPT = P.bufs(4, "PT")
        oTh = [kb.sb(es, [128, 2048], BF16, "oTh") for _ in range(2)]
        BoT = P.bufs(2, "oTh")
        r1 = kb.sb(es, [128, 512], F32, "r1")
        r2 = kb.sb(es, [128, 512], F32, "r2")
        t1 = kb.sb(es, [128, 512], F32, "t1")
        t2 = kb.sb(es, [128, 512], F32, "t2")
        sq = kb.sb(es, [128, 512], BF16, "sq")
        rs = kb.sb(es, [128, 512], F32, "rs")
        Bw = P.buf("work")
        Bsq = P.buf("sq")
        Brs = P.buf("rs")
        pss = [pl, kb.ps(es, [128, 512], F32, "pss"), kb.ps(es, [128, 512], F32, "pss")]
        Bpss = [Bpl, P.buf("pss"), P.buf("pss")]
        pnum = [kb.ps(es, [128, 512], F32, "pnum") for _ in range(2)]
        pden = [kb.ps(es, [128, 512], F32, "pden") for _ in range(2)]
        Bpn = P.bufs(2, "pn")
        Bpd = P.bufs(2, "pd")
        psq = kb.ps(es, [128, 512], F32, "psq")
        Bpsq = P.buf("psq")

        def load_h(h_):
            if h_ >= 16:
                return
            s_ = h_ % 2
            kb.ld(qh[s_][:], qkv_d[h_], (), [Bin[s_]])
            kb.ld(kh[s_][:], qkv_d[16 + h_], (), [Bin[s_]])
            kb.ld(vh[s_][:], vtok_d[:, h_ * 128:(h_ + 1) * 128].rearrange("(t p) e -> p t e", p=128), (), [Bin[s_]])
        load_h(0)
        base = 0
        NPS = 3
        for h in range(16):
            s = h % 2
            load_h(h + 1)
            its = [(c, m, j) for c in range(4) for m in range(2) for j in range(4 * c + 4)]

            def geom(c, j):
                lo = max(j, 4 * c)
                N = (4 * c + 4 - lo) * 128
                if j >= 4 * c:
                    nn, boff = min(N, 256), 0
                elif j == 4 * c - 1:
                    nn, boff = 128, 128
                else:
                    nn, boff = 0, 0
                return lo, N, nn, boff

            def stageA(idx):
                c, m, j = its[idx]
                lo, N, nn, boff = geom(c, j)
                pr = slice(m * 64, (m + 1) * 64)
                mp = 2 * h + m
                q0 = lo * 128
                sl = (base + idx) % NPS
                pt = (base + idx) % 4
                kb.mm(pss[sl][:, 0:N], kh[s][pr, j * 128:(j + 1) * 128], qh[s][pr, q0:q0 + N], True, nn == 0, [Bin[s]], [Bpss[sl]])
                if nn:
                    kb.mm(pss[sl][:, 0:nn], i8[:], bn[:, mp, boff:boff + nn], False, True, [Bi8, Bbn], [Bpss[sl]])
                    kb.act(PT[pt][:, 0:nn], pss[sl][:, 0:nn], AF.Exp, [Bpss[sl]], [BPT[pt]], scale=0.125)
                if N > nn:
                    kb.act(PT[pt][:, nn:N], pss[sl][:, nn:N], AF.Exp, [Bpss[sl], Bc31], [BPT[pt]], scale=0.125, bias=c31[:, mp:mp + 1])

            def stageB(idx):
                c, m, j = its[idx]
                lo, N, nn, boff = geom(c, j)
                off = (lo - 4 * c) * 128
                jl = 4 * c + 3
                pt = (base + idx) % 4
                kb.mm(pnum[m][:, off:512], vh[s][:, j, :], PT[pt][:, 0:N], j == 0, j == jl, [Bin[s], BPT[pt]], [Bpn[m]])
                kb.mm(pden[m][:, off:512], ones[:], PT[pt][:, 0:N], j == 0, j == jl, [Bon, BPT[pt]], [Bpd[m]])
                if m == 1 and j == jl:
                    kb.recip(r1[:], pden[0][:], [Bpd[0]], [Bw])
                    kb.recip(r2[:], pden[1][:], [Bpd[1]], [Bw])
                    kb.tt("dve", t1[:], pnum[0][:], r1[:], ALU.mult, [Bpn[0], Bw], [Bw])
                    kb.tt("dve", t2[:], pnum[1][:], r2[:], ALU.mult, [Bpn[1], Bw], [Bw])
                    kb.stt(t1[:], t2[:], neglam[:, 0:1], t1[:], ALU.mult, ALU.add, [Bw, Bnl], [Bw])
                    kb.act(sq[:], t1[:], AF.Square, [Bw], [Bsq])
                    kb.mm(psq[:], ones[:], sq[:], True, True, [Bon, Bsq], [Bpsq])
                    kb.ts("dve", rs[:], psq[:], 1.0 / 128, ALU.mult, [Bpsq], [Brs], s2=EPS, op1=ALU.add)
                    kb.act(rs[:], rs[:], AF.Sqrt, [Brs], [Brs])
                    kb.recip(rs[:], rs[:], [Brs], [Brs])
                    kb.stt(oTh[s][:, c * 512:(c + 1) * 512], t1[:], gsc[:, 0:1], rs[:], ALU.mult, ALU.mult, [Bw, Bgsc, Brs], [BoT[s]])
            LA = 2
            for i0 in range(min(LA, len(its))):
                stageA(i0)
            for idx in range(len(its)):
                if idx + LA < len(its):
                    stageA(idx + LA)
                stageB(idx)
            base += len(its)
            kb.ld(act_d[h], oTh[s][:], [BoT[s]], (), owner=BoT[s])
        P.end_phase()


def phase_dsa_idx(kb, qkv_d, wi_d, mask_d, cmask_d, ident_d):
    nc, P = kb.nc, kb.P
    BIG = 1.0e30
    with ExitStack() as es:
        kiT = kb.sb(es, [64, 2048], BF16, "kiT")
        qi = kb.sb(es, [64, 16, 2048], BF16, "qi")
        Bqk = P.buf("qiki")
        kb.ld(kiT[:], qkv_d[28, 0:64, :], (), [Bqk])
        for h in range(16):
            kb.ld(qi[:, h, :], qkv_d[20 + h // 2, (h % 2) * 64:(h % 2) * 64 + 64, :], (), [Bqk])
        cm = kb.sb(es, [128, 128], F32, "cm")
        Bcm = P.buf("cm")
        kb.ld(cm[:], cmask_d, (), [Bcm])
        ident = kb.sb(es, [128, 128], BF16, "ident")
        Bid = P.buf("ident")
        kb.ld(ident[:], ident_d, (), [Bid], q="pool")
        zt = kb.sb(es, [128, 256], BF16, "zt")
        Bzt = P.buf("zt")
        kb.memset("pool", zt[:], 0.0, [Bzt])
        kb.ld(mask_d[0, :, 0:256], zt[:], [Bzt], (), owner=Bzt)
        kb.ld(mask_d[1, :, 0:256], zt[:], [Bzt], (), owner=Bzt)
        wi = [kb.sb(es, [128, 16], F32, "wi") for _ in range(2)]
        Bwi = P.bufs(2, "wi")
        dgw = [kb.sb(es, [128, 16, 128], BF16, "dgw") for _ in range(2)]
        Bdg = P.bufs(2, "dgw")
        acc = [kb.sb(es, [128, 2048], F32, "acc") for _ in range(2)]
        Bacc = P.bufs(2, "acc")
        Rb = [kb.sb(es, [128, 512], BF16, "Rb") for _ in range(3)]
        BRb = P.bufs(3, "Rb")
        work = kb.sb(es, [128, 2048], F32, "work")
        Bwork = P.buf("work")
        m8 = kb.sb(es, [128, 8], F32, "m8")
        Bm8 = P.buf("m8")
        Mb = [kb.sb(es, [128, 2048], BF16, "Mb") for _ in range(2)]
        BMb = P.bufs(2, "Mb")
        mT = [kb.sb(es, [128, 8, 128], BF16, "mT") for _ in range(2)]
        BmT = P.bufs(2, "mT")
        pl = [kb.ps(es, [128, 512], F32, "pl") for _ in range(2)]
        Bpl = P.bufs(2, "pl")
        pacc = kb.ps(es, [128, 2048], F32, "pacc")
        Bpacc = P.bufs(4, "pacc")
        pT = [kb.ps(es, [128, 1024], BF16, "pT") for _ in range(2)]
        BpT = P.bufs(2, "pT")
        nl = 0
        nT = 0
        cnt = {"nl": 0, "nT": 0}

        def front(qt):
            if qt >= 16:
                return
            a = qt % 2
            ns = (qt + 1) * 128
            kb.ld(wi[a][:], wi_d[qt * 128:(qt + 1) * 128, :], (), [Bwi[a]])
            for h in range(16):
                kb.ts("dve", dgw[a][:, h, :], ident[:], wi[a][:, h:h + 1], ALU.mult, [Bid, Bwi[a]], [Bdg[a]])
            its = [(sc, h) for sc in range((ns + 511) // 512) for h in range(16)]
            base = cnt["nl"]

            def stA(idx):
                sc, h = its[idx]
                n2 = min(512, ns - sc * 512)
                sl = (base + idx) % 2
                rb = (base + idx) % 3
                kb.mm(pl[sl][:, 0:n2], qi[:, h, qt * 128:(qt + 1) * 128], kiT[:, sc * 512:sc * 512 + n2], True, True, [Bqk], [Bpl[sl]])
                kb.act(Rb[rb][:, 0:n2], pl[sl][:, 0:n2], AF.Relu, [Bpl[sl]], [BRb[rb]])

            def stB(idx):
                sc, h = its[idx]
                n2 = min(512, ns - sc * 512)
                rb = (base + idx) % 3
                kb.mm(pacc[:, sc * 512:sc * 512 + n2], dgw[a][:, h, :], Rb[rb][:, 0:n2], h == 0, h == 15, [Bdg[a], BRb[rb]], [Bpacc[sc]])
            stA(0)
            for idx in range(len(its)):
                if idx + 1 < len(its):
                    stA(idx + 1)
                stB(idx)
            cnt["nl"] += len(its)
            nsc = (ns + 511) // 512
            kb.cp("act", acc[a][:, 0:ns], pacc[:, 0:ns], [Bpacc[i] for i in range(nsc)], [Bacc[a]])

        def back(qt):
            a = qt % 2
            ns = (qt + 1) * 128
            dg = acc[a][:, qt * 128:(qt + 1) * 128]
            kb.tt("dve", dg, dg, cm[:], ALU.add, [Bacc[a], Bcm], [Bacc[a]])
            src, Bsrc = acc[a], Bacc[a]
            for r in range(32):
                sv = src[:, 0:ns]
                P.op("dve", (lambda sv=sv: nc.vector.max(out=m8[:], in_=sv)), [Bsrc], [Bm8])
                if r < 31:
                    wv_ = work[:, 0:ns]
                    P.op("dve", (lambda sv=sv, wv_=wv_: nc.vector.match_replace(out=wv_, in_to_replace=m8[:], in_values=sv, imm_value=-BIG)), [Bsrc, Bm8], [Bwork])
                    src, Bsrc = work, Bwork
            kb.ts("dve", Mb[a][:, 0:ns], acc[a][:, 0:ns], m8[:, 7:8], ALU.is_ge, [Bacc[a], Bm8], [BMb[a]])
            for j0 in range(0, qt + 1, 8):
                njj = min(8, qt + 1 - j0)
                b = cnt["nT"] % 2
                cnt["nT"] += 1
                for i in range(njj):
                    j = j0 + i
                    kb.tr(pT[b][:, i * 128:(i + 1) * 128], Mb[a][:, j * 128:(j + 1) * 128], ident[:], [BMb[a], Bid], [BpT[b]])
                srcv = pT[b][:].rearrange("p (j q) -> p j q", j=8)[:, 0:njj, :]
                kb.ts("dve", mT[b][:, 0:njj, :], srcv, -1.0, ALU.add, [BpT[b]], [BmT[b]], s2=-MASKV, op1=ALU.mult)
                kb.ld(mask_d[j0:j0 + njj, :, qt * 128:(qt + 1) * 128].rearrange("j p q -> p j q"), mT[b][:, 0:njj, :], [BmT[b]], (), owner=BmT[b])
        front(2)
        for qt in range(2, 16):
            front(qt + 1)
            back(qt)
        P.end_phase()


def phase_dsa_attn(kb, qkv_d, vtok_d, act_d, mask_d, biasn_d, i8_d, table_d):
    nc, P = kb.nc, kb.P
    with ExitStack() as es:
        bn = kb.sb(es, [128, 32, 256], BF16, "bn")
        Bbn = P.buf("bn")
        kb.ld(bn[:], biasn_d.rearrange("p (h q) -> p h q", h=32), (), [Bbn], q="pool")
        i8 = kb.sb(es, [128, 128], BF16, "i8")
        Bi8 = P.buf("i8")
        kb.ld(i8[:], i8_d, (), [Bi8], q="pool")
        ones = kb.sb(es, [128, 64], BF16, "ones")
        Bon = P.buf("ones")
        kb.memset("dve", ones[:], 1.0, [Bon])
        c31 = kb.sb(es, [128, 32], F32, "c31")
        Bc31 = P.buf("c31")
        kb.ld(c31[:], table_d[31].partition_broadcast(128), (), [Bc31])
        vt = kb.sb(es, [128, 16, 512], BF16, "vt")
        Bvt = P.buf("vt")
        kb.ld(vt[:], vtok_d[:, 0:512].rearrange("(t p) c -> p t c", p=128), (), [Bvt])
        mk = kb.sb(es, [128, 16, 2048], BF16, "mk")
        Bmk = P.buf("mk")
        for j in range(16):
            kb.ld(mk[:, j, j * 128:2048], mask_d[j, :, j * 128:2048], (), [Bmk])
        kh = [kb.sb(es, [64, 2048], BF16, "kh") for _ in range(2)]
        Bkh = P.bufs(2, "kh")
        qh = [kb.sb(es, [64, 2048], BF16, "qh") for _ in range(2)]
        Bqh = P.bufs(2, "qh")
        PT = [kb.sb(es, [128, 512], BF16, "PT") for _ in range(4)]
        BPT = P.bufs(4, "PT")
        oTh = [kb.sb(es, [64, 2048], BF16, "oTh") for _ in range(2)]
        BoT = P.bufs(2, "oTh")
        r1 = [kb.sb(es, [64, 512], F32, "r1") for _ in range(2)]
        Br1 = P.bufs(2, "r1")
        pss = [kb.ps(es, [128, 512], F32, "pss") for _ in range(4)]
        Bpss = P.bufs(4, "pss")
        pnum = [kb.ps(es, [64, 512], F32, "pnum") for _ in range(2)]
        pden = [kb.ps(es, [64, 512], F32, "pden") for _ in range(2)]
        Bpn = P.bufs(2, "pn")
        Bpd = P.bufs(2, "pd")

        def load_q(hq_):
            if hq_ >= 32:
                return
            kb.ld(qh[hq_ % 2][:], qkv_d[hq_ // 2, (hq_ % 2) * 64:(hq_ % 2) * 64 + 64, :], (), [Bqh[hq_ % 2]])

        def load_k(kv_):
            if kv_ >= 8:
                return
            kb.ld(kh[kv_ % 2][:], qkv_d[16 + kv_ // 2, (kv_ % 2) * 64:(kv_ % 2) * 64 + 64, :], (), [Bkh[kv_ % 2]])
        load_k(0)
        load_q(0)
        nj = 0
        nb = 0
        for kv in range(8):
            ks = kv % 2
            load_k(kv + 1)
            for g in range(4):
                hq = 4 * kv + g
                s = hq % 2
                load_q(hq + 1)
                its = [(c, j) for c in range(4) for j in range(4 * c + 4)]

                def geom(c, j):
                    lo = max(j, 4 * c)
                    N = (4 * c + 4 - lo) * 128
                    if j >= 4 * c:
                        nn, boff = min(N, 256), 0
                    elif j == 4 * c - 1:
                        nn, boff = 128, 128
                    else:
                        nn, boff = 0, 0
                    return lo, N, nn, boff

                def stageA(idx, hq=hq, ks=ks, s=s, base=nj):
                    c, j = its[idx]
                    lo, N, nn, boff = geom(c, j)
                    q0 = lo * 128
                    sl = (base + idx) % 4
                    pt = (base + idx) % 4
                    kb.mm(pss[sl][:, 0:N], kh[ks][:, j * 128:(j + 1) * 128], qh[s][:, q0:q0 + N], True, False, [Bkh[ks], Bqh[s]], [Bpss[sl]])
                    if nn:
                        kb.mm(pss[sl][:, 0:nn], i8[:], bn[:, hq, boff:boff + nn], False, False, [Bi8, Bbn], [Bpss[sl]])
                    kb.mm(pss[sl][:, 0:N], i8[:], mk[:, j, q0:q0 + N], False, True, [Bi8, Bmk], [Bpss[sl]])
                    if nn:
                        kb.act(PT[pt][:, 0:nn], pss[sl][:, 0:nn], AF.Exp, [Bpss[sl]], [BPT[pt]], scale=0.125)
                    if N > nn:
                        kb.act(PT[pt][:, nn:N], pss[sl][:, nn:N], AF.Exp, [Bpss[sl], Bc31], [BPT[pt]], scale=0.125, bias=c31[:, hq:hq + 1])

                def stageB(idx, hq=hq, kv=kv, s=s, base=nj, nb0=nb):
                    c, j = its[idx]
                    lo, N, nn, boff = geom(c, j)
                    off = (lo - 4 * c) * 128
                    jl = 4 * c + 3
                    pt = (base + idx) % 4
                    b = (nb0 + c) % 2
                    kb.mm(pnum[b][:, off:512], vt[:, j, kv * 64:(kv + 1) * 64], PT[pt][:, 0:N], j == 0, j == jl, [Bvt, BPT[pt]], [Bpn[b]])
                    kb.mm(pden[b][:, off:512], ones[:], PT[pt][:, 0:N], j == 0, j == jl, [Bon, BPT[pt]], [Bpd[b]])
                    if j == jl:
                        kb.recip(r1[b][:], pden[b][:], [Bpd[b]], [Br1[b]])
                        kb.tt("dve", oTh[s][:, c * 512:(c + 1) * 512], pnum[b][:], r1[b][:], ALU.mult, [Bpn[b], Br1[b]], [BoT[s]])
                LA = 2
                for i0 in range(min(LA, len(its))):
                    stageA(i0)
                for idx in range(len(its)):
                    if idx + LA < len(its):
                        stageA(idx + LA)
                    stageB(idx)
                nj += len(its)
                nb += 4
                kb.ld(act_d[hq // 2, (hq % 2) * 64:(hq % 2) * 64 + 64, :], oTh[s][:], [BoT[s]], (), owner=BoT[s])
        P.end_phase()


def hgrn_inproj(kb, hT, BhT, w_in, lbl_d, layer_idx, hq_d, hf_d, hk_d, hv_d, qkv_d):
    nc, P = kb.nc, kb.P
    tiles = [(i * 128, 128) for i in range(16)] + [(2048 + i * 128, 128) for i in range(16)] + [(6144 + i * 128, 128) for i in range(16)]

    def setup(es):
        c = {}
        lbl = kb.sb(es, [128, 16, 4], F32, "lbl")
        Bl = P.buf("lbl")
        kb.ld(lbl[:], lbl_d.rearrange("p (t l) -> p t l", l=4), (), [Bl])
        kb.act(lbl[:], lbl[:], AF.Exp, [Bl], [Bl])
        ssum = kb.sb(es, [128, 16], F32, "ssum")
        lb = kb.sb(es, [128, 16], F32, "lb")
        oml = kb.sb(es, [128, 16], F32, "oml")
        Blb = P.buf("lb")
        P.op("dve", lambda: nc.vector.tensor_reduce(out=ssum[:], in_=lbl[:], axis=AX.X, op=ALU.add), [Bl], [Blb])
        kb.recip(ssum[:], ssum[:], [Blb], [Blb])
        kb.cp("dve", lb[:], lbl[:, :, 1], [Bl, Blb], [Blb])
        for j in range(2, layer_idx + 1):
            kb.tt("dve", lb[:], lb[:], lbl[:, :, j], ALU.add, [Bl, Blb], [Blb])
        kb.tt("dve", lb[:], lb[:], ssum[:], ALU.mult, [Blb], [Blb])
        kb.ts("dve", oml[:], lb[:], -1.0, ALU.mult, [Blb], [Blb], s2=1.0, op1=ALU.add)
        c["lb"], c["oml"], c["Blb"] = lb, oml, Blb
        c["fo"] = [kb.sb(es, [128, 2048], F32, "fo") for _ in range(2)]
        c["Bfo"] = P.bufs(2, "fo")
        c["fk"] = [kb.sb(es, [128, 2048], F32, "fk") for _ in range(2)]
        c["Bfk"] = P.bufs(2, "fk")
        c["bo"] = [kb.sb(es, [128, 2048], BF16, "bo") for _ in range(2)]
        c["Bbo"] = P.bufs(2, "bo")
        return c

    def epi(c, ti, c0, w, pb, Bpb):
        kind, h = ti // 16, ti % 16
        a = ti % 2
        if kind == 0:
            kb.act(c["fo"][a][:], pb[:], AF.Silu, [Bpb], [c["Bfo"][a]])
            kb.ld(hq_d[h], c["fo"][a][:], [c["Bfo"][a]], (), owner=c["Bfo"][a])
        elif kind == 1:
            fo, Bfo, fk, Bfk = c["fo"][a], c["Bfo"][a], c["fk"][a], c["Bfk"][a]
            kb.act(fo[:], pb[:], AF.Sigmoid, [Bpb], [Bfo])
            kb.ts("dve", fo[:], fo[:], c["oml"][:, h:h + 1], ALU.mult, [Bfo, c["Blb"]], [Bfo], s2=c["lb"][:, h:h + 1], op1=ALU.add)
            kb.ts("dve", fk[:], fo[:], -1.0, ALU.mult, [Bfo], [Bfk], s2=1.0, op1=ALU.add)
            kb.ld(hk_d[h], fk[:], [Bfk], (), owner=Bfk)
            kb.act(fo[:], fo[:], AF.Ln, [Bfo], [Bfo])
            kb.ld(hf_d[h], fo[:], [Bfo], (), owner=Bfo)
        else:
            kb.act(c["bo"][a][:], pb[:], AF.Silu, [Bpb], [c["Bbo"][a]])
            kb.ld(qkv_d[h], c["bo"][a][:], [c["Bbo"][a]], (), owner=c["Bbo"][a])
    phase_proj_fm(kb, hT, BhT, w_in, tiles, epi, setup)
    phase_proj_tok(kb, hT, BhT, w_in, [(4096 + i * 512, 512) for i in range(4)],
                   epi_store_tok(kb, hv_d, lambda gi: gi * 512), epi_tok_setup(kb, BF16))


def phase_hgrn(kb, hq_d, hf_d, hk_d, hv_d, qkv_d, act_d, ng_d, ident_d, tri_d):
    nc, P = kb.nc, kb.P
    with ExitStack() as es:
        identb = kb.sb(es, [128, 128], BF16, "identb")
        tri = kb.sb(es, [64, 512], F32, "tri")
        ng = kb.sb(es, [128, 1], F32, "ng")
        Bc = P.buf("consts")
        kb.ld(identb[:], ident_d, (), [Bc], q="pool")
        kb.ld(tri[:], tri_d, (), [Bc])
        kb.ld(ng[:], ng_d.rearrange("o e -> e o"), (), [Bc])
        ones = kb.sb(es, [128, 128], BF16, "ones")
        rmask = kb.sb(es, [128, 2048], F32, "rmask")
        Bon = P.buf("ones")
        kb.memset("dve", ones[:], 1.0, [Bon])
        kb.memset("dve", rmask[:], 1.0, [Bon])
        kb.memset("dve", rmask[:].rearrange("p (c t) -> p c t", t=64)[:, :, 0:1], 0.0, [Bon])
        A = kb.sb(es, [128, 2048], F32, "A")
        Bb = kb.sb(es, [128, 2048], F32, "B")
        Cq = kb.sb(es, [128, 2048], F32, "Cq")
        Dd = kb.sb(es, [128, 2048], F32, "Dd")
        BA, BB, BCq, BD = P.buf("A"), P.buf("B"), P.buf("Cq"), P.buf("D")
        Cb = [kb.sb(es, [128, 2048], BF16, "Cb") for _ in range(2)]
        Eb = [kb.sb(es, [128, 2048], BF16, "Eb") for _ in range(2)]
        Fb = [kb.sb(es, [128, 2048], BF16, "Fb") for _ in range(2)]
        vtk = [kb.sb(es, [64, 32, 128], BF16, "vtk") for _ in range(2)]
        gs = [kb.sb(es, [128, 2048], BF16, "gs") for _ in range(2)]
        ebend = [kb.sb(es, [128, 32], F32, "ebend") for _ in range(2)]
        BCb, BEb, BFb, Bvtk, Bgs, Beb = (P.bufs(2, "Cb"), P.bufs(2, "Eb"), P.bufs(2, "Fb"), P.bufs(2, "vtk"), P.bufs(2, "gs"), P.bufs(2, "eb"))
        U = kb.sb(es, [128, 128 * 33], F32, "U")
        Dk = kb.sb(es, [128, 128 * 33], F32, "Dk")
        St = kb.sb(es, [128, 128 * 33], F32, "St")
        BU, BDk, BSt = P.buf("U"), P.buf("Dk"), P.buf("St")
        U3 = U[:].rearrange("p (v c) -> p v c", c=33)
        Dk3 = Dk[:].rearrange("p (v c) -> p v c", c=33)
        St3 = St[:].rearrange("p (v c) -> p v c", c=33)
        Stc = kb.sb(es, [128, 32, 128], BF16, "Stc")
        BStc = P.buf("Stc")
        kb.memset("pool", Dk[:], 0.0, [BDk])
        kb.memset("pool", U[:], 0.0, [BU])
        kendT = kb.sb(es, [64, 32, 128], BF16, "kendT")
        BkT = P.buf("kendT")
        attm = kb.sb(es, [64, 2048], BF16, "attm")
        Batt = P.buf("attm")
        o_h = kb.sb(es, [128, 2048], F32, "o_h")
        Boh = P.buf("o_h")
        oTh = [kb.sb(es, [128, 2048], BF16, "oTh") for _ in range(2)]
        BoT = P.bufs(2, "oTh")
        sq = kb.sb(es, [128, 512], BF16, "sq")
        rs = kb.sb(es, [128, 512], F32, "rs")
        tmp = kb.sb(es, [128, 512], F32, "tmp")
        Bsq, Brs, Btmp = P.buf("sq"), P.buf("rs"), P.buf("tmp")
        ptr = [kb.ps(es, [128, 1024], BF16, "ptr") for _ in range(2)]
        Bptr = P.bufs(2, "ptr")
        patt = kb.ps(es, [128, 512], F32, "patt")
        Bpatt = P.buf("patt")
        pupd = [kb.ps(es, [128, 512], F32, "pupd") for _ in range(2)]
        Bpupd = P.bufs(2, "pupd")
        po = [kb.ps(es, [128, 512], F32, "po") for _ in range(2)]
        Bpo = P.bufs(2, "po")
        psq = kb.ps(es, [128, 512], F32, "psq")
        Bpsq = P.buf("psq")
        B3 = Bb[:].rearrange("p (c t) -> p c t", t=64)
        A3 = A[:].rearrange("p (c t) -> p c t", t=64)

        def load_h(h):
            if h >= 16:
                return
            kb.ld(Cq[:], hq_d[h], (), [BCq])
            kb.ld(A[:], hf_d[h], (), [BA])
            kb.ld(Dd[:], hk_d[h], (), [BD])
            kb.ld(vtk[h % 2][:], hv_d[:, h * 128:(h + 1) * 128].rearrange("(c s) v -> s c v", s=64), (), [Bvtk[h % 2]])
            kb.ld(gs[h % 2][:], qkv_d[h], (), [Bgs[h % 2]])

        def ew(h):
            if h >= 16:
                return
            s = h % 2
            P.op("dve", lambda: nc.vector.tensor_tensor_scan(out=Bb[:], data0=rmask[:], data1=A[:], initial=0.0, op0=ALU.mult, op1=ALU.add),
                 [Bon, BA], [BB])
            kb.act(A[:], Bb[:], AF.Exp, [BB], [BA])
            kb.tt("dve", Cb[s][:], Cq[:], A[:], ALU.mult, [BCq, BA], [BCb[s]])
            kb.act(ebend[s][:], B3[:, :, 63], AF.Exp, [BB], [Beb[s]])
            kb.ts("dve", A[:], Bb[:], -1.0, ALU.mult, [BB, BCb[s]], [BA], s2=80.0, op1=ALU.min)
            kb.act(A[:], A[:], AF.Exp, [BA], [BA])
            kb.tt("dve", Eb[s][:], Dd[:], A[:], ALU.mult, [BD, BA], [BEb[s]])
            kb.tt("dve", A3, B3[:, :, 63:64].to_broadcast([128, 32, 64]), B3, ALU.subtract, [BB, BEb[s]], [BA])
            kb.act(A[:], A[:], AF.Exp, [BA], [BA])
            kb.tt("dve", Fb[s][:], Dd[:], A[:], ALU.mult, [BD, BA], [BFb[s]])

        def pe_front(h):
            s = h % 2
            for c in range(32):
                b = (c // 8) % 2
                kb.tr(ptr[b][0:64, (c % 8) * 128:(c % 8 + 1) * 128], Fb[s][:, c * 64:(c + 1) * 64], identb[:], [BFb[s], Bc], [Bptr[b]])
                if c % 8 == 7:
                    kb.cp("act", kendT[:, c - 7:c + 1, :], ptr[b][0:64, :].rearrange("p (c k) -> p c k", c=8), [Bptr[b]], [BkT])
            for grp in range(4):
                for i in range(8):
                    c = grp * 8 + i
                    kb.mm(patt[0:64, i * 64:(i + 1) * 64], Eb[s][:, c * 64:(c + 1) * 64], Cb[s][:, c * 64:(c + 1) * 64], True, True,
                          [BEb[s], BCb[s]], [Bpatt])
                kb.tt("dve", attm[:, grp * 512:(grp + 1) * 512], patt[0:64, :], tri[:], ALU.mult, [Bpatt, Bc], [Batt])
            kb.memset("dve", U3[:, :, 0:1], 0.0, [BU])
            for grp in range(8):
                b = grp % 2
                for i in range(4):
                    c = grp * 4 + i
                    kb.mm(pupd[b][:, i * 128:(i + 1) * 128], kendT[:, c, :], vtk[s][:, c, :], True, True, [BkT, Bvtk[s]], [Bpupd[b]])
                kb.cp("act" if b == 0 else "dve", U3[:, :, grp * 4 + 1:grp * 4 + 5].rearrange("p v c -> p c v"),
                      pupd[b][:].rearrange("p (c v) -> p c v", c=4), [Bpupd[b]], [BU])
            kb.cp("act", Dk3[:, :, 1:33], ebend[s][:].unsqueeze(1).to_broadcast([128, 128, 32]), [Beb[s]], [BDk])
            P.op("dve", lambda: nc.vector.tensor_tensor_scan(out=St[:], data0=Dk[:], data1=U[:], initial=0.0, op0=ALU.mult, op1=ALU.add),
                 [BDk, BU], [BSt])
            kb.cp("act", Stc[:], St3[:, :, 0:32].rearrange("p v c -> p c v"), [BSt], [BStc])

        def pe_back(h):
            s = h % 2
            for grp in range(4):
                b = grp % 2
                for i in range(8):
                    c = grp * 8 + i
                    kb.mm(po[b][:, i * 64:(i + 1) * 64], vtk[s][:, c, :], attm[:, c * 64:(c + 1) * 64], True, False, [Bvtk[s], Batt], [Bpo[b]])
                    kb.mm(po[b][:, i * 64:(i + 1) * 64], Stc[:, c, :], Cb[s][:, c * 64:(c + 1) * 64], False, True, [BStc, BCb[s]], [Bpo[b]])
                kb.cp("act", o_h[:, grp * 512:(grp + 1) * 512], po[b][:], [Bpo[b]], [Boh])
            for cc in range(4):
                cs = slice(cc * 512, (cc + 1) * 512)
                kb.act(sq[:], o_h[:, cs], AF.Square, [Boh], [Bsq])
                kb.mm(psq[:], ones[:], sq[:], True, True, [Bon, Bsq], [Bpsq])
                kb.ts("dve", rs[:], psq[:], 1.0 / 128, ALU.mult, [Bpsq], [Brs], s2=EPS, op1=ALU.add)
                kb.act(rs[:], rs[:], AF.Sqrt, [Brs], [Brs])
                kb.recip(rs[:], rs[:], [Brs], [Brs])
                kb.stt(tmp[:], o_h[:, cs], ng[:, 0:1], rs[:], ALU.mult, ALU.mult, [Boh, Bc, Brs], [Btmp])
                kb.tt("dve", oTh[s][:, cs], tmp[:], gs[s][:, cs], ALU.mult, [Btmp, Bgs[s]], [BoT[s]])
            kb.ld(act_d[h], oTh[s][:], [BoT[s]], (), owner=BoT[s])
        load_h(0)
        ew(0)
        for h in range(16):
            load_h(h + 1)
            pe_front(h)
            ew(h + 1)
            pe_back(h)
        P.end_phase()


LAYER_NIN = {0: 3072, 1: 6144, 2: 8192, 3: 4176}
MIX_NAMES = {0: "swa", 1: "diff", 2: "hgrn", 3: "dsa"}


def build_program(layers, stop_after=None):
    nc = bass.Bass("TRN2", target_bir_lowering=False)
    dr = {}

    def ext(name, shape, dt=F32):
        dr[name] = nc.dram_tensor(name, list(shape), dt, kind="ExternalInput").ap()
        return dr[name]

    x_in = ext("x", [S, D])
    y_out = nc.dram_tensor("y", [S, D], F32, kind="ExternalOutput").ap()
    normg = ext("norm_g", [16, D])
    ident_d = ext("ident", [128, 128])
    i8_d = ext("i8", [128, 128])
    for l in layers:
        ext("w_in%d" % l, [D, LAYER_NIN[l]])
        ext("w_out%d" % l, [D, D])
        ext("w_up%d" % l, [D, 2 * DFF])
        ext("w_down%d" % l, [DFF, D])
        ext("conv%d" % l, [128, 88, 3])
    if 0 in layers:
        ext("bias_sw", [128, 32 * 256])
        ext("sinkrep", [1, 4096])
    if 1 in layers or 3 in layers:
        ext("biasn", [128, 32 * 256])
        ext("table", [32, 32])
    if 1 in layers:
        ext("diff_lambda", [4, 64])
        ext("diff_subg", [1, 128])
    xres = nc.dram_tensor("xres", [S, D], F32).ap()
    m_d = nc.dram_tensor("m_d", [S, D], F32).ap()
    act_d = nc.dram_tensor("act_d", [KFF, 128, S], BF16).ap()
    qkv_d = nc.dram_tensor("qkv_d", [64, 128, S], BF16).ap()
    vtok_d = nc.dram_tensor("vtok_d", [S, 2048], BF16).ap()
    if 2 in layers:
        ext("lbl", [128, 64])
        ext("hgrn_ng", [1, 128])
        ext("tri", [64, 512])
        hq_d = nc.dram_tensor("hq_d", [16, 128, S], F32).ap()
        hf_d = nc.dram_tensor("hf_d", [16, 128, S], F32).ap()
        hk_d = nc.dram_tensor("hk_d", [16, 128, S], F32).ap()
        hv_d = nc.dram_tensor("hv_d", [S, 2048], BF16).ap()
    if 3 in layers:
        ext("cmask", [128, 128])
        wi_d = nc.dram_tensor("wi_d", [S, 16], F32).ap()
        mask_d = nc.dram_tensor("mask_d", [16, 128, S], BF16).ap()

    with ExitStack() as top:
        P = Prog(nc, top)
        kb = KB(nc, P)
        x_cur = x_in
        pend_m = None
        for li, l in enumerate(layers):
            with ExitStack() as hs:
                hT = kb.sb(hs, [128, 16, S], BF16, "hT")
                BhT = Buf("hT")
                phase_rn(kb, x_cur, xres, m_d if pend_m is not None else None,
                         normg[pend_m] if pend_m is not None else None, normg[4 * l + 0], hT, BhT, ident_d)
                if pend_m is not None:
                    x_cur = xres
                w_in = dr["w_in%d" % l]
                if l == 0:
                    tiles = [(i * 128, 128) for i in range(20)]
                    phase_proj_fm(kb, hT, BhT, w_in, tiles, epi_store_fm(kb, qkv_d, lambda ti: ti), epi_store_setup(kb))
                    phase_proj_tok(kb, hT, BhT, w_in, [(2560, 512)], epi_store_tok(kb, vtok_d, lambda gi: 0), epi_tok_setup(kb))
                elif l == 1:
                    tiles = [(i * 128, 128) for i in range(32)]
                    phase_proj_fm(kb, hT, BhT, w_in, tiles, epi_store_fm(kb, qkv_d, lambda ti: ti), epi_store_setup(kb))
                    phase_proj_tok(kb, hT, BhT, w_in, [(4096 + i * 512, 512) for i in range(4)],
                                   epi_store_tok(kb, vtok_d, lambda gi: gi * 512), epi_tok_setup(kb))
                elif l == 2:
                    hgrn_inproj(kb, hT, BhT, w_in, dr["lbl"], l, hq_d, hf_d, hk_d, hv_d, qkv_d)
                elif l == 3:
                    tiles = [(i * 128, 128) for i in range(20)] + [(3072 + i * 128, 128) for i in range(8)] + [(4096, 64)]
                    phase_proj_fm(kb, hT, BhT, w_in, tiles, epi_store_fm(kb, qkv_d, lambda ti: ti), epi_store_setup(kb))

                    def setup3(es):
                        c_ = epi_tok_setup(kb)(es)
                        c_["tf"] = [kb.sb(es, [128, 16], F32, "etf") for _ in range(2)]
                        c_["Bf"] = kb.P.bufs(2, "etf")
                        return c_

                    def epi3(ctx, gi, t, c0, w, pb, Bpb):
                        if gi == 0:
                            epi_store_tok(kb, vtok_d, lambda gi_: 0)(ctx, gi, t, c0, w, pb, Bpb)
                        else:
                            tl, B = ctx["tf"][t % 2], ctx["Bf"][t % 2]
                            kb.cp("dve", tl[:, 0:16], pb[:, 0:16], [Bpb], [B])
                            kb.ld(wi_d[t * 128:(t + 1) * 128, :], tl[:, 0:16], [B], (), owner=B)
                    phase_proj_tok(kb, hT, BhT, w_in, [(2560, 512), (4160, 16)], epi3, setup3)
            if l == 0:
                phase_swa(kb, qkv_d, vtok_d, act_d, dr["bias_sw"], i8_d, dr["sinkrep"])
            elif l == 2:
                phase_hgrn(kb, hq_d, hf_d, hk_d, hv_d, qkv_d, act_d, dr["hgrn_ng"], ident_d, dr["tri"])
            elif l == 3:
                phase_dsa_idx(kb, qkv_d, wi_d, mask_d, dr["cmask"], ident_d)
                phase_dsa_attn(kb, qkv_d, vtok_d, act_d, mask_d, dr["biasn"], i8_d, dr["table"])
            elif l == 1:
                phase_diff(kb, qkv_d, vtok_d, act_d, dr["biasn"], i8_d, dr["table"], dr["diff_lambda"], dr["diff_subg"], l)
            phase_down(kb, dr["w_out%d" % l], 16, act_d, m_d)
            with ExitStack() as hs:
                hT = kb.sb(hs, [128, 16, S], BF16, "hT")
                BhT = Buf("hT")
                phase_rn(kb, x_cur, xres, m_d, normg[4 * l + 1], normg[4 * l + 2], hT, BhT, ident_d)
                x_cur = xres
                phase_up(kb, hT, BhT, dr["w_up%d" % l], dr["conv%d" % l], act_d)
            phase_down(kb, dr["w_down%d" % l], KFF, act_d, m_d)
            pend_m = 4 * l + 3
        phase_rn(kb, x_cur, y_out, m_d, normg[pend_m], None, None, None, ident_d)
    return nc


def _bucket(n):
    n = np.maximum(n, 0)
    max_exact = 16
    lr = np.log(np.maximum(n, 1).astype(np.float32) / max_exact) / math.log(128 / max_exact)
    large = np.minimum(max_exact + (lr * (32 - max_exact)).astype(np.int32), 31)
    return np.where(n < max_exact, n, large)


def _host_consts(inputs, layers):
    c = {}
    c["ident"] = np.eye(128, dtype=np.float32)
    c["i8"] = (8.0 * np.eye(128)).astype(np.float32)
    table = np.asarray(inputs["rel_bias_table"], np.float32)
    if 0 in layers:
        s_ = np.arange(128)[:, None]
        q_ = np.arange(256)[None, :]
        dist = q_ - s_
        valid = (dist >= 0) & (dist < 128)
        bk = _bucket(dist)
        t = table[bk]
        t = np.where(valid[:, :, None], t, np.float32(MASKV))
        c["bias_sw"] = np.ascontiguousarray(t.transpose(0, 2, 1)).reshape(128, 32 * 256).astype(np.float32)
        c["sinkrep"] = np.ascontiguousarray(np.repeat(np.asarray(inputs["swa_sinks"], np.float32)[0], 128)[None, :])
    if 1 in layers or 3 in layers:
        s_ = np.arange(128)[:, None]
        q_ = np.arange(256)[None, :]
        dist = q_ - s_
        t = table[_bucket(dist)]
        t = np.where((dist >= 0)[:, :, None], t, np.float32(MASKV))
        c["biasn"] = np.ascontiguousarray(t.transpose(0, 2, 1)).reshape(128, 32 * 256).astype(np.float32)
        c["table"] = np.ascontiguousarray(table)
    if 2 in layers:
        lg = np.asarray(inputs["hgrn_lb_logits"], np.float32)
        c["lbl"] = np.ascontiguousarray(lg.reshape(4, 16, 128).transpose(2, 1, 0)).reshape(128, 64)
        c["hgrn_ng"] = np.ascontiguousarray(np.asarray(inputs["hgrn_norm_g"], np.float32)[0][None, :])
        tr_ = (np.arange(64)[:, None] <= np.arange(64)[None, :]).astype(np.float32)
        c["tri"] = np.ascontiguousarray(np.tile(tr_, (1, 8)))
    if 3 in layers:
        c["cmask"] = np.where(np.arange(128)[None, :] > np.arange(128)[:, None], np.float32(-1.0e30), np.float32(0.0)).astype(np.float32)
    if 1 in layers:
        c["diff_lambda"] = np.ascontiguousarray(np.asarray(inputs["diff_lambda"], np.float32)[0])
        c["diff_subg"] = np.ascontiguousarray(np.asarray(inputs["diff_subln_g"], np.float32)[0][None, :])
    return c


_W_KEYS = {0: ("swa_w_in", "swa_w_out"), 1: ("diff_w_in", "diff_w_out"), 2: ("hgrn_w_in", "hgrn_w_out"), 3: ("dsa_w_in", "dsa_w_out")}
LAUNCHES = [[0, 1, 2, 3]]
N_CORES = 4


def _layer_inputs(inputs, layers):
    m = {}
    m["norm_g"] = np.ascontiguousarray(np.asarray(inputs["norm_g"], np.float32).reshape(16, D))
    for l in layers:
        kin, kout = _W_KEYS[l]
        m["w_in%d" % l] = np.ascontiguousarray(np.asarray(inputs[kin], np.float32)[0])
        m["w_out%d" % l] = np.ascontiguousarray(np.asarray(inputs[kout], np.float32)[0])
        m["w_up%d" % l] = np.ascontiguousarray(np.asarray(inputs["ffn_w_up"], np.float32)[l])
        m["w_down%d" % l] = np.ascontiguousarray(np.asarray(inputs["ffn_w_down"], np.float32)[l])
        cv = np.asarray(inputs["ffn_conv"], np.float32)[l]
        m["conv%d" % l] = np.ascontiguousarray(cv.reshape(3, 88, 128).transpose(2, 1, 0))
    m.update(_host_consts(inputs, layers))
    return m


def run_launch(inputs, x, layers, cores=None):
    nc = build_program(layers)
    shared = _layer_inputs(inputs, layers)
    B = x.shape[0]
    cores = cores if cores is not None else N_CORES
    in_maps = []
    for c in range(cores):
        d = dict(shared)
        d["x"] = np.ascontiguousarray(x[c % B])
        in_maps.append(d)
    res = run_bass_kernel_spmd(nc, in_maps, core_ids=list(range(cores)))
    return np.stack([res.results[b]["y"] for b in range(B)], axis=0)


def kernel(**inputs):
    x = np.asarray(inputs["x"], np.float32)
    for layers in LAUNCHES:
        x = run_launch(inputs, x, layers)
    return x.astype(np.float32)
```
